# Optimizing a Trainium2 kernel written in Bass

```python
import jax, jax.numpy as jnp
from jax import lax
import numpy as np

D_MODEL = 1024
BATCH = 16
SEQ = 2048
DEPTH = 1

HEAD_DIM = 64
A_HEADS = 8
A_KV_HEADS = 8
B_HEADS = 8
B_KV_HEADS = 2
N_ATTN_HEADS = A_HEADS + B_HEADS
MOBA_BLOCK = 256
MOBA_TOPK = 3
MOBA_QCHUNK = 128
SWA_WINDOW = 128
D_FF = 4 * D_MODEL
EPS = 1e-6
NEG = -1e30
SCALE = HEAD_DIM ** -0.5

W_QA = A_HEADS * HEAD_DIM
W_KA = A_KV_HEADS * HEAD_DIM
W_VA = A_KV_HEADS * HEAD_DIM
W_QB = B_HEADS * HEAD_DIM
W_KB = B_KV_HEADS * HEAD_DIM
W_VB = B_KV_HEADS * HEAD_DIM
W_GATE = D_MODEL
IN_WIDTH = W_QA + W_KA + W_VA + W_QB + W_KB + W_VB + 2 * W_GATE

kernel_name = "hybrid_moba_swa_gated_block"


def _rmsnorm(x, g):
    xf = x.astype(jnp.float32)
    y = xf * lax.rsqrt(jnp.mean(xf * xf, axis=-1, keepdims=True) + EPS)
    return (y * g.astype(jnp.float32)).astype(x.dtype)


def _alibi_slopes(n):
    return jnp.exp2(-(8.0 / n) * jnp.arange(1, n + 1, dtype=jnp.float32))


def _moba(q, k, v, slopes):
    B, S, H, hd = q.shape
    nb = -(-S // MOBA_BLOCK)
    pad = nb * MOBA_BLOCK - S
    padw = ((0, 0), (0, pad), (0, 0), (0, 0))
    kt = jnp.pad(k, padw).reshape(B, nb, MOBA_BLOCK, H, hd).transpose(0, 3, 1, 2, 4)
    vt = jnp.pad(v, padw).reshape(B, nb, MOBA_BLOCK, H, hd).transpose(0, 3, 1, 2, 4)
    qh = q.transpose(0, 2, 1, 3)

    kmean = jnp.mean(kt.astype(jnp.float32), axis=3)
    gs = jnp.einsum('bhsd,bhnd->bhsn', qh.astype(jnp.float32), kmean)
    qblk = jnp.arange(S) // MOBA_BLOCK
    past = jnp.arange(nb)[None, :] < qblk[:, None]
    gs = jnp.where(past, gs, -jnp.inf)
    ksel = min(MOBA_TOPK, nb)
    _, idx = lax.top_k(gs, ksel)

    nc = S // MOBA_QCHUNK
    qc = qh.reshape(B, H, nc, MOBA_QCHUNK, hd).transpose(0, 2, 1, 3, 4).reshape(B * nc, H, MOBA_QCHUNK, hd)
    ic = idx.reshape(B, H, nc, MOBA_QCHUNK, ksel).transpose(0, 2, 1, 3, 4).reshape(B * nc, H, MOBA_QCHUNK, ksel)
    b_ids = jnp.repeat(jnp.arange(B), nc)
    c_ids = jnp.tile(jnp.arange(nc), B)

    def chunk(args):
        qq, ii, b, c = args
        kb_b = kt[b]
        vb_b = vt[b]
        t = c * MOBA_QCHUNK + jnp.arange(MOBA_QCHUNK)
        own = (c * MOBA_QCHUNK) // MOBA_BLOCK
        gather = jax.vmap(lambda kh, ih: kh[ih])
        k_sel = gather(kb_b, ii)
        v_sel = gather(vb_b, ii)
        s_sel = jnp.einsum('hqd,hqkpd->hqkp', qq, k_sel).astype(jnp.float32) * SCALE
        pos_sel = ii[..., None] * MOBA_BLOCK + jnp.arange(MOBA_BLOCK)
        d_sel = (t[None, :, None, None] - pos_sel).astype(jnp.float32)
        slot_ok = (jnp.arange(ksel) < own)[None, None, :, None]
        s_sel = jnp.where(slot_ok, s_sel - slopes[:, None, None, None] * d_sel, NEG)
        k_own = lax.dynamic_index_in_dim(kb_b, own, axis=1, keepdims=False)
        v_own = lax.dynamic_index_in_dim(vb_b, own, axis=1, keepdims=False)
        s_own = jnp.einsum('hqd,hpd->hqp', qq, k_own).astype(jnp.float32) * SCALE
        d_own = t[:, None] - (own * MOBA_BLOCK + jnp.arange(MOBA_BLOCK))[None, :]
        s_own = jnp.where((d_own >= 0)[None], s_own - slopes[:, None, None] * d_own.astype(jnp.float32)[None], NEG)
        H_, QC = qq.shape[0], qq.shape[1]
        scores = jnp.concatenate([s_sel.reshape(H_, QC, ksel * MOBA_BLOCK), s_own], axis=-1)
        p = jax.nn.softmax(scores, axis=-1).astype(qq.dtype)
        p_sel = p[..., :ksel * MOBA_BLOCK].reshape(H_, QC, ksel, MOBA_BLOCK)
        p_own = p[..., ksel * MOBA_BLOCK:]
        return (jnp.einsum('hqkp,hqkpd->hqd', p_sel, v_sel)
                + jnp.einsum('hqp,hpd->hqd', p_own, v_own))

    o = lax.map(chunk, (qc, ic, b_ids, c_ids))
    return o.reshape(B, nc, H, MOBA_QCHUNK, hd).transpose(0, 1, 3, 2, 4).reshape(B, S, H * hd)


def _swa(q, k, v, slopes, sinks):
    B, S, Hq, hd = q.shape
    Hkv = k.shape[2]
    G = Hq // Hkv
    W = SWA_WINDOW
    nblk = S // W
    qb = q.reshape(B, nblk, W, Hkv, G, hd)
    kb = k.reshape(B, nblk, W, Hkv, hd)
    vb = v.reshape(B, nblk, W, Hkv, hd)
    shift = lambda a: jnp.concatenate([jnp.zeros_like(a[:, :1]), a[:, :-1]], axis=1)
    kc = jnp.concatenate([shift(kb), kb], axis=2)
    vc = jnp.concatenate([shift(vb), vb], axis=2)
    s = jnp.einsum('bnqhgd,bnkhd->bhgnqk', qb, kc).astype(jnp.float32) * SCALE
    dist = jnp.arange(W)[:, None] - jnp.arange(2 * W)[None, :] + W
    kpos = jnp.arange(nblk)[:, None] * W - W + jnp.arange(2 * W)[None, :]
    valid = ((dist >= 0) & (dist < W))[None, :, :] & (kpos >= 0)[:, None, :]
    sl = slopes.reshape(Hkv, G)[:, :, None, None, None]
    s = jnp.where(valid, s - sl * dist.astype(jnp.float32), NEG)
    sink = sinks.astype(jnp.float32).reshape(Hkv, G)[None, :, :, None, None, None]
    m = jnp.maximum(jnp.max(s, axis=-1, keepdims=True), sink)
    e = jnp.exp(s - m)
    p = e / (jnp.sum(e, axis=-1, keepdims=True) + jnp.exp(sink - m))
    o = jnp.einsum('bhgnqk,bnkhd->bnqhgd', p.astype(v.dtype), vc)
    return o.reshape(B, S, Hq * hd)


def setup_inputs(seed: int = 0) -> dict:
    key = jax.random.key(seed)
    ks = jax.random.split(key, 16)
    f = jnp.float32
    nrm = lambda k, shape, fan: jax.random.normal(k, shape, f) * fan ** -0.5
    gain = lambda k, shape: 1.0 + 0.1 * jax.random.normal(k, shape, f)
    return {
        "x": jax.random.normal(ks[0], (BATCH, SEQ, D_MODEL), f),
        "norm_attn": gain(ks[1], (DEPTH, D_MODEL)),
        "w_in": nrm(ks[2], (DEPTH, D_MODEL, IN_WIDTH), D_MODEL),
        "q_norm_a": gain(ks[3], (DEPTH, HEAD_DIM)),
        "k_norm_a": gain(ks[4], (DEPTH, HEAD_DIM)),
        "q_norm_b": gain(ks[5], (DEPTH, HEAD_DIM)),
        "k_norm_b": gain(ks[6], (DEPTH, HEAD_DIM)),
        "sinks_b": 0.5 * jax.random.normal(ks[7], (DEPTH, B_HEADS), f),
        "w_branch_a": nrm(ks[8], (DEPTH, A_HEADS * HEAD_DIM, D_MODEL), A_HEADS * HEAD_DIM),
        "w_branch_b": nrm(ks[9], (DEPTH, B_HEADS * HEAD_DIM, D_MODEL), B_HEADS * HEAD_DIM),
        "w_out": nrm(ks[10], (DEPTH, D_MODEL, D_MODEL), D_MODEL),
        "norm_mlp": gain(ks[11], (DEPTH, D_MODEL)),
        "w_up": nrm(ks[12], (DEPTH, D_MODEL, D_FF), D_MODEL),
        "w_down": nrm(ks[13], (DEPTH, D_FF, D_MODEL), D_FF),
    }


def reference(x, norm_attn, w_in, q_norm_a, k_norm_a, q_norm_b, k_norm_b, sinks_b,
              w_branch_a, w_branch_b, w_out, norm_mlp, w_up, w_down):
    B, S, _ = x.shape
    slopes = _alibi_slopes(N_ATTN_HEADS)
    slopes_b = slopes[:B_HEADS]
    slopes_a = slopes[B_HEADS:]
    widths = (W_QA, W_KA, W_VA, W_QB, W_KB, W_VB, W_GATE)
    splits = []
    acc = 0
    for w in widths:
        acc += w
        splits.append(acc)
    for l in range(DEPTH):
        h = _rmsnorm(x, norm_attn[l])
        proj = h @ w_in[l]
        qa, ka, va, qb, kb, vb, ga, gb = jnp.split(proj, splits, axis=-1)
        qa = _rmsnorm(qa.reshape(B, S, A_HEADS, HEAD_DIM), q_norm_a[l])
        ka = _rmsnorm(ka.reshape(B, S, A_KV_HEADS, HEAD_DIM), k_norm_a[l])
        va = va.reshape(B, S, A_KV_HEADS, HEAD_DIM)
        qb = _rmsnorm(qb.reshape(B, S, B_HEADS, HEAD_DIM), q_norm_b[l])
        kb = _rmsnorm(kb.reshape(B, S, B_KV_HEADS, HEAD_DIM), k_norm_b[l])
        vb = vb.reshape(B, S, B_KV_HEADS, HEAD_DIM)
        oa = _moba(qa, ka, va, slopes_a)
        ob = _swa(qb, kb, vb, slopes_b, sinks_b[l])
        mixed = (jax.nn.sigmoid(ga) * (oa @ w_branch_a[l])
                 + jax.nn.sigmoid(gb) * (ob @ w_branch_b[l]))
        x = x + mixed @ w_out[l]
        h2 = _rmsnorm(x, norm_mlp[l])
        x = x + jnp.square(jax.nn.relu(h2 @ w_up[l])) @ w_down[l]
    return x
```

```python
import os
import numpy as np
from contextlib import ExitStack
import concourse.bass as bass
import concourse.mybir as mybir
from concourse.bass_utils import run_bass_kernel_spmd

F32 = mybir.dt.float32
BF16 = mybir.dt.bfloat16
AF = mybir.ActivationFunctionType
ALU = mybir.AluOpType
AX = mybir.AxisListType

NCORES = 8
D = 1024
S = 2048
NSEQ = 2
T = 512
NT = S // T
HD = 64
DFF = 4096
INW = 4352
NEGM = -30000.0
EPS = 1e-6
SCALE = HD ** -0.5
NS = 4
WSZ = 2048
NCHUNK = 62
ENGS = ("pe", "dve", "act", "pool", "sp")


class Buf:
    __slots__ = ("name", "last_w", "readers")

    def __init__(self, name):
        self.name = name
        self.last_w = None
        self.readers = {}


class Prog:
    def __init__(self):
        self.ops = {e: [] for e in ENGS}
        self.cnt = {e: 0 for e in ENGS}
        self.dcnt = {}
        self.waited = {e: {} for e in ENGS}
        self.semnames = set("e_" + e for e in ENGS)
        self.bufs = {}

    def buf(self, name):
        b = self.bufs.get(name)
        if b is None:
            b = self.bufs[name] = Buf(name)
        return b

    def op(self, eng, fn, reads=(), writes=(), dma=None):
        reads = [self.buf(r) for r in reads]
        writes = [self.buf(w) for w in writes]
        own = "e_" + eng
        need = {}

        def add(sem, val, same_ok):
            if sem == own:
                if eng == "pe" or not same_ok:
                    return
            if need.get(sem, 0) < val:
                need[sem] = val

        for b in reads:
            if b.last_w is not None:
                add(b.last_w[0], b.last_w[1], True)
        for b in writes:
            if b.last_w is not None:
                add(b.last_w[0], b.last_w[1], True)
            for s, v in b.readers.items():
                add(s, v, False)
        waits = []
        wd = self.waited[eng]
        for s, v in need.items():
            if wd.get(s, 0) < v:
                wd[s] = v
                waits.append((s, v))
        if dma is None:
            self.cnt[eng] += 1
            tok = (own, self.cnt[eng])
            inc = (own, 1)
        else:
            sname = "d_" + dma
            self.semnames.add(sname)
            self.dcnt[sname] = self.dcnt.get(sname, 0) + 16
            tok = (sname, self.dcnt[sname])
            inc = (sname, 16)
        self.ops[eng].append((waits, fn, inc))
        for b in writes:
            b.last_w = tok
            b.readers = {}
        for b in reads:
            if b.readers.get(tok[0], 0) < tok[1]:
                b.readers[tok[0]] = tok[1]
        return tok

    def final_wait(self, eng, toks):
        self.ops[eng].append((list(toks), None, None))

    def emit(self, block, sems):
        engmap = {"pe": "tensor", "dve": "vector", "act": "scalar", "pool": "gpsimd", "sp": "sync"}
        for e in ENGS:
            ops = self.ops[e]
            if not ops:
                continue

            def body(engine, ops=ops):
                for waits, fn, inc in ops:
                    for s, v in waits:
                        engine.wait_ge(sems[s], v)
                    if fn is not None:
                        fn(engine).then_inc(sems[inc[0]], inc[1])

            getattr(block, engmap[e])(body)


def _slopes():
    sl = np.exp2(-(8.0 / 16) * np.arange(1, 17, dtype=np.float32)).astype(np.float32)
    return sl[:8], sl[8:]


def _host_tables():
    sl_b, sl_a = _slopes()
    p = np.arange(128, dtype=np.float32)
    k = p[:, None]
    q = p[None, :]
    tri = np.where(k <= q, 0.0, NEGM).astype(np.float32)
    prv = np.where(k > q, 0.0, NEGM).astype(np.float32)
    t = {}
    t["c_ident"] = np.eye(128, dtype=np.float32)
    t["c_tri"] = tri
    t["c_mcur"] = np.tile(tri, (1, 4))
    t["c_mprev"] = np.tile(prv, (1, 4))
    bA = np.zeros((128, 128), np.float32)
    for h in range(8):
        for di in range(16):
            bA[:, h * 16 + di] = sl_a[h] * (p - 128.0 * (di - 3))
    t["c_biasA"] = bA
    bB = np.zeros((128, 16), np.float32)
    for h in range(8):
        bB[:, h * 2 + 0] = sl_b[h] * (p - 64.0)
        bB[:, h * 2 + 1] = sl_b[h] * (p - 192.0)
    t["c_biasB"] = bB
    t1 = np.zeros((128, 2, 2, 128), np.float32)
    for g in range(2):
        for e in range(2):
            t1[64:, g, e, :] = sl_b[4 * g + 2 * e] * (p - 64.0)
            t1[:64, g, e, :] = sl_b[4 * g + 2 * e + 1] * (p - 64.0)
    t["c_t1"] = t1.reshape(128, 512)
    sel = np.zeros((64, 4, 72), np.float32)
    for bi in range(4):
        b = 4 + bi
        for n in range(b):
            for m in range(b):
                if n != m:
                    sel[n * 8 + m, bi, 64 + n] = 1.0
    t["c_sel"] = sel.reshape(64, 288)
    E = np.zeros((8, S), np.float32)
    for n in range(8):
        E[n, n * 256:(n + 1) * 256] = 1.0
    t["c_E"] = E
    t["c_zero"] = np.zeros((8, 8 * T), np.float32)
    return t


def _chunk_pieces(k, w_in, w_a, w_b, w_out, w_up, w_down):
    win = w_in.rearrange("(kc p) n -> p kc n", p=128)
    if k < 10:
        g, kh = divmod(k, 2)
        nc_ = 512 if g < 4 else 256
        return [(0, (4, nc_), win[:, kh * 4:kh * 4 + 4, g * 512:g * 512 + nc_])]
    k -= 10
    if k < 16:
        n, part = divmod(k, 2)
        if part == 0:
            return [(0, (8, 128), win[:, :, 2304 + n * 128:2304 + (n + 1) * 128]),
                    (1024, (8, 128), win[:, :, 3328 + n * 128:3328 + (n + 1) * 128])]
        wa = w_a.rearrange("(kc p) n -> p kc n", p=128)
        wb = w_b.rearrange("(kc p) n -> p kc n", p=128)
        return [(0, (4, 128), wa[:, :, n * 128:(n + 1) * 128]),
                (512, (4, 128), wb[:, :, n * 128:(n + 1) * 128])]
    k -= 16
    if k < 4:
        half, kh = divmod(k, 2)
        wo = w_out.rearrange("(kc p) n -> p kc n", p=128)
        return [(0, (4, 512), wo[:, kh * 4:kh * 4 + 4, half * 512:(half + 1) * 512])]
    k -= 4
    if k < 16:
        gch, kh = divmod(k, 2)
        wu = w_up.rearrange("(kc p) n -> p kc n", p=128)
        return [(0, (4, 512), wu[:, kh * 4:kh * 4 + 4, gch * 512:(gch + 1) * 512])]
    k -= 16
    half, q = divmod(k, 8)
    wd = w_down.rearrange("(fc p) n -> p fc n", p=128)
    return [(0, (4, 512), wd[:, q * 4:q * 4 + 4, half * 512:(half + 1) * 512])]


class _Stop(Exception):
    pass


def build_nc(stop=None, dump=()):
    nc = bass.Bass("TRN2", target_bir_lowering=False)
    dr = lambda n, s, kind="ExternalInput", dt=F32: nc.dram_tensor(n, s, dt, kind=kind).ap()
    x = dr("x", [NSEQ, S, D])
    w_in = dr("w_in", [D, INW])
    w_a = dr("w_a", [512, D])
    w_b = dr("w_b", [512, D])
    w_out = dr("w_out", [D, D])
    w_up = dr("w_up", [D, DFF])
    w_down = dr("w_down", [DFF, D])
    g1_d = dr("g1", [128, 8])
    g2_d = dr("g2", [128, 8])
    gv_d = dr("gv", [64, 4])
    sbc_d = dr("sbc", [128, 4])
    ctab = {n: dr(n, list(a.shape)) for n, a in _host_tables().items()}
    y = dr("y", [NSEQ, S, D], kind="ExternalOutput")
    scr = nc.dram_tensor("scr", [NCHUNK, 128, WSZ], BF16).ap()

    with ExitStack() as es:
        sb = lambda n, s, d: es.enter_context(nc.sbuf_tensor(n, s, d))
        KA = sb("KA", [72, 8, S], BF16)
        VA = sb("VA", [128, 16, 4, 192], BF16)
        KB = sb("KB", [64, 2, 1024], BF16)
        VB = sb("VB", [128, 8, 320], BF16)
        QA = sb("QA", [72, 8, T], BF16)
        QB = sb("QB", [64, 8, T], BF16)
        xs = [sb("xs%d" % i, [128, D], F32) for i in range(2)]
        x1 = sb("x1", [128, 4, D], F32)
        hT = sb("hT", [128, 8, T], BF16)
        xhat = [sb("xhat%d" % i, [128, D], BF16) for i in range(2)]
        qkh = sb("qkh", [128, 1664], BF16)
        sq = sb("sq", [128, 512], F32)
        PT = [sb("PT%d" % i, [128, 512], BF16) for i in range(4)]
        oaT = sb("oaT", [128, 4, T], BF16)
        obT = sb("obT", [128, 4, T], BF16)
        sga = sb("sga", [128, 512], F32)
        sgb = sb("sgb", [128, 512], F32)
        tmpa = sb("tmpa", [128, 512], F32)
        tmpr = sb("tmpr", [128, 512], F32)
        HM = sb("HM", [128, 32, T], BF16)
        W = [sb("W%d" % i, [128, WSZ], BF16) for i in range(NS)]
        R = sb("R", [128, 512], F32)
        ident = sb("ident", [128, 128], BF16)
        tri = sb("tri", [128, 128], BF16)
        mcur = sb("mcur", [128, 512], BF16)
        mprev = sb("mprev", [128, 512], BF16)
        biasA = sb("biasA", [128, 128], F32)
        biasB = sb("biasB", [128, 16], F32)
        t1 = sb("t1", [128, 512], F32)
        sinkT = sb("sinkT", [128, 512], F32)
        sbc = sb("sbc_s", [128, 4], F32)
        sel = sb("sel", [64, 288], BF16)
        g1 = sb("g1_s", [128, 8], F32)
        g2 = sb("g2_s", [128, 8], F32)
        gv = sb("gv_s", [64, 4], F32)
        st = sb("st", [128, 64], F32)
        ksum = sb("ksum", [64, 8, 8], F32)
        dlt = sb("dlt", [64, 8, 64], BF16)
        ind = sb("ind", [64, 512], BF16)
        bank = [es.enter_context(nc.psum_tensor("pb%d" % i, [128, 512], F32)) for i in range(8)]
        tpb = bank[7][:, :].bitcast(BF16)

        P = Prog()

        def op(eng, fn, r=(), w=(), dma=None):
            return P.op(eng, fn, r, w, dma)

        def mm(out, lhsT, rhs, start, stop, r, w):
            op("pe", lambda e: e.matmul(out, lhsT=lhsT, rhs=rhs, start=start, stop=stop), r, w)

        def act(out, in_, func, r, w, **kw):
            op("act", lambda e: e.activation(out=out, in_=in_, func=func, **kw), r, w)

        def tt(eng, out, in0, in1, alu, r, w):
            op(eng, lambda e: e.tensor_tensor(out=out, in0=in0, in1=in1, op=alu), r, w)

        def ts(eng, out, in0, s1, s2, op0, op1, r, w):
            if op1 is None:
                op(eng, lambda e: e.tensor_scalar(out=out, in0=in0, scalar1=s1, scalar2=None, op0=op0), r, w)
            else:
                op(eng, lambda e: e.tensor_scalar(out=out, in0=in0, scalar1=s1, scalar2=s2, op0=op0, op1=op1), r, w)

        cidx = [0]

        def cload(dst, src, name, eng="pool"):
            cidx[0] += 1
            op(eng, lambda e: e.dma_start(out=dst, in_=src), (), [name], dma="c%d" % cidx[0])

        cload(ident[:], ctab["c_ident"], "ident")
        cload(tri[:], ctab["c_tri"], "tri")
        cload(mcur[:], ctab["c_mcur"], "mcur")
        cload(mprev[:], ctab["c_mprev"], "mprev")
        cload(sel[:], ctab["c_sel"], "sel")
        cload(biasA[:], ctab["c_biasA"], "biasA", "sp")
        cload(biasB[:], ctab["c_biasB"], "biasB", "sp")
        cload(t1[:], ctab["c_t1"], "t1", "sp")
        cload(sbc[:], sbc_d, "sbc", "sp")
        cload(g1[:], g1_d, "g1", "sp")
        cload(g2[:], g2_d, "g2", "sp")
        cload(gv[:], gv_d, "gv", "sp")
        for h in range(8):
            cload(KA[64:72, h, :], ctab["c_E"], "KAE")
        op("dve", lambda e: e.memset(VA[:, :, :, 64:128], 1.0), (), ["VAones"])
        op("dve", lambda e: e.memset(VB[:, :, 0:64], 1.0), (), ["VBones"])
        op("dve", lambda e: e.memset(VB[:, :, 128:192], 1.0), (), ["VBones"])
        op("dve", lambda e: e.memset(VB[:, :, 256:320], 1.0), (), ["VBones"])
        op("dve", lambda e: e.memset(ksum[:], 0.0), (), ["ksum"])
        for j in range(4):
            act(sinkT[:, j * 128:(j + 1) * 128], t1[:, j * 128:(j + 1) * 128], AF.Exp, ["t1", "sbc"], ["sinkT"],
                bias=sbc[:, j:j + 1], scale=1.0)

        seq = [(gt, k) for gt in range(NSEQ * NT) for k in range(NCHUNK)]
        wstate = {"next": 0}

        def wload(idx):
            gt, k = seq[idx]
            slot = idx % NS
            wt = W[slot]
            if gt == 0:
                for (off, (kk, ncol), src) in _chunk_pieces(k, w_in, w_a, w_b, w_out, w_up, w_down):
                    dst = wt[:, off:off + kk * ncol].rearrange("p (a b) -> p a b", b=ncol)
                    op("pool", lambda e, dst=dst, src=src: e.dma_start(out=dst, in_=src), (), ["W%d" % slot], dma="w%d" % slot)
                op("sp", lambda e: e.dma_start(out=scr[k], in_=wt[:]), ["W%d" % slot], ["scr%d" % k], dma="ws%d" % slot)
            else:
                op("sp", lambda e: e.dma_start(out=wt[:], in_=scr[k]), ["scr%d" % k], ["W%d" % slot], dma="w%d" % slot)

        def wneed(idx, ahead=NS - 1):
            lim = min(idx + ahead, len(seq) - 1)
            while wstate["next"] <= lim:
                wload(wstate["next"])
                wstate["next"] += 1
            return W[idx % NS], "W%d" % (idx % NS)

        mmb = [0]

        def next_bank():
            b = mmb[0] % 3
            mmb[0] += 1
            return bank[b], "pb%d" % b

        tph = [0]

        def next_tp():
            h = 0
            tph[0] += 1
            return tpb[:, h * 512:(h + 1) * 512], "pb7%s" % "ab"[h]

        def rmsnorm_T(src_ap, src_name, gain, gname, xh, xhn, dst_cols, si):
            c0 = (si % 2) * 8
            act(xh[:], src_ap, AF.Square, [src_name], [xhn, "st%d" % c0], accum_out=st[:, c0:c0 + 1])
            stage('n1_%d' % si)
            ts("dve", st[:, c0 + 1:c0 + 2], st[:, c0:c0 + 1], 1.0 / D, EPS, ALU.mult, ALU.add, ["st%d" % c0], ["st%d" % (c0 + 1)])
            act(st[:, c0 + 2:c0 + 3], st[:, c0 + 1:c0 + 2], AF.Sqrt, ["st%d" % (c0 + 1)], ["st%d" % (c0 + 2)])
            op("dve", lambda e: e.reciprocal(out=st[:, c0 + 3:c0 + 4], in_=st[:, c0 + 2:c0 + 3]), ["st%d" % (c0 + 2)], ["st%d" % (c0 + 3)])
            stage('n2_%d' % si)
            act(xh[:], src_ap, AF.Copy, [src_name, "st%d" % (c0 + 3)], [xhn], scale=st[:, c0 + 3:c0 + 4])
            stage('n3_%d' % si)
            for half in range(2):
                tp, tpn = next_tp()
                for j in range(4):
                    jj = half * 4 + j
                    op("pe", lambda e, jj=jj, j=j, tp=tp: e.transpose(out=tp[:, j * 128:(j + 1) * 128], in_=xh[:, jj * 128:(jj + 1) * 128], identity=ident[:, :]),
                       [xhn, "ident"], [tpn])
                stage('n4_%d_%d' % (si, half))
                for j in range(4):
                    jj = half * 4 + j
                    act(hT[:, jj, dst_cols], tp[:, j * 128:(j + 1) * 128], AF.Copy, [tpn, gname], ["hT"], scale=gain[:, jj:jj + 1])
                stage('n5_%d_%d' % (si, half))
            stage('n6_%d' % si)

        def qk_norm(ps, psn, nh, col0):
            n = nh * 64
            act(sq[:, 0:n], ps, AF.Square, [psn], ["sq"])
            op("dve", lambda e: e.tensor_reduce(out=st[:, 32:32 + nh], in_=sq[:, 0:n].rearrange("p (h d) -> p h d", d=64), axis=AX.X, op=ALU.add),
               ["sq"], ["stq"])
            ts("dve", st[:, 40:40 + nh], st[:, 32:32 + nh], 1.0 / HD, EPS, ALU.mult, ALU.add, ["stq"], ["stq2"])
            act(st[:, 48:48 + nh], st[:, 40:40 + nh], AF.Sqrt, ["stq2"], ["stq3"])
            op("dve", lambda e: e.reciprocal(out=st[:, 56:56 + nh], in_=st[:, 48:48 + nh]), ["stq3"], ["stq4"])
            for h in range(nh):
                act(qkh[:, col0 + h * 64:col0 + (h + 1) * 64], ps[:, h * 64:(h + 1) * 64], AF.Copy, [psn, "stq4"], ["qkh%d" % col0],
                    scale=st[:, 56 + h:57 + h])

        def head_T(col0, nh, dst_fn, gcol, dname):
            for h0 in range(0, nh, 4):
                nhh = min(4, nh - h0)
                tp, tpn = next_tp()
                for j in range(nhh):
                    c = col0 + (h0 + j) * 64
                    op("pe", lambda e, c=c, j=j, tp=tp: e.transpose(out=tp[0:64, j * 128:(j + 1) * 128], in_=qkh[:, c:c + 64], identity=ident[:, :]),
                       ["qkh%d" % col0, "ident"], [tpn])
                act(dst_fn(h0, nhh), tp[0:64, 0:nhh * 128].rearrange("p (j q) -> p j q", q=128), AF.Copy, [tpn, "gv"], [dname],
                    scale=gv[:, gcol:gcol + 1])

        ychunk = [0]
        ytok = None

        def stage(n):
            if stop == n:
                raise _Stop()

        widx = [0]

        try:
          stage('const')
          for s in range(NSEQ):
            for c in range(NT):
                T0 = c * T
                base = widx[0]
                if c == 0:
                    op("pool", lambda e: e.dma_start(out=QA[64:72, :, :].rearrange("p h q -> p (h q)"), in_=ctab["c_zero"]), (), ["QAm"], dma="qam")
                for i in range(4):
                    xb = xs[i % 2]
                    xn = "xs%d" % (i % 2)
                    src = x[s, T0 + i * 128:T0 + (i + 1) * 128, :]
                    op("sp", lambda e, xb=xb, src=src: e.dma_start(out=xb[:], in_=src), (), [xn], dma=xn)
                    rmsnorm_T(xb[:], xn, g1, "g1", xhat[i % 2], "xhat%d" % (i % 2), slice(i * 128, (i + 1) * 128), i)
                stage('A0')
                srcx = x[s, T0:T0 + T, :].rearrange("(i p) d -> p i d", p=128)
                op("sp", lambda e, srcx=srcx: e.dma_start(out=x1[:], in_=srcx), (), ["x1"], dma="x1")
                for g in range(5):
                    w0, w0n = wneed(base + 2 * g)
                    w1, w1n = wneed(base + 2 * g + 1, NS - 2)
                    ncol = 512 if g < 4 else 256
                    for i in range(4):
                        pb, pbn = next_bank()
                        kt = 4 * c + i
                        for k in range(8):
                            ww = (w0 if k < 4 else w1)[:, (k % 4) * ncol:(k % 4 + 1) * ncol]
                            mm(pb[:, 0:ncol], hT[:, k, i * 128:(i + 1) * 128], ww, k == 0, k == 7,
                               ["hT", w0n if k < 4 else w1n], [pbn])
                        qc = slice(i * 128, (i + 1) * 128)
                        if g == 0:
                            qk_norm(pb[:, 0:512], pbn, 8, 0)
                            head_T(0, 8, lambda h0, n: QA[0:64, h0:h0 + n, qc], 0, "QA")
                        elif g == 1:
                            qk_norm(pb[:, 0:512], pbn, 8, 512)
                            kc = slice(T0 + i * 128, T0 + (i + 1) * 128)
                            head_T(512, 8, lambda h0, n: KA[0:64, h0:h0 + n, kc], 1, "KA")
                        elif g == 2:
                            pv = pb[:, 0:512].rearrange("p (pr two d) -> p pr two d", two=2, d=64)
                            act(VA[:, kt, :, 0:64], pv[:, :, 0, :], AF.Copy, [pbn], ["VA"])
                            act(VA[:, kt, :, 128:192], pv[:, :, 1, :], AF.Copy, [pbn], ["VA"])
                        elif g == 3:
                            qk_norm(pb[:, 0:512], pbn, 8, 1024)
                            head_T(1024, 8, lambda h0, n: QB[0:64, h0:h0 + n, qc], 2, "QB")
                        else:
                            qk_norm(pb[:, 0:128], pbn, 2, 1536)
                            ks = kt % 8
                            head_T(1536, 2, lambda h0, n: KB[0:64, h0:h0 + n, ks * 128:(ks + 1) * 128], 3, "KB")
                            act(VB[:, ks, 64:128], pb[:, 128:192], AF.Copy, [pbn], ["VB"])
                            act(VB[:, ks, 192:256], pb[:, 192:256], AF.Copy, [pbn], ["VB"])
                stage('A')
                for h in range(8):
                    ko = ksum[:, h, 2 * c:2 * c + 2]
                    ki = KA[0:64, h, T0:T0 + T].rearrange("p (n k) -> p n k", k=256)
                    op("dve", lambda e, ko=ko, ki=ki: e.tensor_reduce(out=ko, in_=ki, axis=AX.X, op=ALU.add), ["KA"], ["ksum"])
                if c >= 2:
                    for h in range(8):
                        for n in range(8):
                            act(dlt[:, h, n * 8:(n + 1) * 8], ksum[:, h, :], AF.Identity, ["ksum"], ["dlt%d" % h],
                                bias=ksum[:, h, n:n + 1], scale=-1.0)
                        gb_, gbn = bank[3 + h % 2], "pb%d" % (3 + h % 2)
                        mm(gb_[0:64, :], dlt[:, h, :], QA[0:64, h, :], True, True, ["dlt%d" % h, "QA"], [gbn])
                        op("dve", lambda e, gb_=gb_: e.tensor_single_scalar(out=ind[:, :], in_=gb_[0:64, :], scalar=0.0, op=ALU.is_lt), [gbn], ["ind"])
                        for hb in range(2):
                            bi = 2 * c + hb - 4
                            mm(gb_[0:72, hb * 256:(hb + 1) * 256], sel[:, bi * 72:(bi + 1) * 72], ind[:, hb * 256:(hb + 1) * 256], True, True,
                               ["sel", "ind"], [gbn])
                        ts("dve", QA[64:72, h, :], gb_[64:72, :], 2.5, NEGM, ALU.is_ge, ALU.mult, [gbn], ["QAm"])
                stage('gate%d' % c)
                steps = [(h, j) for h in range(8) for j in range(4 * c + 4)]
                nst = len(steps)

                def moba_S(si):
                    h, j = steps[si]
                    qs = 128 * max(0, j - 4 * c)
                    N = T - qs
                    sbk, sbn = bank[3 + si % 2], "pb%d" % (3 + si % 2)
                    diag = j >= 4 * c
                    mm(sbk[:, 0:N], KA[0:72, h, j * 128:(j + 1) * 128], QA[0:72, h, qs:T], True, not diag,
                       ["KA", "KAE", "QA", "QAm"], [sbn])
                    if diag:
                        mm(sbk[:, 0:128], ident[:, :], tri[:, :], False, True, ["ident", "tri"], [sbn])
                    di = 4 * c - j + 3
                    pt = PT[si % 4]
                    act(pt[:, 0:N], sbk[:, 0:N], AF.Exp, [sbn, "biasA"], ["PT%d" % (si % 4)],
                        bias=biasA[:, h * 16 + di:h * 16 + di + 1], scale=SCALE)

                def moba_PV(si):
                    h, j = steps[si]
                    qs = 128 * max(0, j - 4 * c)
                    N = T - qs
                    ob_, obn = bank[5 + h % 2], "pb%d" % (5 + h % 2)
                    pr, od = divmod(h, 2)
                    lhsT = VA[:, j, pr, od * 64:od * 64 + 128]
                    mm(ob_[:, qs:T], lhsT, PT[si % 4][:, 0:N], j == 0, j == 4 * c + 3, ["VA", "VAones", "PT%d" % (si % 4)], [obn])
                    if j == 4 * c + 3:
                        lo, hi = (0, 64) if od == 0 else (64, 128)
                        dl, dh = (64, 128) if od == 0 else (0, 64)
                        act(R[dl:dh, :], ob_[dl:dh, :], AF.Copy, [obn], ["R"])
                        op("dve", lambda e: e.reciprocal(out=R[dl:dh, :], in_=R[dl:dh, :]), ["R"], ["R"])
                        tt("dve", oaT[lo:hi, pr, :], ob_[lo:hi, :], R[dl:dh, :], ALU.mult, [obn, "R"], ["oaT"])

                moba_S(0)
                for si in range(nst):
                    if si + 1 < nst:
                        moba_S(si + 1)
                    moba_PV(si)
                stage('B')
                for i in range(4):
                    kt = 4 * c + i
                    ks = kt % 8
                    kp = (kt - 1) % 8
                    for g in range(2):
                        ob_, obn = bank[5 + (i * 2 + g) % 2], "pb%d" % (5 + (i * 2 + g) % 2)
                        passes = [(ks, mcur, "mcur", 0)] + ([(kp, mprev, "mprev", 1)] if kt >= 1 else [])
                        for pi, (kslot, mk, mkn, which) in enumerate(passes):
                            sbk, sbn = bank[3 + pi], "pb%d" % (3 + pi)
                            mm(sbk[:, :], KB[0:64, g, kslot * 128:(kslot + 1) * 128], QB[0:64, 4 * g:4 * g + 4, i * 128:(i + 1) * 128],
                               True, False, ["KB", "QB"], [sbn])
                            mm(sbk[:, :], ident[:, :], mk[:, :], False, True, ["ident", mkn], [sbn])
                            pt = PT[pi * 2 + g]
                            ptn = "PT%d" % (pi * 2 + g)
                            for hh in range(4):
                                h = 4 * g + hh
                                act(pt[:, hh * 128:(hh + 1) * 128], sbk[:, hh * 128:(hh + 1) * 128], AF.Exp, [sbn, "biasB"], [ptn],
                                    bias=biasB[:, h * 2 + which:h * 2 + which + 1], scale=SCALE)
                            pt3 = pt[:, :].rearrange("p (a e q) -> p a e q", e=2, q=128)
                            mm(ob_[:, 0:256], VB[:, kslot, 64 + 128 * g:64 + 128 * g + 128], pt3[:, :, 0, :], pi == 0, pi == len(passes) - 1,
                               ["VB", "VBones", ptn], [obn])
                            mm(ob_[:, 256:512], VB[:, kslot, 128 * g:128 * g + 128], pt3[:, :, 1, :], False, pi == len(passes) - 1,
                               ["VB", "VBones", ptn], [obn])
                        tt("dve", R[64:128, 0:256], ob_[64:128, 0:256], sinkT[64:128, g * 256:(g + 1) * 256], ALU.add, [obn, "sinkT"], ["R"])
                        tt("dve", R[0:64, 256:512], ob_[0:64, 256:512], sinkT[0:64, g * 256:(g + 1) * 256], ALU.add, [obn, "sinkT"], ["R"])
                        op("dve", lambda e: e.reciprocal(out=R[64:128, 0:256], in_=R[64:128, 0:256]), ["R"], ["R"])
                        op("dve", lambda e: e.reciprocal(out=R[0:64, 256:512], in_=R[0:64, 256:512]), ["R"], ["R"])
                        qc = slice(i * 128, (i + 1) * 128)
                        tt("dve", obT[0:64, 2 * g:2 * g + 2, qc], ob_[0:64, 0:256].rearrange("p (a q) -> p a q", q=128),
                           R[64:128, 0:256].rearrange("p (a q) -> p a q", q=128), ALU.mult, [obn, "R"], ["obT"])
                        tt("dve", obT[64:128, 2 * g:2 * g + 2, qc], ob_[64:128, 256:512].rearrange("p (a q) -> p a q", q=128),
                           R[0:64, 256:512].rearrange("p (a q) -> p a q", q=128), ALU.mult, [obn, "R"], ["obT"])
                stage('C')
                for n in range(8):
                    wg, wgn = wneed(base + 10 + 2 * n)
                    wab, wabn = wneed(base + 10 + 2 * n + 1, NS - 2)
                    pa, pan = next_bank()
                    for k in range(8):
                        mm(pa[:, :], wg[:, k * 128:(k + 1) * 128], hT[:, k, :], k == 0, k == 7, [wgn, "hT"], [pan])
                    act(sga[:], pa[:, :], AF.Sigmoid, [pan], ["sga"])
                    pb2, pb2n = next_bank()
                    for k in range(8):
                        mm(pb2[:, :], wg[:, 1024 + k * 128:1024 + (k + 1) * 128], hT[:, k, :], k == 0, k == 7, [wgn, "hT"], [pb2n])
                    act(sgb[:], pb2[:, :], AF.Sigmoid, [pb2n], ["sgb"])
                    pc, pcn = next_bank()
                    for k in range(4):
                        mm(pc[:, :], wab[:, k * 128:(k + 1) * 128], oaT[:, k, :], k == 0, k == 3, [wabn, "oaT"], [pcn])
                    tt("dve", tmpa[:], pc[:, :], sga[:], ALU.mult, [pcn, "sga"], ["tmpa"])
                    pd, pdn = next_bank()
                    for k in range(4):
                        mm(pd[:, :], wab[:, 512 + k * 128:512 + (k + 1) * 128], obT[:, k, :], k == 0, k == 3, [wabn, "obT"], [pdn])
                    tt("dve", sgb[:], pd[:, :], sgb[:], ALU.mult, [pdn, "sgb"], ["sgb"])
                    tt("dve", HM[:, n, :], tmpa[:], sgb[:], ALU.add, ["tmpa", "sgb"], ["hid%d" % n])
                stage('D')
                for half in range(2):
                    w0, w0n = wneed(base + 26 + 2 * half)
                    w1, w1n = wneed(base + 26 + 2 * half + 1, NS - 2)
                    for i in range(4):
                        pb, pbn = next_bank()
                        for k in range(8):
                            ww = (w0 if k < 4 else w1)[:, (k % 4) * 512:(k % 4 + 1) * 512]
                            mm(pb[:, :], HM[:, k, i * 128:(i + 1) * 128], ww, k == 0, k == 7, ["hid%d" % k, w0n if k < 4 else w1n], [pbn])
                        xo = x1[:, i, half * 512:(half + 1) * 512]
                        tt("dve", xo, pb[:, :], xo, ALU.add, [pbn, "x1"], ["x1"])
                stage('E')
                for i in range(4):
                    rmsnorm_T(x1[:, i, :], "x1", g2, "g2", xhat[i % 2], "xhat%d" % (i % 2), slice(i * 128, (i + 1) * 128), 4 + i)
                stage('F')
                for gch in range(8):
                    w0, w0n = wneed(base + 30 + 2 * gch)
                    w1, w1n = wneed(base + 30 + 2 * gch + 1, NS - 2)
                    for f in range(4):
                        pb, pbn = next_bank()
                        for k in range(8):
                            ww = (w0 if k < 4 else w1)[:, (k % 4) * 512 + f * 128:(k % 4) * 512 + (f + 1) * 128]
                            mm(pb[:, :], ww, hT[:, k, :], k == 0, k == 7, ["hT", w0n if k < 4 else w1n], [pbn])
                        act(tmpr[:], pb[:, :], AF.Relu, [pbn], ["tmpr"])
                        tt("dve", HM[:, gch * 4 + f, :], pb[:, :], tmpr[:], ALU.mult, [pbn, "tmpr"], ["hid%d" % (gch * 4 + f)])
                stage('G')
                for half in range(2):
                    for q in range(8):
                        wd, wdn = wneed(base + 46 + half * 8 + q)
                        for f in range(4):
                            ff = q * 4 + f
                            for i in range(4):
                                mm(bank[i][:, :], HM[:, ff, i * 128:(i + 1) * 128], wd[:, f * 512:(f + 1) * 512], ff == 0, ff == 31,
                                   ["hid%d" % ff, wdn], ["pb%d" % i])
                    for i in range(4):
                        xo = x1[:, i, half * 512:(half + 1) * 512]
                        tt("dve", xo, bank[i][:, :], xo, ALU.add, ["pb%d" % i, "x1"], ["x1"])
                dsty = y[s, T0:T0 + T, :].rearrange("(i p) d -> p i d", p=128)
                ytok = op("pool", lambda e, dsty=dsty: e.dma_start(out=dsty, in_=x1[:]), ["x1"], (), dma="y")
                widx[0] += NCHUNK
                stage('T%d' % (s * NT + c))
        except _Stop:
            pass
        if ytok is not None:
            P.final_wait("pool", [ytok])
        if dump:
            tens = {"hT": hT, "QA": QA, "KA": KA, "VA": VA, "QB": QB, "KB": KB, "VB": VB, "oaT": oaT, "obT": obT,
                    "HM": HM, "x1": x1, "ksum": ksum, "sinkT": sinkT, "R": R, "st": st, "qkh": qkh, "ind": ind, "dlt": dlt}
            allb = list(P.bufs.keys())
            dtoks = []
            for nm in dump:
                tns = tens[nm]
                shp = list(tns.shape)
                flat = tns[:]
                if len(shp) == 3:
                    flat = flat.rearrange("p a b -> p (a b)")
                elif len(shp) == 4:
                    flat = flat.rearrange("p a b c -> p (a b c)")
                fr = int(np.prod(shp[1:]))
                dd = nc.dram_tensor("dbg_" + nm, [shp[0], fr], tns.dtype, kind="ExternalOutput").ap()
                dtoks.append(op("sp", lambda e, dd=dd, flat=flat: e.dma_start(out=dd, in_=flat), allb, (), dma="dbg_" + nm))
            P.final_wait("sp", dtoks)
        sems = {n: es.enter_context(nc.semaphore(n)) for n in sorted(P.semnames)}
        with nc.Block() as block:
            P.emit(block, sems)
    return nc


_NC_CACHE = {}


def kernel(x, norm_attn, w_in, q_norm_a, k_norm_a, q_norm_b, k_norm_b, sinks_b,
           w_branch_a, w_branch_b, w_out, norm_mlp, w_up, w_down):
    f = lambda a: np.ascontiguousarray(np.asarray(a, dtype=np.float32))
    x = f(x)
    tabs = _host_tables()
    g1 = f(np.asarray(norm_attn)[0].reshape(8, 128).T)
    g2 = f(np.asarray(norm_mlp)[0].reshape(8, 128).T)
    gv = f(np.stack([np.asarray(q_norm_a)[0], np.asarray(k_norm_a)[0], np.asarray(q_norm_b)[0], np.asarray(k_norm_b)[0]], axis=1))
    sk = np.asarray(sinks_b)[0]
    sbc = np.zeros((128, 4), np.float32)
    for g in range(2):
        for a in range(2):
            sbc[64:, 2 * g + a] = sk[4 * g + 2 * a]
            sbc[:64, 2 * g + a] = sk[4 * g + 2 * a + 1]
    common = {
        "w_in": f(np.asarray(w_in)[0]), "w_a": f(np.asarray(w_branch_a)[0]), "w_b": f(np.asarray(w_branch_b)[0]),
        "w_out": f(np.asarray(w_out)[0]), "w_up": f(np.asarray(w_up)[0]), "w_down": f(np.asarray(w_down)[0]),
        "g1": g1, "g2": g2, "gv": gv, "sbc": sbc,
    }
    common.update(tabs)
    if "nc" not in _NC_CACHE:
        _NC_CACHE["nc"] = build_nc()
    nc = _NC_CACHE["nc"]
    in_maps = []
    for c in range(NCORES):
        m = dict(common)
        m["x"] = np.ascontiguousarray(x[NSEQ * c:NSEQ * (c + 1)])
        in_maps.append(m)
    res = run_bass_kernel_spmd(nc, in_maps, core_ids=list(range(NCORES)))
    return np.concatenate([r["y"] for r in res.results], axis=0).astype(np.float32)
```

```python
import os
import numpy as np
from contextlib import ExitStack
import concourse.bass as bass
import concourse.mybir as mybir
from concourse.bass_utils import run_bass_kernel_spmd

F32 = mybir.dt.float32
BF16 = mybir.dt.bfloat16
AF = mybir.ActivationFunctionType
ALU = mybir.AluOpType
AX = mybir.AxisListType

NCORES = 8
D = 1024
S = 2048
NSEQ = 2
T = 512
NT = S // T
HD = 64
DFF = 4096
INW = 4352
NEGM = -30000.0
EPS = 1e-6
SCALE = HD ** -0.5
NS = 4
WSZ = 2048
NCHUNK = 62
ENGS = ("pe", "dve", "act", "pool", "sp")


class Buf:
    __slots__ = ("name", "last_w", "readers")

    def __init__(self, name):
        self.name = name
        self.last_w = None
        self.readers = {}


class Prog:
    def __init__(self):
        self.ops = {e: [] for e in ENGS}
        self.cnt = {e: 0 for e in ENGS}
        self.dcnt = {}
        self.waited = {e: {} for e in ENGS}
        self.semnames = set("e_" + e for e in ENGS)
        self.bufs = {}

    def buf(self, name):
        b = self.bufs.get(name)
        if b is None:
            b = self.bufs[name] = Buf(name)
        return b

    def op(self, eng, fn, reads=(), writes=(), dma=None):
        reads = [self.buf(r) for r in reads]
        writes = [self.buf(w) for w in writes]
        own = "e_" + eng
        need = {}

        def add(sem, val, same_ok):
            if sem == own:
                if eng == "pe" or not same_ok:
                    return
            if need.get(sem, 0) < val:
                need[sem] = val

        for b in reads:
            if b.last_w is not None:
                add(b.last_w[0], b.last_w[1], True)
            if b.name.startswith("pb"):
                for s, v in b.readers.items():
                    add(s, v, False)
        for b in writes:
            if b.last_w is not None:
                add(b.last_w[0], b.last_w[1], True)
            for s, v in b.readers.items():
                add(s, v, False)
        waits = []
        wd = self.waited[eng]
        for s, v in need.items():
            if wd.get(s, 0) < v:
                wd[s] = v
                waits.append((s, v))
        if dma is None:
            self.cnt[eng] += 1
            tok = (own, self.cnt[eng])
            inc = (own, 1)
        else:
            sname = "d_" + dma
            self.semnames.add(sname)
            self.dcnt[sname] = self.dcnt.get(sname, 0) + 16
            tok = (sname, self.dcnt[sname])
            inc = (sname, 16)
        self.ops[eng].append((waits, fn, inc))
        for b in writes:
            b.last_w = tok
            b.readers = {}
        for b in reads:
            if b.readers.get(tok[0], 0) < tok[1]:
                b.readers[tok[0]] = tok[1]
        return tok

    def final_wait(self, eng, toks):
        self.ops[eng].append((list(toks), None, None))

    def emit(self, block, sems):
        engmap = {"pe": "tensor", "dve": "vector", "act": "scalar", "pool": "gpsimd", "sp": "sync"}
        for e in ENGS:
            ops = self.ops[e]
            if not ops:
                continue

            def body(engine, ops=ops):
                for waits, fn, inc in ops:
                    for s, v in waits:
                        engine.wait_ge(sems[s], v)
                    if fn is not None:
                        fn(engine).then_inc(sems[inc[0]], inc[1])

            getattr(block, engmap[e])(body)


def _slopes():
    sl = np.exp2(-(8.0 / 16) * np.arange(1, 17, dtype=np.float32)).astype(np.float32)
    return sl[:8], sl[8:]


def _host_tables():
    sl_b, sl_a = _slopes()
    p = np.arange(128, dtype=np.float32)
    k = p[:, None]
    q = p[None, :]
    tri = np.where(k <= q, 0.0, NEGM).astype(np.float32)
    prv = np.where(k > q, 0.0, NEGM).astype(np.float32)
    t = {}
    t["c_ident"] = np.eye(128, dtype=np.float32)
    t["c_tri"] = tri
    t["c_mcur"] = np.tile(tri, (1, 4))
    t["c_mprev"] = np.tile(prv, (1, 4))
    bA = np.zeros((128, 128), np.float32)
    for h in range(8):
        for di in range(16):
            bA[:, h * 16 + di] = sl_a[h] * (p - 128.0 * (di - 3))
    t["c_biasA"] = bA
    bB = np.zeros((128, 16), np.float32)
    for h in range(8):
        bB[:, h * 2 + 0] = sl_b[h] * (p - 64.0)
        bB[:, h * 2 + 1] = sl_b[h] * (p - 192.0)
    t["c_biasB"] = bB
    t1 = np.zeros((128, 2, 2, 128), np.float32)
    for g in range(2):
        for e in range(2):
            t1[64:, g, e, :] = sl_b[4 * g + 2 * e] * (p - 64.0)
            t1[:64, g, e, :] = sl_b[4 * g + 2 * e + 1] * (p - 64.0)
    t["c_t1"] = t1.reshape(128, 512)
    sel = np.zeros((64, 4, 72), np.float32)
    for bi in range(4):
        b = 4 + bi
        for n in range(b):
            for m in range(b):
                if n != m:
                    sel[n * 8 + m, bi, 64 + n] = 1.0
    t["c_sel"] = sel.reshape(64, 288)
    E = np.zeros((8, S), np.float32)
    for n in range(8):
        E[n, n * 256:(n + 1) * 256] = 1.0
    t["c_E"] = E
    t["c_zero"] = np.zeros((8, 8 * T), np.float32)
    return t


def _chunk_pieces(k, w_in, w_a, w_b, w_out, w_up, w_down):
    win = w_in.rearrange("(kc p) n -> p kc n", p=128)
    if k < 10:
        g, kh = divmod(k, 2)
        nc_ = 512 if g < 4 else 256
        return [(0, (4, nc_), win[:, kh * 4:kh * 4 + 4, g * 512:g * 512 + nc_])]
    k -= 10
    if k < 16:
        n, part = divmod(k, 2)
        if part == 0:
            return [(0, (8, 128), win[:, :, 2304 + n * 128:2304 + (n + 1) * 128]),
                    (1024, (8, 128), win[:, :, 3328 + n * 128:3328 + (n + 1) * 128])]
        wa = w_a.rearrange("(kc p) n -> p kc n", p=128)
        wb = w_b.rearrange("(kc p) n -> p kc n", p=128)
        return [(0, (4, 128), wa[:, :, n * 128:(n + 1) * 128]),
                (512, (4, 128), wb[:, :, n * 128:(n + 1) * 128])]
    k -= 16
    if k < 4:
        half, kh = divmod(k, 2)
        wo = w_out.rearrange("(kc p) n -> p kc n", p=128)
        return [(0, (4, 512), wo[:, kh * 4:kh * 4 + 4, half * 512:(half + 1) * 512])]
    k -= 4
    if k < 16:
        gch, kh = divmod(k, 2)
        wu = w_up.rearrange("(kc p) n -> p kc n", p=128)
        return [(0, (4, 512), wu[:, kh * 4:kh * 4 + 4, gch * 512:(gch + 1) * 512])]
    k -= 16
    half, q = divmod(k, 8)
    wd = w_down.rearrange("(fc p) n -> p fc n", p=128)
    return [(0, (4, 512), wd[:, q * 4:q * 4 + 4, half * 512:(half + 1) * 512])]


class _Stop(Exception):
    pass


def build_nc(stop=None, dump=()):
    nc = bass.Bass("TRN2", target_bir_lowering=False)
    dr = lambda n, s, kind="ExternalInput", dt=F32: nc.dram_tensor(n, s, dt, kind=kind).ap()
    x = dr("x", [NSEQ, S, D])
    w_in = dr("w_in", [D, INW])
    w_a = dr("w_a", [512, D])
    w_b = dr("w_b", [512, D])
    w_out = dr("w_out", [D, D])
    w_up = dr("w_up", [D, DFF])
    w_down = dr("w_down", [DFF, D])
    g1_d = dr("g1", [128, 8])
    g2_d = dr("g2", [128, 8])
    gv_d = dr("gv", [64, 4])
    sbc_d = dr("sbc", [128, 4])
    ctab = {n: dr(n, list(a.shape)) for n, a in _host_tables().items()}
    y = dr("y", [NSEQ, S, D], kind="ExternalOutput")
    scr = nc.dram_tensor("scr", [NCHUNK, 128, WSZ], BF16).ap()

    with ExitStack() as es:
        sb = lambda n, s, d: es.enter_context(nc.sbuf_tensor(n, s, d))
        KA = sb("KA", [72, 8, S], BF16)
        VA = sb("VA", [128, 16, 4, 192], BF16)
        KB = sb("KB", [64, 2, 1024], BF16)
        VB = sb("VB", [128, 8, 320], BF16)
        QA = sb("QA", [72, 8, T], BF16)
        QB = sb("QB", [64, 8, T], BF16)
        xs = [sb("xs%d" % i, [128, D], F32) for i in range(2)]
        x1 = sb("x1", [128, 4, D], F32)
        hT = sb("hT", [128, 8, T], BF16)
        xhat = [sb("xhat%d" % i, [128, D], BF16) for i in range(2)]
        qkh = [sb("qkh%d" % i, [128, 1664], BF16) for i in range(2)]
        sq = sb("sq", [128, 512], F32)
        PT = [sb("PT%d" % i, [128, 512], BF16) for i in range(4)]
        oaT = sb("oaT", [128, 4, T], BF16)
        obT = sb("obT", [128, 4, T], BF16)
        sga = sb("sga", [128, 512], F32)
        sgb = sb("sgb", [128, 512], F32)
        tmpa = sb("tmpa", [128, 512], F32)
        tmpr = sb("tmpr", [128, 512], F32)
        HM = sb("HM", [128, 32, T], BF16)
        W = [sb("W%d" % i, [128, WSZ], BF16) for i in range(NS)]
        R = sb("R", [128, 512], F32)
        ident = sb("ident", [128, 128], BF16)
        tri = sb("tri", [128, 128], BF16)
        mcur = sb("mcur", [128, 512], BF16)
        mprev = sb("mprev", [128, 512], BF16)
        biasA = sb("biasA", [128, 128], F32)
        biasB = sb("biasB", [128, 16], F32)
        t1 = sb("t1", [128, 512], F32)
        sinkT = sb("sinkT", [128, 512], F32)
        sbc = sb("sbc_s", [128, 4], F32)
        sel = sb("sel", [64, 288], BF16)
        g1 = sb("g1_s", [128, 8], F32)
        g2 = sb("g2_s", [128, 8], F32)
        gv = sb("gv_s", [64, 4], F32)
        st = sb("st", [128, 64], F32)
        ksum = sb("ksum", [64, 8, 8], F32)
        dlt = sb("dlt", [64, 8, 64], BF16)
        ind = sb("ind", [64, 512], BF16)
        bank = [es.enter_context(nc.psum_tensor("pb%d" % i, [128, 512], F32)) for i in range(8)]
        tpbs = [bank[7][:, :].bitcast(BF16), bank[6][:, :].bitcast(BF16)]

        P = Prog()

        def op(eng, fn, r=(), w=(), dma=None):
            return P.op(eng, fn, r, w, dma)

        def mm(out, lhsT, rhs, start, stop, r, w):
            op("pe", lambda e: e.matmul(out, lhsT=lhsT, rhs=rhs, start=start, stop=stop), r, w)

        def act(out, in_, func, r, w, **kw):
            op("act", lambda e: e.activation(out=out, in_=in_, func=func, **kw), r, w)

        def tt(eng, out, in0, in1, alu, r, w):
            op(eng, lambda e: e.tensor_tensor(out=out, in0=in0, in1=in1, op=alu), r, w)

        def ts(eng, out, in0, s1, s2, op0, op1, r, w):
            if op1 is None:
                op(eng, lambda e: e.tensor_scalar(out=out, in0=in0, scalar1=s1, scalar2=None, op0=op0), r, w)
            else:
                op(eng, lambda e: e.tensor_scalar(out=out, in0=in0, scalar1=s1, scalar2=s2, op0=op0, op1=op1), r, w)

        cidx = [0]

        def cload(dst, src, name, eng="pool"):
            cidx[0] += 1
            op(eng, lambda e: e.dma_start(out=dst, in_=src), (), [name], dma="c%d" % cidx[0])

        cload(ident[:], ctab["c_ident"], "ident")
        cload(tri[:], ctab["c_tri"], "tri")
        cload(mcur[:], ctab["c_mcur"], "mcur")
        cload(mprev[:], ctab["c_mprev"], "mprev")
        cload(sel[:], ctab["c_sel"], "sel")
        cload(biasA[:], ctab["c_biasA"], "biasA", "sp")
        cload(biasB[:], ctab["c_biasB"], "biasB", "sp")
        cload(t1[:], ctab["c_t1"], "t1", "sp")
        cload(sbc[:], sbc_d, "sbc", "sp")
        cload(g1[:], g1_d, "g1", "sp")
        cload(g2[:], g2_d, "g2", "sp")
        cload(gv[:], gv_d, "gv", "sp")
        for h in range(8):
            cload(KA[64:72, h, :], ctab["c_E"], "KAE")
        op("dve", lambda e: e.memset(VA[:, :, :, 64:128], 1.0), (), ["VAones"])
        op("dve", lambda e: e.memset(VB[:, :, 0:64], 1.0), (), ["VBones"])
        op("dve", lambda e: e.memset(VB[:, :, 128:192], 1.0), (), ["VBones"])
        op("dve", lambda e: e.memset(VB[:, :, 256:320], 1.0), (), ["VBones"])
        op("dve", lambda e: e.memset(ksum[:], 0.0), (), ["ksum"])
        for j in range(4):
            act(sinkT[:, j * 128:(j + 1) * 128], t1[:, j * 128:(j + 1) * 128], AF.Exp, ["t1", "sbc"], ["sinkT"],
                bias=sbc[:, j:j + 1], scale=1.0)

        seq = [(gt, k) for gt in range(NSEQ * NT) for k in range(NCHUNK)]
        wstate = {"next": 0}

        def wload(idx):
            gt, k = seq[idx]
            slot = idx % NS
            wt = W[slot]
            if gt == 0:
                for (off, (kk, ncol), src) in _chunk_pieces(k, w_in, w_a, w_b, w_out, w_up, w_down):
                    dst = wt[:, off:off + kk * ncol].rearrange("p (a b) -> p a b", b=ncol)
                    op("pool", lambda e, dst=dst, src=src: e.dma_start(out=dst, in_=src), (), ["W%d" % slot], dma="w%d" % slot)
                op("sp", lambda e: e.dma_start(out=scr[k], in_=wt[:]), ["W%d" % slot], ["scr%d" % k], dma="ws%d" % slot)
            else:
                op("sp", lambda e: e.dma_start(out=wt[:], in_=scr[k]), ["scr%d" % k], ["W%d" % slot], dma="w%d" % slot)

        def wneed(idx, ahead=NS - 1):
            lim = min(idx + ahead, len(seq) - 1)
            while wstate["next"] <= lim:
                wload(wstate["next"])
                wstate["next"] += 1
            return W[idx % NS], "W%d" % (idx % NS)

        mmb = [0]

        def next_bank():
            b = mmb[0] % 3
            mmb[0] += 1
            return bank[b], "pb%d" % b

        tph = [0]

        def next_tp():
            h = tph[0] % 2
            tph[0] += 1
            return tpbs[h][:, 0:512], "pb%d" % (7 - h)

        def rmsnorm_T(src_ap, src_name, gain, gname, xh, xhn, dst_cols, si):
            c0 = (si % 2) * 8
            act(xh[:], src_ap, AF.Square, [src_name], [xhn, "st%d" % c0], accum_out=st[:, c0:c0 + 1])
            stage('n1_%d' % si)
            ts("dve", st[:, c0 + 1:c0 + 2], st[:, c0:c0 + 1], 1.0 / D, EPS, ALU.mult, ALU.add, ["st%d" % c0], ["st%d" % (c0 + 1)])
            act(st[:, c0 + 2:c0 + 3], st[:, c0 + 1:c0 + 2], AF.Sqrt, ["st%d" % (c0 + 1)], ["st%d" % (c0 + 2)])
            op("dve", lambda e: e.reciprocal(out=st[:, c0 + 3:c0 + 4], in_=st[:, c0 + 2:c0 + 3]), ["st%d" % (c0 + 2)], ["st%d" % (c0 + 3)])
            stage('n2_%d' % si)
            act(xh[:], src_ap, AF.Copy, [src_name, "st%d" % (c0 + 3)], [xhn], scale=st[:, c0 + 3:c0 + 4])
            stage('n3_%d' % si)
            for half in range(2):
                tp, tpn = next_tp()
                for j in range(4):
                    jj = half * 4 + j
                    op("pe", lambda e, jj=jj, j=j, tp=tp: e.transpose(out=tp[:, j * 128:(j + 1) * 128], in_=xh[:, jj * 128:(jj + 1) * 128], identity=ident[:, :]),
                       [xhn, "ident"], [tpn])
                stage('n4_%d_%d' % (si, half))
                tt("dve", hT[:, half * 4:half * 4 + 4, dst_cols], tp.rearrange("p (j q) -> p j q", q=128),
                   gain[:, half * 4:half * 4 + 4].unsqueeze(2).broadcast_to([128, 4, 128]), ALU.mult, [tpn, gname], ["hT"])
                stage('n5_%d_%d' % (si, half))
            stage('n6_%d' % si)

        def qk_norm(ps, psn, nh, col0, qb_):
            n = nh * 64
            qk = qkh[qb_]
            act(sq[:, 0:n], ps, AF.Square, [psn], ["sq"])
            op("dve", lambda e: e.tensor_reduce(out=st[:, 32:32 + nh], in_=sq[:, 0:n].rearrange("p (h d) -> p h d", d=64), axis=AX.X, op=ALU.add),
               ["sq"], ["stq"])
            ts("dve", st[:, 40:40 + nh], st[:, 32:32 + nh], 1.0 / HD, EPS, ALU.mult, ALU.add, ["stq"], ["stq2"])
            act(st[:, 48:48 + nh], st[:, 40:40 + nh], AF.Sqrt, ["stq2"], ["stq3"])
            op("dve", lambda e: e.reciprocal(out=st[:, 56:56 + nh], in_=st[:, 48:48 + nh]), ["stq3"], ["stq4"])
            tt("dve", qk[:, col0:col0 + n].rearrange("p (h d) -> p h d", d=64), ps.rearrange("p (h d) -> p h d", d=64),
               st[:, 56:56 + nh].unsqueeze(2).broadcast_to([128, nh, 64]), ALU.mult, [psn, "stq4"], ["qkh%d_%d" % (qb_, col0)])

        def head_T(col0, nh, dst_fn, gcol, dname, qb_):
            qk = qkh[qb_]
            for h0 in range(0, nh, 4):
                nhh = min(4, nh - h0)
                tp, tpn = next_tp()
                for j in range(nhh):
                    c = col0 + (h0 + j) * 64
                    op("pe", lambda e, c=c, j=j, tp=tp: e.transpose(out=tp[0:64, j * 128:(j + 1) * 128], in_=qk[:, c:c + 64], identity=ident[:, :]),
                       ["qkh%d_%d" % (qb_, col0), "ident"], [tpn])
                act(dst_fn(h0, nhh), tp[0:64, 0:nhh * 128].rearrange("p (j q) -> p j q", q=128), AF.Copy, [tpn, "gv"], [dname],
                    scale=gv[:, gcol:gcol + 1])

        ychunk = [0]
        ytok = None

        def stage(n):
            if stop == n:
                raise _Stop()

        widx = [0]

        try:
          stage('const')
          for s in range(NSEQ):
            for c in range(NT):
                T0 = c * T
                base = widx[0]
                if c == 0:
                    op("pool", lambda e: e.dma_start(out=QA[64:72, :, :].rearrange("p h q -> p (h q)"), in_=ctab["c_zero"]), (), ["QAm"], dma="qam")
                for i in range(4):
                    xb = xs[i % 2]
                    xn = "xs%d" % (i % 2)
                    src = x[s, T0 + i * 128:T0 + (i + 1) * 128, :]
                    op("sp", lambda e, xb=xb, src=src: e.dma_start(out=xb[:], in_=src), (), [xn], dma=xn)
                    rmsnorm_T(xb[:], xn, g1, "g1", xhat[i % 2], "xhat%d" % (i % 2), slice(i * 128, (i + 1) * 128), i)
                stage('A0')
                srcx = x[s, T0:T0 + T, :].rearrange("(i p) d -> p i d", p=128)
                op("sp", lambda e, srcx=srcx: e.dma_start(out=x1[:], in_=srcx), (), ["x1"], dma="x1")
                pend = [None]

                def flush():
                    if pend[0] is not None:
                        pend[0]()
                        pend[0] = None

                gi = 0
                for g in range(5):
                    w0, w0n = wneed(base + 2 * g)
                    w1, w1n = wneed(base + 2 * g + 1, NS - 2)
                    ncol = 512 if g < 4 else 256
                    for i in range(4):
                        pb, pbn = next_bank()
                        kt = 4 * c + i
                        qb_ = gi % 2
                        gi += 1
                        for k in range(8):
                            ww = (w0 if k < 4 else w1)[:, (k % 4) * ncol:(k % 4 + 1) * ncol]
                            mm(pb[:, 0:ncol], hT[:, k, i * 128:(i + 1) * 128], ww, k == 0, k == 7,
                               ["hT", w0n if k < 4 else w1n], [pbn])
                        qc = slice(i * 128, (i + 1) * 128)
                        nxt = None
                        if g == 0:
                            qk_norm(pb[:, 0:512], pbn, 8, 0, qb_)
                            nxt = lambda qc=qc, qb_=qb_: head_T(0, 8, lambda h0, n: QA[0:64, h0:h0 + n, qc], 0, "QA", qb_)
                        elif g == 1:
                            qk_norm(pb[:, 0:512], pbn, 8, 512, qb_)
                            kc = slice(T0 + i * 128, T0 + (i + 1) * 128)
                            nxt = lambda kc=kc, qb_=qb_: head_T(512, 8, lambda h0, n: KA[0:64, h0:h0 + n, kc], 1, "KA", qb_)
                        elif g == 2:
                            pv = pb[:, 0:512].rearrange("p (pr two d) -> p pr two d", two=2, d=64)
                            act(VA[:, kt, :, 0:64], pv[:, :, 0, :], AF.Copy, [pbn], ["VA"])
                            act(VA[:, kt, :, 128:192], pv[:, :, 1, :], AF.Copy, [pbn], ["VA"])
                        elif g == 3:
                            qk_norm(pb[:, 0:512], pbn, 8, 1024, qb_)
                            nxt = lambda qc=qc, qb_=qb_: head_T(1024, 8, lambda h0, n: QB[0:64, h0:h0 + n, qc], 2, "QB", qb_)
                        else:
                            qk_norm(pb[:, 0:128], pbn, 2, 1536, qb_)
                            ks = kt % 8
                            nxt = lambda ks=ks, qb_=qb_: head_T(1536, 2, lambda h0, n: KB[0:64, h0:h0 + n, ks * 128:(ks + 1) * 128], 3, "KB", qb_)
                            act(VB[:, ks, 64:128], pb[:, 128:192], AF.Copy, [pbn], ["VB"])
                            act(VB[:, ks, 192:256], pb[:, 192:256], AF.Copy, [pbn], ["VB"])
                        flush()
                        pend[0] = nxt
                flush()
                stage('A')
                for h in range(8):
                    ko = ksum[:, h, 2 * c:2 * c + 2]
                    ki = KA[0:64, h, T0:T0 + T].rearrange("p (n k) -> p n k", k=256)
                    op("dve", lambda e, ko=ko, ki=ki: e.tensor_reduce(out=ko, in_=ki, axis=AX.X, op=ALU.add), ["KA"], ["ksum"])
                if c >= 2:
                    for h in range(8):
                        tt("dve", dlt[:, h, :].rearrange("p (n m) -> p n m", m=8),
                           ksum[:, h, :].unsqueeze(2).broadcast_to([64, 8, 8]),
                           ksum[:, h, :].unsqueeze(1).broadcast_to([64, 8, 8]), ALU.subtract, ["ksum"], ["dlt%d" % h])
                        gb_, gbn = bank[3 + h % 2], "pb%d" % (3 + h % 2)
                        mm(gb_[0:64, :], dlt[:, h, :], QA[0:64, h, :], True, True, ["dlt%d" % h, "QA"], [gbn])
                        op("dve", lambda e, gb_=gb_: e.tensor_single_scalar(out=ind[:, :], in_=gb_[0:64, :], scalar=0.0, op=ALU.is_lt), [gbn], ["ind"])
                        for hb in range(2):
                            bi = 2 * c + hb - 4
                            mm(gb_[0:72, hb * 256:(hb + 1) * 256], sel[:, bi * 72:(bi + 1) * 72], ind[:, hb * 256:(hb + 1) * 256], True, True,
                               ["sel", "ind"], [gbn])
                        ts("dve", QA[64:72, h, :], gb_[64:72, :], 2.5, NEGM, ALU.is_ge, ALU.mult, [gbn], ["QAm"])
                stage('gate%d' % c)
                steps = [(h, j) for h in range(8) for j in range(4 * c + 4)]
                nst = len(steps)

                def moba_S(si):
                    h, j = steps[si]
                    qs = 128 * max(0, j - 4 * c)
                    N = T - qs
                    sbk, sbn = bank[3 + si % 2], "pb%d" % (3 + si % 2)
                    diag = j >= 4 * c
                    mm(sbk[:, 0:N], KA[0:72, h, j * 128:(j + 1) * 128], QA[0:72, h, qs:T], True, not diag,
                       ["KA", "KAE", "QA", "QAm"], [sbn])
                    if diag:
                        mm(sbk[:, 0:128], ident[:, :], tri[:, :], False, True, ["ident", "tri"], [sbn])
                    di = 4 * c - j + 3
                    pt = PT[si % 4]
                    act(pt[:, 0:N], sbk[:, 0:N], AF.Exp, [sbn, "biasA"], ["PT%d" % (si % 4)],
                        bias=biasA[:, h * 16 + di:h * 16 + di + 1], scale=SCALE)

                def moba_PV(si):
                    h, j = steps[si]
                    qs = 128 * max(0, j - 4 * c)
                    N = T - qs
                    ob_, obn = bank[5 + h % 2], "pb%d" % (5 + h % 2)
                    pr, od = divmod(h, 2)
                    lhsT = VA[:, j, pr, od * 64:od * 64 + 128]
                    mm(ob_[:, qs:T], lhsT, PT[si % 4][:, 0:N], j == 0, j == 4 * c + 3, ["VA", "VAones", "PT%d" % (si % 4)], [obn])
                    if j == 4 * c + 3:
                        lo, hi = (0, 64) if od == 0 else (64, 128)
                        dl, dh = (64, 128) if od == 0 else (0, 64)
                        act(R[dl:dh, :], ob_[dl:dh, :], AF.Copy, [obn], ["R"])
                        op("dve", lambda e: e.reciprocal(out=R[dl:dh, :], in_=R[dl:dh, :]), ["R"], ["R"])
                        tt("dve", oaT[lo:hi, pr, :], ob_[lo:hi, :], R[dl:dh, :], ALU.mult, [obn, "R"], ["oaT"])

                moba_S(0)
                for si in range(nst):
                    if si + 1 < nst:
                        moba_S(si + 1)
                    moba_PV(si)
                stage('B')
                sw = []
                for i in range(4):
                    kt = 4 * c + i
                    for g in range(2):
                        passes = [(kt % 8, mcur, "mcur", 0)] + ([((kt - 1) % 8, mprev, "mprev", 1)] if kt >= 1 else [])
                        for pi, (kslot, mk, mkn, which) in enumerate(passes):
                            sw.append((i, g, pi, len(passes), kslot, mk, mkn, which))

                def swa_S(n):
                    i, g, pi, npass, kslot, mk, mkn, which = sw[n]
                    sbk, sbn = bank[3 + n % 2], "pb%d" % (3 + n % 2)
                    mm(sbk[:, :], KB[0:64, g, kslot * 128:(kslot + 1) * 128], QB[0:64, 4 * g:4 * g + 4, i * 128:(i + 1) * 128],
                       True, False, ["KB", "QB"], [sbn])
                    mm(sbk[:, :], ident[:, :], mk[:, :], False, True, ["ident", mkn], [sbn])
                    pt = PT[n % 4]
                    for hh in range(4):
                        h = 4 * g + hh
                        act(pt[:, hh * 128:(hh + 1) * 128], sbk[:, hh * 128:(hh + 1) * 128], AF.Exp, [sbn, "biasB"], ["PT%d" % (n % 4)],
                            bias=biasB[:, h * 2 + which:h * 2 + which + 1], scale=SCALE)

                def swa_PV(n):
                    i, g, pi, npass, kslot, mk, mkn, which = sw[n]
                    ob_, obn = bank[5 + (i * 2 + g) % 2], "pb%d" % (5 + (i * 2 + g) % 2)
                    pt = PT[n % 4]
                    ptn = "PT%d" % (n % 4)
                    pt3 = pt[:, :].rearrange("p (a e q) -> p a e q", e=2, q=128)
                    last = pi == npass - 1
                    mm(ob_[:, 0:256], VB[:, kslot, 64 + 128 * g:64 + 128 * g + 128], pt3[:, :, 0, :], pi == 0, last,
                       ["VB", "VBones", ptn], [obn])
                    mm(ob_[:, 256:512], VB[:, kslot, 128 * g:128 * g + 128], pt3[:, :, 1, :], False, last,
                       ["VB", "VBones", ptn], [obn])
                    if not last:
                        return
                    tt("dve", R[64:128, 0:256], ob_[64:128, 0:256], sinkT[64:128, g * 256:(g + 1) * 256], ALU.add, [obn, "sinkT"], ["R"])
                    tt("dve", R[0:64, 256:512], ob_[0:64, 256:512], sinkT[0:64, g * 256:(g + 1) * 256], ALU.add, [obn, "sinkT"], ["R"])
                    op("dve", lambda e: e.reciprocal(out=R[64:128, 0:256], in_=R[64:128, 0:256]), ["R"], ["R"])
                    op("dve", lambda e: e.reciprocal(out=R[0:64, 256:512], in_=R[0:64, 256:512]), ["R"], ["R"])
                    qc = slice(i * 128, (i + 1) * 128)
                    tt("dve", obT[0:64, 2 * g:2 * g + 2, qc], ob_[0:64, 0:256].rearrange("p (a q) -> p a q", q=128),
                       R[64:128, 0:256].rearrange("p (a q) -> p a q", q=128), ALU.mult, [obn, "R"], ["obT"])
                    tt("dve", obT[64:128, 2 * g:2 * g + 2, qc], ob_[64:128, 256:512].rearrange("p (a q) -> p a q", q=128),
                       R[0:64, 256:512].rearrange("p (a q) -> p a q", q=128), ALU.mult, [obn, "R"], ["obT"])

                swa_S(0)
                for n in range(len(sw)):
                    if n + 1 < len(sw):
                        swa_S(n + 1)
                    swa_PV(n)
                stage('C')
                for n in range(8):
                    wg, wgn = wneed(base + 10 + 2 * n)
                    wab, wabn = wneed(base + 10 + 2 * n + 1, NS - 2)
                    pa, pan = next_bank()
                    for k in range(8):
                        mm(pa[:, :], wg[:, k * 128:(k + 1) * 128], hT[:, k, :], k == 0, k == 7, [wgn, "hT"], [pan])
                    act(sga[:], pa[:, :], AF.Sigmoid, [pan], ["sga"])
                    pb2, pb2n = next_bank()
                    for k in range(8):
                        mm(pb2[:, :], wg[:, 1024 + k * 128:1024 + (k + 1) * 128], hT[:, k, :], k == 0, k == 7, [wgn, "hT"], [pb2n])
                    act(sgb[:], pb2[:, :], AF.Sigmoid, [pb2n], ["sgb"])
                    pc, pcn = next_bank()
                    for k in range(4):
                        mm(pc[:, :], wab[:, k * 128:(k + 1) * 128], oaT[:, k, :], k == 0, k == 3, [wabn, "oaT"], [pcn])
                    tt("dve", tmpa[:], pc[:, :], sga[:], ALU.mult, [pcn, "sga"], ["tmpa"])
                    pd, pdn = next_bank()
                    for k in range(4):
                        mm(pd[:, :], wab[:, 512 + k * 128:512 + (k + 1) * 128], obT[:, k, :], k == 0, k == 3, [wabn, "obT"], [pdn])
                    tt("dve", sgb[:], pd[:, :], sgb[:], ALU.mult, [pdn, "sgb"], ["sgb"])
                    tt("dve", HM[:, n, :], tmpa[:], sgb[:], ALU.add, ["tmpa", "sgb"], ["hid%d" % n])
                stage('D')
                for half in range(2):
                    w0, w0n = wneed(base + 26 + 2 * half)
                    w1, w1n = wneed(base + 26 + 2 * half + 1, NS - 2)
                    for i in range(4):
                        pb, pbn = next_bank()
                        for k in range(8):
                            ww = (w0 if k < 4 else w1)[:, (k % 4) * 512:(k % 4 + 1) * 512]
                            mm(pb[:, :], HM[:, k, i * 128:(i + 1) * 128], ww, k == 0, k == 7, ["hid%d" % k, w0n if k < 4 else w1n], [pbn])
                        xo = x1[:, i, half * 512:(half + 1) * 512]
                        tt("dve", xo, pb[:, :], xo, ALU.add, [pbn, "x1"], ["x1"])
                stage('E')
                for i in range(4):
                    rmsnorm_T(x1[:, i, :], "x1", g2, "g2", xhat[i % 2], "xhat%d" % (i % 2), slice(i * 128, (i + 1) * 128), 4 + i)
                stage('F')
                for gch in range(8):
                    w0, w0n = wneed(base + 30 + 2 * gch)
                    w1, w1n = wneed(base + 30 + 2 * gch + 1, NS - 2)
                    for f in range(4):
                        pb, pbn = next_bank()
                        for k in range(8):
                            ww = (w0 if k < 4 else w1)[:, (k % 4) * 512 + f * 128:(k % 4) * 512 + (f + 1) * 128]
                            mm(pb[:, :], ww, hT[:, k, :], k == 0, k == 7, ["hT", w0n if k < 4 else w1n], [pbn])
                        act(tmpr[:], pb[:, :], AF.Relu, [pbn], ["tmpr"])
                        tt("dve", HM[:, gch * 4 + f, :], pb[:, :], tmpr[:], ALU.mult, [pbn, "tmpr"], ["hid%d" % (gch * 4 + f)])
                stage('G')
                for half in range(2):
                    for q in range(8):
                        wd, wdn = wneed(base + 46 + half * 8 + q)
                        for f in range(4):
                            ff = q * 4 + f
                            for i in range(4):
                                mm(bank[i][:, :], HM[:, ff, i * 128:(i + 1) * 128], wd[:, f * 512:(f + 1) * 512], ff == 0, ff == 31,
                                   ["hid%d" % ff, wdn], ["pb%d" % i])
                    for i in range(4):
                        xo = x1[:, i, half * 512:(half + 1) * 512]
                        tt("dve", xo, bank[i][:, :], xo, ALU.add, ["pb%d" % i, "x1"], ["x1"])
                dsty = y[s, T0:T0 + T, :].rearrange("(i p) d -> p i d", p=128)
                ytok = op("pool", lambda e, dsty=dsty: e.dma_start(out=dsty, in_=x1[:]), ["x1"], (), dma="y")
                widx[0] += NCHUNK
                stage('T%d' % (s * NT + c))
        except _Stop:
            pass
        if ytok is not None:
            P.final_wait("pool", [ytok])
        if dump:
            tens = {"hT": hT, "QA": QA, "KA": KA, "VA": VA, "QB": QB, "KB": KB, "VB": VB, "oaT": oaT, "obT": obT,
                    "HM": HM, "x1": x1, "ksum": ksum, "sinkT": sinkT, "R": R, "st": st, "qkh": qkh[0], "ind": ind, "dlt": dlt}
            allb = list(P.bufs.keys())
            dtoks = []
            for nm in dump:
                tns = tens[nm]
                shp = list(tns.shape)
                flat = tns[:]
                if len(shp) == 3:
                    flat = flat.rearrange("p a b -> p (a b)")
                elif len(shp) == 4:
                    flat = flat.rearrange("p a b c -> p (a b c)")
                fr = int(np.prod(shp[1:]))
                dd = nc.dram_tensor("dbg_" + nm, [shp[0], fr], tns.dtype, kind="ExternalOutput").ap()
                dtoks.append(op("sp", lambda e, dd=dd, flat=flat: e.dma_start(out=dd, in_=flat), allb, (), dma="dbg_" + nm))
            P.final_wait("sp", dtoks)
        sems = {n: es.enter_context(nc.semaphore(n)) for n in sorted(P.semnames)}
        with nc.Block() as block:
            P.emit(block, sems)
    return nc


_NC_CACHE = {}


def kernel(x, norm_attn, w_in, q_norm_a, k_norm_a, q_norm_b, k_norm_b, sinks_b,
           w_branch_a, w_branch_b, w_out, norm_mlp, w_up, w_down):
    f = lambda a: np.ascontiguousarray(np.asarray(a, dtype=np.float32))
    x = f(x)
    tabs = _host_tables()
    g1 = f(np.asarray(norm_attn)[0].reshape(8, 128).T)
    g2 = f(np.asarray(norm_mlp)[0].reshape(8, 128).T)
    gv = f(np.stack([np.asarray(q_norm_a)[0], np.asarray(k_norm_a)[0], np.asarray(q_norm_b)[0], np.asarray(k_norm_b)[0]], axis=1))
    sk = np.asarray(sinks_b)[0]
    sbc = np.zeros((128, 4), np.float32)
    for g in range(2):
        for a in range(2):
            sbc[64:, 2 * g + a] = sk[4 * g + 2 * a]
            sbc[:64, 2 * g + a] = sk[4 * g + 2 * a + 1]
    common = {
        "w_in": f(np.asarray(w_in)[0]), "w_a": f(np.asarray(w_branch_a)[0]), "w_b": f(np.asarray(w_branch_b)[0]),
        "w_out": f(np.asarray(w_out)[0]), "w_up": f(np.asarray(w_up)[0]), "w_down": f(np.asarray(w_down)[0]),
        "g1": g1, "g2": g2, "gv": gv, "sbc": sbc,
    }
    common.update(tabs)
    if "nc" not in _NC_CACHE:
        _NC_CACHE["nc"] = build_nc()
    nc = _NC_CACHE["nc"]
    in_maps = []
    for c in range(NCORES):
        m = dict(common)
        m["x"] = np.ascontiguousarray(x[NSEQ * c:NSEQ * (c + 1)])
        in_maps.append(m)
    res = run_bass_kernel_spmd(nc, in_maps, core_ids=list(range(NCORES)))
    return np.concatenate([r["y"] for r in res.results], axis=0).astype(np.float32)
```

```python
import os
import numpy as np
from contextlib import ExitStack
import concourse.bass as bass
import concourse.mybir as mybir
from concourse.bass_utils import run_bass_kernel_spmd

F32 = mybir.dt.float32
BF16 = mybir.dt.bfloat16
AF = mybir.ActivationFunctionType
ALU = mybir.AluOpType
AX = mybir.AxisListType

NCORES = 8
D = 1024
S = 2048
NSEQ = 2
T = 512
NT = S // T
HD = 64
DFF = 4096
INW = 4352
NEGM = -30000.0
EPS = 1e-6
SCALE = HD ** -0.5
NS = 4
WSZ = 2048
NCHUNK = 62
ENGS = ("pe", "dve", "act", "pool", "sp")


class Buf:
    __slots__ = ("name", "last_w", "readers")

    def __init__(self, name):
        self.name = name
        self.last_w = None
        self.readers = {}


class Prog:
    def __init__(self):
        self.ops = {e: [] for e in ENGS}
        self.cnt = {e: 0 for e in ENGS}
        self.dcnt = {}
        self.waited = {e: {} for e in ENGS}
        self.semnames = set("e_" + e for e in ENGS)
        self.bufs = {}

    def buf(self, name):
        b = self.bufs.get(name)
        if b is None:
            b = self.bufs[name] = Buf(name)
        return b

    def op(self, eng, fn, reads=(), writes=(), dma=None):
        reads = [self.buf(r) for r in reads]
        writes = [self.buf(w) for w in writes]
        own = "e_" + eng
        need = {}

        def add(sem, val, same_ok):
            if sem == own:
                if eng == "pe" or not same_ok:
                    return
            if need.get(sem, 0) < val:
                need[sem] = val

        for b in reads:
            if b.last_w is not None:
                add(b.last_w[0], b.last_w[1], True)
            if b.name.startswith("pb"):
                for s, v in b.readers.items():
                    add(s, v, False)
        for b in writes:
            if b.last_w is not None:
                add(b.last_w[0], b.last_w[1], True)
            for s, v in b.readers.items():
                add(s, v, False)
        waits = []
        wd = self.waited[eng]
        for s, v in need.items():
            if wd.get(s, 0) < v:
                wd[s] = v
                waits.append((s, v))
        if dma is None:
            self.cnt[eng] += 1
            tok = (own, self.cnt[eng])
            inc = (own, 1)
        else:
            sname = "d_" + dma
            self.semnames.add(sname)
            self.dcnt[sname] = self.dcnt.get(sname, 0) + 16
            tok = (sname, self.dcnt[sname])
            inc = (sname, 16)
        self.ops[eng].append((waits, fn, inc))
        for b in writes:
            b.last_w = tok
            b.readers = {}
        for b in reads:
            if b.readers.get(tok[0], 0) < tok[1]:
                b.readers[tok[0]] = tok[1]
        return tok

    def final_wait(self, eng, toks):
        self.ops[eng].append((list(toks), None, None))

    def emit(self, block, sems):
        engmap = {"pe": "tensor", "dve": "vector", "act": "scalar", "pool": "gpsimd", "sp": "sync"}
        for e in ENGS:
            ops = self.ops[e]
            if not ops:
                continue

            def body(engine, ops=ops):
                for waits, fn, inc in ops:
                    for s, v in waits:
                        engine.wait_ge(sems[s], v)
                    if fn is not None:
                        fn(engine).then_inc(sems[inc[0]], inc[1])

            getattr(block, engmap[e])(body)


def _slopes():
    sl = np.exp2(-(8.0 / 16) * np.arange(1, 17, dtype=np.float32)).astype(np.float32)
    return sl[:8], sl[8:]


def _host_tables():
    sl_b, sl_a = _slopes()
    p = np.arange(128, dtype=np.float32)
    k = p[:, None]
    q = p[None, :]
    tri = np.where(k <= q, 0.0, NEGM).astype(np.float32)
    prv = np.where(k > q, 0.0, NEGM).astype(np.float32)
    t = {}
    t["c_ident"] = np.eye(128, dtype=np.float32)
    t["c_tri"] = tri
    t["c_mcur"] = np.tile(tri, (1, 4))
    t["c_mprev"] = np.tile(prv, (1, 4))
    bA = np.zeros((128, 128), np.float32)
    for h in range(8):
        for di in range(16):
            bA[:, h * 16 + di] = sl_a[h] * (p - 128.0 * (di - 3))
    t["c_biasA"] = bA
    bB = np.zeros((128, 16), np.float32)
    for h in range(8):
        bB[:, h * 2 + 0] = sl_b[h] * (p - 64.0)
        bB[:, h * 2 + 1] = sl_b[h] * (p - 192.0)
    t["c_biasB"] = bB
    t1 = np.zeros((128, 2, 2, 128), np.float32)
    for g in range(2):
        for e in range(2):
            t1[64:, g, e, :] = sl_b[4 * g + 2 * e] * (p - 64.0)
            t1[:64, g, e, :] = sl_b[4 * g + 2 * e + 1] * (p - 64.0)
    t["c_t1"] = t1.reshape(128, 512)
    sel = np.zeros((64, 4, 72), np.float32)
    for bi in range(4):
        b = 4 + bi
        for n in range(b):
            for m in range(b):
                if n != m:
                    sel[n * 8 + m, bi, 64 + n] = 1.0
    t["c_sel"] = sel.reshape(64, 288)
    E = np.zeros((8, S), np.float32)
    for n in range(8):
        E[n, n * 256:(n + 1) * 256] = 1.0
    t["c_E"] = E
    t["c_zero"] = np.zeros((8, 8 * T), np.float32)
    return t


def _chunk_pieces(k, w_in, w_a, w_b, w_out, w_up, w_down):
    win = w_in.rearrange("(kc p) n -> p kc n", p=128)
    if k < 10:
        g, kh = divmod(k, 2)
        nc_ = 512 if g < 4 else 256
        return [(0, (4, nc_), win[:, kh * 4:kh * 4 + 4, g * 512:g * 512 + nc_])]
    k -= 10
    if k < 16:
        n, part = divmod(k, 2)
        if part == 0:
            return [(0, (8, 128), win[:, :, 2304 + n * 128:2304 + (n + 1) * 128]),
                    (1024, (8, 128), win[:, :, 3328 + n * 128:3328 + (n + 1) * 128])]
        wa = w_a.rearrange("(kc p) n -> p kc n", p=128)
        wb = w_b.rearrange("(kc p) n -> p kc n", p=128)
        return [(0, (4, 128), wa[:, :, n * 128:(n + 1) * 128]),
                (512, (4, 128), wb[:, :, n * 128:(n + 1) * 128])]
    k -= 16
    if k < 4:
        half, kh = divmod(k, 2)
        wo = w_out.rearrange("(kc p) n -> p kc n", p=128)
        return [(0, (4, 512), wo[:, kh * 4:kh * 4 + 4, half * 512:(half + 1) * 512])]
    k -= 4
    if k < 16:
        gch, kh = divmod(k, 2)
        wu = w_up.rearrange("(kc p) n -> p kc n", p=128)
        return [(0, (4, 512), wu[:, kh * 4:kh * 4 + 4, gch * 512:(gch + 1) * 512])]
    k -= 16
    half, q = divmod(k, 8)
    wd = w_down.rearrange("(fc p) n -> p fc n", p=128)
    return [(0, (4, 512), wd[:, q * 4:q * 4 + 4, half * 512:(half + 1) * 512])]


class _Stop(Exception):
    pass


def build_nc(stop=None, dump=()):
    nc = bass.Bass("TRN2", target_bir_lowering=False)
    dr = lambda n, s, kind="ExternalInput", dt=F32: nc.dram_tensor(n, s, dt, kind=kind).ap()
    x = dr("x", [NSEQ, S, D])
    w_in = dr("w_in", [D, INW])
    w_a = dr("w_a", [512, D])
    w_b = dr("w_b", [512, D])
    w_out = dr("w_out", [D, D])
    w_up = dr("w_up", [D, DFF])
    w_down = dr("w_down", [DFF, D])
    g1_d = dr("g1", [128, 8])
    g2_d = dr("g2", [128, 8])
    gv_d = dr("gv", [64, 4])
    sbc_d = dr("sbc", [128, 4])
    ctab = {n: dr(n, list(a.shape)) for n, a in _host_tables().items()}
    y = dr("y", [NSEQ, S, D], kind="ExternalOutput")
    scr = nc.dram_tensor("scr", [NCHUNK, 128, WSZ], BF16).ap()

    with ExitStack() as es:
        sb = lambda n, s, d: es.enter_context(nc.sbuf_tensor(n, s, d))
        KA = sb("KA", [72, 8, S], BF16)
        VA = sb("VA", [128, 16, 4, 192], BF16)
        KB = sb("KB", [64, 2, 1024], BF16)
        VB = sb("VB", [128, 8, 320], BF16)
        QA = sb("QA", [72, 8, T], BF16)
        QB = sb("QB", [64, 8, T], BF16)
        xs = [sb("xs%d" % i, [128, D], F32) for i in range(2)]
        x1 = sb("x1", [128, 4, D], F32)
        hT = sb("hT", [128, 8, T], BF16)
        xhat = [sb("xhat%d" % i, [128, D], BF16) for i in range(2)]
        qkh = [sb("qkh%d" % i, [128, 1664], BF16) for i in range(2)]
        sq = sb("sq", [128, 512], F32)
        PT = [sb("PT%d" % i, [128, 512], BF16) for i in range(4)]
        oaT = sb("oaT", [128, 4, T], BF16)
        obT = sb("obT", [128, 4, T], BF16)
        sga = sb("sga", [128, 512], F32)
        sgb = sb("sgb", [128, 512], F32)
        tmpa = sb("tmpa", [128, 512], F32)
        tmpr = sb("tmpr", [128, 512], F32)
        HM = sb("HM", [128, 32, T], BF16)
        W = [sb("W%d" % i, [128, WSZ], BF16) for i in range(NS)]
        R = sb("R", [128, 512], F32)
        ident = sb("ident", [128, 128], BF16)
        tri = sb("tri", [128, 128], BF16)
        mcur = sb("mcur", [128, 512], BF16)
        mprev = sb("mprev", [128, 512], BF16)
        biasA = sb("biasA", [128, 128], F32)
        biasB = sb("biasB", [128, 16], F32)
        t1 = sb("t1", [128, 512], F32)
        sinkT = sb("sinkT", [128, 512], F32)
        sbc = sb("sbc_s", [128, 4], F32)
        sel = sb("sel", [64, 288], BF16)
        g1 = sb("g1_s", [128, 8], F32)
        g2 = sb("g2_s", [128, 8], F32)
        gv = sb("gv_s", [64, 4], F32)
        st = sb("st", [128, 64], F32)
        ksum = sb("ksum", [64, 8, 8], F32)
        dlt = sb("dlt", [64, 8, 64], BF16)
        ind = [sb("ind%d" % i, [64, 512], BF16) for i in range(2)]
        bank = [es.enter_context(nc.psum_tensor("pb%d" % i, [128, 512], F32)) for i in range(8)]
        tpbs = [bank[7][:, :].bitcast(BF16), bank[6][:, :].bitcast(BF16)]

        P = Prog()

        def op(eng, fn, r=(), w=(), dma=None):
            return P.op(eng, fn, r, w, dma)

        def mm(out, lhsT, rhs, start, stop, r, w):
            op("pe", lambda e: e.matmul(out, lhsT=lhsT, rhs=rhs, start=start, stop=stop), r, w)

        def act(out, in_, func, r, w, **kw):
            op("act", lambda e: e.activation(out=out, in_=in_, func=func, **kw), r, w)

        def tt(eng, out, in0, in1, alu, r, w):
            op(eng, lambda e: e.tensor_tensor(out=out, in0=in0, in1=in1, op=alu), r, w)

        def ts(eng, out, in0, s1, s2, op0, op1, r, w):
            if op1 is None:
                op(eng, lambda e: e.tensor_scalar(out=out, in0=in0, scalar1=s1, scalar2=None, op0=op0), r, w)
            else:
                op(eng, lambda e: e.tensor_scalar(out=out, in0=in0, scalar1=s1, scalar2=s2, op0=op0, op1=op1), r, w)

        cidx = [0]

        def cload(dst, src, name, eng="pool"):
            cidx[0] += 1
            op(eng, lambda e: e.dma_start(out=dst, in_=src), (), [name], dma="c%d" % cidx[0])

        cload(ident[:], ctab["c_ident"], "ident")
        cload(tri[:], ctab["c_tri"], "tri")
        cload(mcur[:], ctab["c_mcur"], "mcur")
        cload(mprev[:], ctab["c_mprev"], "mprev")
        cload(sel[:], ctab["c_sel"], "sel")
        cload(biasA[:], ctab["c_biasA"], "biasA", "sp")
        cload(biasB[:], ctab["c_biasB"], "biasB", "sp")
        cload(t1[:], ctab["c_t1"], "t1", "sp")
        cload(sbc[:], sbc_d, "sbc", "sp")
        cload(g1[:], g1_d, "g1", "sp")
        cload(g2[:], g2_d, "g2", "sp")
        cload(gv[:], gv_d, "gv", "sp")
        for h in range(8):
            cload(KA[64:72, h, :], ctab["c_E"], "KAE")
        op("dve", lambda e: e.memset(VA[:, :, :, 64:128], 1.0), (), ["VAones"])
        op("dve", lambda e: e.memset(VB[:, :, 0:64], 1.0), (), ["VBones"])
        op("dve", lambda e: e.memset(VB[:, :, 128:192], 1.0), (), ["VBones"])
        op("dve", lambda e: e.memset(VB[:, :, 256:320], 1.0), (), ["VBones"])
        op("dve", lambda e: e.memset(ksum[:], 0.0), (), ["ksum"])
        for j in range(4):
            act(sinkT[:, j * 128:(j + 1) * 128], t1[:, j * 128:(j + 1) * 128], AF.Exp, ["t1", "sbc"], ["sinkT"],
                bias=sbc[:, j:j + 1], scale=1.0)

        seq = [(gt, k) for gt in range(NSEQ * NT) for k in range(NCHUNK)]
        wstate = {"next": 0}

        def wload(idx):
            gt, k = seq[idx]
            slot = idx % NS
            wt = W[slot]
            if gt == 0:
                for (off, (kk, ncol), src) in _chunk_pieces(k, w_in, w_a, w_b, w_out, w_up, w_down):
                    dst = wt[:, off:off + kk * ncol].rearrange("p (a b) -> p a b", b=ncol)
                    op("pool", lambda e, dst=dst, src=src: e.dma_start(out=dst, in_=src), (), ["W%d" % slot], dma="w%d" % slot)
                op("sp", lambda e: e.dma_start(out=scr[k], in_=wt[:]), ["W%d" % slot], ["scr%d" % k], dma="ws%d" % slot)
            else:
                op("sp", lambda e: e.dma_start(out=wt[:], in_=scr[k]), ["scr%d" % k], ["W%d" % slot], dma="w%d" % slot)

        def wneed(idx, ahead=NS - 1):
            lim = min(idx + ahead, len(seq) - 1)
            while wstate["next"] <= lim:
                wload(wstate["next"])
                wstate["next"] += 1
            return W[idx % NS], "W%d" % (idx % NS)

        mmb = [0]

        def next_bank():
            b = mmb[0] % 3
            mmb[0] += 1
            return bank[b], "pb%d" % b

        tph = [0]

        def next_tp():
            h = tph[0] % 2
            tph[0] += 1
            return tpbs[h][:, 0:512], "pb%d" % (7 - h)

        def rmsnorm_T(src_ap, src_name, gain, gname, xh, xhn, dst_cols, si):
            c0 = (si % 2) * 8
            act(xh[:], src_ap, AF.Square, [src_name], [xhn, "st%d" % c0], accum_out=st[:, c0:c0 + 1])
            stage('n1_%d' % si)
            ts("dve", st[:, c0 + 1:c0 + 2], st[:, c0:c0 + 1], 1.0 / D, EPS, ALU.mult, ALU.add, ["st%d" % c0], ["st%d" % (c0 + 1)])
            act(st[:, c0 + 2:c0 + 3], st[:, c0 + 1:c0 + 2], AF.Sqrt, ["st%d" % (c0 + 1)], ["st%d" % (c0 + 2)])
            op("dve", lambda e: e.reciprocal(out=st[:, c0 + 3:c0 + 4], in_=st[:, c0 + 2:c0 + 3]), ["st%d" % (c0 + 2)], ["st%d" % (c0 + 3)])
            stage('n2_%d' % si)
            act(xh[:], src_ap, AF.Copy, [src_name, "st%d" % (c0 + 3)], [xhn], scale=st[:, c0 + 3:c0 + 4])
            stage('n3_%d' % si)
            for half in range(2):
                tp, tpn = next_tp()
                for j in range(4):
                    jj = half * 4 + j
                    op("pe", lambda e, jj=jj, j=j, tp=tp: e.transpose(out=tp[:, j * 128:(j + 1) * 128], in_=xh[:, jj * 128:(jj + 1) * 128], identity=ident[:, :]),
                       [xhn, "ident"], [tpn])
                stage('n4_%d_%d' % (si, half))
                tt("dve", hT[:, half * 4:half * 4 + 4, dst_cols], tp.rearrange("p (j q) -> p j q", q=128),
                   gain[:, half * 4:half * 4 + 4].unsqueeze(2).broadcast_to([128, 4, 128]), ALU.mult, [tpn, gname], ["hT"])
                stage('n5_%d_%d' % (si, half))
            stage('n6_%d' % si)

        def qk_norm(ps, psn, nh, col0, qb_):
            n = nh * 64
            qk = qkh[qb_]
            act(sq[:, 0:n], ps, AF.Square, [psn], ["sq"])
            op("dve", lambda e: e.tensor_reduce(out=st[:, 32:32 + nh], in_=sq[:, 0:n].rearrange("p (h d) -> p h d", d=64), axis=AX.X, op=ALU.add),
               ["sq"], ["stq"])
            ts("dve", st[:, 40:40 + nh], st[:, 32:32 + nh], 1.0 / HD, EPS, ALU.mult, ALU.add, ["stq"], ["stq2"])
            act(st[:, 48:48 + nh], st[:, 40:40 + nh], AF.Sqrt, ["stq2"], ["stq3"])
            op("dve", lambda e: e.reciprocal(out=st[:, 56:56 + nh], in_=st[:, 48:48 + nh]), ["stq3"], ["stq4"])
            tt("dve", qk[:, col0:col0 + n].rearrange("p (h d) -> p h d", d=64), ps.rearrange("p (h d) -> p h d", d=64),
               st[:, 56:56 + nh].unsqueeze(2).broadcast_to([128, nh, 64]), ALU.mult, [psn, "stq4"], ["qkh%d_%d" % (qb_, col0)])

        def head_T(col0, nh, dst_fn, gcol, dname, qb_):
            qk = qkh[qb_]
            for h0 in range(0, nh, 4):
                nhh = min(4, nh - h0)
                tp, tpn = next_tp()
                for j in range(nhh):
                    c = col0 + (h0 + j) * 64
                    op("pe", lambda e, c=c, j=j, tp=tp: e.transpose(out=tp[0:64, j * 128:(j + 1) * 128], in_=qk[:, c:c + 64], identity=ident[:, :]),
                       ["qkh%d_%d" % (qb_, col0), "ident"], [tpn])
                act(dst_fn(h0, nhh), tp[0:64, 0:nhh * 128].rearrange("p (j q) -> p j q", q=128), AF.Copy, [tpn, "gv"], [dname],
                    scale=gv[:, gcol:gcol + 1])

        ychunk = [0]
        ytok = None

        def stage(n):
            if stop == n:
                raise _Stop()

        widx = [0]

        try:
          stage('const')
          for s in range(NSEQ):
            for c in range(NT):
                T0 = c * T
                base = widx[0]
                if c == 0:
                    op("pool", lambda e: e.dma_start(out=QA[64:72, :, :].rearrange("p h q -> p (h q)"), in_=ctab["c_zero"]), (), ["QAm"], dma="qam")
                for i in range(4):
                    xb = xs[i % 2]
                    xn = "xs%d" % (i % 2)
                    src = x[s, T0 + i * 128:T0 + (i + 1) * 128, :]
                    op("sp", lambda e, xb=xb, src=src: e.dma_start(out=xb[:], in_=src), (), [xn], dma=xn)
                    rmsnorm_T(xb[:], xn, g1, "g1", xhat[i % 2], "xhat%d" % (i % 2), slice(i * 128, (i + 1) * 128), i)
                stage('A0')
                srcx = x[s, T0:T0 + T, :].rearrange("(i p) d -> p i d", p=128)
                op("sp", lambda e, srcx=srcx: e.dma_start(out=x1[:], in_=srcx), (), ["x1"], dma="x1")
                pend = [None]

                def flush():
                    if pend[0] is not None:
                        pend[0]()
                        pend[0] = None

                gi = 0
                for g in range(5):
                    w0, w0n = wneed(base + 2 * g)
                    w1, w1n = wneed(base + 2 * g + 1, NS - 2)
                    ncol = 512 if g < 4 else 256
                    for i in range(4):
                        pb, pbn = next_bank()
                        kt = 4 * c + i
                        qb_ = gi % 2
                        gi += 1
                        for k in range(8):
                            ww = (w0 if k < 4 else w1)[:, (k % 4) * ncol:(k % 4 + 1) * ncol]
                            mm(pb[:, 0:ncol], hT[:, k, i * 128:(i + 1) * 128], ww, k == 0, k == 7,
                               ["hT", w0n if k < 4 else w1n], [pbn])
                        qc = slice(i * 128, (i + 1) * 128)
                        nxt = None
                        if g == 0:
                            qk_norm(pb[:, 0:512], pbn, 8, 0, qb_)
                            nxt = lambda qc=qc, qb_=qb_: head_T(0, 8, lambda h0, n: QA[0:64, h0:h0 + n, qc], 0, "QA", qb_)
                        elif g == 1:
                            qk_norm(pb[:, 0:512], pbn, 8, 512, qb_)
                            kc = slice(T0 + i * 128, T0 + (i + 1) * 128)
                            nxt = lambda kc=kc, qb_=qb_: head_T(512, 8, lambda h0, n: KA[0:64, h0:h0 + n, kc], 1, "KA", qb_)
                        elif g == 2:
                            pv = pb[:, 0:512].rearrange("p (pr two d) -> p pr two d", two=2, d=64)
                            act(VA[:, kt, :, 0:64], pv[:, :, 0, :], AF.Copy, [pbn], ["VA"])
                            act(VA[:, kt, :, 128:192], pv[:, :, 1, :], AF.Copy, [pbn], ["VA"])
                        elif g == 3:
                            qk_norm(pb[:, 0:512], pbn, 8, 1024, qb_)
                            nxt = lambda qc=qc, qb_=qb_: head_T(1024, 8, lambda h0, n: QB[0:64, h0:h0 + n, qc], 2, "QB", qb_)
                        else:
                            qk_norm(pb[:, 0:128], pbn, 2, 1536, qb_)
                            ks = kt % 8
                            nxt = lambda ks=ks, qb_=qb_: head_T(1536, 2, lambda h0, n: KB[0:64, h0:h0 + n, ks * 128:(ks + 1) * 128], 3, "KB", qb_)
                            act(VB[:, ks, 64:128], pb[:, 128:192], AF.Copy, [pbn], ["VB"])
                            act(VB[:, ks, 192:256], pb[:, 192:256], AF.Copy, [pbn], ["VB"])
                        flush()
                        pend[0] = nxt
                flush()
                stage('A')
                for h in range(8):
                    ko = ksum[:, h, 2 * c:2 * c + 2]
                    ki = KA[0:64, h, T0:T0 + T].rearrange("p (n k) -> p n k", k=256)
                    op("dve", lambda e, ko=ko, ki=ki: e.tensor_reduce(out=ko, in_=ki, axis=AX.X, op=ALU.add), ["KA"], ["ksum"])
                if c >= 2:
                    gbk = (3, 4, 0, 1)

                    def g1_(h):
                        tt("dve", dlt[:, h, :].rearrange("p (n m) -> p n m", m=8),
                           ksum[:, h, :].unsqueeze(2).broadcast_to([64, 8, 8]),
                           ksum[:, h, :].unsqueeze(1).broadcast_to([64, 8, 8]), ALU.subtract, ["ksum"], ["dlt%d" % h])
                        gb_, gbn = bank[gbk[h % 4]], "pb%d" % gbk[h % 4]
                        mm(gb_[0:64, :], dlt[:, h, :], QA[0:64, h, :], True, True, ["dlt%d" % h, "QA"], [gbn])

                    def g2_(h):
                        gb_, gbn = bank[gbk[h % 4]], "pb%d" % gbk[h % 4]
                        iv, ivn = ind[h % 2], "ind%d" % (h % 2)
                        op("dve", lambda e: e.tensor_single_scalar(out=iv[:, :], in_=gb_[0:64, :], scalar=0.0, op=ALU.is_lt), [gbn], [ivn])
                        for hb in range(2):
                            bi = 2 * c + hb - 4
                            mm(gb_[0:72, hb * 256:(hb + 1) * 256], sel[:, bi * 72:(bi + 1) * 72], iv[:, hb * 256:(hb + 1) * 256], True, True,
                               ["sel", ivn], [gbn])

                    def g3_(h):
                        gb_, gbn = bank[gbk[h % 4]], "pb%d" % gbk[h % 4]
                        ts("dve", QA[64:72, h, :], gb_[64:72, :], 2.5, NEGM, ALU.is_ge, ALU.mult, [gbn], ["QAm"])

                    for k in range(10):
                        if k < 8:
                            g1_(k)
                        if 0 <= k - 1 < 8:
                            g2_(k - 1)
                        if 0 <= k - 2 < 8:
                            g3_(k - 2)
                stage('gate%d' % c)
                steps = [(h, j) for h in range(8) for j in range(4 * c + 4)]
                nst = len(steps)

                def moba_S(si):
                    h, j = steps[si]
                    qs = 128 * max(0, j - 4 * c)
                    N = T - qs
                    sb_i = (3, 4, 0)[si % 3]
                    sbk, sbn = bank[sb_i], "pb%d" % sb_i
                    diag = j >= 4 * c
                    mm(sbk[:, 0:N], KA[0:72, h, j * 128:(j + 1) * 128], QA[0:72, h, qs:T], True, not diag,
                       ["KA", "KAE", "QA", "QAm"], [sbn])
                    if diag:
                        mm(sbk[:, 0:128], ident[:, :], tri[:, :], False, True, ["ident", "tri"], [sbn])
                    di = 4 * c - j + 3
                    pt = PT[si % 4]
                    act(pt[:, 0:N], sbk[:, 0:N], AF.Exp, [sbn, "biasA"], ["PT%d" % (si % 4)],
                        bias=biasA[:, h * 16 + di:h * 16 + di + 1], scale=SCALE)

                def moba_PV(si):
                    h, j = steps[si]
                    qs = 128 * max(0, j - 4 * c)
                    N = T - qs
                    ob_, obn = bank[5 + h % 2], "pb%d" % (5 + h % 2)
                    pr, od = divmod(h, 2)
                    lhsT = VA[:, j, pr, od * 64:od * 64 + 128]
                    mm(ob_[:, qs:T], lhsT, PT[si % 4][:, 0:N], j == 0, j == 4 * c + 3, ["VA", "VAones", "PT%d" % (si % 4)], [obn])
                    if j == 4 * c + 3:
                        lo, hi = (0, 64) if od == 0 else (64, 128)
                        dl, dh = (64, 128) if od == 0 else (0, 64)
                        rn = "R%d" % od
                        act(R[dl:dh, :], ob_[dl:dh, :], AF.Copy, [obn], [rn])
                        op("dve", lambda e: e.reciprocal(out=R[dl:dh, :], in_=R[dl:dh, :]), [rn], [rn])
                        tt("dve", oaT[lo:hi, pr, :], ob_[lo:hi, :], R[dl:dh, :], ALU.mult, [obn, rn], ["oaT%d" % od])

                moba_S(0)
                moba_S(1)
                for si in range(nst):
                    if si + 2 < nst:
                        moba_S(si + 2)
                    moba_PV(si)
                stage('B')
                sw = []
                for i in range(4):
                    kt = 4 * c + i
                    for g in range(2):
                        passes = [(kt % 8, mcur, "mcur", 0)] + ([((kt - 1) % 8, mprev, "mprev", 1)] if kt >= 1 else [])
                        for pi, (kslot, mk, mkn, which) in enumerate(passes):
                            sw.append((i, g, pi, len(passes), kslot, mk, mkn, which))

                def swa_S(n):
                    i, g, pi, npass, kslot, mk, mkn, which = sw[n]
                    sbk, sbn = bank[3 + n % 2], "pb%d" % (3 + n % 2)
                    mm(sbk[:, :], KB[0:64, g, kslot * 128:(kslot + 1) * 128], QB[0:64, 4 * g:4 * g + 4, i * 128:(i + 1) * 128],
                       True, False, ["KB", "QB"], [sbn])
                    mm(sbk[:, :], ident[:, :], mk[:, :], False, True, ["ident", mkn], [sbn])
                    pt = PT[n % 4]
                    for hh in range(4):
                        h = 4 * g + hh
                        act(pt[:, hh * 128:(hh + 1) * 128], sbk[:, hh * 128:(hh + 1) * 128], AF.Exp, [sbn, "biasB"], ["PT%d" % (n % 4)],
                            bias=biasB[:, h * 2 + which:h * 2 + which + 1], scale=SCALE)

                def swa_PV(n):
                    i, g, pi, npass, kslot, mk, mkn, which = sw[n]
                    ob_, obn = bank[5 + (i * 2 + g) % 2], "pb%d" % (5 + (i * 2 + g) % 2)
                    pt = PT[n % 4]
                    ptn = "PT%d" % (n % 4)
                    pt3 = pt[:, :].rearrange("p (a e q) -> p a e q", e=2, q=128)
                    last = pi == npass - 1
                    mm(ob_[:, 0:256], VB[:, kslot, 64 + 128 * g:64 + 128 * g + 128], pt3[:, :, 0, :], pi == 0, last,
                       ["VB", "VBones", ptn], [obn])
                    mm(ob_[:, 256:512], VB[:, kslot, 128 * g:128 * g + 128], pt3[:, :, 1, :], False, last,
                       ["VB", "VBones", ptn], [obn])
                    if not last:
                        return
                    tt("dve", R[64:128, 0:256], ob_[64:128, 0:256], sinkT[64:128, g * 256:(g + 1) * 256], ALU.add, [obn, "sinkT"], ["R0"])
                    tt("dve", R[0:64, 256:512], ob_[0:64, 256:512], sinkT[0:64, g * 256:(g + 1) * 256], ALU.add, [obn, "sinkT"], ["R1"])
                    op("dve", lambda e: e.reciprocal(out=R[64:128, 0:256], in_=R[64:128, 0:256]), ["R0"], ["R0"])
                    op("dve", lambda e: e.reciprocal(out=R[0:64, 256:512], in_=R[0:64, 256:512]), ["R1"], ["R1"])
                    qc = slice(i * 128, (i + 1) * 128)
                    tt("dve", obT[0:64, 2 * g:2 * g + 2, qc], ob_[0:64, 0:256].rearrange("p (a q) -> p a q", q=128),
                       R[64:128, 0:256].rearrange("p (a q) -> p a q", q=128), ALU.mult, [obn, "R0"], ["obT0"])
                    tt("dve", obT[64:128, 2 * g:2 * g + 2, qc], ob_[64:128, 256:512].rearrange("p (a q) -> p a q", q=128),
                       R[0:64, 256:512].rearrange("p (a q) -> p a q", q=128), ALU.mult, [obn, "R1"], ["obT1"])

                swa_S(0)
                for n in range(len(sw)):
                    if n + 1 < len(sw):
                        swa_S(n + 1)
                    swa_PV(n)
                stage('C')
                for n in range(8):
                    wg, wgn = wneed(base + 10 + 2 * n)
                    wab, wabn = wneed(base + 10 + 2 * n + 1, NS - 2)
                    pa, pan = next_bank()
                    for k in range(8):
                        mm(pa[:, :], wg[:, k * 128:(k + 1) * 128], hT[:, k, :], k == 0, k == 7, [wgn, "hT"], [pan])
                    act(sga[:], pa[:, :], AF.Sigmoid, [pan], ["sga"])
                    pb2, pb2n = next_bank()
                    for k in range(8):
                        mm(pb2[:, :], wg[:, 1024 + k * 128:1024 + (k + 1) * 128], hT[:, k, :], k == 0, k == 7, [wgn, "hT"], [pb2n])
                    act(sgb[:], pb2[:, :], AF.Sigmoid, [pb2n], ["sgb"])
                    pc, pcn = next_bank()
                    for k in range(4):
                        mm(pc[:, :], wab[:, k * 128:(k + 1) * 128], oaT[:, k, :], k == 0, k == 3, [wabn, "oaT0", "oaT1"], [pcn])
                    tt("dve", tmpa[:], pc[:, :], sga[:], ALU.mult, [pcn, "sga"], ["tmpa"])
                    pd, pdn = next_bank()
                    for k in range(4):
                        mm(pd[:, :], wab[:, 512 + k * 128:512 + (k + 1) * 128], obT[:, k, :], k == 0, k == 3, [wabn, "obT0", "obT1"], [pdn])
                    tt("dve", sgb[:], pd[:, :], sgb[:], ALU.mult, [pdn, "sgb"], ["sgb"])
                    tt("dve", HM[:, n, :], tmpa[:], sgb[:], ALU.add, ["tmpa", "sgb"], ["hid%d" % n])
                stage('D')
                for half in range(2):
                    w0, w0n = wneed(base + 26 + 2 * half)
                    w1, w1n = wneed(base + 26 + 2 * half + 1, NS - 2)
                    for i in range(4):
                        pb, pbn = next_bank()
                        for k in range(8):
                            ww = (w0 if k < 4 else w1)[:, (k % 4) * 512:(k % 4 + 1) * 512]
                            mm(pb[:, :], HM[:, k, i * 128:(i + 1) * 128], ww, k == 0, k == 7, ["hid%d" % k, w0n if k < 4 else w1n], [pbn])
                        xo = x1[:, i, half * 512:(half + 1) * 512]
                        tt("dve", xo, pb[:, :], xo, ALU.add, [pbn, "x1"], ["x1"])
                stage('E')
                for i in range(4):
                    rmsnorm_T(x1[:, i, :], "x1", g2, "g2", xhat[i % 2], "xhat%d" % (i % 2), slice(i * 128, (i + 1) * 128), 4 + i)
                stage('F')
                for gch in range(8):
                    w0, w0n = wneed(base + 30 + 2 * gch)
                    w1, w1n = wneed(base + 30 + 2 * gch + 1, NS - 2)
                    for f in range(4):
                        pb, pbn = next_bank()
                        for k in range(8):
                            ww = (w0 if k < 4 else w1)[:, (k % 4) * 512 + f * 128:(k % 4) * 512 + (f + 1) * 128]
                            mm(pb[:, :], ww, hT[:, k, :], k == 0, k == 7, ["hT", w0n if k < 4 else w1n], [pbn])
                        act(tmpr[:], pb[:, :], AF.Relu, [pbn], ["tmpr"])
                        tt("dve", HM[:, gch * 4 + f, :], pb[:, :], tmpr[:], ALU.mult, [pbn, "tmpr"], ["hid%d" % (gch * 4 + f)])
                stage('G')
                for half in range(2):
                    for q in range(8):
                        wd, wdn = wneed(base + 46 + half * 8 + q)
                        for f in range(4):
                            ff = q * 4 + f
                            for i in range(4):
                                mm(bank[i][:, :], HM[:, ff, i * 128:(i + 1) * 128], wd[:, f * 512:(f + 1) * 512], ff == 0, ff == 31,
                                   ["hid%d" % ff, wdn], ["pb%d" % i])
                    for i in range(4):
                        xo = x1[:, i, half * 512:(half + 1) * 512]
                        tt("dve", xo, bank[i][:, :], xo, ALU.add, ["pb%d" % i, "x1"], ["x1"])
                dsty = y[s, T0:T0 + T, :].rearrange("(i p) d -> p i d", p=128)
                ytok = op("pool", lambda e, dsty=dsty: e.dma_start(out=dsty, in_=x1[:]), ["x1"], (), dma="y")
                widx[0] += NCHUNK
                stage('T%d' % (s * NT + c))
        except _Stop:
            pass
        if ytok is not None:
            P.final_wait("pool", [ytok])
        if dump:
            tens = {"hT": hT, "QA": QA, "KA": KA, "VA": VA, "QB": QB, "KB": KB, "VB": VB, "oaT": oaT, "obT": obT,
                    "HM": HM, "x1": x1, "ksum": ksum, "sinkT": sinkT, "R": R, "st": st, "qkh": qkh[0], "ind": ind[0], "dlt": dlt}
            allb = list(P.bufs.keys())
            dtoks = []
            for nm in dump:
                tns = tens[nm]
                shp = list(tns.shape)
                flat = tns[:]
                if len(shp) == 3:
                    flat = flat.rearrange("p a b -> p (a b)")
                elif len(shp) == 4:
                    flat = flat.rearrange("p a b c -> p (a b c)")
                fr = int(np.prod(shp[1:]))
                dd = nc.dram_tensor("dbg_" + nm, [shp[0], fr], tns.dtype, kind="ExternalOutput").ap()
                dtoks.append(op("sp", lambda e, dd=dd, flat=flat: e.dma_start(out=dd, in_=flat), allb, (), dma="dbg_" + nm))
            P.final_wait("sp", dtoks)
        sems = {n: es.enter_context(nc.semaphore(n)) for n in sorted(P.semnames)}
        with nc.Block() as block:
            P.emit(block, sems)
    return nc


_NC_CACHE = {}


def kernel(x, norm_attn, w_in, q_norm_a, k_norm_a, q_norm_b, k_norm_b, sinks_b,
           w_branch_a, w_branch_b, w_out, norm_mlp, w_up, w_down):
    f = lambda a: np.ascontiguousarray(np.asarray(a, dtype=np.float32))
    x = f(x)
    tabs = _host_tables()
    g1 = f(np.asarray(norm_attn)[0].reshape(8, 128).T)
    g2 = f(np.asarray(norm_mlp)[0].reshape(8, 128).T)
    gv = f(np.stack([np.asarray(q_norm_a)[0], np.asarray(k_norm_a)[0], np.asarray(q_norm_b)[0], np.asarray(k_norm_b)[0]], axis=1))
    sk = np.asarray(sinks_b)[0]
    sbc = np.zeros((128, 4), np.float32)
    for g in range(2):
        for a in range(2):
            sbc[64:, 2 * g + a] = sk[4 * g + 2 * a]
            sbc[:64, 2 * g + a] = sk[4 * g + 2 * a + 1]
    common = {
        "w_in": f(np.asarray(w_in)[0]), "w_a": f(np.asarray(w_branch_a)[0]), "w_b": f(np.asarray(w_branch_b)[0]),
        "w_out": f(np.asarray(w_out)[0]), "w_up": f(np.asarray(w_up)[0]), "w_down": f(np.asarray(w_down)[0]),
        "g1": g1, "g2": g2, "gv": gv, "sbc": sbc,
    }
    common.update(tabs)
    if "nc" not in _NC_CACHE:
        _NC_CACHE["nc"] = build_nc()
    nc = _NC_CACHE["nc"]
    in_maps = []
    for c in range(NCORES):
        m = dict(common)
        m["x"] = np.ascontiguousarray(x[NSEQ * c:NSEQ * (c + 1)])
        in_maps.append(m)
    res = run_bass_kernel_spmd(nc, in_maps, core_ids=list(range(NCORES)))
    return np.concatenate([r["y"] for r in res.results], axis=0).astype(np.float32)
```

```python
import os
import numpy as np
from contextlib import ExitStack
import concourse.bass as bass
import concourse.mybir as mybir
from concourse.bass_utils import run_bass_kernel_spmd

F32 = mybir.dt.float32
BF16 = mybir.dt.bfloat16
AF = mybir.ActivationFunctionType
ALU = mybir.AluOpType
AX = mybir.AxisListType

NCORES = 8
D = 1024
S = 2048
NSEQ = 2
T = 512
NT = S // T
HD = 64
DFF = 4096
INW = 4352
NEGM = -30000.0
EPS = 1e-6
SCALE = HD ** -0.5
NS = 4
WSZ = 2048
NCHUNK = 62
ENGS = ("pe", "dve", "act", "pool", "sp")


class Buf:
    __slots__ = ("name", "last_w", "readers")

    def __init__(self, name):
        self.name = name
        self.last_w = None
        self.readers = {}


class Prog:
    def __init__(self):
        self.ops = {e: [] for e in ENGS}
        self.cnt = {e: 0 for e in ENGS}
        self.dcnt = {}
        self.waited = {e: {} for e in ENGS}
        self.semnames = set("e_" + e for e in ENGS)
        self.bufs = {}

    def buf(self, name):
        b = self.bufs.get(name)
        if b is None:
            b = self.bufs[name] = Buf(name)
        return b

    def op(self, eng, fn, reads=(), writes=(), dma=None):
        reads = [self.buf(r) for r in reads]
        writes = [self.buf(w) for w in writes]
        own = "e_" + eng
        need = {}

        def add(sem, val, same_ok):
            if sem == own:
                if eng == "pe" or not same_ok:
                    return
            if need.get(sem, 0) < val:
                need[sem] = val

        for b in reads:
            if b.last_w is not None:
                add(b.last_w[0], b.last_w[1], True)
            if b.name.startswith("pb"):
                for s, v in b.readers.items():
                    add(s, v, False)
        for b in writes:
            if b.last_w is not None:
                add(b.last_w[0], b.last_w[1], True)
            for s, v in b.readers.items():
                add(s, v, False)
        waits = []
        wd = self.waited[eng]
        for s, v in need.items():
            if wd.get(s, 0) < v:
                wd[s] = v
                waits.append((s, v))
        if dma is None:
            self.cnt[eng] += 1
            tok = (own, self.cnt[eng])
            inc = (own, 1)
        else:
            sname = "d_" + dma
            self.semnames.add(sname)
            self.dcnt[sname] = self.dcnt.get(sname, 0) + 16
            tok = (sname, self.dcnt[sname])
            inc = (sname, 16)
        self.ops[eng].append((waits, fn, inc))
        for b in writes:
            b.last_w = tok
            b.readers = {}
        for b in reads:
            if b.readers.get(tok[0], 0) < tok[1]:
                b.readers[tok[0]] = tok[1]
        return tok

    def final_wait(self, eng, toks):
        self.ops[eng].append((list(toks), None, None))

    def emit(self, block, sems):
        engmap = {"pe": "tensor", "dve": "vector", "act": "scalar", "pool": "gpsimd", "sp": "sync"}
        for e in ENGS:
            ops = self.ops[e]
            if not ops:
                continue

            def body(engine, ops=ops):
                for waits, fn, inc in ops:
                    for s, v in waits:
                        engine.wait_ge(sems[s], v)
                    if fn is not None:
                        fn(engine).then_inc(sems[inc[0]], inc[1])

            getattr(block, engmap[e])(body)


def _slopes():
    sl = np.exp2(-(8.0 / 16) * np.arange(1, 17, dtype=np.float32)).astype(np.float32)
    return sl[:8], sl[8:]


def _host_tables():
    sl_b, sl_a = _slopes()
    p = np.arange(128, dtype=np.float32)
    k = p[:, None]
    q = p[None, :]
    tri = np.where(k <= q, 0.0, NEGM).astype(np.float32)
    prv = np.where(k > q, 0.0, NEGM).astype(np.float32)
    t = {}
    t["c_ident"] = np.eye(128, dtype=np.float32)
    t["c_tri"] = tri
    t["c_mcur"] = np.tile(tri, (1, 4))
    t["c_mprev"] = np.tile(prv, (1, 4))
    bA = np.zeros((128, 128), np.float32)
    for h in range(8):
        for di in range(16):
            bA[:, h * 16 + di] = sl_a[h] * (p - 128.0 * (di - 3))
    t["c_biasA"] = bA
    bB = np.zeros((128, 16), np.float32)
    for h in range(8):
        bB[:, h * 2 + 0] = sl_b[h] * (p - 64.0)
        bB[:, h * 2 + 1] = sl_b[h] * (p - 192.0)
    t["c_biasB"] = bB
    t1 = np.zeros((128, 2, 2, 128), np.float32)
    for g in range(2):
        for e in range(2):
            t1[64:, g, e, :] = sl_b[4 * g + 2 * e] * (p - 64.0)
            t1[:64, g, e, :] = sl_b[4 * g + 2 * e + 1] * (p - 64.0)
    t["c_t1"] = t1.reshape(128, 512)
    sel = np.zeros((64, 4, 72), np.float32)
    for bi in range(4):
        b = 4 + bi
        for n in range(b):
            for m in range(b):
                if n != m:
                    sel[n * 8 + m, bi, 64 + n] = 1.0
    t["c_sel"] = sel.reshape(64, 288)
    E = np.zeros((8, S), np.float32)
    for n in range(8):
        E[n, n * 256:(n + 1) * 256] = 1.0
    t["c_E"] = E
    t["c_zero"] = np.zeros((8, 8 * T), np.float32)
    return t


def _chunk_pieces(k, w_in, w_a, w_b, w_out, w_up, w_down):
    win = w_in.rearrange("(kc p) n -> p kc n", p=128)
    if k < 10:
        g, kh = divmod(k, 2)
        nc_ = 512 if g < 4 else 256
        return [(0, (4, nc_), win[:, kh * 4:kh * 4 + 4, g * 512:g * 512 + nc_])]
    k -= 10
    if k < 16:
        n, part = divmod(k, 2)
        if part == 0:
            return [(0, (8, 128), win[:, :, 2304 + n * 128:2304 + (n + 1) * 128]),
                    (1024, (8, 128), win[:, :, 3328 + n * 128:3328 + (n + 1) * 128])]
        wa = w_a.rearrange("(kc p) n -> p kc n", p=128)
        wb = w_b.rearrange("(kc p) n -> p kc n", p=128)
        return [(0, (4, 128), wa[:, :, n * 128:(n + 1) * 128]),
                (512, (4, 128), wb[:, :, n * 128:(n + 1) * 128])]
    k -= 16
    if k < 4:
        half, kh = divmod(k, 2)
        wo = w_out.rearrange("(kc p) n -> p kc n", p=128)
        return [(0, (4, 512), wo[:, kh * 4:kh * 4 + 4, half * 512:(half + 1) * 512])]
    k -= 4
    if k < 16:
        gch, kh = divmod(k, 2)
        wu = w_up.rearrange("(kc p) n -> p kc n", p=128)
        return [(0, (4, 512), wu[:, kh * 4:kh * 4 + 4, gch * 512:(gch + 1) * 512])]
    k -= 16
    half, q = divmod(k, 8)
    wd = w_down.rearrange("(fc p) n -> p fc n", p=128)
    return [(0, (4, 512), wd[:, q * 4:q * 4 + 4, half * 512:(half + 1) * 512])]


class _Stop(Exception):
    pass


def build_nc(stop=None, dump=()):
    nc = bass.Bass("TRN2", target_bir_lowering=False)
    dr = lambda n, s, kind="ExternalInput", dt=F32: nc.dram_tensor(n, s, dt, kind=kind).ap()
    x = dr("x", [NSEQ, S, D])
    w_in = dr("w_in", [D, INW])
    w_a = dr("w_a", [512, D])
    w_b = dr("w_b", [512, D])
    w_out = dr("w_out", [D, D])
    w_up = dr("w_up", [D, DFF])
    w_down = dr("w_down", [DFF, D])
    g1_d = dr("g1", [128, 8])
    g2_d = dr("g2", [128, 8])
    gv_d = dr("gv", [64, 4])
    sbc_d = dr("sbc", [128, 4])
    ctab = {n: dr(n, list(a.shape)) for n, a in _host_tables().items()}
    y = dr("y", [NSEQ, S, D], kind="ExternalOutput")
    scr = nc.dram_tensor("scr", [NCHUNK, 128, WSZ], BF16).ap()

    with ExitStack() as es:
        sb = lambda n, s, d: es.enter_context(nc.sbuf_tensor(n, s, d))
        KA = sb("KA", [72, 8, S], BF16)
        VA = sb("VA", [128, 16, 4, 192], BF16)
        KB = sb("KB", [64, 2, 1024], BF16)
        VB = sb("VB", [128, 8, 320], BF16)
        QA = sb("QA", [72, 8, T], BF16)
        QB = sb("QB", [64, 8, T], BF16)
        xs = [sb("xs%d" % i, [128, D], F32) for i in range(2)]
        x1 = sb("x1", [128, 4, D], F32)
        hT = sb("hT", [128, 8, T], BF16)
        xhat = [sb("xhat%d" % i, [128, D], BF16) for i in range(4)]
        qkh = [sb("qkh%d" % i, [128, 512], BF16) for i in range(3)]
        sq = sb("sq", [128, 512], F32)
        PT = [sb("PT%d" % i, [128, 512], BF16) for i in range(4)]
        oaT = sb("oaT", [128, 4, T], BF16)
        obT = sb("obT", [128, 4, T], BF16)
        sga = sb("sga", [128, 512], F32)
        sgb = sb("sgb", [128, 512], F32)
        tmpa = sb("tmpa", [128, 512], F32)
        tmpr = sb("tmpr", [128, 512], F32)
        HM = sb("HM", [128, 32, T], BF16)
        W = [sb("W%d" % i, [128, WSZ], BF16) for i in range(NS)]
        R = sb("R", [128, 512], F32)
        ident = sb("ident", [128, 128], BF16)
        tri = sb("tri", [128, 128], BF16)
        mcur = sb("mcur", [128, 512], BF16)
        mprev = sb("mprev", [128, 512], BF16)
        biasA = sb("biasA", [128, 128], F32)
        biasB = sb("biasB", [128, 16], F32)
        t1 = sb("t1", [128, 512], F32)
        sinkT = sb("sinkT", [128, 512], F32)
        sbc = sb("sbc_s", [128, 4], F32)
        sel = sb("sel", [64, 288], BF16)
        g1 = sb("g1_s", [128, 8], F32)
        g2 = sb("g2_s", [128, 8], F32)
        gv = sb("gv_s", [64, 4], F32)
        st = sb("st", [128, 64], F32)
        ksum = sb("ksum", [64, 8, 8], F32)
        dlt = sb("dlt", [64, 8, 64], BF16)
        ind = [sb("ind%d" % i, [64, 512], BF16) for i in range(2)]
        bank = [es.enter_context(nc.psum_tensor("pb%d" % i, [128, 512], F32)) for i in range(8)]
        tpbs = [bank[7][:, :].bitcast(BF16), bank[6][:, :].bitcast(BF16)]

        P = Prog()

        def op(eng, fn, r=(), w=(), dma=None):
            return P.op(eng, fn, r, w, dma)

        def mm(out, lhsT, rhs, start, stop, r, w):
            op("pe", lambda e: e.matmul(out, lhsT=lhsT, rhs=rhs, start=start, stop=stop), r, w)

        def act(out, in_, func, r, w, **kw):
            op("act", lambda e: e.activation(out=out, in_=in_, func=func, **kw), r, w)

        def tt(eng, out, in0, in1, alu, r, w):
            op(eng, lambda e: e.tensor_tensor(out=out, in0=in0, in1=in1, op=alu), r, w)

        def ts(eng, out, in0, s1, s2, op0, op1, r, w):
            if op1 is None:
                op(eng, lambda e: e.tensor_scalar(out=out, in0=in0, scalar1=s1, scalar2=None, op0=op0), r, w)
            else:
                op(eng, lambda e: e.tensor_scalar(out=out, in0=in0, scalar1=s1, scalar2=s2, op0=op0, op1=op1), r, w)

        cidx = [0]

        def cload(dst, src, name, eng="pool"):
            cidx[0] += 1
            op(eng, lambda e: e.dma_start(out=dst, in_=src), (), [name], dma="c%d" % cidx[0])

        cload(ident[:], ctab["c_ident"], "ident")
        cload(tri[:], ctab["c_tri"], "tri")
        cload(mcur[:], ctab["c_mcur"], "mcur")
        cload(mprev[:], ctab["c_mprev"], "mprev")
        cload(sel[:], ctab["c_sel"], "sel")
        cload(biasA[:], ctab["c_biasA"], "biasA", "sp")
        cload(biasB[:], ctab["c_biasB"], "biasB", "sp")
        cload(t1[:], ctab["c_t1"], "t1", "sp")
        cload(sbc[:], sbc_d, "sbc", "sp")
        cload(g1[:], g1_d, "g1", "sp")
        cload(g2[:], g2_d, "g2", "sp")
        cload(gv[:], gv_d, "gv", "sp")
        for h in range(8):
            cload(KA[64:72, h, :], ctab["c_E"], "KAE")
        op("dve", lambda e: e.memset(VA[:, :, :, 64:128], 1.0), (), ["VAones"])
        op("dve", lambda e: e.memset(VB[:, :, 0:64], 1.0), (), ["VBones"])
        op("dve", lambda e: e.memset(VB[:, :, 128:192], 1.0), (), ["VBones"])
        op("dve", lambda e: e.memset(VB[:, :, 256:320], 1.0), (), ["VBones"])
        op("dve", lambda e: e.memset(ksum[:], 0.0), (), ["ksum"])
        for j in range(4):
            act(sinkT[:, j * 128:(j + 1) * 128], t1[:, j * 128:(j + 1) * 128], AF.Exp, ["t1", "sbc"], ["sinkT"],
                bias=sbc[:, j:j + 1], scale=1.0)

        seq = [(gt, k) for gt in range(NSEQ * NT) for k in range(NCHUNK)]
        wstate = {"next": 0}

        def wload(idx):
            gt, k = seq[idx]
            slot = idx % NS
            wt = W[slot]
            if gt == 0:
                for (off, (kk, ncol), src) in _chunk_pieces(k, w_in, w_a, w_b, w_out, w_up, w_down):
                    dst = wt[:, off:off + kk * ncol].rearrange("p (a b) -> p a b", b=ncol)
                    op("pool", lambda e, dst=dst, src=src: e.dma_start(out=dst, in_=src), (), ["W%d" % slot], dma="w%d" % slot)
                op("sp", lambda e: e.dma_start(out=scr[k], in_=wt[:]), ["W%d" % slot], ["scr%d" % k], dma="ws%d" % slot)
            else:
                op("sp", lambda e: e.dma_start(out=wt[:], in_=scr[k]), ["scr%d" % k], ["W%d" % slot], dma="w%d" % slot)

        def wneed(idx, ahead=NS - 1):
            lim = min(idx + ahead, len(seq) - 1)
            while wstate["next"] <= lim:
                wload(wstate["next"])
                wstate["next"] += 1
            return W[idx % NS], "W%d" % (idx % NS)

        mmb = [0]

        def next_bank():
            b = mmb[0] % 3
            mmb[0] += 1
            return bank[b], "pb%d" % b

        tph = [0]

        def next_tp():
            h = tph[0] % 2
            tph[0] += 1
            return tpbs[h][:, 0:512], "pb%d" % (7 - h)

        def norm_act(src_ap, src_reads, xi, si):
            xh, xhn = xhat[xi], "xhat%d" % xi
            c0 = (si % 4) * 4
            act(xh[:], src_ap, AF.Square, src_reads, [xhn, "st%d" % c0], accum_out=st[:, c0:c0 + 1])
            ts("dve", st[:, c0 + 1:c0 + 2], st[:, c0:c0 + 1], 1.0 / D, EPS, ALU.mult, ALU.add, ["st%d" % c0], ["st%d" % (c0 + 1)])
            act(st[:, c0 + 2:c0 + 3], st[:, c0 + 1:c0 + 2], AF.Sqrt, ["st%d" % (c0 + 1)], ["st%d" % (c0 + 2)])
            op("dve", lambda e: e.reciprocal(out=st[:, c0 + 3:c0 + 4], in_=st[:, c0 + 2:c0 + 3]), ["st%d" % (c0 + 2)], ["st%d" % (c0 + 3)])
            act(xh[:], src_ap, AF.Copy, list(src_reads) + ["st%d" % (c0 + 3)], [xhn], scale=st[:, c0 + 3:c0 + 4])

        def norm_pe_half(xi, gain, gname, dst_cols, half):
            xh, xhn = xhat[xi], "xhat%d" % xi
            tp, tpn = next_tp()
            for j in range(4):
                jj = half * 4 + j
                op("pe", lambda e, jj=jj, j=j, tp=tp: e.transpose(out=tp[:, j * 128:(j + 1) * 128], in_=xh[:, jj * 128:(jj + 1) * 128], identity=ident[:, :]),
                   [xhn, "ident"], [tpn])
            tt("dve", hT[:, half * 4:half * 4 + 4, dst_cols], tp.rearrange("p (j q) -> p j q", q=128),
               gain[:, half * 4:half * 4 + 4].unsqueeze(2).broadcast_to([128, 4, 128]), ALU.mult, [tpn, gname], ["hT"])

        def qk_norm(ps, psn, nh, col0, qb_):
            n = nh * 64
            qk = qkh[qb_]
            act(sq[:, 0:n], ps, AF.Square, [psn], ["sq"])
            op("dve", lambda e: e.tensor_reduce(out=st[:, 32:32 + nh], in_=sq[:, 0:n].rearrange("p (h d) -> p h d", d=64), axis=AX.X, op=ALU.add),
               ["sq"], ["stq"])
            ts("dve", st[:, 40:40 + nh], st[:, 32:32 + nh], 1.0 / HD, EPS, ALU.mult, ALU.add, ["stq"], ["stq2"])
            act(st[:, 48:48 + nh], st[:, 40:40 + nh], AF.Sqrt, ["stq2"], ["stq3"])
            op("dve", lambda e: e.reciprocal(out=st[:, 56:56 + nh], in_=st[:, 48:48 + nh]), ["stq3"], ["stq4"])
            tt("dve", qk[:, 0:n].rearrange("p (h d) -> p h d", d=64), ps.rearrange("p (h d) -> p h d", d=64),
               st[:, 56:56 + nh].unsqueeze(2).broadcast_to([128, nh, 64]), ALU.mult, [psn, "stq4"], ["qkh%d" % qb_])

        def head_T(col0, nh, dst_fn, gcol, dname, qb_):
            qk = qkh[qb_]
            for h0 in range(0, nh, 4):
                nhh = min(4, nh - h0)
                tp, tpn = next_tp()
                for j in range(nhh):
                    c = (h0 + j) * 64
                    op("pe", lambda e, c=c, j=j, tp=tp: e.transpose(out=tp[0:64, j * 128:(j + 1) * 128], in_=qk[:, c:c + 64], identity=ident[:, :]),
                       ["qkh%d" % qb_, "ident"], [tpn])
                act(dst_fn(h0, nhh), tp[0:64, 0:nhh * 128].rearrange("p (j q) -> p j q", q=128), AF.Copy, [tpn, "gv"], [dname],
                    scale=gv[:, gcol:gcol + 1])

        ychunk = [0]
        ytok = None

        def stage(n):
            if stop == n:
                raise _Stop()

        widx = [0]

        tiles = [(s_, c_) for s_ in range(NSEQ) for c_ in range(NT)]

        def A0_act(t, i):
            s_, c_ = tiles[t]
            xb, xn = xs[i % 2], "xs%d" % (i % 2)
            src = x[s_, c_ * T + i * 128:c_ * T + (i + 1) * 128, :]
            op("sp", lambda e: e.dma_start(out=xb[:], in_=src), (), [xn], dma=xn)
            norm_act(xb[:], [xn], i, i)

        def A0_pe(t, i, half):
            norm_pe_half(i, g1, "g1", slice(i * 128, (i + 1) * 128), half)

        try:
          stage('const')
          for i in range(4):
              A0_act(0, i)
          for i in range(4):
              for half in range(2):
                  A0_pe(0, i, half)
          for s in range(NSEQ):
            for c in range(NT):
                T0 = c * T
                base = widx[0]
                tcur = s * NT + c
                has_next = tcur + 1 < len(tiles)
                if c == 0:
                    op("pool", lambda e: e.dma_start(out=QA[64:72, :, :].rearrange("p h q -> p (h q)"), in_=ctab["c_zero"]), (), ["QAm"], dma="qam")
                stage('A0')
                srcx = x[s, T0:T0 + T, :].rearrange("(i p) d -> p i d", p=128)
                op("sp", lambda e, srcx=srcx: e.dma_start(out=x1[:], in_=srcx), (), ["x1_0", "x1_1", "x1_2", "x1_3"], dma="x1")
                pend = []

                def flush(keep):
                    while len(pend) > keep:
                        f_ = pend.pop(0)
                        if f_ is not None:
                            f_()

                gi = 0
                for g in range(5):
                    w0, w0n = wneed(base + 2 * g)
                    w1, w1n = wneed(base + 2 * g + 1, NS - 2)
                    ncol = 512 if g < 4 else 256
                    for i in range(4):
                        pb, pbn = next_bank()
                        kt = 4 * c + i
                        qb_ = gi % 3
                        gi += 1
                        for k in range(8):
                            ww = (w0 if k < 4 else w1)[:, (k % 4) * ncol:(k % 4 + 1) * ncol]
                            mm(pb[:, 0:ncol], hT[:, k, i * 128:(i + 1) * 128], ww, k == 0, k == 7,
                               ["hT", w0n if k < 4 else w1n], [pbn])
                        qc = slice(i * 128, (i + 1) * 128)
                        nxt = None
                        if g == 0:
                            qk_norm(pb[:, 0:512], pbn, 8, 0, qb_)
                            nxt = lambda qc=qc, qb_=qb_: head_T(0, 8, lambda h0, n: QA[0:64, h0:h0 + n, qc], 0, "QA", qb_)
                        elif g == 1:
                            qk_norm(pb[:, 0:512], pbn, 8, 512, qb_)
                            kc = slice(T0 + i * 128, T0 + (i + 1) * 128)
                            nxt = lambda kc=kc, qb_=qb_: head_T(512, 8, lambda h0, n: KA[0:64, h0:h0 + n, kc], 1, "KA", qb_)
                        elif g == 2:
                            pv = pb[:, 0:512].rearrange("p (pr two d) -> p pr two d", two=2, d=64)
                            act(VA[:, kt, :, 0:64], pv[:, :, 0, :], AF.Copy, [pbn], ["VA"])
                            act(VA[:, kt, :, 128:192], pv[:, :, 1, :], AF.Copy, [pbn], ["VA"])
                        elif g == 3:
                            qk_norm(pb[:, 0:512], pbn, 8, 1024, qb_)
                            nxt = lambda qc=qc, qb_=qb_: head_T(1024, 8, lambda h0, n: QB[0:64, h0:h0 + n, qc], 2, "QB", qb_)
                        else:
                            qk_norm(pb[:, 0:128], pbn, 2, 1536, qb_)
                            ks = kt % 8
                            nxt = lambda ks=ks, qb_=qb_: head_T(1536, 2, lambda h0, n: KB[0:64, h0:h0 + n, ks * 128:(ks + 1) * 128], 3, "KB", qb_)
                            act(VB[:, ks, 64:128], pb[:, 128:192], AF.Copy, [pbn], ["VB"])
                            act(VB[:, ks, 192:256], pb[:, 192:256], AF.Copy, [pbn], ["VB"])
                        pend.append(nxt)
                        flush(2)
                flush(0)
                stage('A')
                for h in range(8):
                    ko = ksum[:, h, 2 * c:2 * c + 2]
                    ki = KA[0:64, h, T0:T0 + T].rearrange("p (n k) -> p n k", k=256)
                    op("dve", lambda e, ko=ko, ki=ki: e.tensor_reduce(out=ko, in_=ki, axis=AX.X, op=ALU.add), ["KA"], ["ksum"])
                if c >= 2:
                    gbk = (3, 4, 0, 1)

                    def g1_(h):
                        tt("dve", dlt[:, h, :].rearrange("p (n m) -> p n m", m=8),
                           ksum[:, h, :].unsqueeze(2).broadcast_to([64, 8, 8]),
                           ksum[:, h, :].unsqueeze(1).broadcast_to([64, 8, 8]), ALU.subtract, ["ksum"], ["dlt%d" % h])
                        gb_, gbn = bank[gbk[h % 4]], "pb%d" % gbk[h % 4]
                        mm(gb_[0:64, :], dlt[:, h, :], QA[0:64, h, :], True, True, ["dlt%d" % h, "QA"], [gbn])

                    def g2_(h):
                        gb_, gbn = bank[gbk[h % 4]], "pb%d" % gbk[h % 4]
                        iv, ivn = ind[h % 2], "ind%d" % (h % 2)
                        op("dve", lambda e: e.tensor_single_scalar(out=iv[:, :], in_=gb_[0:64, :], scalar=0.0, op=ALU.is_lt), [gbn], [ivn])
                        for hb in range(2):
                            bi = 2 * c + hb - 4
                            mm(gb_[0:72, hb * 256:(hb + 1) * 256], sel[:, bi * 72:(bi + 1) * 72], iv[:, hb * 256:(hb + 1) * 256], True, True,
                               ["sel", ivn], [gbn])

                    def g3_(h):
                        gb_, gbn = bank[gbk[h % 4]], "pb%d" % gbk[h % 4]
                        ts("dve", QA[64:72, h, :], gb_[64:72, :], 2.5, NEGM, ALU.is_ge, ALU.mult, [gbn], ["QAm"])

                    for k in range(10):
                        if k < 8:
                            g1_(k)
                        if 0 <= k - 1 < 8:
                            g2_(k - 1)
                        if 0 <= k - 2 < 8:
                            g3_(k - 2)
                stage('gate%d' % c)
                steps = [(h, j) for h in range(8) for j in range(4 * c + 4)]
                nst = len(steps)

                def moba_S(si):
                    h, j = steps[si]
                    qs = 128 * max(0, j - 4 * c)
                    N = T - qs
                    sb_i = (3, 4, 0)[si % 3]
                    sbk, sbn = bank[sb_i], "pb%d" % sb_i
                    diag = j >= 4 * c
                    mm(sbk[:, 0:N], KA[0:72, h, j * 128:(j + 1) * 128], QA[0:72, h, qs:T], True, not diag,
                       ["KA", "KAE", "QA", "QAm"], [sbn])
                    if diag:
                        mm(sbk[:, 0:128], ident[:, :], tri[:, :], False, True, ["ident", "tri"], [sbn])
                    di = 4 * c - j + 3
                    pt = PT[si % 4]
                    act(pt[:, 0:N], sbk[:, 0:N], AF.Exp, [sbn, "biasA"], ["PT%d" % (si % 4)],
                        bias=biasA[:, h * 16 + di:h * 16 + di + 1], scale=SCALE)

                def moba_PV(si):
                    h, j = steps[si]
                    qs = 128 * max(0, j - 4 * c)
                    N = T - qs
                    ob_, obn = bank[5 + h % 2], "pb%d" % (5 + h % 2)
                    pr, od = divmod(h, 2)
                    lhsT = VA[:, j, pr, od * 64:od * 64 + 128]
                    mm(ob_[:, qs:T], lhsT, PT[si % 4][:, 0:N], j == 0, j == 4 * c + 3, ["VA", "VAones", "PT%d" % (si % 4)], [obn])
                    if j == 4 * c + 3:
                        lo, hi = (0, 64) if od == 0 else (64, 128)
                        dl, dh = (64, 128) if od == 0 else (0, 64)
                        rn = "R%d" % od
                        act(R[dl:dh, :], ob_[dl:dh, :], AF.Copy, [obn], [rn])
                        op("dve", lambda e: e.reciprocal(out=R[dl:dh, :], in_=R[dl:dh, :]), [rn], [rn])
                        tt("dve", oaT[lo:hi, pr, :], ob_[lo:hi, :], R[dl:dh, :], ALU.mult, [obn, rn], ["oaT%d" % od])

                moba_S(0)
                moba_S(1)
                for si in range(nst):
                    if si + 2 < nst:
                        moba_S(si + 2)
                    moba_PV(si)
                stage('B')
                sw = []
                for i in range(4):
                    kt = 4 * c + i
                    for g in range(2):
                        passes = [(kt % 8, mcur, "mcur", 0)] + ([((kt - 1) % 8, mprev, "mprev", 1)] if kt >= 1 else [])
                        for pi, (kslot, mk, mkn, which) in enumerate(passes):
                            sw.append((i, g, pi, len(passes), kslot, mk, mkn, which))

                def swa_S(n):
                    i, g, pi, npass, kslot, mk, mkn, which = sw[n]
                    sbk, sbn = bank[3 + n % 2], "pb%d" % (3 + n % 2)
                    mm(sbk[:, :], KB[0:64, g, kslot * 128:(kslot + 1) * 128], QB[0:64, 4 * g:4 * g + 4, i * 128:(i + 1) * 128],
                       True, False, ["KB", "QB"], [sbn])
                    mm(sbk[:, :], ident[:, :], mk[:, :], False, True, ["ident", mkn], [sbn])
                    pt = PT[n % 4]
                    for hh in range(4):
                        h = 4 * g + hh
                        act(pt[:, hh * 128:(hh + 1) * 128], sbk[:, hh * 128:(hh + 1) * 128], AF.Exp, [sbn, "biasB"], ["PT%d" % (n % 4)],
                            bias=biasB[:, h * 2 + which:h * 2 + which + 1], scale=SCALE)

                def swa_PV(n):
                    i, g, pi, npass, kslot, mk, mkn, which = sw[n]
                    ob_, obn = bank[5 + (i * 2 + g) % 2], "pb%d" % (5 + (i * 2 + g) % 2)
                    pt = PT[n % 4]
                    ptn = "PT%d" % (n % 4)
                    pt3 = pt[:, :].rearrange("p (a e q) -> p a e q", e=2, q=128)
                    last = pi == npass - 1
                    mm(ob_[:, 0:256], VB[:, kslot, 64 + 128 * g:64 + 128 * g + 128], pt3[:, :, 0, :], pi == 0, last,
                       ["VB", "VBones", ptn], [obn])
                    mm(ob_[:, 256:512], VB[:, kslot, 128 * g:128 * g + 128], pt3[:, :, 1, :], False, last,
                       ["VB", "VBones", ptn], [obn])
                    if not last:
                        return
                    tt("dve", R[64:128, 0:256], ob_[64:128, 0:256], sinkT[64:128, g * 256:(g + 1) * 256], ALU.add, [obn, "sinkT"], ["R0"])
                    tt("dve", R[0:64, 256:512], ob_[0:64, 256:512], sinkT[0:64, g * 256:(g + 1) * 256], ALU.add, [obn, "sinkT"], ["R1"])
                    op("dve", lambda e: e.reciprocal(out=R[64:128, 0:256], in_=R[64:128, 0:256]), ["R0"], ["R0"])
                    op("dve", lambda e: e.reciprocal(out=R[0:64, 256:512], in_=R[0:64, 256:512]), ["R1"], ["R1"])
                    qc = slice(i * 128, (i + 1) * 128)
                    tt("dve", obT[0:64, 2 * g:2 * g + 2, qc], ob_[0:64, 0:256].rearrange("p (a q) -> p a q", q=128),
                       R[64:128, 0:256].rearrange("p (a q) -> p a q", q=128), ALU.mult, [obn, "R0"], ["obT0"])
                    tt("dve", obT[64:128, 2 * g:2 * g + 2, qc], ob_[64:128, 256:512].rearrange("p (a q) -> p a q", q=128),
                       R[0:64, 256:512].rearrange("p (a q) -> p a q", q=128), ALU.mult, [obn, "R1"], ["obT1"])

                swa_S(0)
                for n in range(len(sw)):
                    if n + 1 < len(sw):
                        swa_S(n + 1)
                    swa_PV(n)
                stage('C')
                for n in range(8):
                    wg, wgn = wneed(base + 10 + 2 * n)
                    wab, wabn = wneed(base + 10 + 2 * n + 1, NS - 2)
                    pa, pan = next_bank()
                    for k in range(8):
                        mm(pa[:, :], wg[:, k * 128:(k + 1) * 128], hT[:, k, :], k == 0, k == 7, [wgn, "hT"], [pan])
                    act(sga[:], pa[:, :], AF.Sigmoid, [pan], ["sga"])
                    pb2, pb2n = next_bank()
                    for k in range(8):
                        mm(pb2[:, :], wg[:, 1024 + k * 128:1024 + (k + 1) * 128], hT[:, k, :], k == 0, k == 7, [wgn, "hT"], [pb2n])
                    act(sgb[:], pb2[:, :], AF.Sigmoid, [pb2n], ["sgb"])
                    pc, pcn = next_bank()
                    for k in range(4):
                        mm(pc[:, :], wab[:, k * 128:(k + 1) * 128], oaT[:, k, :], k == 0, k == 3, [wabn, "oaT0", "oaT1"], [pcn])
                    tt("dve", tmpa[:], pc[:, :], sga[:], ALU.mult, [pcn, "sga"], ["tmpa"])
                    pd, pdn = next_bank()
                    for k in range(4):
                        mm(pd[:, :], wab[:, 512 + k * 128:512 + (k + 1) * 128], obT[:, k, :], k == 0, k == 3, [wabn, "obT0", "obT1"], [pdn])
                    tt("dve", sgb[:], pd[:, :], sgb[:], ALU.mult, [pdn, "sgb"], ["sgb"])
                    tt("dve", HM[:, n, :], tmpa[:], sgb[:], ALU.add, ["tmpa", "sgb"], ["hid%d" % n])
                stage('D')
                for half in range(2):
                    w0, w0n = wneed(base + 26 + 2 * half)
                    w1, w1n = wneed(base + 26 + 2 * half + 1, NS - 2)
                    for i in range(4):
                        pb, pbn = next_bank()
                        for k in range(8):
                            ww = (w0 if k < 4 else w1)[:, (k % 4) * 512:(k % 4 + 1) * 512]
                            mm(pb[:, :], HM[:, k, i * 128:(i + 1) * 128], ww, k == 0, k == 7, ["hid%d" % k, w0n if k < 4 else w1n], [pbn])
                        xo = x1[:, i, half * 512:(half + 1) * 512]
                        tt("dve", xo, pb[:, :], xo, ALU.add, [pbn, "x1_%d" % i], ["x1_%d" % i])
                        if half == 1:
                            norm_act(x1[:, i, :], ["x1_%d" % i], i, 4 + i)
                stage('E')
                for i in range(4):
                    for half in range(2):
                        norm_pe_half(i, g2, "g2", slice(i * 128, (i + 1) * 128), half)
                stage('F')
                for gch in range(8):
                    w0, w0n = wneed(base + 30 + 2 * gch)
                    w1, w1n = wneed(base + 30 + 2 * gch + 1, NS - 2)
                    for f in range(4):
                        pb, pbn = next_bank()
                        for k in range(8):
                            ww = (w0 if k < 4 else w1)[:, (k % 4) * 512 + f * 128:(k % 4) * 512 + (f + 1) * 128]
                            mm(pb[:, :], ww, hT[:, k, :], k == 0, k == 7, ["hT", w0n if k < 4 else w1n], [pbn])
                        act(tmpr[:], pb[:, :], AF.Relu, [pbn], ["tmpr"])
                        tt("dve", HM[:, gch * 4 + f, :], pb[:, :], tmpr[:], ALU.mult, [pbn, "tmpr"], ["hid%d" % (gch * 4 + f)])
                        if has_next and f == 3 and gch % 2 == 0:
                            A0_act(tcur + 1, gch // 2)
                stage('G')
                for half in range(2):
                    for q in range(8):
                        wd, wdn = wneed(base + 46 + half * 8 + q)
                        for f in range(4):
                            ff = q * 4 + f
                            for i in range(4):
                                mm(bank[i][:, :], HM[:, ff, i * 128:(i + 1) * 128], wd[:, f * 512:(f + 1) * 512], ff == 0, ff == 31,
                                   ["hid%d" % ff, wdn], ["pb%d" % i])
                        if has_next and half == 0:
                            A0_pe(tcur + 1, q // 2, q % 2)
                    for i in range(4):
                        xo = x1[:, i, half * 512:(half + 1) * 512]
                        tt("dve", xo, bank[i][:, :], xo, ALU.add, ["pb%d" % i, "x1_%d" % i], ["x1_%d" % i])
                dsty = y[s, T0:T0 + T, :].rearrange("(i p) d -> p i d", p=128)
                ytok = op("pool", lambda e, dsty=dsty: e.dma_start(out=dsty, in_=x1[:]), ["x1_0", "x1_1", "x1_2", "x1_3"], (), dma="y")
                widx[0] += NCHUNK
                stage('T%d' % (s * NT + c))
        except _Stop:
            pass
        if ytok is not None:
            P.final_wait("pool", [ytok])
        if dump:
            tens = {"hT": hT, "QA": QA, "KA": KA, "VA": VA, "QB": QB, "KB": KB, "VB": VB, "oaT": oaT, "obT": obT,
                    "HM": HM, "x1": x1, "ksum": ksum, "sinkT": sinkT, "R": R, "st": st, "qkh": qkh[0], "ind": ind[0], "dlt": dlt}
            allb = list(P.bufs.keys())
            dtoks = []
            for nm in dump:
                tns = tens[nm]
                shp = list(tns.shape)
                flat = tns[:]
                if len(shp) == 3:
                    flat = flat.rearrange("p a b -> p (a b)")
                elif len(shp) == 4:
                    flat = flat.rearrange("p a b c -> p (a b c)")
                fr = int(np.prod(shp[1:]))
                dd = nc.dram_tensor("dbg_" + nm, [shp[0], fr], tns.dtype, kind="ExternalOutput").ap()
                dtoks.append(op("sp", lambda e, dd=dd, flat=flat: e.dma_start(out=dd, in_=flat), allb, (), dma="dbg_" + nm))
            P.final_wait("sp", dtoks)
        sems = {n: es.enter_context(nc.semaphore(n)) for n in sorted(P.semnames)}
        with nc.Block() as block:
            P.emit(block, sems)
    return nc


_NC_CACHE = {}


def kernel(x, norm_attn, w_in, q_norm_a, k_norm_a, q_norm_b, k_norm_b, sinks_b,
           w_branch_a, w_branch_b, w_out, norm_mlp, w_up, w_down):
    f = lambda a: np.ascontiguousarray(np.asarray(a, dtype=np.float32))
    x = f(x)
    tabs = _host_tables()
    g1 = f(np.asarray(norm_attn)[0].reshape(8, 128).T)
    g2 = f(np.asarray(norm_mlp)[0].reshape(8, 128).T)
    gv = f(np.stack([np.asarray(q_norm_a)[0], np.asarray(k_norm_a)[0], np.asarray(q_norm_b)[0], np.asarray(k_norm_b)[0]], axis=1))
    sk = np.asarray(sinks_b)[0]
    sbc = np.zeros((128, 4), np.float32)
    for g in range(2):
        for a in range(2):
            sbc[64:, 2 * g + a] = sk[4 * g + 2 * a]
            sbc[:64, 2 * g + a] = sk[4 * g + 2 * a + 1]
    common = {
        "w_in": f(np.asarray(w_in)[0]), "w_a": f(np.asarray(w_branch_a)[0]), "w_b": f(np.asarray(w_branch_b)[0]),
        "w_out": f(np.asarray(w_out)[0]), "w_up": f(np.asarray(w_up)[0]), "w_down": f(np.asarray(w_down)[0]),
        "g1": g1, "g2": g2, "gv": gv, "sbc": sbc,
    }
    common.update(tabs)
    if "nc" not in _NC_CACHE:
        _NC_CACHE["nc"] = build_nc()
    nc = _NC_CACHE["nc"]
    in_maps = []
    for c in range(NCORES):
        m = dict(common)
        m["x"] = np.ascontiguousarray(x[NSEQ * c:NSEQ * (c + 1)])
        in_maps.append(m)
    res = run_bass_kernel_spmd(nc, in_maps, core_ids=list(range(NCORES)))
    return np.concatenate([r["y"] for r in res.results], axis=0).astype(np.float32)
```

```python
import os
import numpy as np
from contextlib import ExitStack
import concourse.bass as bass
import concourse.mybir as mybir
from concourse.bass_utils import run_bass_kernel_spmd

F32 = mybir.dt.float32
BF16 = mybir.dt.bfloat16
AF = mybir.ActivationFunctionType
ALU = mybir.AluOpType
AX = mybir.AxisListType

NCORES = 8
D = 1024
S = 2048
NSEQ = 2
T = 512
NT = S // T
HD = 64
DFF = 4096
INW = 4352
NEGM = -30000.0
EPS = 1e-6
SCALE = HD ** -0.5
NS = 4
WSZ = 2048
NCHUNK = 62
ENGS = ("pe", "dve", "act", "pool", "sp")


class Buf:
    __slots__ = ("name", "last_w", "readers")

    def __init__(self, name):
        self.name = name
        self.last_w = None
        self.readers = {}


class Prog:
    def __init__(self):
        self.ops = {e: [] for e in ENGS}
        self.cnt = {e: 0 for e in ENGS}
        self.dcnt = {}
        self.waited = {e: {} for e in ENGS}
        self.semnames = set("e_" + e for e in ENGS)
        self.bufs = {}

    def buf(self, name):
        b = self.bufs.get(name)
        if b is None:
            b = self.bufs[name] = Buf(name)
        return b

    def op(self, eng, fn, reads=(), writes=(), dma=None):
        reads = [self.buf(r) for r in reads]
        writes = [self.buf(w) for w in writes]
        own = "e_" + eng
        need = {}

        def add(sem, val, same_ok):
            if sem == own:
                if eng == "pe" or not same_ok:
                    return
            if need.get(sem, 0) < val:
                need[sem] = val

        for b in reads:
            if b.last_w is not None:
                add(b.last_w[0], b.last_w[1], True)
            if b.name.startswith("pb"):
                for s, v in b.readers.items():
                    add(s, v, False)
        for b in writes:
            if b.last_w is not None:
                add(b.last_w[0], b.last_w[1], True)
            for s, v in b.readers.items():
                add(s, v, False)
        waits = []
        wd = self.waited[eng]
        for s, v in need.items():
            if wd.get(s, 0) < v:
                wd[s] = v
                waits.append((s, v))
        if dma is None:
            self.cnt[eng] += 1
            tok = (own, self.cnt[eng])
            inc = (own, 1)
        else:
            sname = "d_" + dma
            self.semnames.add(sname)
            self.dcnt[sname] = self.dcnt.get(sname, 0) + 16
            tok = (sname, self.dcnt[sname])
            inc = (sname, 16)
        self.ops[eng].append((waits, fn, inc))
        for b in writes:
            b.last_w = tok
            b.readers = {}
        for b in reads:
            if b.readers.get(tok[0], 0) < tok[1]:
                b.readers[tok[0]] = tok[1]
        return tok

    def final_wait(self, eng, toks):
        self.ops[eng].append((list(toks), None, None))

    def emit(self, block, sems):
        engmap = {"pe": "tensor", "dve": "vector", "act": "scalar", "pool": "gpsimd", "sp": "sync"}
        for e in ENGS:
            ops = self.ops[e]
            if not ops:
                continue

            def body(engine, ops=ops):
                for waits, fn, inc in ops:
                    for s, v in waits:
                        engine.wait_ge(sems[s], v)
                    if fn is not None:
                        fn(engine).then_inc(sems[inc[0]], inc[1])

            getattr(block, engmap[e])(body)


def _slopes():
    sl = np.exp2(-(8.0 / 16) * np.arange(1, 17, dtype=np.float32)).astype(np.float32)
    return sl[:8], sl[8:]


def _host_tables():
    sl_b, sl_a = _slopes()
    p = np.arange(128, dtype=np.float32)
    k = p[:, None]
    q = p[None, :]
    tri = np.where(k <= q, 0.0, NEGM).astype(np.float32)
    prv = np.where(k > q, 0.0, NEGM).astype(np.float32)
    t = {}
    t["c_ident"] = np.eye(128, dtype=np.float32)
    t["c_tri"] = tri
    t["c_mcur"] = np.tile(tri, (1, 4))
    t["c_mprev"] = np.tile(prv, (1, 4))
    bA = np.zeros((128, 128), np.float32)
    for h in range(8):
        for di in range(16):
            bA[:, h * 16 + di] = sl_a[h] * (p - 128.0 * (di - 3))
    t["c_biasA"] = bA
    bB = np.zeros((128, 16), np.float32)
    for h in range(8):
        bB[:, h * 2 + 0] = sl_b[h] * (p - 64.0)
        bB[:, h * 2 + 1] = sl_b[h] * (p - 192.0)
    t["c_biasB"] = bB
    t1 = np.zeros((128, 2, 2, 128), np.float32)
    for g in range(2):
        for e in range(2):
            t1[64:, g, e, :] = sl_b[4 * g + 2 * e] * (p - 64.0)
            t1[:64, g, e, :] = sl_b[4 * g + 2 * e + 1] * (p - 64.0)
    t["c_t1"] = t1.reshape(128, 512)
    sel = np.zeros((64, 4, 72), np.float32)
    for bi in range(4):
        b = 4 + bi
        for n in range(b):
            for m in range(b):
                if n != m:
                    sel[n * 8 + m, bi, 64 + n] = 1.0
    t["c_sel"] = sel.reshape(64, 288)
    E = np.zeros((8, S), np.float32)
    for n in range(8):
        E[n, n * 256:(n + 1) * 256] = 1.0
    t["c_E"] = E
    t["c_zero"] = np.zeros((8, 8 * T), np.float32)
    return t


def _chunk_pieces(k, w_in, w_a, w_b, w_out, w_up, w_down):
    win = w_in.rearrange("(kc p) n -> p kc n", p=128)
    if k < 10:
        g, kh = divmod(k, 2)
        nc_ = 512 if g < 4 else 256
        return [(0, (4, nc_), win[:, kh * 4:kh * 4 + 4, g * 512:g * 512 + nc_])]
    k -= 10
    if k < 16:
        n, part = divmod(k, 2)
        if part == 0:
            return [(0, (8, 128), win[:, :, 2304 + n * 128:2304 + (n + 1) * 128]),
                    (1024, (8, 128), win[:, :, 3328 + n * 128:3328 + (n + 1) * 128])]
        wa = w_a.rearrange("(kc p) n -> p kc n", p=128)
        wb = w_b.rearrange("(kc p) n -> p kc n", p=128)
        return [(0, (4, 128), wa[:, :, n * 128:(n + 1) * 128]),
                (512, (4, 128), wb[:, :, n * 128:(n + 1) * 128])]
    k -= 16
    if k < 4:
        half, kh = divmod(k, 2)
        wo = w_out.rearrange("(kc p) n -> p kc n", p=128)
        return [(0, (4, 512), wo[:, kh * 4:kh * 4 + 4, half * 512:(half + 1) * 512])]
    k -= 4
    if k < 16:
        gch, kh = divmod(k, 2)
        wu = w_up.rearrange("(kc p) n -> p kc n", p=128)
        return [(0, (4, 512), wu[:, kh * 4:kh * 4 + 4, gch * 512:(gch + 1) * 512])]
    k -= 16
    half, q = divmod(k, 8)
    wd = w_down.rearrange("(fc p) n -> p fc n", p=128)
    return [(0, (4, 512), wd[:, q * 4:q * 4 + 4, half * 512:(half + 1) * 512])]


class _Stop(Exception):
    pass


def build_nc(stop=None, dump=()):
    nc = bass.Bass("TRN2", target_bir_lowering=False)
    dr = lambda n, s, kind="ExternalInput", dt=F32: nc.dram_tensor(n, s, dt, kind=kind).ap()
    x = dr("x", [NSEQ, S, D])
    w_in = dr("w_in", [D, INW])
    w_a = dr("w_a", [512, D])
    w_b = dr("w_b", [512, D])
    w_out = dr("w_out", [D, D])
    w_up = dr("w_up", [D, DFF])
    w_down = dr("w_down", [DFF, D])
    g1_d = dr("g1", [128, 8])
    g2_d = dr("g2", [128, 8])
    gv_d = dr("gv", [64, 4])
    sbc_d = dr("sbc", [128, 4])
    ctab = {n: dr(n, list(a.shape)) for n, a in _host_tables().items()}
    y = dr("y", [NSEQ, S, D], kind="ExternalOutput")
    scr = nc.dram_tensor("scr", [NCHUNK, 128, WSZ], BF16).ap()

    with ExitStack() as es:
        sb = lambda n, s, d: es.enter_context(nc.sbuf_tensor(n, s, d))
        KA = sb("KA", [72, 8, S], BF16)
        VA = sb("VA", [128, 16, 4, 192], BF16)
        KB = sb("KB", [64, 2, 1024], BF16)
        VB = sb("VB", [128, 8, 320], BF16)
        QA = sb("QA", [72, 8, T], BF16)
        QB = sb("QB", [64, 8, T], BF16)
        xs = [sb("xs%d" % i, [128, D], F32) for i in range(2)]
        x1 = sb("x1", [128, 4, D], F32)
        hT = sb("hT", [128, 8, T], BF16)
        xhat = [sb("xhat%d" % i, [128, D], BF16) for i in range(4)]
        qkh = [sb("qkh%d" % i, [128, 512], BF16) for i in range(3)]
        sq = sb("sq", [128, 512], F32)
        PT = [sb("PT%d" % i, [128, 512], BF16) for i in range(4)]
        oaT = sb("oaT", [128, 4, T], BF16)
        obT = sb("obT", [128, 4, T], BF16)
        sga = sb("sga", [128, 512], F32)
        sgb = sb("sgb", [128, 512], F32)
        tmpa = sb("tmpa", [128, 512], F32)
        tmpr = sb("tmpr", [128, 512], F32)
        HM = sb("HM", [128, 32, T], BF16)
        W = [sb("W%d" % i, [128, WSZ], BF16) for i in range(NS)]
        R = sb("R", [128, 512], F32)
        ident = sb("ident", [128, 128], BF16)
        tri = sb("tri", [128, 128], BF16)
        mcur = sb("mcur", [128, 512], BF16)
        mprev = sb("mprev", [128, 512], BF16)
        biasA = sb("biasA", [128, 128], F32)
        biasB = sb("biasB", [128, 16], F32)
        t1 = sb("t1", [128, 512], F32)
        sinkT = sb("sinkT", [128, 512], F32)
        sbc = sb("sbc_s", [128, 4], F32)
        sel = sb("sel", [64, 288], BF16)
        g1 = sb("g1_s", [128, 8], F32)
        g2 = sb("g2_s", [128, 8], F32)
        gv = sb("gv_s", [64, 4], F32)
        st = sb("st", [128, 64], F32)
        epsT = sb("epsT", [128, 1], F32)
        ksum = sb("ksum", [64, 8, 8], F32)
        dlt = sb("dlt", [64, 8, 64], BF16)
        ind = [sb("ind%d" % i, [64, 512], BF16) for i in range(2)]
        bank = [es.enter_context(nc.psum_tensor("pb%d" % i, [128, 512], F32)) for i in range(8)]
        tpbs = [bank[7][:, :].bitcast(BF16), bank[6][:, :].bitcast(BF16)]

        P = Prog()

        def op(eng, fn, r=(), w=(), dma=None):
            return P.op(eng, fn, r, w, dma)

        def mm(out, lhsT, rhs, start, stop, r, w):
            op("pe", lambda e: e.matmul(out, lhsT=lhsT, rhs=rhs, start=start, stop=stop), r, w)

        def act(out, in_, func, r, w, **kw):
            op("act", lambda e: e.activation(out=out, in_=in_, func=func, **kw), r, w)

        def tt(eng, out, in0, in1, alu, r, w):
            op(eng, lambda e: e.tensor_tensor(out=out, in0=in0, in1=in1, op=alu), r, w)

        def ts(eng, out, in0, s1, s2, op0, op1, r, w):
            if op1 is None:
                op(eng, lambda e: e.tensor_scalar(out=out, in0=in0, scalar1=s1, scalar2=None, op0=op0), r, w)
            else:
                op(eng, lambda e: e.tensor_scalar(out=out, in0=in0, scalar1=s1, scalar2=s2, op0=op0, op1=op1), r, w)

        cidx = [0]

        def cload(dst, src, name, eng="pool"):
            cidx[0] += 1
            op(eng, lambda e: e.dma_start(out=dst, in_=src), (), [name], dma="c%d" % cidx[0])

        cload(ident[:], ctab["c_ident"], "ident")
        cload(tri[:], ctab["c_tri"], "tri")
        cload(mcur[:], ctab["c_mcur"], "mcur")
        cload(mprev[:], ctab["c_mprev"], "mprev")
        cload(sel[:], ctab["c_sel"], "sel")
        cload(biasA[:], ctab["c_biasA"], "biasA", "sp")
        cload(biasB[:], ctab["c_biasB"], "biasB", "sp")
        cload(t1[:], ctab["c_t1"], "t1", "sp")
        cload(sbc[:], sbc_d, "sbc", "sp")
        cload(g1[:], g1_d, "g1", "sp")
        cload(g2[:], g2_d, "g2", "sp")
        cload(gv[:], gv_d, "gv", "sp")
        for h in range(8):
            cload(KA[64:72, h, :], ctab["c_E"], "KAE")
        op("dve", lambda e: e.memset(VA[:, :, :, 64:128], 1.0), (), ["VAones"])
        op("dve", lambda e: e.memset(VB[:, :, 0:64], 1.0), (), ["VBones"])
        op("dve", lambda e: e.memset(VB[:, :, 128:192], 1.0), (), ["VBones"])
        op("dve", lambda e: e.memset(VB[:, :, 256:320], 1.0), (), ["VBones"])
        op("dve", lambda e: e.memset(ksum[:], 0.0), (), ["ksum"])
        op("dve", lambda e: e.memset(epsT[:], EPS), (), ["epsT"])
        for j in range(4):
            act(sinkT[:, j * 128:(j + 1) * 128], t1[:, j * 128:(j + 1) * 128], AF.Exp, ["t1", "sbc"], ["sinkT"],
                bias=sbc[:, j:j + 1], scale=1.0)

        seq = [(gt, k) for gt in range(NSEQ * NT) for k in range(NCHUNK)]
        wstate = {"next": 0}

        def wload(idx):
            gt, k = seq[idx]
            slot = idx % NS
            wt = W[slot]
            if gt == 0:
                for (off, (kk, ncol), src) in _chunk_pieces(k, w_in, w_a, w_b, w_out, w_up, w_down):
                    dst = wt[:, off:off + kk * ncol].rearrange("p (a b) -> p a b", b=ncol)
                    op("pool", lambda e, dst=dst, src=src: e.dma_start(out=dst, in_=src), (), ["W%d" % slot], dma="w%d" % slot)
                op("sp", lambda e: e.dma_start(out=scr[k], in_=wt[:]), ["W%d" % slot], ["scr%d" % k], dma="ws%d" % slot)
            else:
                op("sp", lambda e: e.dma_start(out=wt[:], in_=scr[k]), ["scr%d" % k], ["W%d" % slot], dma="w%d" % slot)

        def wneed(idx, ahead=NS - 1):
            lim = min(idx + ahead, len(seq) - 1)
            while wstate["next"] <= lim:
                wload(wstate["next"])
                wstate["next"] += 1
            return W[idx % NS], "W%d" % (idx % NS)

        mmb = [0]

        def next_bank():
            b = mmb[0] % 3
            mmb[0] += 1
            return bank[b], "pb%d" % b

        tph = [0]

        def next_tp():
            h = tph[0] % 2
            tph[0] += 1
            return tpbs[h][:, 0:512], "pb%d" % (7 - h)

        def norm_act(src_ap, src_reads, xi, si):
            xh, xhn = xhat[xi], "xhat%d" % xi
            c0 = (si % 4) * 4
            act(xh[:], src_ap, AF.Square, src_reads, [xhn, "st%d" % c0], accum_out=st[:, c0:c0 + 1])
            act(st[:, c0 + 2:c0 + 3], st[:, c0:c0 + 1], AF.Sqrt, ["st%d" % c0, "epsT"], ["st%d" % (c0 + 2)], scale=1.0 / D, bias=epsT[:, 0:1])
            op("dve", lambda e: e.reciprocal(out=st[:, c0 + 3:c0 + 4], in_=st[:, c0 + 2:c0 + 3]), ["st%d" % (c0 + 2)], ["st%d" % (c0 + 3)])
            act(xh[:], src_ap, AF.Copy, list(src_reads) + ["st%d" % (c0 + 3)], [xhn], scale=st[:, c0 + 3:c0 + 4])

        def norm_pe_half(xi, gain, gname, dst_cols, half):
            xh, xhn = xhat[xi], "xhat%d" % xi
            tp, tpn = next_tp()
            for j in range(4):
                jj = half * 4 + j
                op("pe", lambda e, jj=jj, j=j, tp=tp: e.transpose(out=tp[:, j * 128:(j + 1) * 128], in_=xh[:, jj * 128:(jj + 1) * 128], identity=ident[:, :]),
                   [xhn, "ident"], [tpn])
            tt("dve", hT[:, half * 4:half * 4 + 4, dst_cols], tp.rearrange("p (j q) -> p j q", q=128),
               gain[:, half * 4:half * 4 + 4].unsqueeze(2).broadcast_to([128, 4, 128]), ALU.mult, [tpn, gname], ["hT"])

        def qk_norm(ps, psn, nh, col0, qb_):
            n = nh * 64
            qk = qkh[qb_]
            act(sq[:, 0:n], ps, AF.Square, [psn], ["sq"])
            op("dve", lambda e: e.tensor_reduce(out=st[:, 32:32 + nh], in_=sq[:, 0:n].rearrange("p (h d) -> p h d", d=64), axis=AX.X, op=ALU.add),
               ["sq"], ["stq"])
            act(st[:, 48:48 + nh], st[:, 32:32 + nh], AF.Sqrt, ["stq", "epsT"], ["stq3"], scale=1.0 / HD, bias=epsT[:, 0:1])
            op("dve", lambda e: e.reciprocal(out=st[:, 56:56 + nh], in_=st[:, 48:48 + nh]), ["stq3"], ["stq4"])
            tt("dve", qk[:, 0:n].rearrange("p (h d) -> p h d", d=64), ps.rearrange("p (h d) -> p h d", d=64),
               st[:, 56:56 + nh].unsqueeze(2).broadcast_to([128, nh, 64]), ALU.mult, [psn, "stq4"], ["qkh%d" % qb_])

        def head_T(col0, nh, dst_fn, gcol, dname, qb_):
            qk = qkh[qb_]
            for h0 in range(0, nh, 4):
                nhh = min(4, nh - h0)
                tp, tpn = next_tp()
                for j in range(nhh):
                    c = (h0 + j) * 64
                    op("pe", lambda e, c=c, j=j, tp=tp: e.transpose(out=tp[0:64, j * 128:(j + 1) * 128], in_=qk[:, c:c + 64], identity=ident[:, :]),
                       ["qkh%d" % qb_, "ident"], [tpn])
                act(dst_fn(h0, nhh), tp[0:64, 0:nhh * 128].rearrange("p (j q) -> p j q", q=128), AF.Copy, [tpn, "gv"], [dname],
                    scale=gv[:, gcol:gcol + 1])

        ychunk = [0]
        ytok = None

        def stage(n):
            if stop == n:
                raise _Stop()

        widx = [0]

        tiles = [(s_, c_) for s_ in range(NSEQ) for c_ in range(NT)]

        def A0_load(t, i):
            s_, c_ = tiles[t]
            xb, xn = xs[i % 2], "xs%d" % (i % 2)
            src = x[s_, c_ * T + i * 128:c_ * T + (i + 1) * 128, :]
            op("sp", lambda e: e.dma_start(out=xb[:], in_=src), (), [xn], dma=xn)

        def A0_act(t, i):
            norm_act(xs[i % 2][:], ["xs%d" % (i % 2)], i, i)

        def A0_pe(t, i, half):
            norm_pe_half(i, g1, "g1", slice(i * 128, (i + 1) * 128), half)

        try:
          stage('const')
          for i in range(4):
              A0_load(0, i)
              A0_act(0, i)
          for i in range(4):
              for half in range(2):
                  A0_pe(0, i, half)
          for s in range(NSEQ):
            for c in range(NT):
                T0 = c * T
                base = widx[0]
                tcur = s * NT + c
                has_next = tcur + 1 < len(tiles)
                hbk = (3, 4, 5, 0)
                mmb[0] = 1
                if c == 0:
                    op("pool", lambda e: e.dma_start(out=QA[64:72, :, :].rearrange("p h q -> p (h q)"), in_=ctab["c_zero"]), (), ["QAm"], dma="qam")
                stage('A0')
                srcx = x[s, T0:T0 + T, :].rearrange("(i p) d -> p i d", p=128)
                op("sp", lambda e, srcx=srcx: e.dma_start(out=x1[:], in_=srcx), (), ["x1_0", "x1_1", "x1_2", "x1_3"], dma="x1")
                pend = []

                def flush(keep):
                    while len(pend) > keep:
                        f_ = pend.pop(0)
                        if f_ is not None:
                            f_()

                gi = 0
                for g in range(5):
                    w0, w0n = wneed(base + 2 * g)
                    w1, w1n = wneed(base + 2 * g + 1, NS - 2)
                    ncol = 512 if g < 4 else 256
                    for i in range(4):
                        pb, pbn = next_bank()
                        kt = 4 * c + i
                        qb_ = gi % 3
                        gi += 1
                        for k in range(8):
                            ww = (w0 if k < 4 else w1)[:, (k % 4) * ncol:(k % 4 + 1) * ncol]
                            mm(pb[:, 0:ncol], hT[:, k, i * 128:(i + 1) * 128], ww, k == 0, k == 7,
                               ["hT", w0n if k < 4 else w1n], [pbn])
                        qc = slice(i * 128, (i + 1) * 128)
                        nxt = None
                        if g == 0:
                            qk_norm(pb[:, 0:512], pbn, 8, 0, qb_)
                            nxt = lambda qc=qc, qb_=qb_: head_T(0, 8, lambda h0, n: QA[0:64, h0:h0 + n, qc], 0, "QA", qb_)
                        elif g == 1:
                            qk_norm(pb[:, 0:512], pbn, 8, 512, qb_)
                            kc = slice(T0 + i * 128, T0 + (i + 1) * 128)
                            nxt = lambda kc=kc, qb_=qb_: head_T(512, 8, lambda h0, n: KA[0:64, h0:h0 + n, kc], 1, "KA", qb_)
                        elif g == 2:
                            pv = pb[:, 0:512].rearrange("p (pr two d) -> p pr two d", two=2, d=64)
                            act(VA[:, kt, :, 0:64], pv[:, :, 0, :], AF.Copy, [pbn], ["VA"])
                            act(VA[:, kt, :, 128:192], pv[:, :, 1, :], AF.Copy, [pbn], ["VA"])
                        elif g == 3:
                            qk_norm(pb[:, 0:512], pbn, 8, 1024, qb_)
                            nxt = lambda qc=qc, qb_=qb_: head_T(1024, 8, lambda h0, n: QB[0:64, h0:h0 + n, qc], 2, "QB", qb_)
                        else:
                            qk_norm(pb[:, 0:128], pbn, 2, 1536, qb_)
                            ks = kt % 8
                            nxt = lambda ks=ks, qb_=qb_: head_T(1536, 2, lambda h0, n: KB[0:64, h0:h0 + n, ks * 128:(ks + 1) * 128], 3, "KB", qb_)
                            act(VB[:, ks, 64:128], pb[:, 128:192], AF.Copy, [pbn], ["VB"])
                            act(VB[:, ks, 192:256], pb[:, 192:256], AF.Copy, [pbn], ["VB"])
                        pend.append(nxt)
                        flush(2)
                flush(0)
                wneed(base + 10, 3)
                for h in range(8):
                    ko = ksum[:, h, 2 * c:2 * c + 2]
                    ki = KA[0:64, h, T0:T0 + T].rearrange("p (n k) -> p n k", k=256)
                    op("dve", lambda e, ko=ko, ki=ki: e.tensor_reduce(out=ko, in_=ki, axis=AX.X, op=ALU.add), ["KA"], ["ksum"])
                if c >= 2:
                    gbk = (3, 4, 0, 1)

                    def g1_(h):
                        tt("dve", dlt[:, h, :].rearrange("p (n m) -> p n m", m=8),
                           ksum[:, h, :].unsqueeze(2).broadcast_to([64, 8, 8]),
                           ksum[:, h, :].unsqueeze(1).broadcast_to([64, 8, 8]), ALU.subtract, ["ksum"], ["dlt%d" % h])
                        gb_, gbn = bank[gbk[h % 4]], "pb%d" % gbk[h % 4]
                        mm(gb_[0:64, :], dlt[:, h, :], QA[0:64, h, :], True, True, ["dlt%d" % h, "QA"], [gbn])

                    def g2_(h):
                        gb_, gbn = bank[gbk[h % 4]], "pb%d" % gbk[h % 4]
                        iv, ivn = ind[h % 2], "ind%d" % (h % 2)
                        op("dve", lambda e: e.tensor_single_scalar(out=iv[:, :], in_=gb_[0:64, :], scalar=0.0, op=ALU.is_lt), [gbn], [ivn])
                        for hb in range(2):
                            bi = 2 * c + hb - 4
                            mm(gb_[0:72, hb * 256:(hb + 1) * 256], sel[:, bi * 72:(bi + 1) * 72], iv[:, hb * 256:(hb + 1) * 256], True, True,
                               ["sel", ivn], [gbn])

                    def g3_(h):
                        gb_, gbn = bank[gbk[h % 4]], "pb%d" % gbk[h % 4]
                        ts("dve", QA[64:72, h, :], gb_[64:72, :], 2.5, NEGM, ALU.is_ge, ALU.mult, [gbn], ["QAm"])

                    for k in range(10):
                        if k < 8:
                            g1_(k)
                        if 0 <= k - 1 < 8:
                            g2_(k - 1)
                        if 0 <= k - 2 < 8:
                            g3_(k - 2)
                stage('gate%d' % c)
                steps = [(h, j) for h in range(8) for j in range(4 * c + 4)]
                nst = len(steps)

                def moba_S(si):
                    h, j = steps[si]
                    qs = 128 * max(0, j - 4 * c)
                    N = T - qs
                    sb_i = (3, 4, 0)[si % 3]
                    sbk, sbn = bank[sb_i], "pb%d" % sb_i
                    diag = j >= 4 * c
                    mm(sbk[:, 0:N], KA[0:72, h, j * 128:(j + 1) * 128], QA[0:72, h, qs:T], True, not diag,
                       ["KA", "KAE", "QA", "QAm"], [sbn])
                    if diag:
                        mm(sbk[:, 0:128], ident[:, :], tri[:, :], False, True, ["ident", "tri"], [sbn])
                    di = 4 * c - j + 3
                    pt = PT[si % 4]
                    act(pt[:, 0:N], sbk[:, 0:N], AF.Exp, [sbn, "biasA"], ["PT%d" % (si % 4)],
                        bias=biasA[:, h * 16 + di:h * 16 + di + 1], scale=SCALE)

                def moba_PV(si):
                    h, j = steps[si]
                    qs = 128 * max(0, j - 4 * c)
                    N = T - qs
                    ob_, obn = bank[5 + h % 2], "pb%d" % (5 + h % 2)
                    pr, od = divmod(h, 2)
                    lhsT = VA[:, j, pr, od * 64:od * 64 + 128]
                    mm(ob_[:, qs:T], lhsT, PT[si % 4][:, 0:N], j == 0, j == 4 * c + 3, ["VA", "VAones", "PT%d" % (si % 4)], [obn])
                    if j == 4 * c + 3:
                        lo, hi = (0, 64) if od == 0 else (64, 128)
                        dl, dh = (64, 128) if od == 0 else (0, 64)
                        rn = "R%d" % od
                        act(R[dl:dh, :], ob_[dl:dh, :], AF.Copy, [obn], [rn])
                        op("dve", lambda e: e.reciprocal(out=R[dl:dh, :], in_=R[dl:dh, :]), [rn], [rn])
                        tt("dve", oaT[lo:hi, pr, :], ob_[lo:hi, :], R[dl:dh, :], ALU.mult, [obn, rn], ["oaT%d" % od])

                moba_S(0)
                moba_S(1)
                for si in range(nst):
                    if si + 2 < nst:
                        moba_S(si + 2)
                    moba_PV(si)
                stage('B')
                sw = []
                for i in range(4):
                    kt = 4 * c + i
                    for g in range(2):
                        passes = [(kt % 8, mcur, "mcur", 0)] + ([((kt - 1) % 8, mprev, "mprev", 1)] if kt >= 1 else [])
                        for pi, (kslot, mk, mkn, which) in enumerate(passes):
                            sw.append((i, g, pi, len(passes), kslot, mk, mkn, which))

                def swa_S(n):
                    i, g, pi, npass, kslot, mk, mkn, which = sw[n]
                    sbk, sbn = bank[3 + n % 2], "pb%d" % (3 + n % 2)
                    mm(sbk[:, :], KB[0:64, g, kslot * 128:(kslot + 1) * 128], QB[0:64, 4 * g:4 * g + 4, i * 128:(i + 1) * 128],
                       True, False, ["KB", "QB"], [sbn])
                    mm(sbk[:, :], ident[:, :], mk[:, :], False, True, ["ident", mkn], [sbn])
                    pt = PT[n % 4]
                    for hh in range(4):
                        h = 4 * g + hh
                        act(pt[:, hh * 128:(hh + 1) * 128], sbk[:, hh * 128:(hh + 1) * 128], AF.Exp, [sbn, "biasB"], ["PT%d" % (n % 4)],
                            bias=biasB[:, h * 2 + which:h * 2 + which + 1], scale=SCALE)

                def swa_PV(n):
                    i, g, pi, npass, kslot, mk, mkn, which = sw[n]
                    ob_, obn = bank[5 + (i * 2 + g) % 2], "pb%d" % (5 + (i * 2 + g) % 2)
                    pt = PT[n % 4]
                    ptn = "PT%d" % (n % 4)
                    pt3 = pt[:, :].rearrange("p (a e q) -> p a e q", e=2, q=128)
                    last = pi == npass - 1
                    mm(ob_[:, 0:256], VB[:, kslot, 64 + 128 * g:64 + 128 * g + 128], pt3[:, :, 0, :], pi == 0, last,
                       ["VB", "VBones", ptn], [obn])
                    mm(ob_[:, 256:512], VB[:, kslot, 128 * g:128 * g + 128], pt3[:, :, 1, :], False, last,
                       ["VB", "VBones", ptn], [obn])
                    if not last:
                        return
                    tt("dve", R[64:128, 0:256], ob_[64:128, 0:256], sinkT[64:128, g * 256:(g + 1) * 256], ALU.add, [obn, "sinkT"], ["R0"])
                    tt("dve", R[0:64, 256:512], ob_[0:64, 256:512], sinkT[0:64, g * 256:(g + 1) * 256], ALU.add, [obn, "sinkT"], ["R1"])
                    op("dve", lambda e: e.reciprocal(out=R[64:128, 0:256], in_=R[64:128, 0:256]), ["R0"], ["R0"])
                    op("dve", lambda e: e.reciprocal(out=R[0:64, 256:512], in_=R[0:64, 256:512]), ["R1"], ["R1"])
                    qc = slice(i * 128, (i + 1) * 128)
                    tt("dve", obT[0:64, 2 * g:2 * g + 2, qc], ob_[0:64, 0:256].rearrange("p (a q) -> p a q", q=128),
                       R[64:128, 0:256].rearrange("p (a q) -> p a q", q=128), ALU.mult, [obn, "R0"], ["obT0"])
                    tt("dve", obT[64:128, 2 * g:2 * g + 2, qc], ob_[64:128, 256:512].rearrange("p (a q) -> p a q", q=128),
                       R[0:64, 256:512].rearrange("p (a q) -> p a q", q=128), ALU.mult, [obn, "R1"], ["obT1"])

                swa_S(0)
                for n in range(len(sw)):
                    if n + 1 < len(sw):
                        swa_S(n + 1)
                    swa_PV(n)
                stage('C')
                for n in range(8):
                    wg, wgn = wneed(base + 10 + 2 * n)
                    wab, wabn = wneed(base + 10 + 2 * n + 1, NS - 2)
                    pa, pan = next_bank()
                    for k in range(8):
                        mm(pa[:, :], wg[:, k * 128:(k + 1) * 128], hT[:, k, :], k == 0, k == 7, [wgn, "hT"], [pan])
                    act(sga[:], pa[:, :], AF.Sigmoid, [pan], ["sga"])
                    pb2, pb2n = next_bank()
                    for k in range(8):
                        mm(pb2[:, :], wg[:, 1024 + k * 128:1024 + (k + 1) * 128], hT[:, k, :], k == 0, k == 7, [wgn, "hT"], [pb2n])
                    act(sgb[:], pb2[:, :], AF.Sigmoid, [pb2n], ["sgb"])
                    pc, pcn = next_bank()
                    for k in range(4):
                        mm(pc[:, :], wab[:, k * 128:(k + 1) * 128], oaT[:, k, :], k == 0, k == 3, [wabn, "oaT0", "oaT1"], [pcn])
                    tt("dve", tmpa[:], pc[:, :], sga[:], ALU.mult, [pcn, "sga"], ["tmpa"])
                    pd, pdn = next_bank()
                    for k in range(4):
                        mm(pd[:, :], wab[:, 512 + k * 128:512 + (k + 1) * 128], obT[:, k, :], k == 0, k == 3, [wabn, "obT0", "obT1"], [pdn])
                    tt("dve", sgb[:], pd[:, :], sgb[:], ALU.mult, [pdn, "sgb"], ["sgb"])
                    tt("dve", HM[:, n, :], tmpa[:], sgb[:], ALU.add, ["tmpa", "sgb"], ["hid%d" % n])
                stage('D')
                for half in range(2):
                    w0, w0n = wneed(base + 26 + 2 * half)
                    w1, w1n = wneed(base + 26 + 2 * half + 1, NS - 2)
                    for i in range(4):
                        pb, pbn = next_bank()
                        for k in range(8):
                            ww = (w0 if k < 4 else w1)[:, (k % 4) * 512:(k % 4 + 1) * 512]
                            mm(pb[:, :], HM[:, k, i * 128:(i + 1) * 128], ww, k == 0, k == 7, ["hid%d" % k, w0n if k < 4 else w1n], [pbn])
                        xo = x1[:, i, half * 512:(half + 1) * 512]
                        tt("dve", xo, pb[:, :], xo, ALU.add, [pbn, "x1_%d" % i], ["x1_%d" % i])
                        if half == 1:
                            norm_act(x1[:, i, :], ["x1_%d" % i], i, 4 + i)
                stage('E')
                for i in range(4):
                    for half in range(2):
                        norm_pe_half(i, g2, "g2", slice(i * 128, (i + 1) * 128), half)
                stage('F')
                for gch in range(8):
                    w0, w0n = wneed(base + 30 + 2 * gch)
                    w1, w1n = wneed(base + 30 + 2 * gch + 1, NS - 2)
                    for f in range(4):
                        pb, pbn = next_bank()
                        for k in range(8):
                            ww = (w0 if k < 4 else w1)[:, (k % 4) * 512 + f * 128:(k % 4) * 512 + (f + 1) * 128]
                            mm(pb[:, :], ww, hT[:, k, :], k == 0, k == 7, ["hT", w0n if k < 4 else w1n], [pbn])
                        act(tmpr[:], pb[:, :], AF.Relu, [pbn], ["tmpr"])
                        tt("dve", HM[:, gch * 4 + f, :], pb[:, :], tmpr[:], ALU.mult, [pbn, "tmpr"], ["hid%d" % (gch * 4 + f)])
                        if has_next and f == 3:
                            sched = {0: ("L0", "L1"), 2: ("A0", "L2"), 3: ("A1", "L3"), 5: ("A2",), 6: ("A3",)}
                            for what in sched.get(gch, ()):
                                (A0_load if what[0] == "L" else A0_act)(tcur + 1, int(what[1]))
                stage('G')
                for half in range(2):
                    for q in range(8):
                        wd, wdn = wneed(base + 46 + half * 8 + q)
                        for f in range(4):
                            ff = q * 4 + f
                            for i in range(4):
                                mm(bank[hbk[i]][:, :], HM[:, ff, i * 128:(i + 1) * 128], wd[:, f * 512:(f + 1) * 512], ff == 0, ff == 31,
                                   ["hid%d" % ff, wdn], ["pb%d" % hbk[i]])
                        if has_next and half == 0:
                            A0_pe(tcur + 1, q // 2, q % 2)
                    for i in range(4):
                        xo = x1[:, i, half * 512:(half + 1) * 512]
                        tt("dve", xo, bank[hbk[i]][:, :], xo, ALU.add, ["pb%d" % hbk[i], "x1_%d" % i], ["x1_%d" % i])
                dsty = y[s, T0:T0 + T, :].rearrange("(i p) d -> p i d", p=128)
                ytok = op("pool", lambda e, dsty=dsty: e.dma_start(out=dsty, in_=x1[:]), ["x1_0", "x1_1", "x1_2", "x1_3"], (), dma="y")
                widx[0] += NCHUNK
                stage('T%d' % (s * NT + c))
        except _Stop:
            pass
        if ytok is not None:
            P.final_wait("pool", [ytok])
        if dump:
            tens = {"hT": hT, "QA": QA, "KA": KA, "VA": VA, "QB": QB, "KB": KB, "VB": VB, "oaT": oaT, "obT": obT,
                    "HM": HM, "x1": x1, "ksum": ksum, "sinkT": sinkT, "R": R, "st": st, "qkh": qkh[0], "ind": ind[0], "dlt": dlt}
            allb = list(P.bufs.keys())
            dtoks = []
            for nm in dump:
                tns = tens[nm]
                shp = list(tns.shape)
                flat = tns[:]
                if len(shp) == 3:
                    flat = flat.rearrange("p a b -> p (a b)")
                elif len(shp) == 4:
                    flat = flat.rearrange("p a b c -> p (a b c)")
                fr = int(np.prod(shp[1:]))
                dd = nc.dram_tensor("dbg_" + nm, [shp[0], fr], tns.dtype, kind="ExternalOutput").ap()
                dtoks.append(op("sp", lambda e, dd=dd, flat=flat: e.dma_start(out=dd, in_=flat), allb, (), dma="dbg_" + nm))
            P.final_wait("sp", dtoks)
        sems = {n: es.enter_context(nc.semaphore(n)) for n in sorted(P.semnames)}
        with nc.Block() as block:
            P.emit(block, sems)
    return nc


_NC_CACHE = {}


def kernel(x, norm_attn, w_in, q_norm_a, k_norm_a, q_norm_b, k_norm_b, sinks_b,
           w_branch_a, w_branch_b, w_out, norm_mlp, w_up, w_down):
    f = lambda a: np.ascontiguousarray(np.asarray(a, dtype=np.float32))
    x = f(x)
    tabs = _host_tables()
    g1 = f(np.asarray(norm_attn)[0].reshape(8, 128).T)
    g2 = f(np.asarray(norm_mlp)[0].reshape(8, 128).T)
    gv = f(np.stack([np.asarray(q_norm_a)[0], np.asarray(k_norm_a)[0], np.asarray(q_norm_b)[0], np.asarray(k_norm_b)[0]], axis=1))
    sk = np.asarray(sinks_b)[0]
    sbc = np.zeros((128, 4), np.float32)
    for g in range(2):
        for a in range(2):
            sbc[64:, 2 * g + a] = sk[4 * g + 2 * a]
            sbc[:64, 2 * g + a] = sk[4 * g + 2 * a + 1]
    common = {
        "w_in": f(np.asarray(w_in)[0]), "w_a": f(np.asarray(w_branch_a)[0]), "w_b": f(np.asarray(w_branch_b)[0]),
        "w_out": f(np.asarray(w_out)[0]), "w_up": f(np.asarray(w_up)[0]), "w_down": f(np.asarray(w_down)[0]),
        "g1": g1, "g2": g2, "gv": gv, "sbc": sbc,
    }
    common.update(tabs)
    if "nc" not in _NC_CACHE:
        _NC_CACHE["nc"] = build_nc()
    nc = _NC_CACHE["nc"]
    in_maps = []
    for c in range(NCORES):
        m = dict(common)
        m["x"] = np.ascontiguousarray(x[NSEQ * c:NSEQ * (c + 1)])
        in_maps.append(m)
    res = run_bass_kernel_spmd(nc, in_maps, core_ids=list(range(NCORES)))
    return np.concatenate([r["y"] for r in res.results], axis=0).astype(np.float32)
```

```python
import os
import numpy as np
from contextlib import ExitStack
import concourse.bass as bass
import concourse.mybir as mybir
from concourse.bass_utils import run_bass_kernel_spmd

F32 = mybir.dt.float32
BF16 = mybir.dt.bfloat16
AF = mybir.ActivationFunctionType
ALU = mybir.AluOpType
AX = mybir.AxisListType

NCORES = 8
D = 1024
S = 2048
NSEQ = 2
T = 512
NT = S // T
HD = 64
DFF = 4096
INW = 4352
NEGM = -30000.0
EPS = 1e-6
SCALE = HD ** -0.5
NS = 4
WSZ = 2048
NCHUNK = 62
ENGS = ("pe", "dve", "act", "pool", "sp")


class Buf:
    __slots__ = ("name", "last_w", "readers")

    def __init__(self, name):
        self.name = name
        self.last_w = None
        self.readers = {}


class Prog:
    def __init__(self):
        self.ops = {e: [] for e in ENGS}
        self.cnt = {e: 0 for e in ENGS}
        self.dcnt = {}
        self.waited = {e: {} for e in ENGS}
        self.semnames = set("e_" + e for e in ENGS)
        self.bufs = {}

    def buf(self, name):
        b = self.bufs.get(name)
        if b is None:
            b = self.bufs[name] = Buf(name)
        return b

    def op(self, eng, fn, reads=(), writes=(), dma=None):
        reads = [self.buf(r) for r in reads]
        writes = [self.buf(w) for w in writes]
        own = "e_" + eng
        need = {}

        def add(sem, val, same_ok):
            if sem == own:
                if eng == "pe" or not same_ok:
                    return
            if need.get(sem, 0) < val:
                need[sem] = val

        for b in reads:
            if b.last_w is not None:
                add(b.last_w[0], b.last_w[1], True)
            if b.name.startswith("pb"):
                for s, v in b.readers.items():
                    add(s, v, False)
        for b in writes:
            if b.last_w is not None:
                add(b.last_w[0], b.last_w[1], True)
            for s, v in b.readers.items():
                add(s, v, False)
        waits = []
        wd = self.waited[eng]
        for s, v in need.items():
            if wd.get(s, 0) < v:
                wd[s] = v
                waits.append((s, v))
        if dma is None:
            self.cnt[eng] += 1
            tok = (own, self.cnt[eng])
            inc = (own, 1)
        else:
            sname = "d_" + dma
            self.semnames.add(sname)
            self.dcnt[sname] = self.dcnt.get(sname, 0) + 16
            tok = (sname, self.dcnt[sname])
            inc = (sname, 16)
        self.ops[eng].append((waits, fn, inc))
        for b in writes:
            b.last_w = tok
            b.readers = {}
        for b in reads:
            if b.readers.get(tok[0], 0) < tok[1]:
                b.readers[tok[0]] = tok[1]
        return tok

    def final_wait(self, eng, toks):
        self.ops[eng].append((list(toks), None, None))

    def emit(self, block, sems):
        engmap = {"pe": "tensor", "dve": "vector", "act": "scalar", "pool": "gpsimd", "sp": "sync"}
        for e in ENGS:
            ops = self.ops[e]
            if not ops:
                continue

            def body(engine, ops=ops):
                for waits, fn, inc in ops:
                    for s, v in waits:
                        engine.wait_ge(sems[s], v)
                    if fn is not None:
                        fn(engine).then_inc(sems[inc[0]], inc[1])

            getattr(block, engmap[e])(body)


def _slopes():
    sl = np.exp2(-(8.0 / 16) * np.arange(1, 17, dtype=np.float32)).astype(np.float32)
    return sl[:8], sl[8:]


def _host_tables():
    sl_b, sl_a = _slopes()
    p = np.arange(128, dtype=np.float32)
    k = p[:, None]
    q = p[None, :]
    tri = np.where(k <= q, 0.0, NEGM).astype(np.float32)
    prv = np.where(k > q, 0.0, NEGM).astype(np.float32)
    t = {}
    t["c_ident"] = np.eye(128, dtype=np.float32)
    t["c_tri"] = tri
    t["c_mcur"] = np.tile(tri, (1, 4))
    t["c_mprev"] = np.tile(prv, (1, 4))
    bA = np.zeros((128, 128), np.float32)
    for h in range(8):
        for di in range(16):
            bA[:, h * 16 + di] = sl_a[h] * (p - 128.0 * (di - 3))
    t["c_biasA"] = bA
    bB = np.zeros((128, 16), np.float32)
    for h in range(8):
        bB[:, h * 2 + 0] = sl_b[h] * (p - 64.0)
        bB[:, h * 2 + 1] = sl_b[h] * (p - 192.0)
    t["c_biasB"] = bB
    t1 = np.zeros((128, 2, 2, 128), np.float32)
    for g in range(2):
        for e in range(2):
            t1[64:, g, e, :] = sl_b[4 * g + 2 * e] * (p - 64.0)
            t1[:64, g, e, :] = sl_b[4 * g + 2 * e + 1] * (p - 64.0)
    t["c_t1"] = t1.reshape(128, 512)
    sel = np.zeros((64, 4, 72), np.float32)
    for bi in range(4):
        b = 4 + bi
        for n in range(b):
            for m in range(b):
                if n != m:
                    sel[n * 8 + m, bi, 64 + n] = 1.0
    t["c_sel"] = sel.reshape(64, 288)
    E = np.zeros((8, S), np.float32)
    for n in range(8):
        E[n, n * 256:(n + 1) * 256] = 1.0
    t["c_E"] = E
    t["c_zero"] = np.zeros((8, 8 * T), np.float32)
    return t


def _chunk_pieces(k, w_in, w_a, w_b, w_out, w_up, w_down):
    win = w_in.rearrange("(kc p) n -> p kc n", p=128)
    if k < 10:
        g, kh = divmod(k, 2)
        nc_ = 512 if g < 4 else 256
        return [(0, (4, nc_), win[:, kh * 4:kh * 4 + 4, g * 512:g * 512 + nc_])]
    k -= 10
    if k < 16:
        n, part = divmod(k, 2)
        if part == 0:
            return [(0, (8, 128), win[:, :, 2304 + n * 128:2304 + (n + 1) * 128]),
                    (1024, (8, 128), win[:, :, 3328 + n * 128:3328 + (n + 1) * 128])]
        wa = w_a.rearrange("(kc p) n -> p kc n", p=128)
        wb = w_b.rearrange("(kc p) n -> p kc n", p=128)
        return [(0, (4, 128), wa[:, :, n * 128:(n + 1) * 128]),
                (512, (4, 128), wb[:, :, n * 128:(n + 1) * 128])]
    k -= 16
    if k < 4:
        half, kh = divmod(k, 2)
        wo = w_out.rearrange("(kc p) n -> p kc n", p=128)
        return [(0, (4, 512), wo[:, kh * 4:kh * 4 + 4, half * 512:(half + 1) * 512])]
    k -= 4
    if k < 16:
        gch, kh = divmod(k, 2)
        wu = w_up.rearrange("(kc p) n -> p kc n", p=128)
        return [(0, (4, 512), wu[:, kh * 4:kh * 4 + 4, gch * 512:(gch + 1) * 512])]
    k -= 16
    half, q = divmod(k, 8)
    wd = w_down.rearrange("(fc p) n -> p fc n", p=128)
    return [(0, (4, 512), wd[:, q * 4:q * 4 + 4, half * 512:(half + 1) * 512])]


class _Stop(Exception):
    pass


def build_nc(stop=None, dump=()):
    nc = bass.Bass("TRN2", target_bir_lowering=False)
    dr = lambda n, s, kind="ExternalInput", dt=F32: nc.dram_tensor(n, s, dt, kind=kind).ap()
    x = dr("x", [NSEQ, S, D])
    w_in = dr("w_in", [D, INW])
    w_a = dr("w_a", [512, D])
    w_b = dr("w_b", [512, D])
    w_out = dr("w_out", [D, D])
    w_up = dr("w_up", [D, DFF])
    w_down = dr("w_down", [DFF, D])
    g1_d = dr("g1", [128, 8])
    g2_d = dr("g2", [128, 8])
    gv_d = dr("gv", [64, 4])
    sbc_d = dr("sbc", [128, 4])
    ctab = {n: dr(n, list(a.shape)) for n, a in _host_tables().items()}
    y = dr("y", [NSEQ, S, D], kind="ExternalOutput")
    scr = nc.dram_tensor("scr", [NCHUNK, 128, WSZ], BF16).ap()

    with ExitStack() as es:
        sb = lambda n, s, d: es.enter_context(nc.sbuf_tensor(n, s, d))
        KA = sb("KA", [72, 8, S], BF16)
        VA = sb("VA", [128, 16, 4, 192], BF16)
        KB = sb("KB", [64, 2, 1024], BF16)
        VB = sb("VB", [128, 8, 320], BF16)
        QA = sb("QA", [72, 8, T], BF16)
        QB = sb("QB", [64, 8, T], BF16)
        xs = [sb("xs%d" % i, [128, D], F32) for i in range(2)]
        x1 = sb("x1", [128, 4, D], F32)
        hT = sb("hT", [128, 8, T], BF16)
        xhat = [sb("xhat%d" % i, [128, D], BF16) for i in range(4)]
        qkh = [sb("qkh%d" % i, [128, 512], BF16) for i in range(3)]
        sq = sb("sq", [128, 512], F32)
        PT = [sb("PT%d" % i, [128, 512], BF16) for i in range(4)]
        oaT = sb("oaT", [128, 4, T], BF16)
        obT = sb("obT", [128, 4, T], BF16)
        sga = sb("sga", [128, 512], F32)
        sgb = sb("sgb", [128, 512], F32)
        tmpa = sb("tmpa", [128, 512], F32)
        tmpr = sb("tmpr", [128, 512], F32)
        HM = sb("HM", [128, 32, T], BF16)
        W = [sb("W%d" % i, [128, WSZ], BF16) for i in range(NS)]
        R = sb("R", [128, 512], F32)
        ident = sb("ident", [128, 128], BF16)
        tri = sb("tri", [128, 128], BF16)
        mcur = sb("mcur", [128, 512], BF16)
        mprev = sb("mprev", [128, 512], BF16)
        biasA = sb("biasA", [128, 128], F32)
        biasB = sb("biasB", [128, 16], F32)
        t1 = sb("t1", [128, 512], F32)
        sinkT = sb("sinkT", [128, 512], F32)
        sbc = sb("sbc_s", [128, 4], F32)
        sel = sb("sel", [64, 288], BF16)
        g1 = sb("g1_s", [128, 8], F32)
        g2 = sb("g2_s", [128, 8], F32)
        gv = sb("gv_s", [64, 4], F32)
        st = sb("st", [128, 64], F32)
        epsT = sb("epsT", [128, 1], F32)
        ksum = sb("ksum", [64, 8, 8], F32)
        dlt = sb("dlt", [64, 8, 64], BF16)
        ind = [sb("ind%d" % i, [64, 512], BF16) for i in range(2)]
        bank = [es.enter_context(nc.psum_tensor("pb%d" % i, [128, 512], F32)) for i in range(8)]
        tpbs = [bank[7][:, :].bitcast(BF16), bank[6][:, :].bitcast(BF16)]

        P = Prog()

        def op(eng, fn, r=(), w=(), dma=None):
            return P.op(eng, fn, r, w, dma)

        def mm(out, lhsT, rhs, start, stop, r, w):
            op("pe", lambda e: e.matmul(out, lhsT=lhsT, rhs=rhs, start=start, stop=stop), r, w)

        def act(out, in_, func, r, w, **kw):
            op("act", lambda e: e.activation(out=out, in_=in_, func=func, **kw), r, w)

        def tt(eng, out, in0, in1, alu, r, w):
            op(eng, lambda e: e.tensor_tensor(out=out, in0=in0, in1=in1, op=alu), r, w)

        def ts(eng, out, in0, s1, s2, op0, op1, r, w):
            if op1 is None:
                op(eng, lambda e: e.tensor_scalar(out=out, in0=in0, scalar1=s1, scalar2=None, op0=op0), r, w)
            else:
                op(eng, lambda e: e.tensor_scalar(out=out, in0=in0, scalar1=s1, scalar2=s2, op0=op0, op1=op1), r, w)

        cidx = [0]

        def cload(dst, src, name, eng="pool"):
            cidx[0] += 1
            op(eng, lambda e: e.dma_start(out=dst, in_=src), (), [name], dma="c%d" % cidx[0])

        cload(ident[:], ctab["c_ident"], "ident")
        cload(tri[:], ctab["c_tri"], "tri")
        cload(mcur[:], ctab["c_mcur"], "mcur")
        cload(mprev[:], ctab["c_mprev"], "mprev")
        cload(sel[:], ctab["c_sel"], "sel")
        cload(biasA[:], ctab["c_biasA"], "biasA", "sp")
        cload(biasB[:], ctab["c_biasB"], "biasB", "sp")
        cload(t1[:], ctab["c_t1"], "t1", "sp")
        cload(sbc[:], sbc_d, "sbc", "sp")
        cload(g1[:], g1_d, "g1", "sp")
        cload(g2[:], g2_d, "g2", "sp")
        cload(gv[:], gv_d, "gv", "sp")
        for h in range(8):
            cload(KA[64:72, h, :], ctab["c_E"], "KAE")
        op("dve", lambda e: e.memset(VA[:, :, :, 64:128], 1.0), (), ["VAones"])
        op("dve", lambda e: e.memset(VB[:, :, 0:64], 1.0), (), ["VBones"])
        op("dve", lambda e: e.memset(VB[:, :, 128:192], 1.0), (), ["VBones"])
        op("dve", lambda e: e.memset(VB[:, :, 256:320], 1.0), (), ["VBones"])
        op("dve", lambda e: e.memset(ksum[:], 0.0), (), ["ksum"])
        op("dve", lambda e: e.memset(epsT[:], EPS), (), ["epsT"])
        for j in range(4):
            act(sinkT[:, j * 128:(j + 1) * 128], t1[:, j * 128:(j + 1) * 128], AF.Exp, ["t1", "sbc"], ["sinkT"],
                bias=sbc[:, j:j + 1], scale=1.0)

        seq = [(gt, k) for gt in range(NSEQ * NT) for k in range(NCHUNK)]
        wstate = {"next": 0}

        def wload(idx):
            gt, k = seq[idx]
            slot = idx % NS
            wt = W[slot]
            if gt == 0:
                for (off, (kk, ncol), src) in _chunk_pieces(k, w_in, w_a, w_b, w_out, w_up, w_down):
                    dst = wt[:, off:off + kk * ncol].rearrange("p (a b) -> p a b", b=ncol)
                    op("pool", lambda e, dst=dst, src=src: e.dma_start(out=dst, in_=src), (), ["W%d" % slot], dma="w%d" % slot)
                op("sp", lambda e: e.dma_start(out=scr[k], in_=wt[:]), ["W%d" % slot], ["scr%d" % k], dma="ws%d" % slot)
            else:
                op("sp", lambda e: e.dma_start(out=wt[:], in_=scr[k]), ["scr%d" % k], ["W%d" % slot], dma="w%d" % slot)

        def wneed(idx, ahead=NS - 1):
            lim = min(idx + ahead, len(seq) - 1)
            while wstate["next"] <= lim:
                wload(wstate["next"])
                wstate["next"] += 1
            return W[idx % NS], "W%d" % (idx % NS)

        mmb = [0]

        def next_bank():
            b = mmb[0] % 3
            mmb[0] += 1
            return bank[b], "pb%d" % b

        tph = [0]

        def next_tp():
            h = tph[0] % 2
            tph[0] += 1
            return tpbs[h][:, 0:512], "pb%d" % (7 - h)

        def norm_act(src_ap, src_reads, xi, si):
            xh, xhn = xhat[xi], "xhat%d" % xi
            c0 = (si % 4) * 4
            act(xh[:], src_ap, AF.Square, src_reads, [xhn, "st%d" % c0], accum_out=st[:, c0:c0 + 1])
            act(st[:, c0 + 2:c0 + 3], st[:, c0:c0 + 1], AF.Sqrt, ["st%d" % c0, "epsT"], ["st%d" % (c0 + 2)], scale=1.0 / D, bias=epsT[:, 0:1])
            op("dve", lambda e: e.reciprocal(out=st[:, c0 + 3:c0 + 4], in_=st[:, c0 + 2:c0 + 3]), ["st%d" % (c0 + 2)], ["st%d" % (c0 + 3)])
            act(xh[:], src_ap, AF.Copy, list(src_reads) + ["st%d" % (c0 + 3)], [xhn], scale=st[:, c0 + 3:c0 + 4])

        def norm_pe_half(xi, gain, gname, dst_cols, half):
            xh, xhn = xhat[xi], "xhat%d" % xi
            tp, tpn = next_tp()
            for j in range(4):
                jj = half * 4 + j
                op("pe", lambda e, jj=jj, j=j, tp=tp: e.transpose(out=tp[:, j * 128:(j + 1) * 128], in_=xh[:, jj * 128:(jj + 1) * 128], identity=ident[:, :]),
                   [xhn, "ident"], [tpn])
            tt("dve", hT[:, half * 4:half * 4 + 4, dst_cols], tp.rearrange("p (j q) -> p j q", q=128),
               gain[:, half * 4:half * 4 + 4].unsqueeze(2).broadcast_to([128, 4, 128]), ALU.mult, [tpn, gname], ["hT"])

        def qk_norm(ps, psn, nh, col0, qb_):
            n = nh * 64
            qk = qkh[qb_]
            act(sq[:, 0:n], ps, AF.Square, [psn], ["sq"])
            op("dve", lambda e: e.tensor_reduce(out=st[:, 32:32 + nh], in_=sq[:, 0:n].rearrange("p (h d) -> p h d", d=64), axis=AX.X, op=ALU.add),
               ["sq"], ["stq"])
            act(st[:, 48:48 + nh], st[:, 32:32 + nh], AF.Sqrt, ["stq", "epsT"], ["stq3"], scale=1.0 / HD, bias=epsT[:, 0:1])
            op("dve", lambda e: e.reciprocal(out=st[:, 56:56 + nh], in_=st[:, 48:48 + nh]), ["stq3"], ["stq4"])
            tt("dve", qk[:, 0:n].rearrange("p (h d) -> p h d", d=64), ps.rearrange("p (h d) -> p h d", d=64),
               st[:, 56:56 + nh].unsqueeze(2).broadcast_to([128, nh, 64]), ALU.mult, [psn, "stq4"], ["qkh%d" % qb_])

        def head_T(col0, nh, dst_fn, gcol, dname, qb_):
            qk = qkh[qb_]
            for h0 in range(0, nh, 4):
                nhh = min(4, nh - h0)
                tp, tpn = next_tp()
                for j in range(nhh):
                    c = (h0 + j) * 64
                    op("pe", lambda e, c=c, j=j, tp=tp: e.transpose(out=tp[0:64, j * 128:(j + 1) * 128], in_=qk[:, c:c + 64], identity=ident[:, :]),
                       ["qkh%d" % qb_, "ident"], [tpn])
                act(dst_fn(h0, nhh), tp[0:64, 0:nhh * 128].rearrange("p (j q) -> p j q", q=128), AF.Copy, [tpn, "gv"], [dname],
                    scale=gv[:, gcol:gcol + 1])

        ychunk = [0]
        ytok = None

        def stage(n):
            if stop == n:
                raise _Stop()

        widx = [0]

        tiles = [(s_, c_) for s_ in range(NSEQ) for c_ in range(NT)]

        def A0_load(t, i):
            s_, c_ = tiles[t]
            xb, xn = xs[i % 2], "xs%d" % (i % 2)
            src = x[s_, c_ * T + i * 128:c_ * T + (i + 1) * 128, :]
            op("sp", lambda e: e.dma_start(out=xb[:], in_=src), (), [xn], dma=xn)

        def A0_act(t, i):
            norm_act(xs[i % 2][:], ["xs%d" % (i % 2)], i, i)

        def A0_pe(t, i, half):
            norm_pe_half(i, g1, "g1", slice(i * 128, (i + 1) * 128), half)

        try:
          stage('const')
          for i in range(4):
              A0_load(0, i)
              A0_act(0, i)
          for i in range(4):
              for half in range(2):
                  A0_pe(0, i, half)
          for s in range(NSEQ):
            for c in range(NT):
                T0 = c * T
                base = widx[0]
                tcur = s * NT + c
                has_next = tcur + 1 < len(tiles)
                hbk = (3, 4, 5, 0)
                mmb[0] = 1
                if c == 0:
                    op("pool", lambda e: e.dma_start(out=QA[64:72, :, :].rearrange("p h q -> p (h q)"), in_=ctab["c_zero"]), (), ["QAm"], dma="qam")
                stage('A0')
                srcx = x[s, T0:T0 + T, :].rearrange("(i p) d -> p i d", p=128)
                op("sp", lambda e, srcx=srcx: e.dma_start(out=x1[:], in_=srcx), (), ["x1_0", "x1_1", "x1_2", "x1_3"], dma="x1")
                pend = []

                def flush(keep):
                    while len(pend) > keep:
                        f_ = pend.pop(0)
                        if f_ is not None:
                            f_()

                gi = 0
                for g in range(5):
                    w0, w0n = wneed(base + 2 * g)
                    w1, w1n = wneed(base + 2 * g + 1, NS - 2)
                    ncol = 512 if g < 4 else 256
                    for i in range(4):
                        pb, pbn = next_bank()
                        kt = 4 * c + i
                        qb_ = gi % 3
                        gi += 1
                        for k in range(8):
                            ww = (w0 if k < 4 else w1)[:, (k % 4) * ncol:(k % 4 + 1) * ncol]
                            mm(pb[:, 0:ncol], hT[:, k, i * 128:(i + 1) * 128], ww, k == 0, k == 7,
                               ["hT", w0n if k < 4 else w1n], [pbn])
                        qc = slice(i * 128, (i + 1) * 128)
                        nxt = None
                        if g == 0:
                            qk_norm(pb[:, 0:512], pbn, 8, 0, qb_)
                            nxt = lambda qc=qc, qb_=qb_: head_T(0, 8, lambda h0, n: QA[0:64, h0:h0 + n, qc], 0, "QA", qb_)
                        elif g == 1:
                            qk_norm(pb[:, 0:512], pbn, 8, 512, qb_)
                            kc = slice(T0 + i * 128, T0 + (i + 1) * 128)
                            nxt = lambda kc=kc, qb_=qb_: head_T(512, 8, lambda h0, n: KA[0:64, h0:h0 + n, kc], 1, "KA", qb_)
                        elif g == 2:
                            pv = pb[:, 0:512].rearrange("p (pr two d) -> p pr two d", two=2, d=64)
                            act(VA[:, kt, :, 0:64], pv[:, :, 0, :], AF.Copy, [pbn], ["VA"])
                            act(VA[:, kt, :, 128:192], pv[:, :, 1, :], AF.Copy, [pbn], ["VA"])
                        elif g == 3:
                            qk_norm(pb[:, 0:512], pbn, 8, 1024, qb_)
                            nxt = lambda qc=qc, qb_=qb_: head_T(1024, 8, lambda h0, n: QB[0:64, h0:h0 + n, qc], 2, "QB", qb_)
                        else:
                            qk_norm(pb[:, 0:128], pbn, 2, 1536, qb_)
                            ks = kt % 8
                            nxt = lambda ks=ks, qb_=qb_: head_T(1536, 2, lambda h0, n: KB[0:64, h0:h0 + n, ks * 128:(ks + 1) * 128], 3, "KB", qb_)
                            act(VB[:, ks, 64:128], pb[:, 128:192], AF.Copy, [pbn], ["VB"])
                            act(VB[:, ks, 192:256], pb[:, 192:256], AF.Copy, [pbn], ["VB"])
                        pend.append(nxt)
                        flush(2)
                flush(0)
                wneed(base + 10, 3)
                for h in range(8):
                    ko = ksum[:, h, 2 * c:2 * c + 2]
                    ki = KA[0:64, h, T0:T0 + T].rearrange("p (n k) -> p n k", k=256)
                    op("dve", lambda e, ko=ko, ki=ki: e.tensor_reduce(out=ko, in_=ki, axis=AX.X, op=ALU.add), ["KA"], ["ksum"])
                if c >= 2:
                    gbk = (3, 4, 0, 1)

                    def g1_(h):
                        tt("dve", dlt[:, h, :].rearrange("p (n m) -> p n m", m=8),
                           ksum[:, h, :].unsqueeze(2).broadcast_to([64, 8, 8]),
                           ksum[:, h, :].unsqueeze(1).broadcast_to([64, 8, 8]), ALU.subtract, ["ksum"], ["dlt%d" % h])
                        gb_, gbn = bank[gbk[h % 4]], "pb%d" % gbk[h % 4]
                        mm(gb_[0:64, :], dlt[:, h, :], QA[0:64, h, :], True, True, ["dlt%d" % h, "QA"], [gbn])

                    def g2_(h):
                        gb_, gbn = bank[gbk[h % 4]], "pb%d" % gbk[h % 4]
                        iv, ivn = ind[h % 2], "ind%d" % (h % 2)
                        op("dve", lambda e: e.tensor_single_scalar(out=iv[:, :], in_=gb_[0:64, :], scalar=0.0, op=ALU.is_lt), [gbn], [ivn])
                        for hb in range(2):
                            bi = 2 * c + hb - 4
                            mm(gb_[0:72, hb * 256:(hb + 1) * 256], sel[:, bi * 72:(bi + 1) * 72], iv[:, hb * 256:(hb + 1) * 256], True, True,
                               ["sel", ivn], [gbn])

                    def g3_(h):
                        gb_, gbn = bank[gbk[h % 4]], "pb%d" % gbk[h % 4]
                        ts("dve", QA[64:72, h, :], gb_[64:72, :], 2.5, NEGM, ALU.is_ge, ALU.mult, [gbn], ["QAm"])

                    for k in range(10):
                        if k < 8:
                            g1_(k)
                        if 0 <= k - 1 < 8:
                            g2_(k - 1)
                        if 0 <= k - 2 < 8:
                            g3_(k - 2)
                stage('gate%d' % c)
                steps = [(h, j) for h in range(8) for j in range(4 * c + 4)]
                nst = len(steps)

                def moba_S(si):
                    h, j = steps[si]
                    qs = 128 * max(0, j - 4 * c)
                    N = T - qs
                    sb_i = (3, 4, 0)[si % 3]
                    sbk, sbn = bank[sb_i], "pb%d" % sb_i
                    diag = j >= 4 * c
                    mm(sbk[:, 0:N], KA[0:72, h, j * 128:(j + 1) * 128], QA[0:72, h, qs:T], True, not diag,
                       ["KA", "KAE", "QA", "QAm"], [sbn])
                    if diag:
                        mm(sbk[:, 0:128], ident[:, :], tri[:, :], False, True, ["ident", "tri"], [sbn])
                    di = 4 * c - j + 3
                    pt = PT[si % 4]
                    act(pt[:, 0:N], sbk[:, 0:N], AF.Exp, [sbn, "biasA"], ["PT%d" % (si % 4)],
                        bias=biasA[:, h * 16 + di:h * 16 + di + 1], scale=SCALE)

                def moba_PV(si):
                    h, j = steps[si]
                    qs = 128 * max(0, j - 4 * c)
                    N = T - qs
                    ob_, obn = bank[5 + h % 2], "pb%d" % (5 + h % 2)
                    pr, od = divmod(h, 2)
                    lhsT = VA[:, j, pr, od * 64:od * 64 + 128]
                    mm(ob_[:, qs:T], lhsT, PT[si % 4][:, 0:N], j == 0, j == 4 * c + 3, ["VA", "VAones", "PT%d" % (si % 4)], [obn])
                    if j == 4 * c + 3:
                        lo, hi = (0, 64) if od == 0 else (64, 128)
                        dl, dh = (64, 128) if od == 0 else (0, 64)
                        rn = "R%d" % od
                        act(R[dl:dh, :], ob_[dl:dh, :], AF.Copy, [obn], [rn])
                        op("dve", lambda e: e.reciprocal(out=R[dl:dh, :], in_=R[dl:dh, :]), [rn], [rn])
                        tt("dve", oaT[lo:hi, pr, :], ob_[lo:hi, :], R[dl:dh, :], ALU.mult, [obn, rn], ["oaT%d" % od])

                moba_S(0)
                moba_S(1)
                for si in range(nst):
                    if si + 2 < nst:
                        moba_S(si + 2)
                    moba_PV(si)
                stage('B')
                sw = []
                for i in range(4):
                    kt = 4 * c + i
                    for g in range(2):
                        passes = [(kt % 8, mcur, "mcur", 0)] + ([((kt - 1) % 8, mprev, "mprev", 1)] if kt >= 1 else [])
                        for pi, (kslot, mk, mkn, which) in enumerate(passes):
                            sw.append((i, g, pi, len(passes), kslot, mk, mkn, which))

                def swa_S(n):
                    i, g, pi, npass, kslot, mk, mkn, which = sw[n]
                    sbk, sbn = bank[3 + n % 2], "pb%d" % (3 + n % 2)
                    mm(sbk[:, :], KB[0:64, g, kslot * 128:(kslot + 1) * 128], QB[0:64, 4 * g:4 * g + 4, i * 128:(i + 1) * 128],
                       True, False, ["KB", "QB"], [sbn])
                    mm(sbk[:, :], ident[:, :], mk[:, :], False, True, ["ident", mkn], [sbn])
                    pt = PT[n % 4]
                    for hh in range(4):
                        h = 4 * g + hh
                        act(pt[:, hh * 128:(hh + 1) * 128], sbk[:, hh * 128:(hh + 1) * 128], AF.Exp, [sbn, "biasB"], ["PT%d" % (n % 4)],
                            bias=biasB[:, h * 2 + which:h * 2 + which + 1], scale=SCALE)

                def swa_PV(n):
                    i, g, pi, npass, kslot, mk, mkn, which = sw[n]
                    be, bo = (5, 6) if i % 2 == 0 else (0, 1)
                    oe, oen, oo, oon = bank[be], "pb%d" % be, bank[bo], "pb%d" % bo
                    pt = PT[n % 4]
                    ptn = "PT%d" % (n % 4)
                    pt3 = pt[:, :].rearrange("p (a e q) -> p a e q", e=2, q=128)
                    last = pi == npass - 1
                    cs = slice(g * 256, (g + 1) * 256)
                    mm(oe[:, cs], VB[:, kslot, 64 + 128 * g:64 + 128 * g + 128], pt3[:, :, 0, :], pi == 0, last,
                       ["VB", "VBones", ptn], [oen])
                    mm(oo[:, cs], VB[:, kslot, 128 * g:128 * g + 128], pt3[:, :, 1, :], pi == 0, last,
                       ["VB", "VBones", ptn], [oon])
                    if not (last and g == 1):
                        return
                    tt("dve", R[64:128, :], oe[64:128, :], sinkT[64:128, :], ALU.add, [oen, "sinkT"], ["R0"])
                    tt("dve", R[0:64, :], oo[0:64, :], sinkT[0:64, :], ALU.add, [oon, "sinkT"], ["R1"])
                    op("dve", lambda e: e.reciprocal(out=R[:, :], in_=R[:, :]), ["R0", "R1"], ["R0", "R1"])
                    qc = slice(i * 128, (i + 1) * 128)
                    tt("dve", obT[0:64, :, qc], oe[0:64, :].rearrange("p (a q) -> p a q", q=128),
                       R[64:128, :].rearrange("p (a q) -> p a q", q=128), ALU.mult, [oen, "R0"], ["obT0"])
                    tt("dve", obT[64:128, :, qc], oo[64:128, :].rearrange("p (a q) -> p a q", q=128),
                       R[0:64, :].rearrange("p (a q) -> p a q", q=128), ALU.mult, [oon, "R1"], ["obT1"])

                swa_S(0)
                for n in range(len(sw)):
                    if n + 1 < len(sw):
                        swa_S(n + 1)
                    swa_PV(n)
                stage('C')
                for n in range(8):
                    wg, wgn = wneed(base + 10 + 2 * n)
                    wab, wabn = wneed(base + 10 + 2 * n + 1, NS - 2)
                    pa, pan = next_bank()
                    for k in range(8):
                        mm(pa[:, :], wg[:, k * 128:(k + 1) * 128], hT[:, k, :], k == 0, k == 7, [wgn, "hT"], [pan])
                    act(sga[:], pa[:, :], AF.Sigmoid, [pan], ["sga"])
                    pb2, pb2n = next_bank()
                    for k in range(8):
                        mm(pb2[:, :], wg[:, 1024 + k * 128:1024 + (k + 1) * 128], hT[:, k, :], k == 0, k == 7, [wgn, "hT"], [pb2n])
                    act(sgb[:], pb2[:, :], AF.Sigmoid, [pb2n], ["sgb"])
                    pc, pcn = next_bank()
                    for k in range(4):
                        mm(pc[:, :], wab[:, k * 128:(k + 1) * 128], oaT[:, k, :], k == 0, k == 3, [wabn, "oaT0", "oaT1"], [pcn])
                    tt("dve", tmpa[:], pc[:, :], sga[:], ALU.mult, [pcn, "sga"], ["tmpa"])
                    pd, pdn = next_bank()
                    for k in range(4):
                        mm(pd[:, :], wab[:, 512 + k * 128:512 + (k + 1) * 128], obT[:, k, :], k == 0, k == 3, [wabn, "obT0", "obT1"], [pdn])
                    tt("dve", sgb[:], pd[:, :], sgb[:], ALU.mult, [pdn, "sgb"], ["sgb"])
                    tt("dve", HM[:, n, :], tmpa[:], sgb[:], ALU.add, ["tmpa", "sgb"], ["hid%d" % n])
                stage('D')
                for half in range(2):
                    w0, w0n = wneed(base + 26 + 2 * half)
                    w1, w1n = wneed(base + 26 + 2 * half + 1, NS - 2)
                    for i in range(4):
                        pb, pbn = next_bank()
                        for k in range(8):
                            ww = (w0 if k < 4 else w1)[:, (k % 4) * 512:(k % 4 + 1) * 512]
                            mm(pb[:, :], HM[:, k, i * 128:(i + 1) * 128], ww, k == 0, k == 7, ["hid%d" % k, w0n if k < 4 else w1n], [pbn])
                        xo = x1[:, i, half * 512:(half + 1) * 512]
                        tt("dve", xo, pb[:, :], xo, ALU.add, [pbn, "x1_%d" % i], ["x1_%d" % i])
                        if half == 1:
                            norm_act(x1[:, i, :], ["x1_%d" % i], i, 4 + i)
                stage('E')
                for i in range(4):
                    for half in range(2):
                        norm_pe_half(i, g2, "g2", slice(i * 128, (i + 1) * 128), half)
                stage('F')
                for gch in range(8):
                    w0, w0n = wneed(base + 30 + 2 * gch)
                    w1, w1n = wneed(base + 30 + 2 * gch + 1, NS - 2)
                    for f in range(4):
                        pb, pbn = next_bank()
                        for k in range(8):
                            ww = (w0 if k < 4 else w1)[:, (k % 4) * 512 + f * 128:(k % 4) * 512 + (f + 1) * 128]
                            mm(pb[:, :], ww, hT[:, k, :], k == 0, k == 7, ["hT", w0n if k < 4 else w1n], [pbn])
                        act(tmpr[:], pb[:, :], AF.Relu, [pbn], ["tmpr"])
                        tt("dve", HM[:, gch * 4 + f, :], pb[:, :], tmpr[:], ALU.mult, [pbn, "tmpr"], ["hid%d" % (gch * 4 + f)])
                        if has_next and f == 3:
                            sched = {0: ("L0", "L1"), 2: ("A0", "L2"), 3: ("A1", "L3"), 5: ("A2",), 6: ("A3",)}
                            for what in sched.get(gch, ()):
                                (A0_load if what[0] == "L" else A0_act)(tcur + 1, int(what[1]))
                stage('G')
                for half in range(2):
                    for q in range(8):
                        wd, wdn = wneed(base + 46 + half * 8 + q)
                        for f in range(4):
                            ff = q * 4 + f
                            for i in range(4):
                                mm(bank[hbk[i]][:, :], HM[:, ff, i * 128:(i + 1) * 128], wd[:, f * 512:(f + 1) * 512], ff == 0, ff == 31,
                                   ["hid%d" % ff, wdn], ["pb%d" % hbk[i]])
                        if has_next and half == 0:
                            A0_pe(tcur + 1, q // 2, q % 2)
                    for i in range(4):
                        xo = x1[:, i, half * 512:(half + 1) * 512]
                        tt("dve", xo, bank[hbk[i]][:, :], xo, ALU.add, ["pb%d" % hbk[i], "x1_%d" % i], ["x1_%d" % i])
                dsty = y[s, T0:T0 + T, :].rearrange("(i p) d -> p i d", p=128)
                ytok = op("pool", lambda e, dsty=dsty: e.dma_start(out=dsty, in_=x1[:]), ["x1_0", "x1_1", "x1_2", "x1_3"], (), dma="y")
                widx[0] += NCHUNK
                stage('T%d' % (s * NT + c))
        except _Stop:
            pass
        if ytok is not None:
            P.final_wait("pool", [ytok])
        if dump:
            tens = {"hT": hT, "QA": QA, "KA": KA, "VA": VA, "QB": QB, "KB": KB, "VB": VB, "oaT": oaT, "obT": obT,
                    "HM": HM, "x1": x1, "ksum": ksum, "sinkT": sinkT, "R": R, "st": st, "qkh": qkh[0], "ind": ind[0], "dlt": dlt}
            allb = list(P.bufs.keys())
            dtoks = []
            for nm in dump:
                tns = tens[nm]
                shp = list(tns.shape)
                flat = tns[:]
                if len(shp) == 3:
                    flat = flat.rearrange("p a b -> p (a b)")
                elif len(shp) == 4:
                    flat = flat.rearrange("p a b c -> p (a b c)")
                fr = int(np.prod(shp[1:]))
                dd = nc.dram_tensor("dbg_" + nm, [shp[0], fr], tns.dtype, kind="ExternalOutput").ap()
                dtoks.append(op("sp", lambda e, dd=dd, flat=flat: e.dma_start(out=dd, in_=flat), allb, (), dma="dbg_" + nm))
            P.final_wait("sp", dtoks)
        sems = {n: es.enter_context(nc.semaphore(n)) for n in sorted(P.semnames)}
        with nc.Block() as block:
            P.emit(block, sems)
    return nc


_NC_CACHE = {}


def kernel(x, norm_attn, w_in, q_norm_a, k_norm_a, q_norm_b, k_norm_b, sinks_b,
           w_branch_a, w_branch_b, w_out, norm_mlp, w_up, w_down):
    f = lambda a: np.ascontiguousarray(np.asarray(a, dtype=np.float32))
    x = f(x)
    tabs = _host_tables()
    g1 = f(np.asarray(norm_attn)[0].reshape(8, 128).T)
    g2 = f(np.asarray(norm_mlp)[0].reshape(8, 128).T)
    gv = f(np.stack([np.asarray(q_norm_a)[0], np.asarray(k_norm_a)[0], np.asarray(q_norm_b)[0], np.asarray(k_norm_b)[0]], axis=1))
    sk = np.asarray(sinks_b)[0]
    sbc = np.zeros((128, 4), np.float32)
    for g in range(2):
        for a in range(2):
            sbc[64:, 2 * g + a] = sk[4 * g + 2 * a]
            sbc[:64, 2 * g + a] = sk[4 * g + 2 * a + 1]
    common = {
        "w_in": f(np.asarray(w_in)[0]), "w_a": f(np.asarray(w_branch_a)[0]), "w_b": f(np.asarray(w_branch_b)[0]),
        "w_out": f(np.asarray(w_out)[0]), "w_up": f(np.asarray(w_up)[0]), "w_down": f(np.asarray(w_down)[0]),
        "g1": g1, "g2": g2, "gv": gv, "sbc": sbc,
    }
    common.update(tabs)
    if "nc" not in _NC_CACHE:
        _NC_CACHE["nc"] = build_nc()
    nc = _NC_CACHE["nc"]
    in_maps = []
    for c in range(NCORES):
        m = dict(common)
        m["x"] = np.ascontiguousarray(x[NSEQ * c:NSEQ * (c + 1)])
        in_maps.append(m)
    res = run_bass_kernel_spmd(nc, in_maps, core_ids=list(range(NCORES)))
    return np.concatenate([r["y"] for r in res.results], axis=0).astype(np.float32)
```

```python
import os
import numpy as np
from contextlib import ExitStack
import concourse.bass as bass
import concourse.mybir as mybir
from concourse.bass_utils import run_bass_kernel_spmd

F32 = mybir.dt.float32
BF16 = mybir.dt.bfloat16
AF = mybir.ActivationFunctionType
ALU = mybir.AluOpType
AX = mybir.AxisListType

NCORES = 8
D = 1024
S = 2048
NSEQ = 2
T = 512
NT = S // T
HD = 64
DFF = 4096
INW = 4352
NEGM = -30000.0
EPS = 1e-6
SCALE = HD ** -0.5
NS = 4
WSZ = 2048
NCHUNK = 62
ENGS = ("pe", "dve", "act", "pool", "sp")


class Buf:
    __slots__ = ("name", "last_w", "readers")

    def __init__(self, name):
        self.name = name
        self.last_w = None
        self.readers = {}


class Prog:
    def __init__(self):
        self.ops = {e: [] for e in ENGS}
        self.cnt = {e: 0 for e in ENGS}
        self.dcnt = {}
        self.waited = {e: {} for e in ENGS}
        self.semnames = set("e_" + e for e in ENGS)
        self.bufs = {}

    def buf(self, name):
        b = self.bufs.get(name)
        if b is None:
            b = self.bufs[name] = Buf(name)
        return b

    def op(self, eng, fn, reads=(), writes=(), dma=None):
        reads = [self.buf(r) for r in reads]
        writes = [self.buf(w) for w in writes]
        own = "e_" + eng
        need = {}

        def add(sem, val, same_ok):
            if sem == own:
                if eng == "pe" or not same_ok:
                    return
            if need.get(sem, 0) < val:
                need[sem] = val

        for b in reads:
            if b.last_w is not None:
                add(b.last_w[0], b.last_w[1], True)
            if b.name.startswith("pb"):
                for s, v in b.readers.items():
                    add(s, v, False)
        for b in writes:
            if b.last_w is not None:
                add(b.last_w[0], b.last_w[1], True)
            for s, v in b.readers.items():
                add(s, v, False)
        waits = []
        wd = self.waited[eng]
        for s, v in need.items():
            if wd.get(s, 0) < v:
                wd[s] = v
                waits.append((s, v))
        if dma is None:
            self.cnt[eng] += 1
            tok = (own, self.cnt[eng])
            inc = (own, 1)
        else:
            sname = "d_" + dma
            self.semnames.add(sname)
            self.dcnt[sname] = self.dcnt.get(sname, 0) + 16
            tok = (sname, self.dcnt[sname])
            inc = (sname, 16)
        self.ops[eng].append((waits, fn, inc))
        for b in writes:
            b.last_w = tok
            b.readers = {}
        for b in reads:
            if b.readers.get(tok[0], 0) < tok[1]:
                b.readers[tok[0]] = tok[1]
        return tok

    def final_wait(self, eng, toks):
        self.ops[eng].append((list(toks), None, None))

    def emit(self, block, sems):
        engmap = {"pe": "tensor", "dve": "vector", "act": "scalar", "pool": "gpsimd", "sp": "sync"}
        for e in ENGS:
            ops = self.ops[e]
            if not ops:
                continue

            def body(engine, ops=ops):
                for waits, fn, inc in ops:
                    for s, v in waits:
                        engine.wait_ge(sems[s], v)
                    if fn is not None:
                        fn(engine).then_inc(sems[inc[0]], inc[1])

            getattr(block, engmap[e])(body)


def _slopes():
    sl = np.exp2(-(8.0 / 16) * np.arange(1, 17, dtype=np.float32)).astype(np.float32)
    return sl[:8], sl[8:]


def _host_tables():
    sl_b, sl_a = _slopes()
    p = np.arange(128, dtype=np.float32)
    k = p[:, None]
    q = p[None, :]
    tri = np.where(k <= q, 0.0, NEGM).astype(np.float32)
    prv = np.where(k > q, 0.0, NEGM).astype(np.float32)
    t = {}
    t["c_ident"] = np.eye(128, dtype=np.float32)
    t["c_tri"] = tri
    t["c_mcur"] = np.tile(tri, (1, 4))
    t["c_mprev"] = np.tile(prv, (1, 4))
    bA = np.zeros((128, 128), np.float32)
    for h in range(8):
        for di in range(16):
            bA[:, h * 16 + di] = sl_a[h] * (p - 128.0 * (di - 3))
    t["c_biasA"] = bA
    bB = np.zeros((128, 16), np.float32)
    for h in range(8):
        bB[:, h * 2 + 0] = sl_b[h] * (p - 64.0)
        bB[:, h * 2 + 1] = sl_b[h] * (p - 192.0)
    t["c_biasB"] = bB
    t1 = np.zeros((128, 2, 2, 128), np.float32)
    for g in range(2):
        for e in range(2):
            t1[64:, g, e, :] = sl_b[4 * g + 2 * e] * (p - 64.0)
            t1[:64, g, e, :] = sl_b[4 * g + 2 * e + 1] * (p - 64.0)
    t["c_t1"] = t1.reshape(128, 512)
    sel = np.zeros((64, 4, 72), np.float32)
    for bi in range(4):
        b = 4 + bi
        for n in range(b):
            for m in range(b):
                if n != m:
                    sel[n * 8 + m, bi, 64 + n] = 1.0
    t["c_sel"] = sel.reshape(64, 288)
    E = np.zeros((8, S), np.float32)
    for n in range(8):
        E[n, n * 256:(n + 1) * 256] = 1.0
    t["c_E"] = E
    t["c_zero"] = np.zeros((8, 8 * T), np.float32)
    return t


def _chunk_pieces(k, w_in, w_a, w_b, w_out, w_up, w_down):
    win = w_in.rearrange("(kc p) n -> p kc n", p=128)
    if k < 10:
        g, kh = divmod(k, 2)
        nc_ = 512 if g < 4 else 256
        return [(0, (4, nc_), win[:, kh * 4:kh * 4 + 4, g * 512:g * 512 + nc_])]
    k -= 10
    if k < 16:
        n, part = divmod(k, 2)
        if part == 0:
            return [(0, (8, 128), win[:, :, 2304 + n * 128:2304 + (n + 1) * 128]),
                    (1024, (8, 128), win[:, :, 3328 + n * 128:3328 + (n + 1) * 128])]
        wa = w_a.rearrange("(kc p) n -> p kc n", p=128)
        wb = w_b.rearrange("(kc p) n -> p kc n", p=128)
        return [(0, (4, 128), wa[:, :, n * 128:(n + 1) * 128]),
                (512, (4, 128), wb[:, :, n * 128:(n + 1) * 128])]
    k -= 16
    if k < 4:
        half, kh = divmod(k, 2)
        wo = w_out.rearrange("(kc p) n -> p kc n", p=128)
        return [(0, (4, 512), wo[:, kh * 4:kh * 4 + 4, half * 512:(half + 1) * 512])]
    k -= 4
    if k < 16:
        gch, kh = divmod(k, 2)
        wu = w_up.rearrange("(kc p) n -> p kc n", p=128)
        return [(0, (4, 512), wu[:, kh * 4:kh * 4 + 4, gch * 512:(gch + 1) * 512])]
    k -= 16
    half, q = divmod(k, 8)
    wd = w_down.rearrange("(fc p) n -> p fc n", p=128)
    return [(0, (4, 512), wd[:, q * 4:q * 4 + 4, half * 512:(half + 1) * 512])]


class _Stop(Exception):
    pass


def build_nc(stop=None, dump=()):
    nc = bass.Bass("TRN2", target_bir_lowering=False)
    dr = lambda n, s, kind="ExternalInput", dt=F32: nc.dram_tensor(n, s, dt, kind=kind).ap()
    x = dr("x", [NSEQ, S, D])
    w_in = dr("w_in", [D, INW])
    w_a = dr("w_a", [512, D])
    w_b = dr("w_b", [512, D])
    w_out = dr("w_out", [D, D])
    w_up = dr("w_up", [D, DFF])
    w_down = dr("w_down", [DFF, D])
    g1_d = dr("g1", [128, 8])
    g2_d = dr("g2", [128, 8])
    gv_d = dr("gv", [64, 4])
    sbc_d = dr("sbc", [128, 4])
    ctab = {n: dr(n, list(a.shape)) for n, a in _host_tables().items()}
    y = dr("y", [NSEQ, S, D], kind="ExternalOutput")
    scr = nc.dram_tensor("scr", [NCHUNK, 128, WSZ], BF16).ap()

    with ExitStack() as es:
        sb = lambda n, s, d: es.enter_context(nc.sbuf_tensor(n, s, d))
        KA = sb("KA", [72, 8, S], BF16)
        VA = sb("VA", [128, 16, 4, 192], BF16)
        KB = sb("KB", [64, 2, 1024], BF16)
        VB = sb("VB", [128, 8, 320], BF16)
        QA = sb("QA", [72, 8, T], BF16)
        QB = sb("QB", [64, 8, T], BF16)
        xs = [sb("xs%d" % i, [128, D], F32) for i in range(2)]
        x1 = sb("x1", [128, 4, D], F32)
        hT = sb("hT", [128, 8, T], BF16)
        xhat = [sb("xhat%d" % i, [128, D], BF16) for i in range(4)]
        qkh = [sb("qkh%d" % i, [128, 512], BF16) for i in range(4)]
        sq = sb("sq", [128, 512], F32)
        PT = [sb("PT%d" % i, [128, 512], BF16) for i in range(4)]
        oaT = sb("oaT", [128, 4, T], BF16)
        obT = sb("obT", [128, 4, T], BF16)
        sga = sb("sga", [128, 512], F32)
        sgb = sb("sgb", [128, 512], F32)
        tmpa = sb("tmpa", [128, 512], F32)
        tmpr = sb("tmpr", [128, 512], F32)
        HM = sb("HM", [128, 32, T], BF16)
        W = [sb("W%d" % i, [128, WSZ], BF16) for i in range(NS)]
        R = sb("R", [128, 512], F32)
        ident = sb("ident", [128, 128], BF16)
        tri = sb("tri", [128, 128], BF16)
        mcur = sb("mcur", [128, 512], BF16)
        mprev = sb("mprev", [128, 512], BF16)
        biasA = sb("biasA", [128, 128], F32)
        biasB = sb("biasB", [128, 16], F32)
        t1 = sb("t1", [128, 512], F32)
        sinkT = t1
        sbc = sb("sbc_s", [128, 4], F32)
        sel = sb("sel", [64, 288], BF16)
        g1 = sb("g1_s", [128, 8], F32)
        g2 = sb("g2_s", [128, 8], F32)
        gv = sb("gv_s", [64, 4], F32)
        st = sb("st", [128, 64], F32)
        epsT = sb("epsT", [128, 1], F32)
        ksum = sb("ksum", [64, 8, 8], F32)
        dlt = sb("dlt", [64, 8, 64], BF16)
        ind = [sb("ind%d" % i, [64, 512], BF16) for i in range(2)]
        bank = [es.enter_context(nc.psum_tensor("pb%d" % i, [128, 512], F32)) for i in range(8)]
        tpbs = [bank[7][:, :].bitcast(BF16), bank[6][:, :].bitcast(BF16)]

        P = Prog()

        def op(eng, fn, r=(), w=(), dma=None):
            return P.op(eng, fn, r, w, dma)

        def mm(out, lhsT, rhs, start, stop, r, w):
            op("pe", lambda e: e.matmul(out, lhsT=lhsT, rhs=rhs, start=start, stop=stop), r, w)

        def act(out, in_, func, r, w, **kw):
            op("act", lambda e: e.activation(out=out, in_=in_, func=func, **kw), r, w)

        def tt(eng, out, in0, in1, alu, r, w):
            op(eng, lambda e: e.tensor_tensor(out=out, in0=in0, in1=in1, op=alu), r, w)

        def ts(eng, out, in0, s1, s2, op0, op1, r, w):
            if op1 is None:
                op(eng, lambda e: e.tensor_scalar(out=out, in0=in0, scalar1=s1, scalar2=None, op0=op0), r, w)
            else:
                op(eng, lambda e: e.tensor_scalar(out=out, in0=in0, scalar1=s1, scalar2=s2, op0=op0, op1=op1), r, w)

        cidx = [0]

        def cload(dst, src, name, eng="pool"):
            cidx[0] += 1
            op(eng, lambda e: e.dma_start(out=dst, in_=src), (), [name], dma="c%d" % cidx[0])

        cload(ident[:], ctab["c_ident"], "ident")
        cload(tri[:], ctab["c_tri"], "tri")
        cload(mcur[:], ctab["c_mcur"], "mcur")
        cload(mprev[:], ctab["c_mprev"], "mprev")
        cload(sel[:], ctab["c_sel"], "sel")
        cload(biasA[:], ctab["c_biasA"], "biasA", "sp")
        cload(biasB[:], ctab["c_biasB"], "biasB", "sp")
        cload(t1[:], ctab["c_t1"], "t1", "sp")
        cload(sbc[:], sbc_d, "sbc", "sp")
        cload(g1[:], g1_d, "g1", "sp")
        cload(g2[:], g2_d, "g2", "sp")
        cload(gv[:], gv_d, "gv", "sp")
        for h in range(8):
            cload(KA[64:72, h, :], ctab["c_E"], "KAE")
        op("dve", lambda e: e.memset(VA[:, :, :, 64:128], 1.0), (), ["VAones"])
        op("dve", lambda e: e.memset(VB[:, :, 0:64], 1.0), (), ["VBones"])
        op("dve", lambda e: e.memset(VB[:, :, 128:192], 1.0), (), ["VBones"])
        op("dve", lambda e: e.memset(VB[:, :, 256:320], 1.0), (), ["VBones"])
        op("dve", lambda e: e.memset(ksum[:], 0.0), (), ["ksum"])
        op("dve", lambda e: e.memset(epsT[:], EPS), (), ["epsT"])
        for j in range(4):
            act(sinkT[:, j * 128:(j + 1) * 128], t1[:, j * 128:(j + 1) * 128], AF.Exp, ["t1", "sbc"], ["t1", "sinkT"],
                bias=sbc[:, j:j + 1], scale=1.0)

        seq = [(gt, k) for gt in range(NSEQ * NT) for k in range(NCHUNK)]
        wstate = {"next": 0}

        def wload(idx):
            gt, k = seq[idx]
            slot = idx % NS
            wt = W[slot]
            if gt == 0:
                for (off, (kk, ncol), src) in _chunk_pieces(k, w_in, w_a, w_b, w_out, w_up, w_down):
                    dst = wt[:, off:off + kk * ncol].rearrange("p (a b) -> p a b", b=ncol)
                    op("pool", lambda e, dst=dst, src=src: e.dma_start(out=dst, in_=src), (), ["W%d" % slot], dma="w%d" % slot)
                op("sp", lambda e: e.dma_start(out=scr[k], in_=wt[:]), ["W%d" % slot], ["scr%d" % k], dma="ws%d" % slot)
            else:
                op("sp", lambda e: e.dma_start(out=wt[:], in_=scr[k]), ["scr%d" % k], ["W%d" % slot], dma="w%d" % slot)

        def wneed(idx, ahead=NS - 1):
            lim = min(idx + ahead, len(seq) - 1)
            while wstate["next"] <= lim:
                wload(wstate["next"])
                wstate["next"] += 1
            return W[idx % NS], "W%d" % (idx % NS)

        mmb = [0]

        def next_bank():
            b = mmb[0] % 3
            mmb[0] += 1
            return bank[b], "pb%d" % b

        tph = [0]

        def next_tp():
            h = tph[0] % 2
            tph[0] += 1
            return tpbs[h][:, 0:512], "pb%d" % (7 - h)

        def norm_act(src_ap, src_reads, xi, si):
            xh, xhn = xhat[xi], "xhat%d" % xi
            c0 = (si % 4) * 4
            act(xh[:], src_ap, AF.Square, src_reads, [xhn, "st%d" % c0], accum_out=st[:, c0:c0 + 1])
            act(st[:, c0 + 2:c0 + 3], st[:, c0:c0 + 1], AF.Sqrt, ["st%d" % c0, "epsT"], ["st%d" % (c0 + 2)], scale=1.0 / D, bias=epsT[:, 0:1])
            op("dve", lambda e: e.reciprocal(out=st[:, c0 + 3:c0 + 4], in_=st[:, c0 + 2:c0 + 3]), ["st%d" % (c0 + 2)], ["st%d" % (c0 + 3)])
            act(xh[:], src_ap, AF.Copy, list(src_reads) + ["st%d" % (c0 + 3)], [xhn], scale=st[:, c0 + 3:c0 + 4])

        def norm_pe_half(xi, gain, gname, dst_cols, half):
            xh, xhn = xhat[xi], "xhat%d" % xi
            tp, tpn = next_tp()
            for j in range(4):
                jj = half * 4 + j
                op("pe", lambda e, jj=jj, j=j, tp=tp: e.transpose(out=tp[:, j * 128:(j + 1) * 128], in_=xh[:, jj * 128:(jj + 1) * 128], identity=ident[:, :]),
                   [xhn, "ident"], [tpn])
            tt("dve", hT[:, half * 4:half * 4 + 4, dst_cols], tp.rearrange("p (j q) -> p j q", q=128),
               gain[:, half * 4:half * 4 + 4].unsqueeze(2).broadcast_to([128, 4, 128]), ALU.mult, [tpn, gname], ["hT"])

        def qk_norm(ps, psn, nh, col0, qb_):
            n = nh * 64
            qk = qkh[qb_]
            act(sq[:, 0:n], ps, AF.Square, [psn], ["sq"])
            op("dve", lambda e: e.tensor_reduce(out=st[:, 32:32 + nh], in_=sq[:, 0:n].rearrange("p (h d) -> p h d", d=64), axis=AX.X, op=ALU.add),
               ["sq"], ["stq"])
            act(st[:, 48:48 + nh], st[:, 32:32 + nh], AF.Sqrt, ["stq", "epsT"], ["stq3"], scale=1.0 / HD, bias=epsT[:, 0:1])
            op("dve", lambda e: e.reciprocal(out=st[:, 56:56 + nh], in_=st[:, 48:48 + nh]), ["stq3"], ["stq4"])
            tt("dve", qk[:, 0:n].rearrange("p (h d) -> p h d", d=64), ps.rearrange("p (h d) -> p h d", d=64),
               st[:, 56:56 + nh].unsqueeze(2).broadcast_to([128, nh, 64]), ALU.mult, [psn, "stq4"], ["qkh%d" % qb_])

        def head_T(col0, nh, dst_fn, gcol, dname, qb_):
            qk = qkh[qb_]
            for h0 in range(0, nh, 4):
                nhh = min(4, nh - h0)
                tp, tpn = next_tp()
                for j in range(nhh):
                    c = (h0 + j) * 64
                    op("pe", lambda e, c=c, j=j, tp=tp: e.transpose(out=tp[0:64, j * 128:(j + 1) * 128], in_=qk[:, c:c + 64], identity=ident[:, :]),
                       ["qkh%d" % qb_, "ident"], [tpn])
                act(dst_fn(h0, nhh), tp[0:64, 0:nhh * 128].rearrange("p (j q) -> p j q", q=128), AF.Copy, [tpn, "gv"], [dname],
                    scale=gv[:, gcol:gcol + 1])

        ychunk = [0]
        ytok = None

        def stage(n):
            if stop == n:
                raise _Stop()

        widx = [0]

        tiles = [(s_, c_) for s_ in range(NSEQ) for c_ in range(NT)]

        def A0_load(t, i):
            s_, c_ = tiles[t]
            xb, xn = xs[i % 2], "xs%d" % (i % 2)
            src = x[s_, c_ * T + i * 128:c_ * T + (i + 1) * 128, :]
            op("sp", lambda e: e.dma_start(out=xb[:], in_=src), (), [xn], dma=xn)

        def A0_act(t, i):
            norm_act(xs[i % 2][:], ["xs%d" % (i % 2)], i, i)

        def A0_pe(t, i, half):
            norm_pe_half(i, g1, "g1", slice(i * 128, (i + 1) * 128), half)

        try:
          stage('const')
          for i in range(4):
              A0_load(0, i)
              A0_act(0, i)
          for i in range(4):
              for half in range(2):
                  A0_pe(0, i, half)
          for s in range(NSEQ):
            for c in range(NT):
                T0 = c * T
                base = widx[0]
                tcur = s * NT + c
                has_next = tcur + 1 < len(tiles)
                hbk = (3, 4, 5, 0)
                mmb[0] = 1
                if c == 0:
                    op("pool", lambda e: e.dma_start(out=QA[64:72, :, :].rearrange("p h q -> p (h q)"), in_=ctab["c_zero"]), (), ["QAm"], dma="qam")
                stage('A0')
                srcx = x[s, T0:T0 + T, :].rearrange("(i p) d -> p i d", p=128)
                op("sp", lambda e, srcx=srcx: e.dma_start(out=x1[:], in_=srcx), (), ["x1_0", "x1_1", "x1_2", "x1_3"], dma="x1")
                pend = []

                def flush(keep):
                    while len(pend) > keep:
                        f_ = pend.pop(0)
                        if f_ is not None:
                            f_()

                gi = 0
                for g in range(5):
                    w0, w0n = wneed(base + 2 * g)
                    w1, w1n = wneed(base + 2 * g + 1, NS - 2)
                    ncol = 512 if g < 4 else 256
                    for i in range(4):
                        ab_ = (1, 2, 0, 3, 4)[gi % 5]
                        pb, pbn = bank[ab_], "pb%d" % ab_
                        kt = 4 * c + i
                        qb_ = gi % 4
                        gi += 1
                        for k in range(8):
                            ww = (w0 if k < 4 else w1)[:, (k % 4) * ncol:(k % 4 + 1) * ncol]
                            mm(pb[:, 0:ncol], hT[:, k, i * 128:(i + 1) * 128], ww, k == 0, k == 7,
                               ["hT", w0n if k < 4 else w1n], [pbn])
                        qc = slice(i * 128, (i + 1) * 128)
                        nxt = None
                        if g == 0:
                            qk_norm(pb[:, 0:512], pbn, 8, 0, qb_)
                            nxt = lambda qc=qc, qb_=qb_: head_T(0, 8, lambda h0, n: QA[0:64, h0:h0 + n, qc], 0, "QA", qb_)
                        elif g == 1:
                            qk_norm(pb[:, 0:512], pbn, 8, 512, qb_)
                            kc = slice(T0 + i * 128, T0 + (i + 1) * 128)
                            nxt = lambda kc=kc, qb_=qb_: head_T(512, 8, lambda h0, n: KA[0:64, h0:h0 + n, kc], 1, "KA", qb_)
                        elif g == 2:
                            pv = pb[:, 0:512].rearrange("p (pr two d) -> p pr two d", two=2, d=64)
                            act(VA[:, kt, :, 0:64], pv[:, :, 0, :], AF.Copy, [pbn], ["VA"])
                            act(VA[:, kt, :, 128:192], pv[:, :, 1, :], AF.Copy, [pbn], ["VA"])
                        elif g == 3:
                            qk_norm(pb[:, 0:512], pbn, 8, 1024, qb_)
                            nxt = lambda qc=qc, qb_=qb_: head_T(1024, 8, lambda h0, n: QB[0:64, h0:h0 + n, qc], 2, "QB", qb_)
                        else:
                            qk_norm(pb[:, 0:128], pbn, 2, 1536, qb_)
                            ks = kt % 8
                            nxt = lambda ks=ks, qb_=qb_: head_T(1536, 2, lambda h0, n: KB[0:64, h0:h0 + n, ks * 128:(ks + 1) * 128], 3, "KB", qb_)
                            act(VB[:, ks, 64:128], pb[:, 128:192], AF.Copy, [pbn], ["VB"])
                            act(VB[:, ks, 192:256], pb[:, 192:256], AF.Copy, [pbn], ["VB"])
                        pend.append(nxt)
                        flush(3)
                flush(0)
                wneed(base + 10, 3)
                for h in range(8):
                    ko = ksum[:, h, 2 * c:2 * c + 2]
                    ki = KA[0:64, h, T0:T0 + T].rearrange("p (n k) -> p n k", k=256)
                    op("dve", lambda e, ko=ko, ki=ki: e.tensor_reduce(out=ko, in_=ki, axis=AX.X, op=ALU.add), ["KA"], ["ksum"])
                if c >= 2:
                    gbk = (3, 4, 0, 1)

                    def g1_(h):
                        tt("dve", dlt[:, h, :].rearrange("p (n m) -> p n m", m=8),
                           ksum[:, h, :].unsqueeze(2).broadcast_to([64, 8, 8]),
                           ksum[:, h, :].unsqueeze(1).broadcast_to([64, 8, 8]), ALU.subtract, ["ksum"], ["dlt%d" % h])
                        gb_, gbn = bank[gbk[h % 4]], "pb%d" % gbk[h % 4]
                        mm(gb_[0:64, :], dlt[:, h, :], QA[0:64, h, :], True, True, ["dlt%d" % h, "QA"], [gbn])

                    def g2_(h):
                        gb_, gbn = bank[gbk[h % 4]], "pb%d" % gbk[h % 4]
                        iv, ivn = ind[h % 2], "ind%d" % (h % 2)
                        op("dve", lambda e: e.tensor_single_scalar(out=iv[:, :], in_=gb_[0:64, :], scalar=0.0, op=ALU.is_lt), [gbn], [ivn])
                        for hb in range(2):
                            bi = 2 * c + hb - 4
                            mm(gb_[0:72, hb * 256:(hb + 1) * 256], sel[:, bi * 72:(bi + 1) * 72], iv[:, hb * 256:(hb + 1) * 256], True, True,
                               ["sel", ivn], [gbn])

                    def g3_(h):
                        gb_, gbn = bank[gbk[h % 4]], "pb%d" % gbk[h % 4]
                        ts("dve", QA[64:72, h, :], gb_[64:72, :], 2.5, NEGM, ALU.is_ge, ALU.mult, [gbn], ["QAm"])

                    for k in range(10):
                        if k < 8:
                            g1_(k)
                        if 0 <= k - 1 < 8:
                            g2_(k - 1)
                        if 0 <= k - 2 < 8:
                            g3_(k - 2)
                stage('gate%d' % c)
                steps = [(h, j) for h in range(8) for j in range(4 * c + 4)]
                nst = len(steps)

                def moba_S(si):
                    h, j = steps[si]
                    qs = 128 * max(0, j - 4 * c)
                    N = T - qs
                    sb_i = (3, 4, 0)[si % 3]
                    sbk, sbn = bank[sb_i], "pb%d" % sb_i
                    diag = j >= 4 * c
                    mm(sbk[:, 0:N], KA[0:72, h, j * 128:(j + 1) * 128], QA[0:72, h, qs:T], True, not diag,
                       ["KA", "KAE", "QA", "QAm"], [sbn])
                    if diag:
                        mm(sbk[:, 0:128], ident[:, :], tri[:, :], False, True, ["ident", "tri"], [sbn])
                    di = 4 * c - j + 3
                    pt = PT[si % 4]
                    act(pt[:, 0:N], sbk[:, 0:N], AF.Exp, [sbn, "biasA"], ["PT%d" % (si % 4)],
                        bias=biasA[:, h * 16 + di:h * 16 + di + 1], scale=SCALE)

                def moba_PV(si):
                    h, j = steps[si]
                    qs = 128 * max(0, j - 4 * c)
                    N = T - qs
                    ob_, obn = bank[5 + h % 2], "pb%d" % (5 + h % 2)
                    pr, od = divmod(h, 2)
                    lhsT = VA[:, j, pr, od * 64:od * 64 + 128]
                    mm(ob_[:, qs:T], lhsT, PT[si % 4][:, 0:N], j == 0, j == 4 * c + 3, ["VA", "VAones", "PT%d" % (si % 4)], [obn])
                    if j == 4 * c + 3:
                        lo, hi = (0, 64) if od == 0 else (64, 128)
                        dl, dh = (64, 128) if od == 0 else (0, 64)
                        rn = "R%d" % od
                        op("dve", lambda e: e.tensor_copy(out=R[dl:dh, :], in_=ob_[dl:dh, :]), [obn], [rn])
                        op("dve", lambda e: e.reciprocal(out=R[dl:dh, :], in_=R[dl:dh, :]), [rn], [rn])
                        tt("dve", oaT[lo:hi, pr, :], ob_[lo:hi, :], R[dl:dh, :], ALU.mult, [obn, rn], ["oaT%d" % od])

                moba_S(0)
                moba_S(1)
                for si in range(nst):
                    if si + 2 < nst:
                        moba_S(si + 2)
                    moba_PV(si)
                stage('B')
                sw = []
                for i in range(4):
                    kt = 4 * c + i
                    for g in range(2):
                        passes = [(kt % 8, mcur, "mcur", 0)] + ([((kt - 1) % 8, mprev, "mprev", 1)] if kt >= 1 else [])
                        for pi, (kslot, mk, mkn, which) in enumerate(passes):
                            sw.append((i, g, pi, len(passes), kslot, mk, mkn, which))

                def swa_S(n):
                    i, g, pi, npass, kslot, mk, mkn, which = sw[n]
                    sb_i = (3, 4, 2)[n % 3]
                    sbk, sbn = bank[sb_i], "pb%d" % sb_i
                    mm(sbk[:, :], KB[0:64, g, kslot * 128:(kslot + 1) * 128], QB[0:64, 4 * g:4 * g + 4, i * 128:(i + 1) * 128],
                       True, False, ["KB", "QB"], [sbn])
                    mm(sbk[:, :], ident[:, :], mk[:, :], False, True, ["ident", mkn], [sbn])
                    pt = PT[n % 4]
                    for hh in range(4):
                        h = 4 * g + hh
                        act(pt[:, hh * 128:(hh + 1) * 128], sbk[:, hh * 128:(hh + 1) * 128], AF.Exp, [sbn, "biasB"], ["PT%d" % (n % 4)],
                            bias=biasB[:, h * 2 + which:h * 2 + which + 1], scale=SCALE)

                def swa_PV(n):
                    i, g, pi, npass, kslot, mk, mkn, which = sw[n]
                    be, bo = (5, 6) if i % 2 == 0 else (0, 1)
                    oe, oen, oo, oon = bank[be], "pb%d" % be, bank[bo], "pb%d" % bo
                    pt = PT[n % 4]
                    ptn = "PT%d" % (n % 4)
                    pt3 = pt[:, :].rearrange("p (a e q) -> p a e q", e=2, q=128)
                    last = pi == npass - 1
                    cs = slice(g * 256, (g + 1) * 256)
                    mm(oe[:, cs], VB[:, kslot, 64 + 128 * g:64 + 128 * g + 128], pt3[:, :, 0, :], pi == 0, last,
                       ["VB", "VBones", ptn], [oen])
                    mm(oo[:, cs], VB[:, kslot, 128 * g:128 * g + 128], pt3[:, :, 1, :], pi == 0, last,
                       ["VB", "VBones", ptn], [oon])
                    if not (last and g == 1):
                        return
                    tt("dve", R[64:128, :], oe[64:128, :], sinkT[64:128, :], ALU.add, [oen, "sinkT"], ["R0"])
                    tt("dve", R[0:64, :], oo[0:64, :], sinkT[0:64, :], ALU.add, [oon, "sinkT"], ["R1"])
                    op("dve", lambda e: e.reciprocal(out=R[:, :], in_=R[:, :]), ["R0", "R1"], ["R0", "R1"])
                    qc = slice(i * 128, (i + 1) * 128)
                    tt("dve", obT[0:64, :, qc], oe[0:64, :].rearrange("p (a q) -> p a q", q=128),
                       R[64:128, :].rearrange("p (a q) -> p a q", q=128), ALU.mult, [oen, "R0"], ["obT0"])
                    tt("dve", obT[64:128, :, qc], oo[64:128, :].rearrange("p (a q) -> p a q", q=128),
                       R[0:64, :].rearrange("p (a q) -> p a q", q=128), ALU.mult, [oon, "R1"], ["obT1"])

                swa_S(0)
                swa_S(1)
                for n in range(len(sw)):
                    if n + 2 < len(sw):
                        swa_S(n + 2)
                    swa_PV(n)
                stage('C')
                for n in range(8):
                    wg, wgn = wneed(base + 10 + 2 * n)
                    wab, wabn = wneed(base + 10 + 2 * n + 1, NS - 2)
                    pa, pan = next_bank()
                    for k in range(8):
                        mm(pa[:, :], wg[:, k * 128:(k + 1) * 128], hT[:, k, :], k == 0, k == 7, [wgn, "hT"], [pan])
                    act(sga[:], pa[:, :], AF.Sigmoid, [pan], ["sga"])
                    pb2, pb2n = next_bank()
                    for k in range(8):
                        mm(pb2[:, :], wg[:, 1024 + k * 128:1024 + (k + 1) * 128], hT[:, k, :], k == 0, k == 7, [wgn, "hT"], [pb2n])
                    act(sgb[:], pb2[:, :], AF.Sigmoid, [pb2n], ["sgb"])
                    pc, pcn = next_bank()
                    for k in range(4):
                        mm(pc[:, :], wab[:, k * 128:(k + 1) * 128], oaT[:, k, :], k == 0, k == 3, [wabn, "oaT0", "oaT1"], [pcn])
                    tt("dve", tmpa[:], pc[:, :], sga[:], ALU.mult, [pcn, "sga"], ["tmpa"])
                    pd, pdn = next_bank()
                    for k in range(4):
                        mm(pd[:, :], wab[:, 512 + k * 128:512 + (k + 1) * 128], obT[:, k, :], k == 0, k == 3, [wabn, "obT0", "obT1"], [pdn])
                    tt("dve", sgb[:], pd[:, :], sgb[:], ALU.mult, [pdn, "sgb"], ["sgb"])
                    tt("dve", HM[:, n, :], tmpa[:], sgb[:], ALU.add, ["tmpa", "sgb"], ["hid%d" % n])
                stage('D')
                for half in range(2):
                    w0, w0n = wneed(base + 26 + 2 * half)
                    w1, w1n = wneed(base + 26 + 2 * half + 1, NS - 2)
                    for i in range(4):
                        pb, pbn = next_bank()
                        for k in range(8):
                            ww = (w0 if k < 4 else w1)[:, (k % 4) * 512:(k % 4 + 1) * 512]
                            mm(pb[:, :], HM[:, k, i * 128:(i + 1) * 128], ww, k == 0, k == 7, ["hid%d" % k, w0n if k < 4 else w1n], [pbn])
                        xo = x1[:, i, half * 512:(half + 1) * 512]
                        tt("dve", xo, pb[:, :], xo, ALU.add, [pbn, "x1_%d" % i], ["x1_%d" % i])
                        if half == 1:
                            norm_act(x1[:, i, :], ["x1_%d" % i], i, 4 + i)
                stage('E')
                for i in range(4):
                    for half in range(2):
                        norm_pe_half(i, g2, "g2", slice(i * 128, (i + 1) * 128), half)
                stage('F')
                for gch in range(8):
                    w0, w0n = wneed(base + 30 + 2 * gch)
                    w1, w1n = wneed(base + 30 + 2 * gch + 1, NS - 2)
                    for f in range(4):
                        pb, pbn = next_bank()
                        for k in range(8):
                            ww = (w0 if k < 4 else w1)[:, (k % 4) * 512 + f * 128:(k % 4) * 512 + (f + 1) * 128]
                            mm(pb[:, :], ww, hT[:, k, :], k == 0, k == 7, ["hT", w0n if k < 4 else w1n], [pbn])
                        act(tmpr[:], pb[:, :], AF.Relu, [pbn], ["tmpr"])
                        tt("dve", HM[:, gch * 4 + f, :], pb[:, :], tmpr[:], ALU.mult, [pbn, "tmpr"], ["hid%d" % (gch * 4 + f)])
                        if has_next and f == 3:
                            sched = {0: ("L0", "L1"), 2: ("A0", "L2"), 3: ("A1", "L3"), 5: ("A2",), 6: ("A3",)}
                            for what in sched.get(gch, ()):
                                (A0_load if what[0] == "L" else A0_act)(tcur + 1, int(what[1]))
                stage('G')
                for half in range(2):
                    for q in range(8):
                        wd, wdn = wneed(base + 46 + half * 8 + q)
                        for f in range(4):
                            ff = q * 4 + f
                            for i in range(4):
                                mm(bank[hbk[i]][:, :], HM[:, ff, i * 128:(i + 1) * 128], wd[:, f * 512:(f + 1) * 512], ff == 0, ff == 31,
                                   ["hid%d" % ff, wdn], ["pb%d" % hbk[i]])
                        if has_next and half == 0:
                            A0_pe(tcur + 1, q // 2, q % 2)
                    for i in range(4):
                        xo = x1[:, i, half * 512:(half + 1) * 512]
                        tt("dve", xo, bank[hbk[i]][:, :], xo, ALU.add, ["pb%d" % hbk[i], "x1_%d" % i], ["x1_%d" % i])
                dsty = y[s, T0:T0 + T, :].rearrange("(i p) d -> p i d", p=128)
                ytok = op("pool", lambda e, dsty=dsty: e.dma_start(out=dsty, in_=x1[:]), ["x1_0", "x1_1", "x1_2", "x1_3"], (), dma="y")
                widx[0] += NCHUNK
                stage('T%d' % (s * NT + c))
        except _Stop:
            pass
        if ytok is not None:
            P.final_wait("pool", [ytok])
        if dump:
            tens = {"hT": hT, "QA": QA, "KA": KA, "VA": VA, "QB": QB, "KB": KB, "VB": VB, "oaT": oaT, "obT": obT,
                    "HM": HM, "x1": x1, "ksum": ksum, "sinkT": sinkT, "R": R, "st": st, "qkh": qkh[0], "ind": ind[0], "dlt": dlt}
            allb = list(P.bufs.keys())
            dtoks = []
            for nm in dump:
                tns = tens[nm]
                shp = list(tns.shape)
                flat = tns[:]
                if len(shp) == 3:
                    flat = flat.rearrange("p a b -> p (a b)")
                elif len(shp) == 4:
                    flat = flat.rearrange("p a b c -> p (a b c)")
                fr = int(np.prod(shp[1:]))
                dd = nc.dram_tensor("dbg_" + nm, [shp[0], fr], tns.dtype, kind="ExternalOutput").ap()
                dtoks.append(op("sp", lambda e, dd=dd, flat=flat: e.dma_start(out=dd, in_=flat), allb, (), dma="dbg_" + nm))
            P.final_wait("sp", dtoks)
        sems = {n: es.enter_context(nc.semaphore(n)) for n in sorted(P.semnames)}
        with nc.Block() as block:
            P.emit(block, sems)
    return nc


_NC_CACHE = {}


def kernel(x, norm_attn, w_in, q_norm_a, k_norm_a, q_norm_b, k_norm_b, sinks_b,
           w_branch_a, w_branch_b, w_out, norm_mlp, w_up, w_down):
    f = lambda a: np.ascontiguousarray(np.asarray(a, dtype=np.float32))
    x = f(x)
    tabs = _host_tables()
    g1 = f(np.asarray(norm_attn)[0].reshape(8, 128).T)
    g2 = f(np.asarray(norm_mlp)[0].reshape(8, 128).T)
    gv = f(np.stack([np.asarray(q_norm_a)[0], np.asarray(k_norm_a)[0], np.asarray(q_norm_b)[0], np.asarray(k_norm_b)[0]], axis=1))
    sk = np.asarray(sinks_b)[0]
    sbc = np.zeros((128, 4), np.float32)
    for g in range(2):
        for a in range(2):
            sbc[64:, 2 * g + a] = sk[4 * g + 2 * a]
            sbc[:64, 2 * g + a] = sk[4 * g + 2 * a + 1]
    common = {
        "w_in": f(np.asarray(w_in)[0]), "w_a": f(np.asarray(w_branch_a)[0]), "w_b": f(np.asarray(w_branch_b)[0]),
        "w_out": f(np.asarray(w_out)[0]), "w_up": f(np.asarray(w_up)[0]), "w_down": f(np.asarray(w_down)[0]),
        "g1": g1, "g2": g2, "gv": gv, "sbc": sbc,
    }
    common.update(tabs)
    if "nc" not in _NC_CACHE:
        _NC_CACHE["nc"] = build_nc()
    nc = _NC_CACHE["nc"]
    in_maps = []
    for c in range(NCORES):
        m = dict(common)
        m["x"] = np.ascontiguousarray(x[NSEQ * c:NSEQ * (c + 1)])
        in_maps.append(m)
    res = run_bass_kernel_spmd(nc, in_maps, core_ids=list(range(NCORES)))
    return np.concatenate([r["y"] for r in res.results], axis=0).astype(np.float32)
```

```python
import os
import numpy as np
from contextlib import ExitStack
import concourse.bass as bass
import concourse.mybir as mybir
from concourse.bass_utils import run_bass_kernel_spmd

F32 = mybir.dt.float32
BF16 = mybir.dt.bfloat16
AF = mybir.ActivationFunctionType
ALU = mybir.AluOpType
AX = mybir.AxisListType

NCORES = 8
D = 1024
S = 2048
NSEQ = 2
T = 512
NT = S // T
HD = 64
DFF = 4096
INW = 4352
NEGM = -30000.0
EPS = 1e-6
SCALE = HD ** -0.5
NS = 4
WSZ = 2048
NCHUNK = 62
ENGS = ("pe", "dve", "act", "pool", "sp")


class Buf:
    __slots__ = ("name", "last_w", "readers")

    def __init__(self, name):
        self.name = name
        self.last_w = None
        self.readers = {}


class Prog:
    def __init__(self):
        self.ops = {e: [] for e in ENGS}
        self.cnt = {e: 0 for e in ENGS}
        self.dcnt = {}
        self.waited = {e: {} for e in ENGS}
        self.semnames = set("e_" + e for e in ENGS)
        self.bufs = {}

    def buf(self, name):
        b = self.bufs.get(name)
        if b is None:
            b = self.bufs[name] = Buf(name)
        return b

    def op(self, eng, fn, reads=(), writes=(), dma=None):
        reads = [self.buf(r) for r in reads]
        writes = [self.buf(w) for w in writes]
        own = "e_" + eng
        need = {}

        def add(sem, val, same_ok):
            if sem == own:
                if eng == "pe" or not same_ok:
                    return
            if need.get(sem, 0) < val:
                need[sem] = val

        for b in reads:
            if b.last_w is not None:
                add(b.last_w[0], b.last_w[1], True)
            if b.name.startswith("pb"):
                for s, v in b.readers.items():
                    add(s, v, False)
        for b in writes:
            if b.last_w is not None:
                add(b.last_w[0], b.last_w[1], True)
            for s, v in b.readers.items():
                add(s, v, False)
        waits = []
        wd = self.waited[eng]
        for s, v in need.items():
            if wd.get(s, 0) < v:
                wd[s] = v
                waits.append((s, v))
        if dma is None:
            self.cnt[eng] += 1
            tok = (own, self.cnt[eng])
            inc = (own, 1)
        else:
            sname = "d_" + dma
            self.semnames.add(sname)
            self.dcnt[sname] = self.dcnt.get(sname, 0) + 16
            tok = (sname, self.dcnt[sname])
            inc = (sname, 16)
        self.ops[eng].append((waits, fn, inc))
        for b in writes:
            b.last_w = tok
            b.readers = {}
        for b in reads:
            if b.readers.get(tok[0], 0) < tok[1]:
                b.readers[tok[0]] = tok[1]
        return tok

    def final_wait(self, eng, toks):
        self.ops[eng].append((list(toks), None, None))

    def emit(self, block, sems):
        engmap = {"pe": "tensor", "dve": "vector", "act": "scalar", "pool": "gpsimd", "sp": "sync"}
        for e in ENGS:
            ops = self.ops[e]
            if not ops:
                continue

            def body(engine, ops=ops):
                for waits, fn, inc in ops:
                    for s, v in waits:
                        engine.wait_ge(sems[s], v)
                    if fn is not None:
                        fn(engine).then_inc(sems[inc[0]], inc[1])

            getattr(block, engmap[e])(body)


def _slopes():
    sl = np.exp2(-(8.0 / 16) * np.arange(1, 17, dtype=np.float32)).astype(np.float32)
    return sl[:8], sl[8:]


def _host_tables():
    sl_b, sl_a = _slopes()
    p = np.arange(128, dtype=np.float32)
    k = p[:, None]
    q = p[None, :]
    tri = np.where(k <= q, 0.0, NEGM).astype(np.float32)
    prv = np.where(k > q, 0.0, NEGM).astype(np.float32)
    t = {}
    t["c_ident"] = np.eye(128, dtype=np.float32)
    t["c_tri"] = tri
    t["c_mcur"] = np.tile(tri, (1, 4))
    t["c_mprev"] = np.tile(prv, (1, 4))
    bA = np.zeros((128, 128), np.float32)
    for h in range(8):
        for di in range(16):
            bA[:, h * 16 + di] = sl_a[h] * (p - 128.0 * (di - 3))
    t["c_biasA"] = bA
    bB = np.zeros((128, 16), np.float32)
    for h in range(8):
        bB[:, h * 2 + 0] = sl_b[h] * (p - 64.0)
        bB[:, h * 2 + 1] = sl_b[h] * (p - 192.0)
    t["c_biasB"] = bB
    t1 = np.zeros((128, 2, 2, 128), np.float32)
    for g in range(2):
        for e in range(2):
            t1[64:, g, e, :] = sl_b[4 * g + 2 * e] * (p - 64.0)
            t1[:64, g, e, :] = sl_b[4 * g + 2 * e + 1] * (p - 64.0)
    t["c_t1"] = t1.reshape(128, 512)
    sel = np.zeros((64, 4, 72), np.float32)
    for bi in range(4):
        b = 4 + bi
        for n in range(b):
            for m in range(b):
                if n != m:
                    sel[n * 8 + m, bi, 64 + n] = 1.0
    t["c_sel"] = sel.reshape(64, 288)
    E = np.zeros((8, S), np.float32)
    for n in range(8):
        E[n, n * 256:(n + 1) * 256] = 1.0
    t["c_E"] = E
    t["c_zero"] = np.zeros((8, 8 * T), np.float32)
    return t


def _chunk_pieces(k, w_in, w_a, w_b, w_out, w_up, w_down):
    win = w_in.rearrange("(kc p) n -> p kc n", p=128)
    if k < 10:
        g, kh = divmod(k, 2)
        nc_ = 512 if g < 4 else 256
        return [(0, (4, nc_), win[:, kh * 4:kh * 4 + 4, g * 512:g * 512 + nc_])]
    k -= 10
    if k < 16:
        n, part = divmod(k, 2)
        if part == 0:
            return [(0, (8, 128), win[:, :, 2304 + n * 128:2304 + (n + 1) * 128]),
                    (1024, (8, 128), win[:, :, 3328 + n * 128:3328 + (n + 1) * 128])]
        wa = w_a.rearrange("(kc p) n -> p kc n", p=128)
        wb = w_b.rearrange("(kc p) n -> p kc n", p=128)
        return [(0, (4, 128), wa[:, :, n * 128:(n + 1) * 128]),
                (512, (4, 128), wb[:, :, n * 128:(n + 1) * 128])]
    k -= 16
    if k < 4:
        half, kh = divmod(k, 2)
        wo = w_out.rearrange("(kc p) n -> p kc n", p=128)
        return [(0, (4, 512), wo[:, kh * 4:kh * 4 + 4, half * 512:(half + 1) * 512])]
    k -= 4
    if k < 16:
        gch, kh = divmod(k, 2)
        wu = w_up.rearrange("(kc p) n -> p kc n", p=128)
        return [(0, (4, 512), wu[:, kh * 4:kh * 4 + 4, gch * 512:(gch + 1) * 512])]
    k -= 16
    half, q = divmod(k, 8)
    wd = w_down.rearrange("(fc p) n -> p fc n", p=128)
    return [(0, (4, 512), wd[:, q * 4:q * 4 + 4, half * 512:(half + 1) * 512])]


class _Stop(Exception):
    pass


def build_nc(stop=None, dump=()):
    nc = bass.Bass("TRN2", target_bir_lowering=False)
    dr = lambda n, s, kind="ExternalInput", dt=F32: nc.dram_tensor(n, s, dt, kind=kind).ap()
    x = dr("x", [NSEQ, S, D])
    w_in = dr("w_in", [D, INW])
    w_a = dr("w_a", [512, D])
    w_b = dr("w_b", [512, D])
    w_out = dr("w_out", [D, D])
    w_up = dr("w_up", [D, DFF])
    w_down = dr("w_down", [DFF, D])
    g1_d = dr("g1", [128, 8])
    g2_d = dr("g2", [128, 8])
    gv_d = dr("gv", [64, 4])
    sbc_d = dr("sbc", [128, 4])
    ctab = {n: dr(n, list(a.shape)) for n, a in _host_tables().items()}
    y = dr("y", [NSEQ, S, D], kind="ExternalOutput")
    scr = nc.dram_tensor("scr", [NCHUNK, 128, WSZ], BF16).ap()

    with ExitStack() as es:
        sb = lambda n, s, d: es.enter_context(nc.sbuf_tensor(n, s, d))
        KA = sb("KA", [72, 8, S], BF16)
        VA = sb("VA", [128, 16, 4, 192], BF16)
        KB = sb("KB", [64, 2, 1024], BF16)
        VB = sb("VB", [128, 8, 320], BF16)
        QA = sb("QA", [72, 8, T], BF16)
        QB = sb("QB", [64, 8, T], BF16)
        xs = [sb("xs%d" % i, [128, D], F32) for i in range(2)]
        x1 = sb("x1", [128, 4, D], F32)
        hT = sb("hT", [128, 8, T], BF16)
        xhat = [sb("xhat%d" % i, [128, D], BF16) for i in range(4)]
        qkh = [sb("qkh%d" % i, [128, 512], BF16) for i in range(4)]
        sq = sb("sq", [128, 512], F32)
        PT = [sb("PT%d" % i, [128, 512], BF16) for i in range(4)]
        oaT = sb("oaT", [128, 4, T], BF16)
        obT = sb("obT", [128, 4, T], BF16)
        sga = sb("sga", [128, 512], F32)
        sgb = sb("sgb", [128, 512], F32)
        tmpa = sb("tmpa", [128, 512], F32)
        tmpr = sb("tmpr", [128, 512], F32)
        HM = sb("HM", [128, 32, T], BF16)
        W = [sb("W%d" % i, [128, WSZ], BF16) for i in range(NS)]
        R = sb("R", [128, 512], F32)
        ident = sb("ident", [128, 128], BF16)
        tri = sb("tri", [128, 128], BF16)
        mcur = sb("mcur", [128, 512], BF16)
        mprev = sb("mprev", [128, 512], BF16)
        biasA = sb("biasA", [128, 128], F32)
        biasB = sb("biasB", [128, 16], F32)
        t1 = sb("t1", [128, 512], F32)
        sinkT = t1
        sbc = sb("sbc_s", [128, 4], F32)
        sel = sb("sel", [64, 288], BF16)
        g1 = sb("g1_s", [128, 8], F32)
        g2 = sb("g2_s", [128, 8], F32)
        gv = sb("gv_s", [64, 4], F32)
        st = sb("st", [128, 64], F32)
        epsT = sb("epsT", [128, 1], F32)
        ksum = sb("ksum", [64, 8, 8], F32)
        dlt = sb("dlt", [64, 8, 64], BF16)
        ind = [sb("ind%d" % i, [64, 512], BF16) for i in range(2)]
        bank = [es.enter_context(nc.psum_tensor("pb%d" % i, [128, 512], F32)) for i in range(8)]
        tpbs = [bank[7][:, :].bitcast(BF16), bank[6][:, :].bitcast(BF16)]

        P = Prog()

        def op(eng, fn, r=(), w=(), dma=None):
            return P.op(eng, fn, r, w, dma)

        def mm(out, lhsT, rhs, start, stop, r, w):
            op("pe", lambda e: e.matmul(out, lhsT=lhsT, rhs=rhs, start=start, stop=stop), r, w)

        def act(out, in_, func, r, w, **kw):
            op("act", lambda e: e.activation(out=out, in_=in_, func=func, **kw), r, w)

        def tt(eng, out, in0, in1, alu, r, w):
            op(eng, lambda e: e.tensor_tensor(out=out, in0=in0, in1=in1, op=alu), r, w)

        def ts(eng, out, in0, s1, s2, op0, op1, r, w):
            if op1 is None:
                op(eng, lambda e: e.tensor_scalar(out=out, in0=in0, scalar1=s1, scalar2=None, op0=op0), r, w)
            else:
                op(eng, lambda e: e.tensor_scalar(out=out, in0=in0, scalar1=s1, scalar2=s2, op0=op0, op1=op1), r, w)

        cidx = [0]

        def cload(dst, src, name, eng="pool"):
            cidx[0] += 1
            op(eng, lambda e: e.dma_start(out=dst, in_=src), (), [name], dma="c%d" % cidx[0])

        cload(ident[:], ctab["c_ident"], "ident")
        cload(tri[:], ctab["c_tri"], "tri")
        cload(mcur[:], ctab["c_mcur"], "mcur")
        cload(mprev[:], ctab["c_mprev"], "mprev")
        cload(sel[:], ctab["c_sel"], "sel")
        cload(biasA[:], ctab["c_biasA"], "biasA", "sp")
        cload(biasB[:], ctab["c_biasB"], "biasB", "sp")
        cload(t1[:], ctab["c_t1"], "t1", "sp")
        cload(sbc[:], sbc_d, "sbc", "sp")
        cload(g1[:], g1_d, "g1", "sp")
        cload(g2[:], g2_d, "g2", "sp")
        cload(gv[:], gv_d, "gv", "sp")
        for h in range(8):
            cload(KA[64:72, h, :], ctab["c_E"], "KAE")
        op("dve", lambda e: e.memset(VA[:, :, :, 64:128], 1.0), (), ["VAones"])
        op("dve", lambda e: e.memset(VB[:, :, 0:64], 1.0), (), ["VBones"])
        op("dve", lambda e: e.memset(VB[:, :, 128:192], 1.0), (), ["VBones"])
        op("dve", lambda e: e.memset(VB[:, :, 256:320], 1.0), (), ["VBones"])
        op("dve", lambda e: e.memset(ksum[:], 0.0), (), ["ksum"])
        op("dve", lambda e: e.memset(epsT[:], EPS), (), ["epsT"])
        for j in range(4):
            act(sinkT[:, j * 128:(j + 1) * 128], t1[:, j * 128:(j + 1) * 128], AF.Exp, ["t1", "sbc"], ["t1", "sinkT"],
                bias=sbc[:, j:j + 1], scale=1.0)

        seq = [(gt, k) for gt in range(NSEQ * NT) for k in range(NCHUNK)]
        wstate = {"next": 0}

        def wload(idx):
            gt, k = seq[idx]
            slot = idx % NS
            wt = W[slot]
            used = WSZ // 2 if (k in (8, 9) or (10 <= k < 26 and (k - 10) % 2 == 1)) else WSZ
            if gt == 0:
                for (off, (kk, ncol), src) in _chunk_pieces(k, w_in, w_a, w_b, w_out, w_up, w_down):
                    dst = wt[:, off:off + kk * ncol].rearrange("p (a b) -> p a b", b=ncol)
                    op("pool", lambda e, dst=dst, src=src: e.dma_start(out=dst, in_=src), (), ["W%d" % slot], dma="w%d" % slot)
                op("sp", lambda e: e.dma_start(out=scr[k, :, 0:used], in_=wt[:, 0:used]), ["W%d" % slot], ["scr%d" % k], dma="ws%d" % slot)
            else:
                op("sp", lambda e: e.dma_start(out=wt[:, 0:used], in_=scr[k, :, 0:used]), ["scr%d" % k], ["W%d" % slot], dma="w%d" % slot)

        def wneed(idx, ahead=NS - 1):
            lim = min(idx + ahead, len(seq) - 1)
            while wstate["next"] <= lim:
                wload(wstate["next"])
                wstate["next"] += 1
            return W[idx % NS], "W%d" % (idx % NS)

        mmb = [0]

        def next_bank():
            b = mmb[0] % 3
            mmb[0] += 1
            return bank[b], "pb%d" % b

        tph = [0]

        def next_tp():
            h = tph[0] % 2
            tph[0] += 1
            return tpbs[h][:, 0:512], "pb%d" % (7 - h)

        def norm_act(src_ap, src_reads, xi, si):
            xh, xhn = xhat[xi], "xhat%d" % xi
            c0 = (si % 4) * 4
            act(xh[:], src_ap, AF.Square, src_reads, [xhn, "st%d" % c0], accum_out=st[:, c0:c0 + 1])
            act(st[:, c0 + 2:c0 + 3], st[:, c0:c0 + 1], AF.Sqrt, ["st%d" % c0, "epsT"], ["st%d" % (c0 + 2)], scale=1.0 / D, bias=epsT[:, 0:1])
            op("dve", lambda e: e.reciprocal(out=st[:, c0 + 3:c0 + 4], in_=st[:, c0 + 2:c0 + 3]), ["st%d" % (c0 + 2)], ["st%d" % (c0 + 3)])
            act(xh[:], src_ap, AF.Copy, list(src_reads) + ["st%d" % (c0 + 3)], [xhn], scale=st[:, c0 + 3:c0 + 4])

        def norm_pe_half(xi, gain, gname, dst_cols, half):
            xh, xhn = xhat[xi], "xhat%d" % xi
            tp, tpn = next_tp()
            for j in range(4):
                jj = half * 4 + j
                op("pe", lambda e, jj=jj, j=j, tp=tp: e.transpose(out=tp[:, j * 128:(j + 1) * 128], in_=xh[:, jj * 128:(jj + 1) * 128], identity=ident[:, :]),
                   [xhn, "ident"], [tpn])
            tt("dve", hT[:, half * 4:half * 4 + 4, dst_cols], tp.rearrange("p (j q) -> p j q", q=128),
               gain[:, half * 4:half * 4 + 4].unsqueeze(2).broadcast_to([128, 4, 128]), ALU.mult, [tpn, gname], ["hT"])

        def qk_norm(ps, psn, nh, col0, qb_):
            n = nh * 64
            qk = qkh[qb_]
            act(sq[:, 0:n], ps, AF.Square, [psn], ["sq"])
            op("dve", lambda e: e.tensor_reduce(out=st[:, 32:32 + nh], in_=sq[:, 0:n].rearrange("p (h d) -> p h d", d=64), axis=AX.X, op=ALU.add),
               ["sq"], ["stq"])
            act(st[:, 48:48 + nh], st[:, 32:32 + nh], AF.Sqrt, ["stq", "epsT"], ["stq3"], scale=1.0 / HD, bias=epsT[:, 0:1])
            op("dve", lambda e: e.reciprocal(out=st[:, 56:56 + nh], in_=st[:, 48:48 + nh]), ["stq3"], ["stq4"])
            tt("dve", qk[:, 0:n].rearrange("p (h d) -> p h d", d=64), ps.rearrange("p (h d) -> p h d", d=64),
               st[:, 56:56 + nh].unsqueeze(2).broadcast_to([128, nh, 64]), ALU.mult, [psn, "stq4"], ["qkh%d" % qb_])

        def head_T(col0, nh, dst_fn, gcol, dname, qb_):
            qk = qkh[qb_]
            for h0 in range(0, nh, 4):
                nhh = min(4, nh - h0)
                tp, tpn = next_tp()
                for j in range(nhh):
                    c = (h0 + j) * 64
                    op("pe", lambda e, c=c, j=j, tp=tp: e.transpose(out=tp[0:64, j * 128:(j + 1) * 128], in_=qk[:, c:c + 64], identity=ident[:, :]),
                       ["qkh%d" % qb_, "ident"], [tpn])
                act(dst_fn(h0, nhh), tp[0:64, 0:nhh * 128].rearrange("p (j q) -> p j q", q=128), AF.Copy, [tpn, "gv"], [dname],
                    scale=gv[:, gcol:gcol + 1])

        ychunk = [0]
        ytok = None

        def stage(n):
            if stop == n:
                raise _Stop()

        widx = [0]

        tiles = [(s_, c_) for s_ in range(NSEQ) for c_ in range(NT)]

        def A0_load(t, i):
            s_, c_ = tiles[t]
            xb, xn = xs[i % 2], "xs%d" % (i % 2)
            src = x[s_, c_ * T + i * 128:c_ * T + (i + 1) * 128, :]
            op("sp", lambda e: e.dma_start(out=xb[:], in_=src), (), [xn], dma=xn)

        def A0_act(t, i):
            norm_act(xs[i % 2][:], ["xs%d" % (i % 2)], i, i)

        def A0_pe(t, i, half):
            norm_pe_half(i, g1, "g1", slice(i * 128, (i + 1) * 128), half)

        try:
          stage('const')
          for i in range(4):
              A0_load(0, i)
              A0_act(0, i)
          for i in range(4):
              for half in range(2):
                  A0_pe(0, i, half)
          for s in range(NSEQ):
            for c in range(NT):
                T0 = c * T
                base = widx[0]
                tcur = s * NT + c
                has_next = tcur + 1 < len(tiles)
                hbk = (3, 4, 5, 0)
                mmb[0] = 1
                if c == 0:
                    op("pool", lambda e: e.dma_start(out=QA[64:72, :, :].rearrange("p h q -> p (h q)"), in_=ctab["c_zero"]), (), ["QAm"], dma="qam")
                stage('A0')
                srcx = x[s, T0:T0 + T, :].rearrange("(i p) d -> p i d", p=128)
                op("sp", lambda e, srcx=srcx: e.dma_start(out=x1[:], in_=srcx), (), ["x1_0", "x1_1", "x1_2", "x1_3"], dma="x1")
                pend = []

                def flush(keep):
                    while len(pend) > keep:
                        f_ = pend.pop(0)
                        if f_ is not None:
                            f_()

                gi = 0
                for g in range(5):
                    w0, w0n = wneed(base + 2 * g)
                    w1, w1n = wneed(base + 2 * g + 1, NS - 2)
                    ncol = 512 if g < 4 else 256
                    for i in range(4):
                        ab_ = (1, 2, 0, 3, 4)[gi % 5]
                        pb, pbn = bank[ab_], "pb%d" % ab_
                        kt = 4 * c + i
                        qb_ = gi % 4
                        gi += 1
                        for k in range(8):
                            ww = (w0 if k < 4 else w1)[:, (k % 4) * ncol:(k % 4 + 1) * ncol]
                            mm(pb[:, 0:ncol], hT[:, k, i * 128:(i + 1) * 128], ww, k == 0, k == 7,
                               ["hT", w0n if k < 4 else w1n], [pbn])
                        qc = slice(i * 128, (i + 1) * 128)
                        nxt = None
                        if g == 0:
                            qk_norm(pb[:, 0:512], pbn, 8, 0, qb_)
                            nxt = lambda qc=qc, qb_=qb_: head_T(0, 8, lambda h0, n: QA[0:64, h0:h0 + n, qc], 0, "QA", qb_)
                        elif g == 1:
                            qk_norm(pb[:, 0:512], pbn, 8, 512, qb_)
                            kc = slice(T0 + i * 128, T0 + (i + 1) * 128)
                            nxt = lambda kc=kc, qb_=qb_: head_T(512, 8, lambda h0, n: KA[0:64, h0:h0 + n, kc], 1, "KA", qb_)
                        elif g == 2:
                            pv = pb[:, 0:512].rearrange("p (pr two d) -> p pr two d", two=2, d=64)
                            act(VA[:, kt, :, 0:64], pv[:, :, 0, :], AF.Copy, [pbn], ["VA"])
                            act(VA[:, kt, :, 128:192], pv[:, :, 1, :], AF.Copy, [pbn], ["VA"])
                        elif g == 3:
                            qk_norm(pb[:, 0:512], pbn, 8, 1024, qb_)
                            nxt = lambda qc=qc, qb_=qb_: head_T(1024, 8, lambda h0, n: QB[0:64, h0:h0 + n, qc], 2, "QB", qb_)
                        else:
                            qk_norm(pb[:, 0:128], pbn, 2, 1536, qb_)
                            ks = kt % 8
                            nxt = lambda ks=ks, qb_=qb_: head_T(1536, 2, lambda h0, n: KB[0:64, h0:h0 + n, ks * 128:(ks + 1) * 128], 3, "KB", qb_)
                            act(VB[:, ks, 64:128], pb[:, 128:192], AF.Copy, [pbn], ["VB"])
                            act(VB[:, ks, 192:256], pb[:, 192:256], AF.Copy, [pbn], ["VB"])
                        pend.append(nxt)
                        flush(3)
                flush(0)
                wneed(base + 10, 3)
                for h in range(8):
                    ko = ksum[:, h, 2 * c:2 * c + 2]
                    ki = KA[0:64, h, T0:T0 + T].rearrange("p (n k) -> p n k", k=256)
                    op("dve", lambda e, ko=ko, ki=ki: e.tensor_reduce(out=ko, in_=ki, axis=AX.X, op=ALU.add), ["KA"], ["ksum"])
                if c >= 2:
                    gbk = (3, 4, 0, 1)

                    def g1_(h):
                        tt("dve", dlt[:, h, :].rearrange("p (n m) -> p n m", m=8),
                           ksum[:, h, :].unsqueeze(2).broadcast_to([64, 8, 8]),
                           ksum[:, h, :].unsqueeze(1).broadcast_to([64, 8, 8]), ALU.subtract, ["ksum"], ["dlt%d" % h])
                        gb_, gbn = bank[gbk[h % 4]], "pb%d" % gbk[h % 4]
                        mm(gb_[0:64, :], dlt[:, h, :], QA[0:64, h, :], True, True, ["dlt%d" % h, "QA"], [gbn])

                    def g2_(h):
                        gb_, gbn = bank[gbk[h % 4]], "pb%d" % gbk[h % 4]
                        iv, ivn = ind[h % 2], "ind%d" % (h % 2)
                        op("dve", lambda e: e.tensor_single_scalar(out=iv[:, :], in_=gb_[0:64, :], scalar=0.0, op=ALU.is_lt), [gbn], [ivn])
                        for hb in range(2):
                            bi = 2 * c + hb - 4
                            mm(gb_[0:72, hb * 256:(hb + 1) * 256], sel[:, bi * 72:(bi + 1) * 72], iv[:, hb * 256:(hb + 1) * 256], True, True,
                               ["sel", ivn], [gbn])

                    def g3_(h):
                        gb_, gbn = bank[gbk[h % 4]], "pb%d" % gbk[h % 4]
                        ts("dve", QA[64:72, h, :], gb_[64:72, :], 2.5, NEGM, ALU.is_ge, ALU.mult, [gbn], ["QAm"])

                    for k in range(10):
                        if k < 8:
                            g1_(k)
                        if 0 <= k - 1 < 8:
                            g2_(k - 1)
                        if 0 <= k - 2 < 8:
                            g3_(k - 2)
                stage('gate%d' % c)
                steps = [(h, j) for h in range(8) for j in range(4 * c + 4)]
                nst = len(steps)

                def moba_S(si):
                    h, j = steps[si]
                    qs = 128 * max(0, j - 4 * c)
                    N = T - qs
                    sb_i = (3, 4, 0)[si % 3]
                    sbk, sbn = bank[sb_i], "pb%d" % sb_i
                    diag = j >= 4 * c
                    mm(sbk[:, 0:N], KA[0:72, h, j * 128:(j + 1) * 128], QA[0:72, h, qs:T], True, not diag,
                       ["KA", "KAE", "QA", "QAm"], [sbn])
                    if diag:
                        mm(sbk[:, 0:128], ident[:, :], tri[:, :], False, True, ["ident", "tri"], [sbn])
                    di = 4 * c - j + 3
                    pt = PT[si % 4]
                    act(pt[:, 0:N], sbk[:, 0:N], AF.Exp, [sbn, "biasA"], ["PT%d" % (si % 4)],
                        bias=biasA[:, h * 16 + di:h * 16 + di + 1], scale=SCALE)

                def moba_PV(si):
                    h, j = steps[si]
                    qs = 128 * max(0, j - 4 * c)
                    N = T - qs
                    ob_, obn = bank[5 + h % 2], "pb%d" % (5 + h % 2)
                    pr, od = divmod(h, 2)
                    lhsT = VA[:, j, pr, od * 64:od * 64 + 128]
                    mm(ob_[:, qs:T], lhsT, PT[si % 4][:, 0:N], j == 0, j == 4 * c + 3, ["VA", "VAones", "PT%d" % (si % 4)], [obn])
                    if j == 4 * c + 3:
                        lo, hi = (0, 64) if od == 0 else (64, 128)
                        dl, dh = (64, 128) if od == 0 else (0, 64)
                        rn = "R%d" % od
                        op("dve", lambda e: e.tensor_copy(out=R[dl:dh, :], in_=ob_[dl:dh, :]), [obn], [rn])
                        op("dve", lambda e: e.reciprocal(out=R[dl:dh, :], in_=R[dl:dh, :]), [rn], [rn])
                        tt("dve", oaT[lo:hi, pr, :], ob_[lo:hi, :], R[dl:dh, :], ALU.mult, [obn, rn], ["oaT%d" % od])

                moba_S(0)
                moba_S(1)
                for si in range(nst):
                    if si + 2 < nst:
                        moba_S(si + 2)
                    moba_PV(si)
                stage('B')
                sw = []
                for i in range(4):
                    kt = 4 * c + i
                    for g in range(2):
                        passes = [(kt % 8, mcur, "mcur", 0)] + ([((kt - 1) % 8, mprev, "mprev", 1)] if kt >= 1 else [])
                        for pi, (kslot, mk, mkn, which) in enumerate(passes):
                            sw.append((i, g, pi, len(passes), kslot, mk, mkn, which))

                def swa_S(n):
                    i, g, pi, npass, kslot, mk, mkn, which = sw[n]
                    sb_i = (3, 4, 2)[n % 3]
                    sbk, sbn = bank[sb_i], "pb%d" % sb_i
                    mm(sbk[:, :], KB[0:64, g, kslot * 128:(kslot + 1) * 128], QB[0:64, 4 * g:4 * g + 4, i * 128:(i + 1) * 128],
                       True, False, ["KB", "QB"], [sbn])
                    mm(sbk[:, :], ident[:, :], mk[:, :], False, True, ["ident", mkn], [sbn])
                    pt = PT[n % 4]
                    for hh in range(4):
                        h = 4 * g + hh
                        act(pt[:, hh * 128:(hh + 1) * 128], sbk[:, hh * 128:(hh + 1) * 128], AF.Exp, [sbn, "biasB"], ["PT%d" % (n % 4)],
                            bias=biasB[:, h * 2 + which:h * 2 + which + 1], scale=SCALE)

                def swa_PV(n):
                    i, g, pi, npass, kslot, mk, mkn, which = sw[n]
                    be, bo = (5, 6) if i % 2 == 0 else (0, 1)
                    oe, oen, oo, oon = bank[be], "pb%d" % be, bank[bo], "pb%d" % bo
                    pt = PT[n % 4]
                    ptn = "PT%d" % (n % 4)
                    pt3 = pt[:, :].rearrange("p (a e q) -> p a e q", e=2, q=128)
                    last = pi == npass - 1
                    cs = slice(g * 256, (g + 1) * 256)
                    mm(oe[:, cs], VB[:, kslot, 64 + 128 * g:64 + 128 * g + 128], pt3[:, :, 0, :], pi == 0, last,
                       ["VB", "VBones", ptn], [oen])
                    mm(oo[:, cs], VB[:, kslot, 128 * g:128 * g + 128], pt3[:, :, 1, :], pi == 0, last,
                       ["VB", "VBones", ptn], [oon])
                    if not (last and g == 1):
                        return
                    tt("dve", R[64:128, :], oe[64:128, :], sinkT[64:128, :], ALU.add, [oen, "sinkT"], ["R0"])
                    tt("dve", R[0:64, :], oo[0:64, :], sinkT[0:64, :], ALU.add, [oon, "sinkT"], ["R1"])
                    op("dve", lambda e: e.reciprocal(out=R[:, :], in_=R[:, :]), ["R0", "R1"], ["R0", "R1"])
                    qc = slice(i * 128, (i + 1) * 128)
                    tt("dve", obT[0:64, :, qc], oe[0:64, :].rearrange("p (a q) -> p a q", q=128),
                       R[64:128, :].rearrange("p (a q) -> p a q", q=128), ALU.mult, [oen, "R0"], ["obT0"])
                    tt("dve", obT[64:128, :, qc], oo[64:128, :].rearrange("p (a q) -> p a q", q=128),
                       R[0:64, :].rearrange("p (a q) -> p a q", q=128), ALU.mult, [oon, "R1"], ["obT1"])

                swa_S(0)
                swa_S(1)
                for n in range(len(sw)):
                    if n + 2 < len(sw):
                        swa_S(n + 2)
                    swa_PV(n)
                stage('C')
                for n in range(8):
                    wg, wgn = wneed(base + 10 + 2 * n)
                    wab, wabn = wneed(base + 10 + 2 * n + 1, NS - 2)
                    pa, pan = next_bank()
                    for k in range(8):
                        mm(pa[:, :], wg[:, k * 128:(k + 1) * 128], hT[:, k, :], k == 0, k == 7, [wgn, "hT"], [pan])
                    act(sga[:], pa[:, :], AF.Sigmoid, [pan], ["sga"])
                    pb2, pb2n = next_bank()
                    for k in range(8):
                        mm(pb2[:, :], wg[:, 1024 + k * 128:1024 + (k + 1) * 128], hT[:, k, :], k == 0, k == 7, [wgn, "hT"], [pb2n])
                    act(sgb[:], pb2[:, :], AF.Sigmoid, [pb2n], ["sgb"])
                    pc, pcn = next_bank()
                    for k in range(4):
                        mm(pc[:, :], wab[:, k * 128:(k + 1) * 128], oaT[:, k, :], k == 0, k == 3, [wabn, "oaT0", "oaT1"], [pcn])
                    tt("dve", tmpa[:], pc[:, :], sga[:], ALU.mult, [pcn, "sga"], ["tmpa"])
                    pd, pdn = next_bank()
                    for k in range(4):
                        mm(pd[:, :], wab[:, 512 + k * 128:512 + (k + 1) * 128], obT[:, k, :], k == 0, k == 3, [wabn, "obT0", "obT1"], [pdn])
                    tt("dve", sgb[:], pd[:, :], sgb[:], ALU.mult, [pdn, "sgb"], ["sgb"])
                    tt("dve", HM[:, n, :], tmpa[:], sgb[:], ALU.add, ["tmpa", "sgb"], ["hid%d" % n])
                stage('D')
                for half in range(2):
                    w0, w0n = wneed(base + 26 + 2 * half)
                    w1, w1n = wneed(base + 26 + 2 * half + 1, NS - 2)
                    for i in range(4):
                        pb, pbn = next_bank()
                        for k in range(8):
                            ww = (w0 if k < 4 else w1)[:, (k % 4) * 512:(k % 4 + 1) * 512]
                            mm(pb[:, :], HM[:, k, i * 128:(i + 1) * 128], ww, k == 0, k == 7, ["hid%d" % k, w0n if k < 4 else w1n], [pbn])
                        xo = x1[:, i, half * 512:(half + 1) * 512]
                        tt("dve", xo, pb[:, :], xo, ALU.add, [pbn, "x1_%d" % i], ["x1_%d" % i])
                        if half == 1:
                            norm_act(x1[:, i, :], ["x1_%d" % i], i, 4 + i)
                stage('E')
                for i in range(4):
                    for half in range(2):
                        norm_pe_half(i, g2, "g2", slice(i * 128, (i + 1) * 128), half)
                stage('F')
                for gch in range(8):
                    w0, w0n = wneed(base + 30 + 2 * gch)
                    w1, w1n = wneed(base + 30 + 2 * gch + 1, NS - 2)
                    for f in range(4):
                        pb, pbn = next_bank()
                        for k in range(8):
                            ww = (w0 if k < 4 else w1)[:, (k % 4) * 512 + f * 128:(k % 4) * 512 + (f + 1) * 128]
                            mm(pb[:, :], ww, hT[:, k, :], k == 0, k == 7, ["hT", w0n if k < 4 else w1n], [pbn])
                        act(tmpr[:], pb[:, :], AF.Relu, [pbn], ["tmpr"])
                        tt("dve", HM[:, gch * 4 + f, :], pb[:, :], tmpr[:], ALU.mult, [pbn, "tmpr"], ["hid%d" % (gch * 4 + f)])
                        if has_next and f == 3:
                            sched = {0: ("L0", "L1"), 2: ("A0", "L2"), 3: ("A1", "L3"), 5: ("A2",), 6: ("A3",)}
                            for what in sched.get(gch, ()):
                                (A0_load if what[0] == "L" else A0_act)(tcur + 1, int(what[1]))
                stage('G')
                for half in range(2):
                    for q in range(8):
                        wd, wdn = wneed(base + 46 + half * 8 + q)
                        for f in range(4):
                            ff = q * 4 + f
                            for i in range(4):
                                mm(bank[hbk[i]][:, :], HM[:, ff, i * 128:(i + 1) * 128], wd[:, f * 512:(f + 1) * 512], ff == 0, ff == 31,
                                   ["hid%d" % ff, wdn], ["pb%d" % hbk[i]])
                        if has_next and half == 0:
                            A0_pe(tcur + 1, q // 2, q % 2)
                    for i in range(4):
                        xo = x1[:, i, half * 512:(half + 1) * 512]
                        tt("dve", xo, bank[hbk[i]][:, :], xo, ALU.add, ["pb%d" % hbk[i], "x1_%d" % i], ["x1_%d" % i])
                dsty = y[s, T0:T0 + T, :].rearrange("(i p) d -> p i d", p=128)
                ytok = op("pool", lambda e, dsty=dsty: e.dma_start(out=dsty, in_=x1[:]), ["x1_0", "x1_1", "x1_2", "x1_3"], (), dma="y")
                widx[0] += NCHUNK
                stage('T%d' % (s * NT + c))
        except _Stop:
            pass
        if ytok is not None:
            P.final_wait("pool", [ytok])
        if dump:
            tens = {"hT": hT, "QA": QA, "KA": KA, "VA": VA, "QB": QB, "KB": KB, "VB": VB, "oaT": oaT, "obT": obT,
                    "HM": HM, "x1": x1, "ksum": ksum, "sinkT": sinkT, "R": R, "st": st, "qkh": qkh[0], "ind": ind[0], "dlt": dlt}
            allb = list(P.bufs.keys())
            dtoks = []
            for nm in dump:
                tns = tens[nm]
                shp = list(tns.shape)
                flat = tns[:]
                if len(shp) == 3:
                    flat = flat.rearrange("p a b -> p (a b)")
                elif len(shp) == 4:
                    flat = flat.rearrange("p a b c -> p (a b c)")
                fr = int(np.prod(shp[1:]))
                dd = nc.dram_tensor("dbg_" + nm, [shp[0], fr], tns.dtype, kind="ExternalOutput").ap()
                dtoks.append(op("sp", lambda e, dd=dd, flat=flat: e.dma_start(out=dd, in_=flat), allb, (), dma="dbg_" + nm))
            P.final_wait("sp", dtoks)
        sems = {n: es.enter_context(nc.semaphore(n)) for n in sorted(P.semnames)}
        with nc.Block() as block:
            P.emit(block, sems)
    return nc


_NC_CACHE = {}


def kernel(x, norm_attn, w_in, q_norm_a, k_norm_a, q_norm_b, k_norm_b, sinks_b,
           w_branch_a, w_branch_b, w_out, norm_mlp, w_up, w_down):
    f = lambda a: np.ascontiguousarray(np.asarray(a, dtype=np.float32))
    x = f(x)
    tabs = _host_tables()
    g1 = f(np.asarray(norm_attn)[0].reshape(8, 128).T)
    g2 = f(np.asarray(norm_mlp)[0].reshape(8, 128).T)
    gv = f(np.stack([np.asarray(q_norm_a)[0], np.asarray(k_norm_a)[0], np.asarray(q_norm_b)[0], np.asarray(k_norm_b)[0]], axis=1))
    sk = np.asarray(sinks_b)[0]
    sbc = np.zeros((128, 4), np.float32)
    for g in range(2):
        for a in range(2):
            sbc[64:, 2 * g + a] = sk[4 * g + 2 * a]
            sbc[:64, 2 * g + a] = sk[4 * g + 2 * a + 1]
    common = {
        "w_in": f(np.asarray(w_in)[0]), "w_a": f(np.asarray(w_branch_a)[0]), "w_b": f(np.asarray(w_branch_b)[0]),
        "w_out": f(np.asarray(w_out)[0]), "w_up": f(np.asarray(w_up)[0]), "w_down": f(np.asarray(w_down)[0]),
        "g1": g1, "g2": g2, "gv": gv, "sbc": sbc,
    }
    common.update(tabs)
    if "nc" not in _NC_CACHE:
        _NC_CACHE["nc"] = build_nc()
    nc = _NC_CACHE["nc"]
    in_maps = []
    for c in range(NCORES):
        m = dict(common)
        m["x"] = np.ascontiguousarray(x[NSEQ * c:NSEQ * (c + 1)])
        in_maps.append(m)
    res = run_bass_kernel_spmd(nc, in_maps, core_ids=list(range(NCORES)))
    return np.concatenate([r["y"] for r in res.results], axis=0).astype(np.float32)
```

```python
import os
import numpy as np
from contextlib import ExitStack
import concourse.bass as bass
import concourse.mybir as mybir
from concourse.bass_utils import run_bass_kernel_spmd

F32 = mybir.dt.float32
BF16 = mybir.dt.bfloat16
AF = mybir.ActivationFunctionType
ALU = mybir.AluOpType
AX = mybir.AxisListType

NCORES = 8
D = 1024
S = 2048
NSEQ = 2
T = 512
NT = S // T
HD = 64
DFF = 4096
INW = 4352
NEGM = -30000.0
EPS = 1e-6
SCALE = HD ** -0.5
NS = 5
WSZ = 2048
NCHUNK = 62
ENGS = ("pe", "dve", "act", "pool", "sp")


class Buf:
    __slots__ = ("name", "last_w", "readers")

    def __init__(self, name):
        self.name = name
        self.last_w = None
        self.readers = {}


class Prog:
    def __init__(self):
        self.ops = {e: [] for e in ENGS}
        self.cnt = {e: 0 for e in ENGS}
        self.dcnt = {}
        self.waited = {e: {} for e in ENGS}
        self.semnames = set("e_" + e for e in ENGS)
        self.bufs = {}

    def buf(self, name):
        b = self.bufs.get(name)
        if b is None:
            b = self.bufs[name] = Buf(name)
        return b

    def op(self, eng, fn, reads=(), writes=(), dma=None):
        reads = [self.buf(r) for r in reads]
        writes = [self.buf(w) for w in writes]
        own = "e_" + eng
        need = {}

        def add(sem, val, same_ok):
            if sem == own:
                if eng == "pe" or not same_ok:
                    return
            if need.get(sem, 0) < val:
                need[sem] = val

        for b in reads:
            if b.last_w is not None:
                add(b.last_w[0], b.last_w[1], True)
            if b.name.startswith("pb"):
                for s, v in b.readers.items():
                    add(s, v, False)
        for b in writes:
            if b.last_w is not None:
                add(b.last_w[0], b.last_w[1], True)
            for s, v in b.readers.items():
                add(s, v, False)
        waits = []
        wd = self.waited[eng]
        for s, v in need.items():
            if wd.get(s, 0) < v:
                wd[s] = v
                waits.append((s, v))
        if dma is None:
            self.cnt[eng] += 1
            tok = (own, self.cnt[eng])
            inc = (own, 1)
        else:
            sname = "d_" + dma
            self.semnames.add(sname)
            self.dcnt[sname] = self.dcnt.get(sname, 0) + 16
            tok = (sname, self.dcnt[sname])
            inc = (sname, 16)
        self.ops[eng].append((waits, fn, inc))
        for b in writes:
            b.last_w = tok
            b.readers = {}
        for b in reads:
            if b.readers.get(tok[0], 0) < tok[1]:
                b.readers[tok[0]] = tok[1]
        return tok

    def final_wait(self, eng, toks):
        self.ops[eng].append((list(toks), None, None))

    def emit(self, block, sems):
        engmap = {"pe": "tensor", "dve": "vector", "act": "scalar", "pool": "gpsimd", "sp": "sync"}
        for e in ENGS:
            ops = self.ops[e]
            if not ops:
                continue

            def body(engine, ops=ops):
                for waits, fn, inc in ops:
                    for s, v in waits:
                        engine.wait_ge(sems[s], v)
                    if fn is not None:
                        fn(engine).then_inc(sems[inc[0]], inc[1])

            getattr(block, engmap[e])(body)


def _slopes():
    sl = np.exp2(-(8.0 / 16) * np.arange(1, 17, dtype=np.float32)).astype(np.float32)
    return sl[:8], sl[8:]


def _host_tables():
    sl_b, sl_a = _slopes()
    p = np.arange(128, dtype=np.float32)
    k = p[:, None]
    q = p[None, :]
    tri = np.where(k <= q, 0.0, NEGM).astype(np.float32)
    prv = np.where(k > q, 0.0, NEGM).astype(np.float32)
    t = {}
    t["c_ident"] = np.eye(128, dtype=np.float32)
    t["c_tri"] = tri
    t["c_mcur"] = np.tile(tri, (1, 4))
    t["c_mprev"] = np.tile(prv, (1, 4))
    bA = np.zeros((128, 128), np.float32)
    for h in range(8):
        for di in range(16):
            bA[:, h * 16 + di] = sl_a[h] * (p - 128.0 * (di - 3))
    t["c_biasA"] = bA
    bB = np.zeros((128, 16), np.float32)
    for h in range(8):
        bB[:, h * 2 + 0] = sl_b[h] * (p - 64.0)
        bB[:, h * 2 + 1] = sl_b[h] * (p - 192.0)
    t["c_biasB"] = bB
    t1 = np.zeros((128, 2, 2, 128), np.float32)
    for g in range(2):
        for e in range(2):
            t1[64:, g, e, :] = sl_b[4 * g + 2 * e] * (p - 64.0)
            t1[:64, g, e, :] = sl_b[4 * g + 2 * e + 1] * (p - 64.0)
    t["c_t1"] = t1.reshape(128, 512)
    sel = np.zeros((64, 4, 72), np.float32)
    for bi in range(4):
        b = 4 + bi
        for n in range(b):
            for m in range(b):
                if n != m:
                    sel[n * 8 + m, bi, 64 + n] = 1.0
    t["c_sel"] = sel.reshape(64, 288)
    E = np.zeros((8, S), np.float32)
    for n in range(8):
        E[n, n * 256:(n + 1) * 256] = 1.0
    t["c_E"] = E
    t["c_zero"] = np.zeros((8, 8 * T), np.float32)
    return t


def _chunk_pieces(k, w_in, w_a, w_b, w_out, w_up, w_down):
    win = w_in.rearrange("(kc p) n -> p kc n", p=128)
    if k < 10:
        g, kh = divmod(k, 2)
        nc_ = 512 if g < 4 else 256
        return [(0, (4, nc_), win[:, kh * 4:kh * 4 + 4, g * 512:g * 512 + nc_])]
    k -= 10
    if k < 16:
        n, part = divmod(k, 2)
        if part == 0:
            return [(0, (8, 128), win[:, :, 2304 + n * 128:2304 + (n + 1) * 128]),
                    (1024, (8, 128), win[:, :, 3328 + n * 128:3328 + (n + 1) * 128])]
        wa = w_a.rearrange("(kc p) n -> p kc n", p=128)
        wb = w_b.rearrange("(kc p) n -> p kc n", p=128)
        return [(0, (4, 128), wa[:, :, n * 128:(n + 1) * 128]),
                (512, (4, 128), wb[:, :, n * 128:(n + 1) * 128])]
    k -= 16
    if k < 4:
        half, kh = divmod(k, 2)
        wo = w_out.rearrange("(kc p) n -> p kc n", p=128)
        return [(0, (4, 512), wo[:, kh * 4:kh * 4 + 4, half * 512:(half + 1) * 512])]
    k -= 4
    if k < 16:
        gch, kh = divmod(k, 2)
        wu = w_up.rearrange("(kc p) n -> p kc n", p=128)
        return [(0, (4, 512), wu[:, kh * 4:kh * 4 + 4, gch * 512:(gch + 1) * 512])]
    k -= 16
    half, q = divmod(k, 8)
    wd = w_down.rearrange("(fc p) n -> p fc n", p=128)
    return [(0, (4, 512), wd[:, q * 4:q * 4 + 4, half * 512:(half + 1) * 512])]


class _Stop(Exception):
    pass


def build_nc(stop=None, dump=()):
    nc = bass.Bass("TRN2", target_bir_lowering=False)
    dr = lambda n, s, kind="ExternalInput", dt=F32: nc.dram_tensor(n, s, dt, kind=kind).ap()
    x = dr("x", [NSEQ, S, D])
    w_in = dr("w_in", [D, INW])
    w_a = dr("w_a", [512, D])
    w_b = dr("w_b", [512, D])
    w_out = dr("w_out", [D, D])
    w_up = dr("w_up", [D, DFF])
    w_down = dr("w_down", [DFF, D])
    g1_d = dr("g1", [128, 8])
    g2_d = dr("g2", [128, 8])
    gv_d = dr("gv", [64, 4])
    sbc_d = dr("sbc", [128, 4])
    ctab = {n: dr(n, list(a.shape)) for n, a in _host_tables().items()}
    y = dr("y", [NSEQ, S, D], kind="ExternalOutput")
    scr = nc.dram_tensor("scr", [NCHUNK, 128, WSZ], BF16).ap()

    with ExitStack() as es:
        sb = lambda n, s, d: es.enter_context(nc.sbuf_tensor(n, s, d))
        KA = sb("KA", [72, 8, S], BF16)
        VA = sb("VA", [128, 16, 4, 192], BF16)
        KB = sb("KB", [64, 2, 1024], BF16)
        VB = sb("VB", [128, 8, 320], BF16)
        QA = sb("QA", [72, 8, T], BF16)
        QB = sb("QB", [64, 8, T], BF16)
        xs = [sb("xs0", [128, D], F32)]
        x1 = sb("x1", [128, 4, D], F32)
        hT = sb("hT", [128, 8, T], BF16)
        xhat = [sb("xhat%d" % i, [128, D], BF16) for i in range(4)]
        qkh = [sb("qkh%d" % i, [128, 512], BF16) for i in range(4)]
        sq = sb("sq", [128, 512], F32)
        PT = [sb("PT%d" % i, [128, 512], BF16) for i in range(4)]
        oaT = sb("oaT", [128, 4, T], BF16)
        obT = sb("obT", [128, 4, T], BF16)
        sga = sb("sga", [128, 512], F32)
        sgb = sb("sgb", [128, 512], F32)
        tmpa = sb("tmpa", [128, 512], F32)
        tmpr = sb("tmpr", [128, 512], F32)
        HM = sb("HM", [128, 32, T], BF16)
        W = [sb("W%d" % i, [128, WSZ], BF16) for i in range(NS)]
        R = sb("R", [128, 512], F32)
        ident = sb("ident", [128, 128], BF16)
        tri = sb("tri", [128, 128], BF16)
        mcur = sb("mcur", [128, 512], BF16)
        mprev = sb("mprev", [128, 512], BF16)
        biasA = sb("biasA", [128, 128], F32)
        biasB = sb("biasB", [128, 16], F32)
        t1 = sb("t1", [128, 512], F32)
        sinkT = t1
        sbc = sb("sbc_s", [128, 4], F32)
        sel = sb("sel", [64, 288], BF16)
        g1 = sb("g1_s", [128, 8], F32)
        g2 = sb("g2_s", [128, 8], F32)
        gv = sb("gv_s", [64, 4], F32)
        st = sb("st", [128, 64], F32)
        epsT = sb("epsT", [128, 1], F32)
        ksum = sb("ksum", [64, 8, 8], F32)
        dlt = sb("dlt", [64, 8, 64], BF16)
        ind = [sb("ind%d" % i, [64, 512], BF16) for i in range(2)]
        bank = [es.enter_context(nc.psum_tensor("pb%d" % i, [128, 512], F32)) for i in range(8)]
        tpbs = [bank[7][:, :].bitcast(BF16), bank[6][:, :].bitcast(BF16)]

        P = Prog()

        def op(eng, fn, r=(), w=(), dma=None):
            return P.op(eng, fn, r, w, dma)

        def mm(out, lhsT, rhs, start, stop, r, w):
            op("pe", lambda e: e.matmul(out, lhsT=lhsT, rhs=rhs, start=start, stop=stop), r, w)

        def act(out, in_, func, r, w, **kw):
            op("act", lambda e: e.activation(out=out, in_=in_, func=func, **kw), r, w)

        def tt(eng, out, in0, in1, alu, r, w):
            op(eng, lambda e: e.tensor_tensor(out=out, in0=in0, in1=in1, op=alu), r, w)

        def ts(eng, out, in0, s1, s2, op0, op1, r, w):
            if op1 is None:
                op(eng, lambda e: e.tensor_scalar(out=out, in0=in0, scalar1=s1, scalar2=None, op0=op0), r, w)
            else:
                op(eng, lambda e: e.tensor_scalar(out=out, in0=in0, scalar1=s1, scalar2=s2, op0=op0, op1=op1), r, w)

        cidx = [0]

        def cload(dst, src, name, eng="pool"):
            cidx[0] += 1
            op(eng, lambda e: e.dma_start(out=dst, in_=src), (), [name], dma="c%d" % cidx[0])

        cload(ident[:], ctab["c_ident"], "ident")
        cload(tri[:], ctab["c_tri"], "tri")
        cload(mcur[:], ctab["c_mcur"], "mcur")
        cload(mprev[:], ctab["c_mprev"], "mprev")
        cload(sel[:], ctab["c_sel"], "sel")
        cload(biasA[:], ctab["c_biasA"], "biasA", "sp")
        cload(biasB[:], ctab["c_biasB"], "biasB", "sp")
        cload(t1[:], ctab["c_t1"], "t1", "sp")
        cload(sbc[:], sbc_d, "sbc", "sp")
        cload(g1[:], g1_d, "g1", "sp")
        cload(g2[:], g2_d, "g2", "sp")
        cload(gv[:], gv_d, "gv", "sp")
        for h in range(8):
            cload(KA[64:72, h, :], ctab["c_E"], "KAE")
        op("dve", lambda e: e.memset(VA[:, :, :, 64:128], 1.0), (), ["VAones"])
        op("dve", lambda e: e.memset(VB[:, :, 0:64], 1.0), (), ["VBones"])
        op("dve", lambda e: e.memset(VB[:, :, 128:192], 1.0), (), ["VBones"])
        op("dve", lambda e: e.memset(VB[:, :, 256:320], 1.0), (), ["VBones"])
        op("dve", lambda e: e.memset(ksum[:], 0.0), (), ["ksum"])
        op("dve", lambda e: e.memset(epsT[:], EPS), (), ["epsT"])
        for j in range(4):
            act(sinkT[:, j * 128:(j + 1) * 128], t1[:, j * 128:(j + 1) * 128], AF.Exp, ["t1", "sbc"], ["t1", "sinkT"],
                bias=sbc[:, j:j + 1], scale=1.0)

        seq = [(gt, k) for gt in range(NSEQ * NT) for k in range(NCHUNK)]
        wstate = {"next": 0}

        def wload(idx):
            gt, k = seq[idx]
            slot = idx % NS
            wt = W[slot]
            used = WSZ // 2 if (k in (8, 9) or (10 <= k < 26 and (k - 10) % 2 == 1)) else WSZ
            if gt == 0:
                for (off, (kk, ncol), src) in _chunk_pieces(k, w_in, w_a, w_b, w_out, w_up, w_down):
                    dst = wt[:, off:off + kk * ncol].rearrange("p (a b) -> p a b", b=ncol)
                    op("pool", lambda e, dst=dst, src=src: e.dma_start(out=dst, in_=src), (), ["W%d" % slot], dma="w%d" % slot)
                op("sp", lambda e: e.dma_start(out=scr[k, :, 0:used], in_=wt[:, 0:used]), ["W%d" % slot], ["scr%d" % k], dma="ws%d" % slot)
            else:
                op("sp", lambda e: e.dma_start(out=wt[:, 0:used], in_=scr[k, :, 0:used]), ["scr%d" % k], ["W%d" % slot], dma="w%d" % slot)

        def wneed(idx, ahead=NS - 1):
            lim = min(idx + ahead, len(seq) - 1)
            while wstate["next"] <= lim:
                wload(wstate["next"])
                wstate["next"] += 1
            return W[idx % NS], "W%d" % (idx % NS)

        mmb = [0]

        def next_bank():
            b = mmb[0] % 3
            mmb[0] += 1
            return bank[b], "pb%d" % b

        tph = [0]

        def next_tp():
            h = tph[0] % 2
            tph[0] += 1
            return tpbs[h][:, 0:512], "pb%d" % (7 - h)

        def norm_act(src_ap, src_reads, xi, si):
            xh, xhn = xhat[xi], "xhat%d" % xi
            c0 = (si % 4) * 4
            act(xh[:], src_ap, AF.Square, src_reads, [xhn, "st%d" % c0], accum_out=st[:, c0:c0 + 1])
            act(st[:, c0 + 2:c0 + 3], st[:, c0:c0 + 1], AF.Sqrt, ["st%d" % c0, "epsT"], ["st%d" % (c0 + 2)], scale=1.0 / D, bias=epsT[:, 0:1])
            op("dve", lambda e: e.reciprocal(out=st[:, c0 + 3:c0 + 4], in_=st[:, c0 + 2:c0 + 3]), ["st%d" % (c0 + 2)], ["st%d" % (c0 + 3)])
            act(xh[:], src_ap, AF.Copy, list(src_reads) + ["st%d" % (c0 + 3)], [xhn], scale=st[:, c0 + 3:c0 + 4])

        def norm_pe_half(xi, gain, gname, dst_cols, half):
            xh, xhn = xhat[xi], "xhat%d" % xi
            tp, tpn = next_tp()
            for j in range(4):
                jj = half * 4 + j
                op("pe", lambda e, jj=jj, j=j, tp=tp: e.transpose(out=tp[:, j * 128:(j + 1) * 128], in_=xh[:, jj * 128:(jj + 1) * 128], identity=ident[:, :]),
                   [xhn, "ident"], [tpn])
            tt("dve", hT[:, half * 4:half * 4 + 4, dst_cols], tp.rearrange("p (j q) -> p j q", q=128),
               gain[:, half * 4:half * 4 + 4].unsqueeze(2).broadcast_to([128, 4, 128]), ALU.mult, [tpn, gname], ["hT"])

        def qk_norm(ps, psn, nh, col0, qb_):
            n = nh * 64
            qk = qkh[qb_]
            act(sq[:, 0:n], ps, AF.Square, [psn], ["sq"])
            op("dve", lambda e: e.tensor_reduce(out=st[:, 32:32 + nh], in_=sq[:, 0:n].rearrange("p (h d) -> p h d", d=64), axis=AX.X, op=ALU.add),
               ["sq"], ["stq"])
            act(st[:, 48:48 + nh], st[:, 32:32 + nh], AF.Sqrt, ["stq", "epsT"], ["stq3"], scale=1.0 / HD, bias=epsT[:, 0:1])
            op("dve", lambda e: e.reciprocal(out=st[:, 56:56 + nh], in_=st[:, 48:48 + nh]), ["stq3"], ["stq4"])
            tt("dve", qk[:, 0:n].rearrange("p (h d) -> p h d", d=64), ps.rearrange("p (h d) -> p h d", d=64),
               st[:, 56:56 + nh].unsqueeze(2).broadcast_to([128, nh, 64]), ALU.mult, [psn, "stq4"], ["qkh%d" % qb_])

        def head_T(col0, nh, dst_fn, gcol, dname, qb_):
            qk = qkh[qb_]
            for h0 in range(0, nh, 4):
                nhh = min(4, nh - h0)
                tp, tpn = next_tp()
                for j in range(nhh):
                    c = (h0 + j) * 64
                    op("pe", lambda e, c=c, j=j, tp=tp: e.transpose(out=tp[0:64, j * 128:(j + 1) * 128], in_=qk[:, c:c + 64], identity=ident[:, :]),
                       ["qkh%d" % qb_, "ident"], [tpn])
                act(dst_fn(h0, nhh), tp[0:64, 0:nhh * 128].rearrange("p (j q) -> p j q", q=128), AF.Copy, [tpn, "gv"], [dname],
                    scale=gv[:, gcol:gcol + 1])

        ychunk = [0]
        ytok = None

        def stage(n):
            if stop == n:
                raise _Stop()

        widx = [0]

        tiles = [(s_, c_) for s_ in range(NSEQ) for c_ in range(NT)]

        def A0_load(t, i):
            s_, c_ = tiles[t]
            xb, xn = xs[0], "xs0"
            src = x[s_, c_ * T + i * 128:c_ * T + (i + 1) * 128, :]
            op("sp", lambda e: e.dma_start(out=xb[:], in_=src), (), [xn], dma=xn)

        def A0_act(t, i):
            norm_act(xs[0][:], ["xs0"], i, i)

        def A0_pe(t, i, half):
            norm_pe_half(i, g1, "g1", slice(i * 128, (i + 1) * 128), half)

        try:
          stage('const')
          for i in range(4):
              A0_load(0, i)
              A0_act(0, i)
          for i in range(4):
              for half in range(2):
                  A0_pe(0, i, half)
          for s in range(NSEQ):
            for c in range(NT):
                T0 = c * T
                base = widx[0]
                tcur = s * NT + c
                has_next = tcur + 1 < len(tiles)
                hbk = (3, 4, 5, 0)
                mmb[0] = 1
                if c == 0:
                    op("pool", lambda e: e.dma_start(out=QA[64:72, :, :].rearrange("p h q -> p (h q)"), in_=ctab["c_zero"]), (), ["QAm"], dma="qam")
                stage('A0')
                srcx = x[s, T0:T0 + T, :].rearrange("(i p) d -> p i d", p=128)
                op("sp", lambda e, srcx=srcx: e.dma_start(out=x1[:], in_=srcx), (), ["x1_0", "x1_1", "x1_2", "x1_3"], dma="x1")
                pend = []

                def flush(keep):
                    while len(pend) > keep:
                        f_ = pend.pop(0)
                        if f_ is not None:
                            f_()

                gi = 0
                for g in range(5):
                    w0, w0n = wneed(base + 2 * g)
                    w1, w1n = wneed(base + 2 * g + 1, NS - 2)
                    ncol = 512 if g < 4 else 256
                    for i in range(4):
                        ab_ = (1, 2, 0, 3, 4)[gi % 5]
                        pb, pbn = bank[ab_], "pb%d" % ab_
                        kt = 4 * c + i
                        qb_ = gi % 4
                        gi += 1
                        for k in range(8):
                            ww = (w0 if k < 4 else w1)[:, (k % 4) * ncol:(k % 4 + 1) * ncol]
                            mm(pb[:, 0:ncol], hT[:, k, i * 128:(i + 1) * 128], ww, k == 0, k == 7,
                               ["hT", w0n if k < 4 else w1n], [pbn])
                        qc = slice(i * 128, (i + 1) * 128)
                        nxt = None
                        if g == 0:
                            qk_norm(pb[:, 0:512], pbn, 8, 0, qb_)
                            nxt = lambda qc=qc, qb_=qb_: head_T(0, 8, lambda h0, n: QA[0:64, h0:h0 + n, qc], 0, "QA", qb_)
                        elif g == 1:
                            qk_norm(pb[:, 0:512], pbn, 8, 512, qb_)
                            kc = slice(T0 + i * 128, T0 + (i + 1) * 128)
                            nxt = lambda kc=kc, qb_=qb_: head_T(512, 8, lambda h0, n: KA[0:64, h0:h0 + n, kc], 1, "KA", qb_)
                        elif g == 2:
                            pv = pb[:, 0:512].rearrange("p (pr two d) -> p pr two d", two=2, d=64)
                            act(VA[:, kt, :, 0:64], pv[:, :, 0, :], AF.Copy, [pbn], ["VA"])
                            act(VA[:, kt, :, 128:192], pv[:, :, 1, :], AF.Copy, [pbn], ["VA"])
                        elif g == 3:
                            qk_norm(pb[:, 0:512], pbn, 8, 1024, qb_)
                            nxt = lambda qc=qc, qb_=qb_: head_T(1024, 8, lambda h0, n: QB[0:64, h0:h0 + n, qc], 2, "QB", qb_)
                        else:
                            qk_norm(pb[:, 0:128], pbn, 2, 1536, qb_)
                            ks = kt % 8
                            nxt = lambda ks=ks, qb_=qb_: head_T(1536, 2, lambda h0, n: KB[0:64, h0:h0 + n, ks * 128:(ks + 1) * 128], 3, "KB", qb_)
                            act(VB[:, ks, 64:128], pb[:, 128:192], AF.Copy, [pbn], ["VB"])
                            act(VB[:, ks, 192:256], pb[:, 192:256], AF.Copy, [pbn], ["VB"])
                        pend.append(nxt)
                        flush(3)
                flush(0)
                wneed(base + 10, 3)
                for h in range(8):
                    ko = ksum[:, h, 2 * c:2 * c + 2]
                    ki = KA[0:64, h, T0:T0 + T].rearrange("p (n k) -> p n k", k=256)
                    op("dve", lambda e, ko=ko, ki=ki: e.tensor_reduce(out=ko, in_=ki, axis=AX.X, op=ALU.add), ["KA"], ["ksum"])
                if c >= 2:
                    gbk = (3, 4, 0, 1)

                    def g1_(h):
                        tt("dve", dlt[:, h, :].rearrange("p (n m) -> p n m", m=8),
                           ksum[:, h, :].unsqueeze(2).broadcast_to([64, 8, 8]),
                           ksum[:, h, :].unsqueeze(1).broadcast_to([64, 8, 8]), ALU.subtract, ["ksum"], ["dlt%d" % h])
                        gb_, gbn = bank[gbk[h % 4]], "pb%d" % gbk[h % 4]
                        mm(gb_[0:64, :], dlt[:, h, :], QA[0:64, h, :], True, True, ["dlt%d" % h, "QA"], [gbn])

                    def g2_(h):
                        gb_, gbn = bank[gbk[h % 4]], "pb%d" % gbk[h % 4]
                        iv, ivn = ind[h % 2], "ind%d" % (h % 2)
                        op("dve", lambda e: e.tensor_single_scalar(out=iv[:, :], in_=gb_[0:64, :], scalar=0.0, op=ALU.is_lt), [gbn], [ivn])
                        for hb in range(2):
                            bi = 2 * c + hb - 4
                            mm(gb_[0:72, hb * 256:(hb + 1) * 256], sel[:, bi * 72:(bi + 1) * 72], iv[:, hb * 256:(hb + 1) * 256], True, True,
                               ["sel", ivn], [gbn])

                    def g3_(h):
                        gb_, gbn = bank[gbk[h % 4]], "pb%d" % gbk[h % 4]
                        ts("dve", QA[64:72, h, :], gb_[64:72, :], 2.5, NEGM, ALU.is_ge, ALU.mult, [gbn], ["QAm"])

                    for k in range(10):
                        if k < 8:
                            g1_(k)
                        if 0 <= k - 1 < 8:
                            g2_(k - 1)
                        if 0 <= k - 2 < 8:
                            g3_(k - 2)
                stage('gate%d' % c)
                steps = [(h, j) for h in range(8) for j in range(4 * c + 4)]
                nst = len(steps)

                def moba_S(si):
                    h, j = steps[si]
                    qs = 128 * max(0, j - 4 * c)
                    N = T - qs
                    sb_i = (3, 4, 0)[si % 3]
                    sbk, sbn = bank[sb_i], "pb%d" % sb_i
                    diag = j >= 4 * c
                    mm(sbk[:, 0:N], KA[0:72, h, j * 128:(j + 1) * 128], QA[0:72, h, qs:T], True, not diag,
                       ["KA", "KAE", "QA", "QAm"], [sbn])
                    if diag:
                        mm(sbk[:, 0:128], ident[:, :], tri[:, :], False, True, ["ident", "tri"], [sbn])
                    di = 4 * c - j + 3
                    pt = PT[si % 4]
                    act(pt[:, 0:N], sbk[:, 0:N], AF.Exp, [sbn, "biasA"], ["PT%d" % (si % 4)],
                        bias=biasA[:, h * 16 + di:h * 16 + di + 1], scale=SCALE)

                def moba_PV(si):
                    h, j = steps[si]
                    qs = 128 * max(0, j - 4 * c)
                    N = T - qs
                    ob_, obn = bank[5 + h % 2], "pb%d" % (5 + h % 2)
                    pr, od = divmod(h, 2)
                    lhsT = VA[:, j, pr, od * 64:od * 64 + 128]
                    mm(ob_[:, qs:T], lhsT, PT[si % 4][:, 0:N], j == 0, j == 4 * c + 3, ["VA", "VAones", "PT%d" % (si % 4)], [obn])
                    if j == 4 * c + 3:
                        lo, hi = (0, 64) if od == 0 else (64, 128)
                        dl, dh = (64, 128) if od == 0 else (0, 64)
                        rn = "R%d" % od
                        op("dve", lambda e: e.tensor_copy(out=R[dl:dh, :], in_=ob_[dl:dh, :]), [obn], [rn])
                        op("dve", lambda e: e.reciprocal(out=R[dl:dh, :], in_=R[dl:dh, :]), [rn], [rn])
                        tt("dve", oaT[lo:hi, pr, :], ob_[lo:hi, :], R[dl:dh, :], ALU.mult, [obn, rn], ["oaT%d" % od])

                moba_S(0)
                moba_S(1)
                for si in range(nst):
                    if si + 2 < nst:
                        moba_S(si + 2)
                    moba_PV(si)
                stage('B')
                sw = []
                for i in range(4):
                    kt = 4 * c + i
                    for g in range(2):
                        passes = [(kt % 8, mcur, "mcur", 0)] + ([((kt - 1) % 8, mprev, "mprev", 1)] if kt >= 1 else [])
                        for pi, (kslot, mk, mkn, which) in enumerate(passes):
                            sw.append((i, g, pi, len(passes), kslot, mk, mkn, which))

                def swa_S(n):
                    i, g, pi, npass, kslot, mk, mkn, which = sw[n]
                    sb_i = (3, 4, 2)[n % 3]
                    sbk, sbn = bank[sb_i], "pb%d" % sb_i
                    mm(sbk[:, :], KB[0:64, g, kslot * 128:(kslot + 1) * 128], QB[0:64, 4 * g:4 * g + 4, i * 128:(i + 1) * 128],
                       True, False, ["KB", "QB"], [sbn])
                    mm(sbk[:, :], ident[:, :], mk[:, :], False, True, ["ident", mkn], [sbn])
                    pt = PT[n % 4]
                    for hh in range(4):
                        h = 4 * g + hh
                        act(pt[:, hh * 128:(hh + 1) * 128], sbk[:, hh * 128:(hh + 1) * 128], AF.Exp, [sbn, "biasB"], ["PT%d" % (n % 4)],
                            bias=biasB[:, h * 2 + which:h * 2 + which + 1], scale=SCALE)

                def swa_PV(n):
                    i, g, pi, npass, kslot, mk, mkn, which = sw[n]
                    be, bo = (5, 6) if i % 2 == 0 else (0, 1)
                    oe, oen, oo, oon = bank[be], "pb%d" % be, bank[bo], "pb%d" % bo
                    pt = PT[n % 4]
                    ptn = "PT%d" % (n % 4)
                    pt3 = pt[:, :].rearrange("p (a e q) -> p a e q", e=2, q=128)
                    last = pi == npass - 1
                    cs = slice(g * 256, (g + 1) * 256)
                    mm(oe[:, cs], VB[:, kslot, 64 + 128 * g:64 + 128 * g + 128], pt3[:, :, 0, :], pi == 0, last,
                       ["VB", "VBones", ptn], [oen])
                    mm(oo[:, cs], VB[:, kslot, 128 * g:128 * g + 128], pt3[:, :, 1, :], pi == 0, last,
                       ["VB", "VBones", ptn], [oon])
                    if not (last and g == 1):
                        return
                    tt("dve", R[64:128, :], oe[64:128, :], sinkT[64:128, :], ALU.add, [oen, "sinkT"], ["R0"])
                    tt("dve", R[0:64, :], oo[0:64, :], sinkT[0:64, :], ALU.add, [oon, "sinkT"], ["R1"])
                    op("dve", lambda e: e.reciprocal(out=R[:, :], in_=R[:, :]), ["R0", "R1"], ["R0", "R1"])
                    qc = slice(i * 128, (i + 1) * 128)
                    tt("dve", obT[0:64, :, qc], oe[0:64, :].rearrange("p (a q) -> p a q", q=128),
                       R[64:128, :].rearrange("p (a q) -> p a q", q=128), ALU.mult, [oen, "R0"], ["obT0"])
                    tt("dve", obT[64:128, :, qc], oo[64:128, :].rearrange("p (a q) -> p a q", q=128),
                       R[0:64, :].rearrange("p (a q) -> p a q", q=128), ALU.mult, [oon, "R1"], ["obT1"])

                swa_S(0)
                swa_S(1)
                for n in range(len(sw)):
                    if n + 2 < len(sw):
                        swa_S(n + 2)
                    swa_PV(n)
                stage('C')
                for n in range(8):
                    wg, wgn = wneed(base + 10 + 2 * n)
                    wab, wabn = wneed(base + 10 + 2 * n + 1, NS - 2)
                    pa, pan = next_bank()
                    for k in range(8):
                        mm(pa[:, :], wg[:, k * 128:(k + 1) * 128], hT[:, k, :], k == 0, k == 7, [wgn, "hT"], [pan])
                    act(sga[:], pa[:, :], AF.Sigmoid, [pan], ["sga"])
                    pb2, pb2n = next_bank()
                    for k in range(8):
                        mm(pb2[:, :], wg[:, 1024 + k * 128:1024 + (k + 1) * 128], hT[:, k, :], k == 0, k == 7, [wgn, "hT"], [pb2n])
                    act(sgb[:], pb2[:, :], AF.Sigmoid, [pb2n], ["sgb"])
                    pc, pcn = next_bank()
                    for k in range(4):
                        mm(pc[:, :], wab[:, k * 128:(k + 1) * 128], oaT[:, k, :], k == 0, k == 3, [wabn, "oaT0", "oaT1"], [pcn])
                    tt("dve", tmpa[:], pc[:, :], sga[:], ALU.mult, [pcn, "sga"], ["tmpa"])
                    pd, pdn = next_bank()
                    for k in range(4):
                        mm(pd[:, :], wab[:, 512 + k * 128:512 + (k + 1) * 128], obT[:, k, :], k == 0, k == 3, [wabn, "obT0", "obT1"], [pdn])
                    tt("dve", sgb[:], pd[:, :], sgb[:], ALU.mult, [pdn, "sgb"], ["sgb"])
                    tt("dve", HM[:, n, :], tmpa[:], sgb[:], ALU.add, ["tmpa", "sgb"], ["hid%d" % n])
                stage('D')
                for half in range(2):
                    w0, w0n = wneed(base + 26 + 2 * half)
                    w1, w1n = wneed(base + 26 + 2 * half + 1, NS - 2)
                    for i in range(4):
                        pb, pbn = next_bank()
                        for k in range(8):
                            ww = (w0 if k < 4 else w1)[:, (k % 4) * 512:(k % 4 + 1) * 512]
                            mm(pb[:, :], HM[:, k, i * 128:(i + 1) * 128], ww, k == 0, k == 7, ["hid%d" % k, w0n if k < 4 else w1n], [pbn])
                        xo = x1[:, i, half * 512:(half + 1) * 512]
                        tt("dve", xo, pb[:, :], xo, ALU.add, [pbn, "x1_%d" % i], ["x1_%d" % i])
                        if half == 1:
                            norm_act(x1[:, i, :], ["x1_%d" % i], i, 4 + i)
                stage('E')
                for i in range(4):
                    for half in range(2):
                        norm_pe_half(i, g2, "g2", slice(i * 128, (i + 1) * 128), half)
                stage('F')
                for gch in range(8):
                    w0, w0n = wneed(base + 30 + 2 * gch)
                    w1, w1n = wneed(base + 30 + 2 * gch + 1, NS - 2)
                    for f in range(4):
                        pb, pbn = next_bank()
                        for k in range(8):
                            ww = (w0 if k < 4 else w1)[:, (k % 4) * 512 + f * 128:(k % 4) * 512 + (f + 1) * 128]
                            mm(pb[:, :], ww, hT[:, k, :], k == 0, k == 7, ["hT", w0n if k < 4 else w1n], [pbn])
                        act(tmpr[:], pb[:, :], AF.Relu, [pbn], ["tmpr"])
                        tt("dve", HM[:, gch * 4 + f, :], pb[:, :], tmpr[:], ALU.mult, [pbn, "tmpr"], ["hid%d" % (gch * 4 + f)])
                        if has_next and f == 3:
                            sched = {0: ("L0",), 1: ("A0", "L1"), 2: ("A1", "L2"), 4: ("A2", "L3"), 6: ("A3",)}
                            for what in sched.get(gch, ()):
                                (A0_load if what[0] == "L" else A0_act)(tcur + 1, int(what[1]))
                stage('G')
                for half in range(2):
                    for q in range(8):
                        wd, wdn = wneed(base + 46 + half * 8 + q)
                        for f in range(4):
                            ff = q * 4 + f
                            for i in range(4):
                                mm(bank[hbk[i]][:, :], HM[:, ff, i * 128:(i + 1) * 128], wd[:, f * 512:(f + 1) * 512], ff == 0, ff == 31,
                                   ["hid%d" % ff, wdn], ["pb%d" % hbk[i]])
                        if has_next and half == 0:
                            A0_pe(tcur + 1, q // 2, q % 2)
                    for i in range(4):
                        xo = x1[:, i, half * 512:(half + 1) * 512]
                        tt("dve", xo, bank[hbk[i]][:, :], xo, ALU.add, ["pb%d" % hbk[i], "x1_%d" % i], ["x1_%d" % i])
                dsty = y[s, T0:T0 + T, :].rearrange("(i p) d -> p i d", p=128)
                ytok = op("pool", lambda e, dsty=dsty: e.dma_start(out=dsty, in_=x1[:]), ["x1_0", "x1_1", "x1_2", "x1_3"], (), dma="y")
                widx[0] += NCHUNK
                stage('T%d' % (s * NT + c))
        except _Stop:
            pass
        if ytok is not None:
            P.final_wait("pool", [ytok])
        if dump:
            tens = {"hT": hT, "QA": QA, "KA": KA, "VA": VA, "QB": QB, "KB": KB, "VB": VB, "oaT": oaT, "obT": obT,
                    "HM": HM, "x1": x1, "ksum": ksum, "sinkT": sinkT, "R": R, "st": st, "qkh": qkh[0], "ind": ind[0], "dlt": dlt}
            allb = list(P.bufs.keys())
            dtoks = []
            for nm in dump:
                tns = tens[nm]
                shp = list(tns.shape)
                flat = tns[:]
                if len(shp) == 3:
                    flat = flat.rearrange("p a b -> p (a b)")
                elif len(shp) == 4:
                    flat = flat.rearrange("p a b c -> p (a b c)")
                fr = int(np.prod(shp[1:]))
                dd = nc.dram_tensor("dbg_" + nm, [shp[0], fr], tns.dtype, kind="ExternalOutput").ap()
                dtoks.append(op("sp", lambda e, dd=dd, flat=flat: e.dma_start(out=dd, in_=flat), allb, (), dma="dbg_" + nm))
            P.final_wait("sp", dtoks)
        sems = {n: es.enter_context(nc.semaphore(n)) for n in sorted(P.semnames)}
        with nc.Block() as block:
            P.emit(block, sems)
    return nc


_NC_CACHE = {}


def kernel(x, norm_attn, w_in, q_norm_a, k_norm_a, q_norm_b, k_norm_b, sinks_b,
           w_branch_a, w_branch_b, w_out, norm_mlp, w_up, w_down):
    f = lambda a: np.ascontiguousarray(np.asarray(a, dtype=np.float32))
    x = f(x)
    tabs = _host_tables()
    g1 = f(np.asarray(norm_attn)[0].reshape(8, 128).T)
    g2 = f(np.asarray(norm_mlp)[0].reshape(8, 128).T)
    gv = f(np.stack([np.asarray(q_norm_a)[0], np.asarray(k_norm_a)[0], np.asarray(q_norm_b)[0], np.asarray(k_norm_b)[0]], axis=1))
    sk = np.asarray(sinks_b)[0]
    sbc = np.zeros((128, 4), np.float32)
    for g in range(2):
        for a in range(2):
            sbc[64:, 2 * g + a] = sk[4 * g + 2 * a]
            sbc[:64, 2 * g + a] = sk[4 * g + 2 * a + 1]
    common = {
        "w_in": f(np.asarray(w_in)[0]), "w_a": f(np.asarray(w_branch_a)[0]), "w_b": f(np.asarray(w_branch_b)[0]),
        "w_out": f(np.asarray(w_out)[0]), "w_up": f(np.asarray(w_up)[0]), "w_down": f(np.asarray(w_down)[0]),
        "g1": g1, "g2": g2, "gv": gv, "sbc": sbc,
    }
    common.update(tabs)
    if "nc" not in _NC_CACHE:
        _NC_CACHE["nc"] = build_nc()
    nc = _NC_CACHE["nc"]
    in_maps = []
    for c in range(NCORES):
        m = dict(common)
        m["x"] = np.ascontiguousarray(x[NSEQ * c:NSEQ * (c + 1)])
        in_maps.append(m)
    res = run_bass_kernel_spmd(nc, in_maps, core_ids=list(range(NCORES)))
    return np.concatenate([r["y"] for r in res.results], axis=0).astype(np.float32)
```

```python
import os
import numpy as np
from contextlib import ExitStack
import concourse.bass as bass
import concourse.mybir as mybir
from concourse.bass_utils import run_bass_kernel_spmd

F32 = mybir.dt.float32
BF16 = mybir.dt.bfloat16
AF = mybir.ActivationFunctionType
ALU = mybir.AluOpType
AX = mybir.AxisListType

NCORES = 8
D = 1024
S = 2048
NSEQ = 2
T = 512
NT = S // T
HD = 64
DFF = 4096
INW = 4352
NEGM = -30000.0
EPS = 1e-6
SCALE = HD ** -0.5
NS = 6
WSZ = 2048
NCHUNK = 62
ENGS = ("pe", "dve", "act", "pool", "sp")


class Buf:
    __slots__ = ("name", "last_w", "readers")

    def __init__(self, name):
        self.name = name
        self.last_w = None
        self.readers = {}


class Prog:
    def __init__(self):
        self.ops = {e: [] for e in ENGS}
        self.cnt = {e: 0 for e in ENGS}
        self.dcnt = {}
        self.waited = {e: {} for e in ENGS}
        self.semnames = set("e_" + e for e in ENGS)
        self.bufs = {}

    def buf(self, name):
        b = self.bufs.get(name)
        if b is None:
            b = self.bufs[name] = Buf(name)
        return b

    def op(self, eng, fn, reads=(), writes=(), dma=None):
        reads = [self.buf(r) for r in reads]
        writes = [self.buf(w) for w in writes]
        own = "e_" + eng
        need = {}

        def add(sem, val, same_ok):
            if sem == own:
                if eng == "pe" or not same_ok:
                    return
            if need.get(sem, 0) < val:
                need[sem] = val

        for b in reads:
            if b.last_w is not None:
                add(b.last_w[0], b.last_w[1], True)
            if b.name.startswith("pb"):
                for s, v in b.readers.items():
                    add(s, v, False)
        for b in writes:
            if b.last_w is not None:
                add(b.last_w[0], b.last_w[1], True)
            for s, v in b.readers.items():
                add(s, v, False)
        waits = []
        wd = self.waited[eng]
        for s, v in need.items():
            if wd.get(s, 0) < v:
                wd[s] = v
                waits.append((s, v))
        if dma is None:
            self.cnt[eng] += 1
            tok = (own, self.cnt[eng])
            inc = (own, 1)
        else:
            sname = "d_" + dma
            self.semnames.add(sname)
            self.dcnt[sname] = self.dcnt.get(sname, 0) + 16
            tok = (sname, self.dcnt[sname])
            inc = (sname, 16)
        self.ops[eng].append((waits, fn, inc))
        for b in writes:
            b.last_w = tok
            b.readers = {}
        for b in reads:
            if b.readers.get(tok[0], 0) < tok[1]:
                b.readers[tok[0]] = tok[1]
        return tok

    def final_wait(self, eng, toks):
        self.ops[eng].append((list(toks), None, None))

    def emit(self, block, sems):
        engmap = {"pe": "tensor", "dve": "vector", "act": "scalar", "pool": "gpsimd", "sp": "sync"}
        for e in ENGS:
            ops = self.ops[e]
            if not ops:
                continue

            def body(engine, ops=ops):
                for waits, fn, inc in ops:
                    for s, v in waits:
                        engine.wait_ge(sems[s], v)
                    if fn is not None:
                        fn(engine).then_inc(sems[inc[0]], inc[1])

            getattr(block, engmap[e])(body)


def _slopes():
    sl = np.exp2(-(8.0 / 16) * np.arange(1, 17, dtype=np.float32)).astype(np.float32)
    return sl[:8], sl[8:]


def _host_tables():
    sl_b, sl_a = _slopes()
    p = np.arange(128, dtype=np.float32)
    k = p[:, None]
    q = p[None, :]
    tri = np.where(k <= q, 0.0, NEGM).astype(np.float32)
    prv = np.where(k > q, 0.0, NEGM).astype(np.float32)
    t = {}
    t["c_ident"] = np.eye(128, dtype=np.float32)
    t["c_tri"] = tri
    t["c_mcur"] = np.tile(tri, (1, 4))
    t["c_mprev"] = np.tile(prv, (1, 4))
    bA = np.zeros((128, 128), np.float32)
    for h in range(8):
        for di in range(16):
            bA[:, h * 16 + di] = sl_a[h] * (p - 128.0 * (di - 3))
    t["c_biasA"] = bA
    bB = np.zeros((128, 16), np.float32)
    for h in range(8):
        bB[:, h * 2 + 0] = sl_b[h] * (p - 64.0)
        bB[:, h * 2 + 1] = sl_b[h] * (p - 192.0)
    t["c_biasB"] = bB
    t1 = np.zeros((128, 2, 2, 128), np.float32)
    for g in range(2):
        for e in range(2):
            t1[64:, g, e, :] = sl_b[4 * g + 2 * e] * (p - 64.0)
            t1[:64, g, e, :] = sl_b[4 * g + 2 * e + 1] * (p - 64.0)
    t["c_t1"] = t1.reshape(128, 512)
    sel = np.zeros((64, 4, 72), np.float32)
    for bi in range(4):
        b = 4 + bi
        for n in range(b):
            for m in range(b):
                if n != m:
                    sel[n * 8 + m, bi, 64 + n] = 1.0
    t["c_sel"] = sel.reshape(64, 288)
    E = np.zeros((8, S), np.float32)
    for n in range(8):
        E[n, n * 256:(n + 1) * 256] = 1.0
    t["c_E"] = E
    t["c_zero"] = np.zeros((8, 8 * T), np.float32)
    return t


def _chunk_pieces(k, w_in, w_a, w_b, w_out, w_up, w_down):
    win = w_in.rearrange("(kc p) n -> p kc n", p=128)
    if k < 10:
        g, kh = divmod(k, 2)
        nc_ = 512 if g < 4 else 256
        return [(0, (4, nc_), win[:, kh * 4:kh * 4 + 4, g * 512:g * 512 + nc_])]
    k -= 10
    if k < 16:
        n, part = divmod(k, 2)
        if part == 0:
            return [(0, (8, 128), win[:, :, 2304 + n * 128:2304 + (n + 1) * 128]),
                    (1024, (8, 128), win[:, :, 3328 + n * 128:3328 + (n + 1) * 128])]
        wa = w_a.rearrange("(kc p) n -> p kc n", p=128)
        wb = w_b.rearrange("(kc p) n -> p kc n", p=128)
        return [(0, (4, 128), wa[:, :, n * 128:(n + 1) * 128]),
                (512, (4, 128), wb[:, :, n * 128:(n + 1) * 128])]
    k -= 16
    if k < 4:
        half, kh = divmod(k, 2)
        wo = w_out.rearrange("(kc p) n -> p kc n", p=128)
        return [(0, (4, 512), wo[:, kh * 4:kh * 4 + 4, half * 512:(half + 1) * 512])]
    k -= 4
    if k < 16:
        gch, kh = divmod(k, 2)
        wu = w_up.rearrange("(kc p) n -> p kc n", p=128)
        return [(0, (4, 512), wu[:, kh * 4:kh * 4 + 4, gch * 512:(gch + 1) * 512])]
    k -= 16
    half, q = divmod(k, 8)
    wd = w_down.rearrange("(fc p) n -> p fc n", p=128)
    return [(0, (4, 512), wd[:, q * 4:q * 4 + 4, half * 512:(half + 1) * 512])]


class _Stop(Exception):
    pass


def build_nc(stop=None, dump=()):
    nc = bass.Bass("TRN2", target_bir_lowering=False)
    dr = lambda n, s, kind="ExternalInput", dt=F32: nc.dram_tensor(n, s, dt, kind=kind).ap()
    x = dr("x", [NSEQ, S, D])
    w_in = dr("w_in", [D, INW])
    w_a = dr("w_a", [512, D])
    w_b = dr("w_b", [512, D])
    w_out = dr("w_out", [D, D])
    w_up = dr("w_up", [D, DFF])
    w_down = dr("w_down", [DFF, D])
    g1_d = dr("g1", [128, 8])
    g2_d = dr("g2", [128, 8])
    gv_d = dr("gv", [64, 4])
    sbc_d = dr("sbc", [128, 4])
    ctab = {n: dr(n, list(a.shape)) for n, a in _host_tables().items()}
    y = dr("y", [NSEQ, S, D], kind="ExternalOutput")
    scr = nc.dram_tensor("scr", [NCHUNK, 128, WSZ], BF16).ap()

    with ExitStack() as es:
        sb = lambda n, s, d: es.enter_context(nc.sbuf_tensor(n, s, d))
        KA = sb("KA", [72, 8, S], BF16)
        VA = sb("VA", [128, 16, 4, 192], BF16)
        KB = sb("KB", [64, 2, 1024], BF16)
        VB = sb("VB", [128, 8, 320], BF16)
        QA = sb("QA", [72, 8, T], BF16)
        QB = sb("QB", [64, 8, T], BF16)
        xs = [sb("xs0", [128, D], F32)]
        x1 = sb("x1", [128, 4, D], F32)
        hT = sb("hT", [128, 8, T], BF16)
        xhat = [sb("xhat%d" % i, [128, D], BF16) for i in range(4)]
        qkh = [sb("qkh%d" % i, [128, 512], BF16) for i in range(4)]
        sq = sb("sq", [128, 512], F32)
        PT = [sb("PT%d" % i, [128, 512], BF16) for i in range(3)]
        oaT = sb("oaT", [128, 4, T], BF16)
        obT = sb("obT", [128, 4, T], BF16)
        sga = sb("sga", [128, 512], F32)
        sgb = sb("sgb", [128, 512], F32)
        tmpr = sb("tmpr", [128, 512], F32)
        HM = sb("HM", [128, 32, T], BF16)
        W = [sb("W%d" % i, [128, WSZ], BF16) for i in range(NS)]
        R = sb("R", [128, 512], F32)
        ident = sb("ident", [128, 128], BF16)
        tri = sb("tri", [128, 128], BF16)
        mcur = sb("mcur", [128, 512], BF16)
        mprev = sb("mprev", [128, 512], BF16)
        biasA = sb("biasA", [128, 128], F32)
        biasB = sb("biasB", [128, 16], F32)
        t1 = sb("t1", [128, 512], F32)
        sinkT = t1
        sbc = sb("sbc_s", [128, 4], F32)
        sel = sb("sel", [64, 288], BF16)
        g1 = sb("g1_s", [128, 8], F32)
        g2 = sb("g2_s", [128, 8], F32)
        gv = sb("gv_s", [64, 4], F32)
        st = sb("st", [128, 64], F32)
        epsT = sb("epsT", [128, 1], F32)
        ksum = sb("ksum", [64, 8, 8], F32)
        dlt = sb("dlt", [64, 8, 64], BF16)
        ind = [sb("ind%d" % i, [64, 512], BF16) for i in range(2)]
        bank = [es.enter_context(nc.psum_tensor("pb%d" % i, [128, 512], F32)) for i in range(8)]
        tpbs = [bank[7][:, :].bitcast(BF16), bank[6][:, :].bitcast(BF16)]

        P = Prog()

        def op(eng, fn, r=(), w=(), dma=None):
            return P.op(eng, fn, r, w, dma)

        def mm(out, lhsT, rhs, start, stop, r, w):
            op("pe", lambda e: e.matmul(out, lhsT=lhsT, rhs=rhs, start=start, stop=stop), r, w)

        def act(out, in_, func, r, w, **kw):
            op("act", lambda e: e.activation(out=out, in_=in_, func=func, **kw), r, w)

        def tt(eng, out, in0, in1, alu, r, w):
            op(eng, lambda e: e.tensor_tensor(out=out, in0=in0, in1=in1, op=alu), r, w)

        def ts(eng, out, in0, s1, s2, op0, op1, r, w):
            if op1 is None:
                op(eng, lambda e: e.tensor_scalar(out=out, in0=in0, scalar1=s1, scalar2=None, op0=op0), r, w)
            else:
                op(eng, lambda e: e.tensor_scalar(out=out, in0=in0, scalar1=s1, scalar2=s2, op0=op0, op1=op1), r, w)

        cidx = [0]

        def cload(dst, src, name, eng="pool"):
            cidx[0] += 1
            op(eng, lambda e: e.dma_start(out=dst, in_=src), (), [name], dma="c%d" % cidx[0])

        cload(ident[:], ctab["c_ident"], "ident")
        cload(tri[:], ctab["c_tri"], "tri")
        cload(mcur[:], ctab["c_mcur"], "mcur")
        cload(mprev[:], ctab["c_mprev"], "mprev")
        cload(sel[:], ctab["c_sel"], "sel")
        cload(biasA[:], ctab["c_biasA"], "biasA", "sp")
        cload(biasB[:], ctab["c_biasB"], "biasB", "sp")
        cload(t1[:], ctab["c_t1"], "t1", "sp")
        cload(sbc[:], sbc_d, "sbc", "sp")
        cload(g1[:], g1_d, "g1", "sp")
        cload(g2[:], g2_d, "g2", "sp")
        cload(gv[:], gv_d, "gv", "sp")
        for h in range(8):
            cload(KA[64:72, h, :], ctab["c_E"], "KAE")
        op("dve", lambda e: e.memset(VA[:, :, :, 64:128], 1.0), (), ["VAones"])
        op("dve", lambda e: e.memset(VB[:, :, 0:64], 1.0), (), ["VBones"])
        op("dve", lambda e: e.memset(VB[:, :, 128:192], 1.0), (), ["VBones"])
        op("dve", lambda e: e.memset(VB[:, :, 256:320], 1.0), (), ["VBones"])
        op("dve", lambda e: e.memset(ksum[:], 0.0), (), ["ksum"])
        op("dve", lambda e: e.memset(epsT[:], EPS), (), ["epsT"])
        for j in range(4):
            act(sinkT[:, j * 128:(j + 1) * 128], t1[:, j * 128:(j + 1) * 128], AF.Exp, ["t1", "sbc"], ["t1", "sinkT"],
                bias=sbc[:, j:j + 1], scale=1.0)

        seq = [(gt, k) for gt in range(NSEQ * NT) for k in range(NCHUNK)]
        wstate = {"next": 0}

        def wload(idx):
            gt, k = seq[idx]
            slot = idx % NS
            wt = W[slot]
            used = WSZ // 2 if (k in (8, 9) or (10 <= k < 26 and (k - 10) % 2 == 1)) else WSZ
            if gt == 0:
                for (off, (kk, ncol), src) in _chunk_pieces(k, w_in, w_a, w_b, w_out, w_up, w_down):
                    dst = wt[:, off:off + kk * ncol].rearrange("p (a b) -> p a b", b=ncol)
                    op("pool", lambda e, dst=dst, src=src: e.dma_start(out=dst, in_=src), (), ["W%d" % slot], dma="w%d" % slot)
                op("sp", lambda e: e.dma_start(out=scr[k, :, 0:used], in_=wt[:, 0:used]), ["W%d" % slot], ["scr%d" % k], dma="ws%d" % slot)
            else:
                op("sp", lambda e: e.dma_start(out=wt[:, 0:used], in_=scr[k, :, 0:used]), ["scr%d" % k], ["W%d" % slot], dma="w%d" % slot)

        def wneed(idx, ahead=NS - 1):
            lim = min(idx + ahead, len(seq) - 1)
            while wstate["next"] <= lim:
                wload(wstate["next"])
                wstate["next"] += 1
            return W[idx % NS], "W%d" % (idx % NS)

        mmb = [0]

        def next_bank():
            b = mmb[0] % 3
            mmb[0] += 1
            return bank[b], "pb%d" % b

        tph = [0]

        def next_tp():
            h = tph[0] % 2
            tph[0] += 1
            return tpbs[h][:, 0:512], "pb%d" % (7 - h)

        def norm_act(src_ap, src_reads, xi, si):
            xh, xhn = xhat[xi], "xhat%d" % xi
            c0 = (si % 4) * 4
            act(xh[:], src_ap, AF.Square, src_reads, [xhn, "st%d" % c0], accum_out=st[:, c0:c0 + 1])
            act(st[:, c0 + 2:c0 + 3], st[:, c0:c0 + 1], AF.Sqrt, ["st%d" % c0, "epsT"], ["st%d" % (c0 + 2)], scale=1.0 / D, bias=epsT[:, 0:1])
            op("dve", lambda e: e.reciprocal(out=st[:, c0 + 3:c0 + 4], in_=st[:, c0 + 2:c0 + 3]), ["st%d" % (c0 + 2)], ["st%d" % (c0 + 3)])
            act(xh[:], src_ap, AF.Copy, list(src_reads) + ["st%d" % (c0 + 3)], [xhn], scale=st[:, c0 + 3:c0 + 4])

        def norm_pe_half(xi, gain, gname, dst_cols, half):
            xh, xhn = xhat[xi], "xhat%d" % xi
            tp, tpn = next_tp()
            for j in range(4):
                jj = half * 4 + j
                op("pe", lambda e, jj=jj, j=j, tp=tp: e.transpose(out=tp[:, j * 128:(j + 1) * 128], in_=xh[:, jj * 128:(jj + 1) * 128], identity=ident[:, :]),
                   [xhn, "ident"], [tpn])
            tt("dve", hT[:, half * 4:half * 4 + 4, dst_cols], tp.rearrange("p (j q) -> p j q", q=128),
               gain[:, half * 4:half * 4 + 4].unsqueeze(2).broadcast_to([128, 4, 128]), ALU.mult, [tpn, gname], ["hT"])

        def qk_norm(ps, psn, nh, col0, qb_):
            n = nh * 64
            qk = qkh[qb_]
            act(sq[:, 0:n], ps, AF.Square, [psn], ["sq"])
            op("dve", lambda e: e.tensor_reduce(out=st[:, 32:32 + nh], in_=sq[:, 0:n].rearrange("p (h d) -> p h d", d=64), axis=AX.X, op=ALU.add),
               ["sq"], ["stq"])
            act(st[:, 48:48 + nh], st[:, 32:32 + nh], AF.Sqrt, ["stq", "epsT"], ["stq3"], scale=1.0 / HD, bias=epsT[:, 0:1])
            op("dve", lambda e: e.reciprocal(out=st[:, 56:56 + nh], in_=st[:, 48:48 + nh]), ["stq3"], ["stq4"])
            tt("dve", qk[:, 0:n].rearrange("p (h d) -> p h d", d=64), ps.rearrange("p (h d) -> p h d", d=64),
               st[:, 56:56 + nh].unsqueeze(2).broadcast_to([128, nh, 64]), ALU.mult, [psn, "stq4"], ["qkh%d" % qb_])

        def head_T(col0, nh, dst_fn, gcol, dname, qb_):
            qk = qkh[qb_]
            for h0 in range(0, nh, 4):
                nhh = min(4, nh - h0)
                tp, tpn = next_tp()
                for j in range(nhh):
                    c = (h0 + j) * 64
                    op("pe", lambda e, c=c, j=j, tp=tp: e.transpose(out=tp[0:64, j * 128:(j + 1) * 128], in_=qk[:, c:c + 64], identity=ident[:, :]),
                       ["qkh%d" % qb_, "ident"], [tpn])
                act(dst_fn(h0, nhh), tp[0:64, 0:nhh * 128].rearrange("p (j q) -> p j q", q=128), AF.Copy, [tpn, "gv"], [dname],
                    scale=gv[:, gcol:gcol + 1])

        ychunk = [0]
        ytok = None

        def stage(n):
            if stop == n:
                raise _Stop()

        widx = [0]

        tiles = [(s_, c_) for s_ in range(NSEQ) for c_ in range(NT)]

        def A0_load(t, i):
            s_, c_ = tiles[t]
            xb, xn = xs[0], "xs0"
            src = x[s_, c_ * T + i * 128:c_ * T + (i + 1) * 128, :]
            op("sp", lambda e: e.dma_start(out=xb[:], in_=src), (), [xn], dma=xn)

        def A0_act(t, i):
            norm_act(xs[0][:], ["xs0"], i, i)

        def A0_pe(t, i, half):
            norm_pe_half(i, g1, "g1", slice(i * 128, (i + 1) * 128), half)

        try:
          stage('const')
          for i in range(4):
              A0_load(0, i)
              A0_act(0, i)
          for i in range(4):
              for half in range(2):
                  A0_pe(0, i, half)
          for s in range(NSEQ):
            for c in range(NT):
                T0 = c * T
                base = widx[0]
                tcur = s * NT + c
                has_next = tcur + 1 < len(tiles)
                hbk = (3, 4, 5, 0)
                mmb[0] = 1
                if c == 0:
                    op("pool", lambda e: e.dma_start(out=QA[64:72, :, :].rearrange("p h q -> p (h q)"), in_=ctab["c_zero"]), (), ["QAm%d" % h_ for h_ in range(8)], dma="qam")
                stage('A0')
                srcx = x[s, T0:T0 + T, :].rearrange("(i p) d -> p i d", p=128)
                op("sp", lambda e, srcx=srcx: e.dma_start(out=x1[:], in_=srcx), (), ["x1_0", "x1_1", "x1_2", "x1_3"], dma="x1")
                pend = []

                def flush(keep):
                    while len(pend) > keep:
                        f_ = pend.pop(0)
                        if f_ is not None:
                            f_()

                gi = 0
                for g in range(5):
                    w0, w0n = wneed(base + 2 * g)
                    w1, w1n = wneed(base + 2 * g + 1, NS - 2)
                    ncol = 512 if g < 4 else 256
                    for i in range(4):
                        ab_ = (1, 2, 0, 3, 4)[gi % 5]
                        pb, pbn = bank[ab_], "pb%d" % ab_
                        kt = 4 * c + i
                        qb_ = gi % 4
                        gi += 1
                        for k in range(8):
                            ww = (w0 if k < 4 else w1)[:, (k % 4) * ncol:(k % 4 + 1) * ncol]
                            mm(pb[:, 0:ncol], hT[:, k, i * 128:(i + 1) * 128], ww, k == 0, k == 7,
                               ["hT", w0n if k < 4 else w1n], [pbn])
                        qc = slice(i * 128, (i + 1) * 128)
                        nxt = None
                        if g == 0:
                            qk_norm(pb[:, 0:512], pbn, 8, 0, qb_)
                            nxt = lambda qc=qc, qb_=qb_: head_T(0, 8, lambda h0, n: QA[0:64, h0:h0 + n, qc], 0, "QA", qb_)
                        elif g == 1:
                            qk_norm(pb[:, 0:512], pbn, 8, 512, qb_)
                            kc = slice(T0 + i * 128, T0 + (i + 1) * 128)
                            nxt = lambda kc=kc, qb_=qb_: head_T(512, 8, lambda h0, n: KA[0:64, h0:h0 + n, kc], 1, "KA", qb_)
                        elif g == 2:
                            pv = pb[:, 0:512].rearrange("p (pr two d) -> p pr two d", two=2, d=64)
                            act(VA[:, kt, :, 0:64], pv[:, :, 0, :], AF.Copy, [pbn], ["VA"])
                            act(VA[:, kt, :, 128:192], pv[:, :, 1, :], AF.Copy, [pbn], ["VA"])
                        elif g == 3:
                            qk_norm(pb[:, 0:512], pbn, 8, 1024, qb_)
                            nxt = lambda qc=qc, qb_=qb_: head_T(1024, 8, lambda h0, n: QB[0:64, h0:h0 + n, qc], 2, "QB", qb_)
                        else:
                            qk_norm(pb[:, 0:128], pbn, 2, 1536, qb_)
                            ks = kt % 8
                            nxt = lambda ks=ks, qb_=qb_: head_T(1536, 2, lambda h0, n: KB[0:64, h0:h0 + n, ks * 128:(ks + 1) * 128], 3, "KB", qb_)
                            act(VB[:, ks, 64:128], pb[:, 128:192], AF.Copy, [pbn], ["VB"])
                            act(VB[:, ks, 192:256], pb[:, 192:256], AF.Copy, [pbn], ["VB"])
                        pend.append(nxt)
                        flush(3)
                flush(0)
                wneed(base + 10, 3)
                for h in range(8):
                    ko = ksum[:, h, 2 * c:2 * c + 2]
                    ki = KA[0:64, h, T0:T0 + T].rearrange("p (n k) -> p n k", k=256)
                    op("dve", lambda e, ko=ko, ki=ki: e.tensor_reduce(out=ko, in_=ki, axis=AX.X, op=ALU.add), ["KA"], ["ksum"])
                if c >= 2:
                    gbk = (3, 4, 0, 1)

                    def g1_(h):
                        tt("dve", dlt[:, h, :].rearrange("p (n m) -> p n m", m=8),
                           ksum[:, h, :].unsqueeze(2).broadcast_to([64, 8, 8]),
                           ksum[:, h, :].unsqueeze(1).broadcast_to([64, 8, 8]), ALU.subtract, ["ksum"], ["dlt%d" % h])
                        gb_, gbn = bank[gbk[h % 4]], "pb%d" % gbk[h % 4]
                        mm(gb_[0:64, :], dlt[:, h, :], QA[0:64, h, :], True, True, ["dlt%d" % h, "QA"], [gbn])

                    def g2_(h):
                        gb_, gbn = bank[gbk[h % 4]], "pb%d" % gbk[h % 4]
                        iv, ivn = ind[h % 2], "ind%d" % (h % 2)
                        op("dve", lambda e: e.tensor_single_scalar(out=iv[:, :], in_=gb_[0:64, :], scalar=0.0, op=ALU.is_lt), [gbn], [ivn])
                        for hb in range(2):
                            bi = 2 * c + hb - 4
                            mm(gb_[0:72, hb * 256:(hb + 1) * 256], sel[:, bi * 72:(bi + 1) * 72], iv[:, hb * 256:(hb + 1) * 256], True, True,
                               ["sel", ivn], [gbn])

                    def g3_(h):
                        gb_, gbn = bank[gbk[h % 4]], "pb%d" % gbk[h % 4]
                        ts("dve", QA[64:72, h, :], gb_[64:72, :], 2.5, NEGM, ALU.is_ge, ALU.mult, [gbn], ["QAm%d" % h])

                    for k in range(10):
                        if k < 8:
                            g1_(k)
                        if 0 <= k - 1 < 8:
                            g2_(k - 1)
                        if 0 <= k - 2 < 8:
                            g3_(k - 2)
                stage('gate%d' % c)
                steps = [(h, j) for h in range(8) for j in range(4 * c + 4)]
                nst = len(steps)

                def moba_S(si):
                    h, j = steps[si]
                    qs = 128 * max(0, j - 4 * c)
                    N = T - qs
                    sb_i = (3, 4, 0)[si % 3]
                    sbk, sbn = bank[sb_i], "pb%d" % sb_i
                    diag = j >= 4 * c
                    mm(sbk[:, 0:N], KA[0:72, h, j * 128:(j + 1) * 128], QA[0:72, h, qs:T], True, not diag,
                       ["KA", "KAE", "QA", "QAm%d" % h], [sbn])
                    if diag:
                        mm(sbk[:, 0:128], ident[:, :], tri[:, :], False, True, ["ident", "tri"], [sbn])
                    di = 4 * c - j + 3
                    pt = PT[si % 3]
                    act(pt[:, 0:N], sbk[:, 0:N], AF.Exp, [sbn, "biasA"], ["PT%d" % (si % 3)],
                        bias=biasA[:, h * 16 + di:h * 16 + di + 1], scale=SCALE)

                def moba_PV(si):
                    h, j = steps[si]
                    qs = 128 * max(0, j - 4 * c)
                    N = T - qs
                    ob_, obn = bank[5 + h % 2], "pb%d" % (5 + h % 2)
                    pr, od = divmod(h, 2)
                    lhsT = VA[:, j, pr, od * 64:od * 64 + 128]
                    mm(ob_[:, qs:T], lhsT, PT[si % 3][:, 0:N], j == 0, j == 4 * c + 3, ["VA", "VAones", "PT%d" % (si % 3)], [obn])
                    if j == 4 * c + 3:
                        lo, hi = (0, 64) if od == 0 else (64, 128)
                        dl, dh = (64, 128) if od == 0 else (0, 64)
                        rn = "R%d" % od
                        op("dve", lambda e: e.tensor_copy(out=R[dl:dh, :], in_=ob_[dl:dh, :]), [obn], [rn])
                        op("dve", lambda e: e.reciprocal(out=R[dl:dh, :], in_=R[dl:dh, :]), [rn], [rn])
                        tt("dve", oaT[lo:hi, pr, :], ob_[lo:hi, :], R[dl:dh, :], ALU.mult, [obn, rn], ["oaT%d" % od])

                moba_S(0)
                moba_S(1)
                for si in range(nst):
                    if si + 2 < nst:
                        moba_S(si + 2)
                    moba_PV(si)
                stage('B')
                sw = []
                for i in range(4):
                    kt = 4 * c + i
                    for g in range(2):
                        passes = [(kt % 8, mcur, "mcur", 0)] + ([((kt - 1) % 8, mprev, "mprev", 1)] if kt >= 1 else [])
                        for pi, (kslot, mk, mkn, which) in enumerate(passes):
                            sw.append((i, g, pi, len(passes), kslot, mk, mkn, which))

                def swa_S(n):
                    i, g, pi, npass, kslot, mk, mkn, which = sw[n]
                    sb_i = (3, 4, 2)[n % 3]
                    sbk, sbn = bank[sb_i], "pb%d" % sb_i
                    mm(sbk[:, :], KB[0:64, g, kslot * 128:(kslot + 1) * 128], QB[0:64, 4 * g:4 * g + 4, i * 128:(i + 1) * 128],
                       True, False, ["KB", "QB"], [sbn])
                    mm(sbk[:, :], ident[:, :], mk[:, :], False, True, ["ident", mkn], [sbn])
                    pt = PT[n % 3]
                    for hh in range(4):
                        h = 4 * g + hh
                        act(pt[:, hh * 128:(hh + 1) * 128], sbk[:, hh * 128:(hh + 1) * 128], AF.Exp, [sbn, "biasB"], ["PT%d" % (n % 3)],
                            bias=biasB[:, h * 2 + which:h * 2 + which + 1], scale=SCALE)

                def swa_PV(n):
                    i, g, pi, npass, kslot, mk, mkn, which = sw[n]
                    be, bo = (5, 6) if i % 2 == 0 else (0, 1)
                    oe, oen, oo, oon = bank[be], "pb%d" % be, bank[bo], "pb%d" % bo
                    pt = PT[n % 3]
                    ptn = "PT%d" % (n % 3)
                    pt3 = pt[:, :].rearrange("p (a e q) -> p a e q", e=2, q=128)
                    last = pi == npass - 1
                    cs = slice(g * 256, (g + 1) * 256)
                    mm(oe[:, cs], VB[:, kslot, 64 + 128 * g:64 + 128 * g + 128], pt3[:, :, 0, :], pi == 0, last,
                       ["VB", "VBones", ptn], [oen])
                    mm(oo[:, cs], VB[:, kslot, 128 * g:128 * g + 128], pt3[:, :, 1, :], pi == 0, last,
                       ["VB", "VBones", ptn], [oon])
                    if not (last and g == 1):
                        return
                    tt("dve", R[64:128, :], oe[64:128, :], sinkT[64:128, :], ALU.add, [oen, "sinkT"], ["R0"])
                    tt("dve", R[0:64, :], oo[0:64, :], sinkT[0:64, :], ALU.add, [oon, "sinkT"], ["R1"])
                    op("dve", lambda e: e.reciprocal(out=R[:, :], in_=R[:, :]), ["R0", "R1"], ["R0", "R1"])
                    qc = slice(i * 128, (i + 1) * 128)
                    tt("dve", obT[0:64, :, qc], oe[0:64, :].rearrange("p (a q) -> p a q", q=128),
                       R[64:128, :].rearrange("p (a q) -> p a q", q=128), ALU.mult, [oen, "R0"], ["obT0"])
                    tt("dve", obT[64:128, :, qc], oo[64:128, :].rearrange("p (a q) -> p a q", q=128),
                       R[0:64, :].rearrange("p (a q) -> p a q", q=128), ALU.mult, [oon, "R1"], ["obT1"])

                swa_S(0)
                swa_S(1)
                for n in range(len(sw)):
                    if n + 2 < len(sw):
                        swa_S(n + 2)
                    swa_PV(n)
                stage('C')
                for n in range(8):
                    wg, wgn = wneed(base + 10 + 2 * n)
                    wab, wabn = wneed(base + 10 + 2 * n + 1, NS - 2)
                    pa, pan = next_bank()
                    for k in range(8):
                        mm(pa[:, :], wg[:, k * 128:(k + 1) * 128], hT[:, k, :], k == 0, k == 7, [wgn, "hT"], [pan])
                    act(sga[:], pa[:, :], AF.Sigmoid, [pan], ["sga"])
                    pb2, pb2n = next_bank()
                    for k in range(8):
                        mm(pb2[:, :], wg[:, 1024 + k * 128:1024 + (k + 1) * 128], hT[:, k, :], k == 0, k == 7, [wgn, "hT"], [pb2n])
                    act(sgb[:], pb2[:, :], AF.Sigmoid, [pb2n], ["sgb"])
                    pc, pcn = next_bank()
                    for k in range(4):
                        mm(pc[:, :], wab[:, k * 128:(k + 1) * 128], oaT[:, k, :], k == 0, k == 3, [wabn, "oaT0", "oaT1"], [pcn])
                    tt("dve", sga[:], pc[:, :], sga[:], ALU.mult, [pcn, "sga"], ["sga"])
                    pd, pdn = next_bank()
                    for k in range(4):
                        mm(pd[:, :], wab[:, 512 + k * 128:512 + (k + 1) * 128], obT[:, k, :], k == 0, k == 3, [wabn, "obT0", "obT1"], [pdn])
                    tt("dve", sgb[:], pd[:, :], sgb[:], ALU.mult, [pdn, "sgb"], ["sgb"])
                    tt("dve", HM[:, n, :], sga[:], sgb[:], ALU.add, ["sga", "sgb"], ["hid%d" % n])
                stage('D')
                for half in range(2):
                    w0, w0n = wneed(base + 26 + 2 * half)
                    w1, w1n = wneed(base + 26 + 2 * half + 1, NS - 2)
                    for i in range(4):
                        pb, pbn = next_bank()
                        for k in range(8):
                            ww = (w0 if k < 4 else w1)[:, (k % 4) * 512:(k % 4 + 1) * 512]
                            mm(pb[:, :], HM[:, k, i * 128:(i + 1) * 128], ww, k == 0, k == 7, ["hid%d" % k, w0n if k < 4 else w1n], [pbn])
                        xo = x1[:, i, half * 512:(half + 1) * 512]
                        tt("dve", xo, pb[:, :], xo, ALU.add, [pbn, "x1_%d" % i], ["x1_%d" % i])
                        if half == 1:
                            norm_act(x1[:, i, :], ["x1_%d" % i], i, 4 + i)
                stage('E')
                for i in range(4):
                    for half in range(2):
                        norm_pe_half(i, g2, "g2", slice(i * 128, (i + 1) * 128), half)
                stage('F')
                for gch in range(8):
                    w0, w0n = wneed(base + 30 + 2 * gch)
                    w1, w1n = wneed(base + 30 + 2 * gch + 1, NS - 2)
                    for f in range(4):
                        pb, pbn = next_bank()
                        for k in range(8):
                            ww = (w0 if k < 4 else w1)[:, (k % 4) * 512 + f * 128:(k % 4) * 512 + (f + 1) * 128]
                            mm(pb[:, :], ww, hT[:, k, :], k == 0, k == 7, ["hT", w0n if k < 4 else w1n], [pbn])
                        act(tmpr[:], pb[:, :], AF.Relu, [pbn], ["tmpr"])
                        tt("dve", HM[:, gch * 4 + f, :], pb[:, :], tmpr[:], ALU.mult, [pbn, "tmpr"], ["hid%d" % (gch * 4 + f)])
                        if has_next and f == 3:
                            sched = {0: ("L0",), 1: ("A0", "L1"), 2: ("A1", "L2"), 4: ("A2", "L3"), 6: ("A3",)}
                            for what in sched.get(gch, ()):
                                (A0_load if what[0] == "L" else A0_act)(tcur + 1, int(what[1]))
                stage('G')
                for half in range(2):
                    for q in range(8):
                        wd, wdn = wneed(base + 46 + half * 8 + q)
                        for f in range(4):
                            ff = q * 4 + f
                            for i in range(4):
                                mm(bank[hbk[i]][:, :], HM[:, ff, i * 128:(i + 1) * 128], wd[:, f * 512:(f + 1) * 512], ff == 0, ff == 31,
                                   ["hid%d" % ff, wdn], ["pb%d" % hbk[i]])
                        if has_next and half == 0:
                            A0_pe(tcur + 1, q // 2, q % 2)
                    for i in range(4):
                        xo = x1[:, i, half * 512:(half + 1) * 512]
                        tt("dve", xo, bank[hbk[i]][:, :], xo, ALU.add, ["pb%d" % hbk[i], "x1_%d" % i], ["x1_%d" % i])
                dsty = y[s, T0:T0 + T, :].rearrange("(i p) d -> p i d", p=128)
                ytok = op("pool", lambda e, dsty=dsty: e.dma_start(out=dsty, in_=x1[:]), ["x1_0", "x1_1", "x1_2", "x1_3"], (), dma="y")
                widx[0] += NCHUNK
                stage('T%d' % (s * NT + c))
        except _Stop:
            pass
        if ytok is not None:
            P.final_wait("pool", [ytok])
        if dump:
            tens = {"hT": hT, "QA": QA, "KA": KA, "VA": VA, "QB": QB, "KB": KB, "VB": VB, "oaT": oaT, "obT": obT,
                    "HM": HM, "x1": x1, "ksum": ksum, "sinkT": sinkT, "R": R, "st": st, "qkh": qkh[0], "ind": ind[0], "dlt": dlt}
            allb = list(P.bufs.keys())
            dtoks = []
            for nm in dump:
                tns = tens[nm]
                shp = list(tns.shape)
                flat = tns[:]
                if len(shp) == 3:
                    flat = flat.rearrange("p a b -> p (a b)")
                elif len(shp) == 4:
                    flat = flat.rearrange("p a b c -> p (a b c)")
                fr = int(np.prod(shp[1:]))
                dd = nc.dram_tensor("dbg_" + nm, [shp[0], fr], tns.dtype, kind="ExternalOutput").ap()
                dtoks.append(op("sp", lambda e, dd=dd, flat=flat: e.dma_start(out=dd, in_=flat), allb, (), dma="dbg_" + nm))
            P.final_wait("sp", dtoks)
        sems = {n: es.enter_context(nc.semaphore(n)) for n in sorted(P.semnames)}
        with nc.Block() as block:
            P.emit(block, sems)
    return nc


_NC_CACHE = {}


def kernel(x, norm_attn, w_in, q_norm_a, k_norm_a, q_norm_b, k_norm_b, sinks_b,
           w_branch_a, w_branch_b, w_out, norm_mlp, w_up, w_down):
    f = lambda a: np.ascontiguousarray(np.asarray(a, dtype=np.float32))
    x = f(x)
    tabs = _host_tables()
    g1 = f(np.asarray(norm_attn)[0].reshape(8, 128).T)
    g2 = f(np.asarray(norm_mlp)[0].reshape(8, 128).T)
    gv = f(np.stack([np.asarray(q_norm_a)[0], np.asarray(k_norm_a)[0], np.asarray(q_norm_b)[0], np.asarray(k_norm_b)[0]], axis=1))
    sk = np.asarray(sinks_b)[0]
    sbc = np.zeros((128, 4), np.float32)
    for g in range(2):
        for a in range(2):
            sbc[64:, 2 * g + a] = sk[4 * g + 2 * a]
            sbc[:64, 2 * g + a] = sk[4 * g + 2 * a + 1]
    common = {
        "w_in": f(np.asarray(w_in)[0]), "w_a": f(np.asarray(w_branch_a)[0]), "w_b": f(np.asarray(w_branch_b)[0]),
        "w_out": f(np.asarray(w_out)[0]), "w_up": f(np.asarray(w_up)[0]), "w_down": f(np.asarray(w_down)[0]),
        "g1": g1, "g2": g2, "gv": gv, "sbc": sbc,
    }
    common.update(tabs)
    if "nc" not in _NC_CACHE:
        _NC_CACHE["nc"] = build_nc()
    nc = _NC_CACHE["nc"]
    in_maps = []
    for c in range(NCORES):
        m = dict(common)
        m["x"] = np.ascontiguousarray(x[NSEQ * c:NSEQ * (c + 1)])
        in_maps.append(m)
    res = run_bass_kernel_spmd(nc, in_maps, core_ids=list(range(NCORES)))
    return np.concatenate([r["y"] for r in res.results], axis=0).astype(np.float32)
```
